# Optimizing a Trainium2 kernel written in Bass

```python
import math
import jax
import jax.numpy as jnp
from jax import lax
import numpy as np

D_MODEL = 1024
BATCH = 8
SEQ = 2048
DEPTH = 4

GRID_W = 64
CTX_LEN = 256
EPS = 1e-6

GROUP_W = D_MODEL // 4
D_MIX = 4 * GROUP_W
HEAD_DIM = 64
N_HEADS = GROUP_W // HEAD_DIM

HGRN_DK = 64
HGRN_DV = 64
HGRN_CHUNK = 64

HYENA_ORDER = 2
HYENA_SHORT = 3
HYENA_BANDS = 16
HYENA_EMB = 1 + 2 * HYENA_BANDS
HYENA_FFN = 64
HYENA_DECAY_MIN = 3.07
HYENA_DECAY_MAX = 15.35

NA_ROWS = 8
NA_COLS = 16

MLA_Q_RANK = 256
MLA_KV_RANK = 128
MLA_NOPE = 64
MLA_ROPE = 32
MLA_V = 64
MLA_QBLOCK = 128
ROPE_BASE = 10000.0

N_GROUPS = 4
EXPERTS_PER_GROUP = 8
N_EXPERTS = N_GROUPS * EXPERTS_PER_GROUP
TOP_K = 2
D_EXPERT = 512
MOE_BLOCK = 128

IN_SPLITS = (GROUP_W, GROUP_W, GROUP_W, GROUP_W, GROUP_W, 3 * GROUP_W, 3 * GROUP_W, MLA_Q_RANK, MLA_KV_RANK, MLA_ROPE)
D_IN = sum(IN_SPLITS)

kernel_name = "hybrid_parallel_heads_diffusion_trunk"


def rmsnorm(x, g):
    xf = x.astype(jnp.float32)
    y = xf * lax.rsqrt(jnp.mean(xf * xf, axis=-1, keepdims=True) + EPS)
    return (y * g.astype(jnp.float32)).astype(x.dtype)


def _heads(a, d):
    return a.reshape(a.shape[0], a.shape[1], -1, d)


def dense_attention(q, k, v):
    s = jnp.einsum('bqhd,bkhd->bhqk', q.astype(jnp.float32), k.astype(jnp.float32)) * (q.shape[-1] ** -0.5)
    p = jax.nn.softmax(s, axis=-1)
    return jnp.einsum('bhqk,bkhd->bqhd', p, v.astype(jnp.float32)).astype(v.dtype)


def blocked_attention(q, k, v):
    B, L, H, dq = q.shape
    nb = L // MLA_QBLOCK
    qb = q.reshape(B, nb, MLA_QBLOCK, H, dq).swapaxes(0, 1)
    o = lax.map(lambda qi: dense_attention(qi, k, v), qb)
    return o.swapaxes(0, 1).reshape(B, L, H, v.shape[-1])


def rope_2d(x):
    L = x.shape[1]
    t = jnp.arange(L)
    row = (t // GRID_W).astype(jnp.float32)
    col = (t % GRID_W).astype(jnp.float32)
    half = MLA_ROPE // 2
    inv = ROPE_BASE ** (-jnp.arange(0, half, 2, dtype=jnp.float32) / half)

    def rot(part, pos):
        ang = pos[:, None] * inv[None, :]
        cos = jnp.cos(ang)[None, :, None, :]
        sin = jnp.sin(ang)[None, :, None, :]
        a, b = part[..., :half // 2], part[..., half // 2:]
        return jnp.concatenate([a * cos - b * sin, a * sin + b * cos], axis=-1)

    xf = x.astype(jnp.float32)
    return jnp.concatenate([rot(xf[..., :half], row), rot(xf[..., half:], col)], axis=-1).astype(x.dtype)


def hgrn_gates(z, lb):
    zf = z.astype(jnp.float32)
    lbf = lb.astype(jnp.float32)
    logf = jnp.logaddexp(jnp.log(lbf), jnp.log1p(-lbf) + jax.nn.log_sigmoid(zf))
    k = -jnp.expm1(logf)
    return _heads(logf, HGRN_DK), _heads(k, HGRN_DK)


def hgrn_chunk_scan(q, k, logf, v, s0):
    B, L, H, dk = q.shape
    dv = v.shape[-1]
    C = HGRN_CHUNK
    n = L // C

    def chunks(a):
        return a.reshape(B, n, C, H, a.shape[-1]).transpose(1, 0, 3, 2, 4)

    causal = jnp.tril(jnp.ones((C, C), dtype=bool))

    def step(S, inp):
        qc, kc, fc, vc = inp
        b = jnp.cumsum(fc, axis=2)
        o_inter = jnp.einsum('bhtk,bhkv->bhtv', qc * jnp.exp(b), S)
        diff = b[:, :, :, None, :] - b[:, :, None, :, :]
        decay = jnp.exp(jnp.where(causal[None, None, :, :, None], diff, -jnp.inf))
        attn = jnp.einsum('bhtk,bhsk,bhtsk->bhts', qc, kc, decay)
        o_intra = jnp.einsum('bhts,bhsv->bhtv', attn, vc)
        b_last = b[:, :, -1:, :]
        S_new = jnp.exp(b_last[:, :, 0, :])[..., None] * S + jnp.einsum('bhsk,bhsv->bhkv', kc * jnp.exp(b_last - b), vc)
        return S_new, o_inter + o_intra

    S_fin, o = lax.scan(step, s0, (chunks(q), chunks(k), chunks(logf), chunks(v)))
    return o.transpose(1, 0, 3, 2, 4).reshape(B, L, H, dv), S_fin


def hgrn_readout(o, g, norm_g):
    B, L = g.shape[0], g.shape[1]
    o = rmsnorm(o, norm_g).reshape(B, L, -1)
    return (o * jax.nn.silu(g.astype(jnp.float32))).astype(g.dtype)


def hgrn_mixer(q, zf, zb, i, g, qc, zfc, zbc, ic, gc, lb, norm_g, need_ctx):
    f32 = jnp.float32
    B = q.shape[0]
    qh, ih = _heads(q.astype(f32), HGRN_DK), _heads(i.astype(f32), HGRN_DV)
    qch, ich = _heads(qc.astype(f32), HGRN_DK), _heads(ic.astype(f32), HGRN_DV)
    lf_f, k_f = hgrn_gates(zf, lb[0])
    lf_b, k_b = hgrn_gates(zb, lb[1])
    lfc_f, kc_f = hgrn_gates(zfc, lb[0])
    lfc_b, kc_b = hgrn_gates(zbc, lb[1])
    s0 = jnp.zeros((B, N_HEADS, HGRN_DK, HGRN_DV), f32)
    rev = lambda a: jnp.flip(a, axis=1)
    oc_f, s_cf = hgrn_chunk_scan(qch, kc_f, lfc_f, ich, s0)
    o_f, _ = hgrn_chunk_scan(qh, k_f, lf_f, ih, s_cf)
    oc_b, s_cb = hgrn_chunk_scan(rev(qch), rev(kc_b), rev(lfc_b), rev(ich), s0)
    o_b, _ = hgrn_chunk_scan(rev(qh), rev(k_b), rev(lf_b), rev(ih), s_cb)
    out = hgrn_readout(o_f + rev(o_b), g, norm_g)
    outc = hgrn_readout(oc_f + rev(oc_b), gc, norm_g) if need_ctx else None
    return out, outc


def hyena_filters(L, w1, b1, freq, w2, b2, w3, b3, decay):
    f32 = jnp.float32
    t = jnp.arange(L, dtype=f32)
    t_unit = jnp.linspace(0.0, 1.0, L, dtype=f32)
    bands = jnp.linspace(1e-4, HYENA_BANDS - 1, HYENA_BANDS, dtype=f32)
    ang = (2.0 * math.pi / L) * t[:, None] * bands[None, :]
    feats = jnp.concatenate([t_unit[:, None], jnp.cos(ang), -jnp.sin(ang)], axis=-1)
    fr = freq.astype(f32)
    hdn = jnp.sin(fr * (feats @ w1.astype(f32) + b1.astype(f32)))
    hdn = jnp.sin(fr * (hdn @ w2.astype(f32) + b2.astype(f32)))
    filt = (hdn @ w3.astype(f32) + b3.astype(f32)).reshape(L, HYENA_ORDER, 2, GROUP_W)
    filt = filt * jnp.exp(-t_unit[:, None, None, None] * decay.astype(f32)[None])
    fwd, bwd = filt[:, :, 0], filt[:, :, 1]
    kern = jnp.concatenate([fwd, jnp.zeros((1, HYENA_ORDER, GROUP_W), f32), bwd[:0:-1]], axis=0)
    return kern * lax.rsqrt(jnp.sum(kern * kern, axis=0, keepdims=True) + EPS)


def hyena_mixer(u, short_w, short_b, w1, b1, freq, w2, b2, w3, b3, decay, dbias):
    B, L, Cin = u.shape
    uc = lax.conv_general_dilated(u, short_w[:, None, :].astype(u.dtype), (1,), 'SAME',
                                  dimension_numbers=('NWC', 'WIO', 'NWC'),
                                  feature_group_count=Cin) + short_b.astype(u.dtype)
    v, x1, x2 = jnp.split(uc.astype(jnp.float32), 3, axis=-1)
    kern = hyena_filters(L, w1, b1, freq, w2, b2, w3, b3, decay)
    kf = jnp.fft.rfft(kern, axis=0)
    db = dbias.astype(jnp.float32)
    z = v
    for o, gate in enumerate((x1, x2)):
        zf = jnp.fft.rfft(z, n=2 * L, axis=1)
        y = jnp.fft.irfft(zf * kf[None, :, o], n=2 * L, axis=1)[:, :L]
        z = gate * (y + db[o] * z)
    return z.astype(u.dtype)


def neighborhood_attention(q, k, v, k_ctx, v_ctx, rpb):
    B, L, H, d = q.shape
    rows = L // GRID_W
    wr = min(NA_ROWS, rows)
    f32 = jnp.float32
    qg = q.astype(f32).reshape(B, rows, GRID_W, H, d)
    kg = k.astype(f32).reshape(B, rows, GRID_W, H, d)
    vg = v.astype(f32).reshape(B, rows, GRID_W, H, d)
    r = jnp.arange(rows)
    rs = jnp.clip(r - wr // 2, 0, rows - wr)
    row_idx = rs[:, None] + jnp.arange(wr)[None, :]
    kb = kg[:, row_idx]
    vb = vg[:, row_idx]
    cq = jnp.arange(GRID_W)
    cs = jnp.clip(cq - NA_COLS // 2, 0, GRID_W - NA_COLS)
    col_ok = (cq[None, :] >= cs[:, None]) & (cq[None, :] < cs[:, None] + NA_COLS)
    dr = row_idx - r[:, None] + (NA_ROWS - 1)
    dc = jnp.clip(cq[None, :] - cq[:, None] + (NA_COLS - 1), 0, 2 * NA_COLS - 2)
    bias = rpb.astype(f32)[:, dr[:, None, :, None], dc[None, :, None, :]]
    scale = d ** -0.5
    s_loc = jnp.einsum('brqhd,brjkhd->bhrqjk', qg, kb) * scale + bias[None]
    s_loc = jnp.where(col_ok[None, None, None, :, None, :], s_loc, -jnp.inf)
    s_ctx = jnp.einsum('brqhd,bchd->bhrqc', qg, k_ctx.astype(f32)) * scale
    s = jnp.concatenate([s_loc.reshape(B, H, rows, GRID_W, wr * GRID_W), s_ctx], axis=-1)
    p = jax.nn.softmax(s, axis=-1)
    p_loc = p[..., :wr * GRID_W].reshape(B, H, rows, GRID_W, wr, GRID_W)
    p_ctx = p[..., wr * GRID_W:]
    out = jnp.einsum('bhrqjk,brjkhd->brqhd', p_loc, vb) + jnp.einsum('bhrqc,bchd->brqhd', p_ctx, v_ctx.astype(f32))
    return out.reshape(B, L, H, d).astype(v.dtype)


def na_mixer(qkv, qkvc, rpb, q_g, k_g, need_ctx):
    q, k, v = [_heads(a, HEAD_DIM) for a in jnp.split(qkv, 3, axis=-1)]
    qc, kc, vc = [_heads(a, HEAD_DIM) for a in jnp.split(qkvc, 3, axis=-1)]
    q, k = rmsnorm(q, q_g), rmsnorm(k, k_g)
    qc, kc = rmsnorm(qc, q_g), rmsnorm(kc, k_g)
    B, L = qkv.shape[0], qkv.shape[1]
    out = neighborhood_attention(q, k, v, kc, vc, rpb).reshape(B, L, GROUP_W)
    outc = dense_attention(qc, kc, vc).reshape(B, qkvc.shape[1], GROUP_W) if need_ctx else None
    return out, outc


def mla_project(cq, ckv, kr, q_a_g, kv_a_g, w_uq, w_ukv, q_g, k_g, use_rope):
    B, L = cq.shape[0], cq.shape[1]
    q = (rmsnorm(cq, q_a_g) @ w_uq).reshape(B, L, N_HEADS, MLA_NOPE + MLA_ROPE)
    kv = (rmsnorm(ckv, kv_a_g) @ w_ukv).reshape(B, L, N_HEADS, MLA_NOPE + MLA_V)
    k_nope, v = kv[..., :MLA_NOPE], kv[..., MLA_NOPE:]
    k = jnp.concatenate([k_nope, jnp.broadcast_to(kr[:, :, None, :], (B, L, N_HEADS, MLA_ROPE))], axis=-1)
    q, k = rmsnorm(q, q_g), rmsnorm(k, k_g)
    if use_rope:
        q = jnp.concatenate([q[..., :MLA_NOPE], rope_2d(q[..., MLA_NOPE:])], axis=-1)
        k = jnp.concatenate([k[..., :MLA_NOPE], rope_2d(k[..., MLA_NOPE:])], axis=-1)
    return q, k, v


def mla_mixer(cq, ckv, kr, cqc, ckvc, krc, q_a_g, kv_a_g, w_uq, w_ukv, q_g, k_g, need_ctx):
    q, k, v = mla_project(cq, ckv, kr, q_a_g, kv_a_g, w_uq, w_ukv, q_g, k_g, True)
    qc, kc, vc = mla_project(cqc, ckvc, krc, q_a_g, kv_a_g, w_uq, w_ukv, q_g, k_g, False)
    B, L = cq.shape[0], cq.shape[1]
    k_all = jnp.concatenate([k, kc], axis=1)
    v_all = jnp.concatenate([v, vc], axis=1)
    out = blocked_attention(q, k_all, v_all).reshape(B, L, N_HEADS * MLA_V)
    outc = dense_attention(qc, kc, vc).reshape(B, cqc.shape[1], N_HEADS * MLA_V) if need_ctx else None
    return out, outc


def moe_ffn(h, wg, bg, we, be, w_gate, w_up, w_down):
    T, D = h.shape
    f32 = jnp.float32
    hf = h.astype(f32)
    lg = hf @ wg.astype(f32) + bg.astype(f32)
    pg = jax.nn.softmax(lg, axis=-1)
    g_sel = jnp.argmax(lg, axis=-1).astype(jnp.int32)
    p_sel = jnp.take_along_axis(pg, g_sel[:, None], axis=-1)[:, 0]
    le = (hf @ we.astype(f32) + be.astype(f32)).reshape(T, N_GROUPS, EXPERTS_PER_GROUP)
    le_sel = le[jnp.arange(T), g_sel]
    top_v, top_i = lax.top_k(le_sel, TOP_K)
    wts = jax.nn.softmax(top_v, axis=-1) * p_sel[:, None]
    eid = g_sel[:, None] * EXPERTS_PER_GROUP + top_i.astype(jnp.int32)
    N = T * TOP_K
    flat_e = eid.reshape(N)
    flat_t = jnp.repeat(jnp.arange(T, dtype=jnp.int32), TOP_K)
    flat_w = wts.reshape(N).astype(h.dtype)
    order = jnp.argsort(flat_e)
    se = flat_e[order]
    counts = jnp.bincount(flat_e, length=N_EXPERTS)
    starts = jnp.cumsum(counts) - counts
    pcounts = ((counts + MOE_BLOCK - 1) // MOE_BLOCK) * MOE_BLOCK
    pends = jnp.cumsum(pcounts)
    pstarts = pends - pcounts
    dest = pstarts[se] + (jnp.arange(N) - starts[se])
    NB = -(-N // MOE_BLOCK) + N_EXPERTS
    P = NB * MOE_BLOCK
    buf_t = jnp.full((P,), T, dtype=jnp.int32).at[dest].set(flat_t[order])
    buf_w = jnp.zeros((P,), h.dtype).at[dest].set(flat_w[order])
    block_e = jnp.minimum(jnp.searchsorted(pends, jnp.arange(NB) * MOE_BLOCK, side='right'), N_EXPERTS - 1)
    xpad = jnp.concatenate([h, jnp.zeros((1, D), h.dtype)], axis=0)
    xb = xpad[buf_t].reshape(NB, MOE_BLOCK, D)

    def run(args):
        xblk, e = args
        return (jax.nn.silu(xblk @ w_gate[e]) * (xblk @ w_up[e])) @ w_down[e]

    yb = lax.map(run, (xb, block_e)).reshape(P, D)
    return jax.ops.segment_sum(yb * buf_w[:, None], buf_t, num_segments=T + 1)[:T]


def setup_inputs(seed: int = 0) -> dict:
    key = jax.random.key(seed)
    ks = iter(jax.random.split(key, 48))
    f32 = jnp.float32
    nrm = lambda shape, scale: jax.random.normal(next(ks), shape, f32) * scale
    gain = lambda shape: 1.0 + 0.02 * jax.random.normal(next(ks), shape, f32)
    D = D_MODEL
    C = GROUP_W
    return {
        "x": nrm((BATCH, SEQ, D), 1.0),
        "c": nrm((BATCH, D), 1.0),
        "ctx": nrm((BATCH, CTX_LEN, D), 1.0),
        "c_ctx": nrm((D,), 1.0),
        "w_ada": nrm((DEPTH, D, 6 * D), 0.3 * D ** -0.5),
        "b_ada": nrm((DEPTH, 6 * D), 0.02),
        "norm1_g": gain((DEPTH, D)),
        "norm2_g": gain((DEPTH, D)),
        "w_in": nrm((DEPTH, D, D_IN), D ** -0.5),
        "w_out": nrm((DEPTH, D_MIX, D), D_MIX ** -0.5),
        "hgrn_lb_logits": nrm((DEPTH, 2, N_HEADS * HGRN_DK), 1.0),
        "hgrn_norm_g": gain((DEPTH, HGRN_DV)),
        "hy_short_w": nrm((DEPTH, HYENA_SHORT, 3 * C), HYENA_SHORT ** -0.5),
        "hy_short_b": nrm((DEPTH, 3 * C), 0.02),
        "hy_w1": nrm((DEPTH, HYENA_EMB, HYENA_FFN), HYENA_EMB ** -0.5),
        "hy_b1": nrm((DEPTH, HYENA_FFN), 0.02),
        "hy_freq": gain((DEPTH, HYENA_FFN)),
        "hy_w2": nrm((DEPTH, HYENA_FFN, HYENA_FFN), HYENA_FFN ** -0.5),
        "hy_b2": nrm((DEPTH, HYENA_FFN), 0.02),
        "hy_w3": nrm((DEPTH, HYENA_FFN, HYENA_ORDER * 2 * C), HYENA_FFN ** -0.5),
        "hy_b3": nrm((DEPTH, HYENA_ORDER * 2 * C), 0.02),
        "hy_decay": jnp.exp(jax.random.uniform(next(ks), (DEPTH, HYENA_ORDER, 2, C), f32,
                                               math.log(HYENA_DECAY_MIN), math.log(HYENA_DECAY_MAX))),
        "hy_bias": nrm((DEPTH, HYENA_ORDER, C), 0.1),
        "na_rpb": nrm((DEPTH, N_HEADS, 2 * NA_ROWS - 1, 2 * NA_COLS - 1), 0.02),
        "na_q_g": gain((DEPTH, HEAD_DIM)),
        "na_k_g": gain((DEPTH, HEAD_DIM)),
        "mla_q_a_g": gain((DEPTH, MLA_Q_RANK)),
        "mla_kv_a_g": gain((DEPTH, MLA_KV_RANK)),
        "mla_w_uq": nrm((DEPTH, MLA_Q_RANK, N_HEADS * (MLA_NOPE + MLA_ROPE)), MLA_Q_RANK ** -0.5),
        "mla_w_ukv": nrm((DEPTH, MLA_KV_RANK, N_HEADS * (MLA_NOPE + MLA_V)), MLA_KV_RANK ** -0.5),
        "mla_q_g": gain((DEPTH, MLA_NOPE + MLA_ROPE)),
        "mla_k_g": gain((DEPTH, MLA_NOPE + MLA_ROPE)),
        "moe_wg": nrm((DEPTH, D, N_GROUPS), D ** -0.5),
        "moe_bg": nrm((DEPTH, N_GROUPS), 0.01),
        "moe_we": nrm((DEPTH, D, N_EXPERTS), D ** -0.5),
        "moe_be": nrm((DEPTH, N_EXPERTS), 0.01),
        "moe_w_gate": nrm((DEPTH, N_EXPERTS, D, D_EXPERT), D ** -0.5),
        "moe_w_up": nrm((DEPTH, N_EXPERTS, D, D_EXPERT), D ** -0.5),
        "moe_w_down": nrm((DEPTH, N_EXPERTS, D_EXPERT, D), D_EXPERT ** -0.5),
    }


def reference(x, c, ctx, c_ctx, w_ada, b_ada, norm1_g, norm2_g, w_in, w_out,
              hgrn_lb_logits, hgrn_norm_g,
              hy_short_w, hy_short_b, hy_w1, hy_b1, hy_freq, hy_w2, hy_b2, hy_w3, hy_b3, hy_decay, hy_bias,
              na_rpb, na_q_g, na_k_g,
              mla_q_a_g, mla_kv_a_g, mla_w_uq, mla_w_ukv, mla_q_g, mla_k_g,
              moe_wg, moe_bg, moe_we, moe_be, moe_w_gate, moe_w_up, moe_w_down):
    B, L, D = x.shape
    lb_cum = jnp.cumsum(jax.nn.softmax(hgrn_lb_logits.astype(jnp.float32), axis=0), axis=0)
    lower_bounds = lb_cum - lb_cum[0:1]
    sc = jax.nn.silu(c)
    scc = jax.nn.silu(c_ctx)
    split_at = [int(s) for s in np.cumsum(IN_SPLITS)[:-1]]
    xc = ctx
    for l in range(DEPTH):
        need_ctx = l < DEPTH - 1
        mod = sc @ w_ada[l] + b_ada[l]
        modc = scc @ w_ada[l] + b_ada[l]
        sh1, s1, g1, sh2, s2, g2 = jnp.split(mod[:, None, :], 6, axis=-1)
        sh1c, s1c, g1c, sh2c, s2c, g2c = jnp.split(modc, 6, axis=-1)
        h = rmsnorm(x, norm1_g[l]) * (1 + s1) + sh1
        hc = rmsnorm(xc, norm1_g[l]) * (1 + s1c) + sh1c
        p = jnp.split(h @ w_in[l], split_at, axis=-1)
        pc = jnp.split(hc @ w_in[l], split_at, axis=-1)
        o_a, oc_a = hgrn_mixer(p[0], p[1], p[2], p[3], p[4], pc[0], pc[1], pc[2], pc[3], pc[4],
                               lower_bounds[l], hgrn_norm_g[l], need_ctx)
        hy_args = (hy_short_w[l], hy_short_b[l], hy_w1[l], hy_b1[l], hy_freq[l], hy_w2[l], hy_b2[l],
                   hy_w3[l], hy_b3[l], hy_decay[l], hy_bias[l])
        o_b = hyena_mixer(p[5], *hy_args)
        o_c, oc_c = na_mixer(p[6], pc[6], na_rpb[l], na_q_g[l], na_k_g[l], need_ctx)
        o_d, oc_d = mla_mixer(p[7], p[8], p[9], pc[7], pc[8], pc[9], mla_q_a_g[l], mla_kv_a_g[l],
                              mla_w_uq[l], mla_w_ukv[l], mla_q_g[l], mla_k_g[l], need_ctx)
        x = x + g1 * (jnp.concatenate([o_a, o_b, o_c, o_d], axis=-1) @ w_out[l])
        h2 = rmsnorm(x, norm2_g[l]) * (1 + s2) + sh2
        moe_args = (moe_wg[l], moe_bg[l], moe_we[l], moe_be[l], moe_w_gate[l], moe_w_up[l], moe_w_down[l])
        if need_ctx:
            oc_b = hyena_mixer(pc[5], *hy_args)
            xc = xc + g1c * (jnp.concatenate([oc_a, oc_b, oc_c, oc_d], axis=-1) @ w_out[l])
            h2c = rmsnorm(xc, norm2_g[l]) * (1 + s2c) + sh2c
            y = moe_ffn(jnp.concatenate([h2.reshape(-1, D), h2c.reshape(-1, D)], axis=0), *moe_args)
            x = x + g2 * y[:B * L].reshape(B, L, D)
            xc = xc + g2c * y[B * L:].reshape(xc.shape)
        else:
            x = x + g2 * moe_ffn(h2.reshape(-1, D), *moe_args).reshape(B, L, D)
    return x
```

```python
import contextlib
import numpy as np
import concourse.bass as bass
import concourse.mybir as mybir

F32 = mybir.dt.float32
F32R = mybir.dt.float32r
I32 = mybir.dt.int32
U32 = mybir.dt.uint32
ALU = mybir.AluOpType
AF = mybir.ActivationFunctionType
AX = mybir.AxisListType

SEM_ROT = 30000


class MK:
    def __init__(self, nc):
        self.nc = nc
        self.root = contextlib.ExitStack()
        self.stack = [self.root]
        self.eng = {"pe": nc.tensor, "dve": nc.vector, "act": nc.scalar,
                    "pool": nc.gpsimd, "sp": nc.sync}
        self.esem = {}
        self.ecnt = {}
        self.nsem = 0
        for k in self.eng:
            self.esem[k] = self._newsem("e_" + k)
            self.ecnt[k] = 0
        self.waited = {}
        self.ts = {}
        self.uid = 0
        self.dq = {}
        for q, n in (("sp", 10), ("pool", 6), ("act", 4)):
            self.dq[q] = {"sems": [[self._newsem("d_%s%d" % (q, i)), 0] for i in range(n)], "i": 0}
        self.ninstr = 0

    def _newsem(self, name):
        self.nsem += 1
        return self.root.enter_context(self.nc.semaphore("%s_%d" % (name, self.nsem)))

    def _nm(self, name):
        self.uid += 1
        return "%s_%d" % (name, self.uid)

    def sb(self, name, shape, dtype=F32):
        return self.stack[-1].enter_context(self.nc.sbuf_tensor(self._nm(name), list(shape), dtype))

    def ps(self, name, shape, dtype=F32):
        return self.stack[-1].enter_context(self.nc.psum_tensor(self._nm(name), list(shape), dtype))

    def dram(self, name, shape, dtype=F32, kind="Internal"):
        return self.nc.dram_tensor(name, list(shape), dtype, kind=kind)

    @contextlib.contextmanager
    def scope(self):
        es = contextlib.ExitStack()
        self.stack.append(es)
        try:
            yield
        finally:
            self.barrier()
            self.stack.pop()
            es.close()

    def _wait(self, en, ev):
        if ev is None:
            return
        sem, val = ev
        if en == "pe" and sem.name.startswith("e_pe"):
            return
        key = (en, sem.name if hasattr(sem, "name") else id(sem))
        if self.waited.get(key, 0) >= val:
            return
        self.waited[key] = val
        self.eng[en].wait_ge(sem, val)

    def _tn(self, x):
        if isinstance(x, str):
            return x
        if hasattr(x, "tensor"):
            return x.tensor.name
        return x.name

    def _region(self, x):
        if isinstance(x, str) or not hasattr(x, "tensor"):
            return (self._tn(x), None)
        t = x.tensor
        name = t.name
        try:
            apl = [(int(a[0]), int(a[1])) for a in x.ap]
            off = int(x.offset)
            if any(st < 0 for st, _ in apl):
                return (name, None)
            if type(t).__name__ == "PSumTensorHandle":
                return (name, None)
            if type(t).__name__ == "DRamTensorHandle":
                ext = sum((n - 1) * st for st, n in apl)
                return (name, (0, 0, off, off + ext))
            row = 1
            for d in list(t.shape)[1:]:
                row *= int(d)
            pst, pn = apl[0]
            if pst != row:
                return (name, None)
            p0 = off // row
            f0 = off % row
            ext = sum((n - 1) * st for st, n in apl[1:])
            return (name, (p0, p0 + pn - 1, f0, f0 + ext))
        except Exception:
            return (name, None)

    @staticmethod
    def _ovl(a, b):
        if a is None or b is None:
            return True
        return not (a[1] < b[0] or b[1] < a[0] or a[3] < b[2] or b[3] < a[2])

    @staticmethod
    def _contains(a, b):
        if a is None:
            return True
        if b is None:
            return False
        return a[0] <= b[0] and a[1] >= b[1] and a[2] <= b[2] and a[3] >= b[3]

    def _deps(self, en, reads, writes):
        for r in reads:
            name, box = self._region(r)
            st = self.ts.get(name)
            if st is not None:
                for b, ev in st["w"]:
                    if self._ovl(box, b):
                        self._wait(en, ev)
        for w in writes:
            name, box = self._region(w)
            st = self.ts.get(name)
            if st is not None:
                for b, ev in st["w"]:
                    if self._ovl(box, b):
                        self._wait(en, ev)
                for b, ev in st["r"]:
                    if self._ovl(box, b):
                        self._wait(en, ev)

    @staticmethod
    def _compress(lst):
        d = {}
        for b, (s, v) in lst:
            k = id(s)
            if k not in d or d[k][1] < v:
                d[k] = (s, v)
        return [(None, ev) for ev in d.values()]

    def _record(self, ev, reads, writes):
        for r in reads:
            name, box = self._region(r)
            st = self.ts.setdefault(name, {"w": [], "r": []})
            st["r"].append((box, ev))
            if len(st["r"]) > 40:
                st["r"] = self._compress(st["r"])
        for w in writes:
            name, box = self._region(w)
            st = self.ts.setdefault(name, {"w": [], "r": []})
            st["w"] = [(b, e) for b, e in st["w"] if not self._contains(box, b)]
            st["r"] = [(b, e) for b, e in st["r"] if not self._contains(box, b)]
            st["w"].append((box, ev))
            if len(st["w"]) > 40:
                st["w"] = self._compress(st["w"])

    def op(self, en, fn, reads=(), writes=()):
        reads = [r for r in reads if r is not None and not isinstance(r, (int, float))]
        writes = [w for w in writes if w is not None]
        self._deps(en, reads, writes)
        ins = fn(self.eng[en])
        if self.ecnt[en] >= SEM_ROT:
            self.esem[en] = self._newsem("e_" + en)
            self.ecnt[en] = 0
        self.ecnt[en] += 1
        ins.then_inc(self.esem[en], 1)
        ev = (self.esem[en], self.ecnt[en])
        self._record(ev, reads, writes)
        self.ninstr += 1
        return ev

    def dma(self, q, out, in_, fn=None, extra_reads=(), **kw):
        dq = self.dq[q]
        slot = dq["sems"][dq["i"] % len(dq["sems"])]
        dq["i"] += 1
        sem, cnt = slot
        if cnt > 0:
            self._wait(q, (sem, cnt))
        reads = [in_] + list(extra_reads)
        writes = [out]
        self._deps(q, reads, writes)
        if fn is None:
            ins = self.eng[q].dma_start(out=out, in_=in_, **kw)
        else:
            ins = fn(self.eng[q])
        slot[1] = cnt + 16
        ins.then_inc(sem, 16)
        ev = (sem, slot[1])
        self._record(ev, reads, writes)
        self.ninstr += 1
        return ev

    def barrier(self):
        evs = [(self.esem[k], self.ecnt[k]) for k in self.eng if self.ecnt[k] > 0]
        for q in self.dq.values():
            for sem, cnt in q["sems"]:
                if cnt > 0:
                    evs.append((sem, cnt))
        for en in self.eng:
            for ev in evs:
                self._wait(en, ev)
        self.ts = {}

    def finish(self):
        self.barrier()
        self.root.close()

    def mm(self, out, lhsT, rhs, start=True, stop=True):
        return self.op("pe", lambda e: e.matmul(out, lhsT=lhsT, rhs=rhs, start=start, stop=stop),
                       reads=[lhsT, rhs], writes=[out])

    def tr(self, out, in_, ident):
        return self.op("pe", lambda e: e.transpose(out, in_, ident), reads=[in_, ident], writes=[out])

    def act(self, out, in_, func, bias=None, scale=1.0, accum_out=None, en="act"):
        kw = {}
        if bias is not None:
            kw["bias"] = bias
        if accum_out is not None:
            kw["accum_out"] = accum_out
        rd = [in_]
        if isinstance(bias, bass.AP):
            rd.append(bias)
        if isinstance(scale, bass.AP):
            rd.append(scale)
        return self.op("act", lambda e: e.activation(out=out, in_=in_, func=func, scale=scale, **kw),
                       reads=rd, writes=[out, accum_out])

    def tt(self, out, in0, in1, op, en="dve"):
        return self.op(en, lambda e: e.tensor_tensor(out=out, in0=in0, in1=in1, op=op),
                       reads=[in0, in1], writes=[out])

    def tsc(self, out, in0, s1, op0, s2=None, op1=None, accum_out=None, en="dve"):
        kw = {}
        if op1 is not None:
            kw["op1"] = op1
        if accum_out is not None:
            kw["accum_out"] = accum_out
        rd = [in0] + [s for s in (s1, s2) if isinstance(s, bass.AP)]
        return self.op(en, lambda e: e.tensor_scalar(out=out, in0=in0, scalar1=s1, scalar2=s2, op0=op0, **kw),
                       reads=rd, writes=[out, accum_out])

    def stt(self, out, in0, scalar, in1, op0, op1):
        rd = [in0, in1] + ([scalar] if isinstance(scalar, bass.AP) else [])
        return self.op("dve", lambda e: e.scalar_tensor_tensor(out=out, in0=in0, scalar=scalar, in1=in1,
                                                               op0=op0, op1=op1),
                       reads=rd, writes=[out])

    def copy(self, out, in_, en="dve"):
        if en == "act":
            return self.op("act", lambda e: e.copy(out=out, in_=in_), reads=[in_], writes=[out])
        return self.op(en, lambda e: e.tensor_copy(out=out, in_=in_), reads=[in_], writes=[out])

    def memset(self, out, val, en="dve"):
        return self.op(en, lambda e: e.memset(out, val), reads=[], writes=[out])

    def recip(self, out, in_):
        return self.op("dve", lambda e: e.reciprocal(out=out, in_=in_), reads=[in_], writes=[out])

    def reduce(self, out, in_, op, axis=AX.X):
        return self.op("dve", lambda e: e.tensor_reduce(out=out, in_=in_, op=op, axis=axis),
                       reads=[in_], writes=[out])


T_LAT = 2048
T_CTX = 256
T_ALL = 2304
NT = 18
D = 1024
D_IN = 3232
DEPTH = 4
TB = [(0, 512), (512, 512), (1024, 512), (1536, 512), (2048, 256)]
EPS = 1e-6


class Prog:
    def __init__(self, debug=(), depth=DEPTH, stop=None, phases=("front", "hgrn", "hyena", "na", "mla", "outproj", "moe")):
        self.phases = phases
        self.nc = bass.Bass("TRN2", target_bir_lowering=False)
        self.mk = MK(self.nc)
        self.debug = set(debug)
        self.depth = depth
        self.stop = stop
        self.inputs = {}
        self.outs = []
        self._evi = 0

    def din(self, name, shape, dtype=F32):
        t = self.nc.dram_tensor(name, list(shape), dtype, kind="ExternalInput")
        self.inputs[name] = (tuple(shape), dtype)
        return t

    def dscr(self, name, shape, dtype=F32):
        if ("in:" + name) in self.debug:
            return self.din(name, shape, dtype)
        if name in self.debug:
            self.outs.append(name)
            return self.nc.dram_tensor(name, list(shape), dtype, kind="ExternalOutput")
        return self.nc.dram_tensor(name, list(shape), dtype, kind="Internal")

    def dout(self, name, shape, dtype=F32):
        self.outs.append(name)
        return self.nc.dram_tensor(name, list(shape), dtype, kind="ExternalOutput")

    def evac(self, out, in_):
        self._evi += 1
        if self._evi % 2:
            return self.mk.copy(out, in_, en="dve")
        return self.mk.copy(out, in_, en="act")

    def declare(self):
        p = self
        L = DEPTH
        p.xin = p.din("xin", [T_ALL, D])
        p.cvecT = p.din("cvecT", [D, 2])
        p.w_ada = p.din("w_ada", [L, D, 6 * D])
        p.b_adaT = p.din("b_adaT", [L, 128, 48])
        p.n1g = p.din("n1g", [L, 128, 8])
        p.n2g = p.din("n2g", [L, 128, 8])
        p.w_in = p.din("w_in", [L, D, D_IN])
        p.w_out = p.din("w_out", [L, D, D])
        p.ident = p.din("ident", [128, 128])
        p.xT = p.dscr("xT", [D, T_ALL])
        p.ptok = p.dscr("ptok", [T_ALL, D_IN])
        p.uT = p.dscr("uT", [768, T_ALL])
        p.catT = p.dscr("catT", [D, T_ALL])
        p.h2T = p.dscr("h2T", [D, T_ALL])
        p.hof = p.dscr("hof", [T_ALL, 256])
        p.Xb = p.dscr("Xb", [NSLOT + 128, D])
        p.Yb = p.dscr("Yb", [NSLOT + 128, D])
        p.Ksp = p.dscr("Ksp", [2, 2, 2048, 256])
        p.Kspc = p.dscr("Kspc", [2, 2, 256, 256])
        p.gtok = p.dscr("gtok", [2, T_ALL, 256])
        p.xout = p.dout("xout", [T_LAT, D])
        for nm, shp in (("hgrn_lb", [L, 2, 256]), ("hgrn_norm_g", [L, 64]), ("hy_swT", [L, 128, 6, 3]), ("hy_sbT", [L, 128, 6]),
                        ("hy_w1", [L, 33, 64]), ("hy_b1", [L, 64]), ("hy_freq", [L, 64]), ("hy_w2", [L, 64, 64]),
                        ("hy_b2", [L, 64]), ("hy_w3", [L, 64, 1024]), ("hy_b3", [L, 1024]), ("hy_decay", [L, 2, 2, 256]),
                        ("hy_bias", [L, 2, 256]), ("na_biasT", [L, 4, 128, 5, 5, 128]), ("na_q_g", [L, 64]), ("na_k_g", [L, 64]),
                        ("mla_q_a_g", [L, 256]), ("mla_kv_a_g", [L, 128]), ("mla_w_uq", [L, 256, 384]), ("mla_w_ukv", [L, 128, 512]),
                        ("mla_q_g", [L, 96]), ("mla_k_g", [L, 96]), ("moe_wg", [L, D, 4]), ("moe_bg", [L, 4]),
                        ("moe_we", [L, D, 32]), ("moe_be", [L, 32]), ("moe_w_gate", [L, 32, D, 512]),
                        ("moe_w_up", [L, 32, D, 512]), ("moe_w_down", [L, 32, 512, D]),
                        ("c_lstrict", [128, 128]), ("c_iotaE", [128, 32]), ("c_trif", [64, 64]), ("c_trib", [64, 64]),
                        ("c_exch", [128, 128]), ("c_ropecos", [128, 16, 32]), ("c_ropesin", [128, 16, 32]),
                        ("c_feats", [2, 33, 2048]), ("c_ntun", [128, 16]), ("c_featsc", [2, 33, 256]), ("c_ntunc", [128, 2]),
                        ("c_dftB", [16, 128, 3, 16, 128]), ("c_dftBc", [2, 128, 3, 2, 128])):
            setattr(p, nm, p.din(nm, shp))

    def build(self):
        p, mk = self, self.mk
        p.declare()
        p.identS = mk.sb("ident", [128, 128])
        mk.dma("sp", p.identS[:], p.ident[:, :])
        p.onesF = mk.sb("onesF", [128, 128])
        mk.memset(p.onesF[:], 1.0)
        p.onesR = mk.sb("onesR", [128, 128], F32R)
        mk.copy(p.onesR[:], p.onesF[:])
        p.psum = [mk.ps("ps%d" % i, [128, 512]) for i in range(8)]
        p.breg = p.nc.gpsimd.to_reg(NSLOT - 1)
        p.load_x()
        phases = p.phases
        if "moe" in phases:
            p.moe_init()
        for l in range(p.depth):
            if "front" in phases:
                p.front(l)
            else:
                p.mod = p.modulation(l)
            p.bg_hgrn = False
            for ph in ("hgrn", "hyena", "na", "mla", "outproj", "moe"):
                if ph == "hgrn" and p.bg_hgrn:
                    continue
                if ph in phases:
                    getattr(p, ph)(l)
        p.store_x()
        mk.finish()

    def load_x(self):
        p, mk = self, self.mk
        with mk.scope():
            xt = [mk.sb("xt%d" % i, [128, D]) for i in range(2)]
            xTt = mk.sb("xTt", [128, 8, 512])
            xTv = p.xT.ap().rearrange("(k q) t -> q k t", q=128)
            for (t0, w) in TB:
                for j in range(w // 128):
                    n = t0 // 128 + j
                    xb = xt[n % 2]
                    mk.dma("sp", xb[:], p.xin[n * 128:(n + 1) * 128, :])
                    for k in range(8):
                        ps = p.psum[k % 8]
                        mk.tr(ps[:, 0:128], xb[:, k * 128:(k + 1) * 128], p.identS[:])
                        p.evac(xTt[:, k, j * 128:(j + 1) * 128], ps[:, 0:128])
                mk.dma("sp", xTv[:, :, t0:t0 + w], xTt[:, :, 0:w])

    def store_x(self):
        p, mk = self, self.mk
        with mk.scope():
            xTt = mk.sb("xTt", [128, 8, 512])
            xo = [mk.sb("xo%d" % i, [128, D]) for i in range(2)]
            xTv = p.xT.ap().rearrange("(k q) t -> q k t", q=128)
            for (t0, w) in TB[:4]:
                mk.dma("sp", xTt[:, :, 0:w], xTv[:, :, t0:t0 + w])
                for j in range(w // 128):
                    n = t0 // 128 + j
                    xb = xo[n % 2]
                    for k in range(8):
                        ps = p.psum[k % 8]
                        mk.tr(ps[:, 0:128], xTt[:, k, j * 128:(j + 1) * 128], p.identS[:])
                        p.evac(xb[:, k * 128:(k + 1) * 128], ps[:, 0:128])
                    mk.dma("sp", p.xout[n * 128:(n + 1) * 128, :], xb[:])

    def modulation(self, l):
        p, mk = self, self.mk
        mod = mk.sb("mod%d" % l, [128, 48, 2])
        with mk.scope():
            cT = mk.sb("cT", [128, 8, 2])
            mk.dma("sp", cT[:], p.cvecT.ap().rearrange("(k q) r -> q k r", q=128))
            scT = mk.sb("scT", [128, 8, 2])
            mk.act(scT[:], cT[:], AF.Silu)
            bT = mk.sb("bT", [128, 48])
            mk.dma("sp", bT[:], p.b_adaT[l, :, :])
            wv = p.w_ada[l, :, :].rearrange("(k q) n -> q k n", q=128)
            wb = [mk.sb("wada%d" % i, [128, 8, 512]) for i in range(2)]
            ps = p.psum[0]
            psv = ps[:, 0:96].rearrange("q (j r) -> q j r", r=2)
            for jb in range(12):
                w = wb[jb % 2]
                mk.dma("sp" if jb % 2 == 0 else "pool", w[:], wv[:, :, jb * 512:(jb + 1) * 512])
                for jj in range(4):
                    j = jb * 4 + jj
                    for k in range(8):
                        mk.mm(psv[:, j, :], lhsT=w[:, k, jj * 128:(jj + 1) * 128], rhs=scT[:, k, :],
                              start=(k == 0), stop=(k == 7))
            for r in range(2):
                mk.tt(mod[:, :, r], psv[:, :, r], bT[:], ALU.add)
        return mod

    def front(self, l):
        p, mk = self, self.mk
        mod = p.modulation(l)
        p.mod = mod
        with mk.scope():
            g1n = mk.sb("g1n", [128, 8])
            mk.dma("sp", g1n[:], p.n1g[l, :, :])
            A1 = mk.sb("A1", [128, 8, 2])
            for r in range(2):
                mk.stt(A1[:, :, r], mod[:, 8:16, r], 1.0, g1n[:], ALU.add, ALU.mult)
            hT = mk.sb("hT", [128, 8, T_ALL], F32R)
            p.norm_mod(p.xT, A1, mod[:, 0:8, :], hT)
            p.inproj(l, hT)

    def norm_mod(self, src, A, B, hT, dst_dram=None):
        p, mk = self, self.mk
        with mk.scope():
            xb = [mk.sb("nx%d" % i, [128, 8, 512]) for i in range(2)]
            sq = mk.sb("nsq", [128, 8, 512], F32R)
            rstd = mk.sb("rstd", [128, 512])
            tmp = [mk.sb("ntmp%d" % i, [128, 512]) for i in range(2)]
            xv = src.ap().rearrange("(k q) t -> q k t", q=128)
            for bi, (t0, w) in enumerate(TB):
                r = 0 if t0 < T_LAT else 1
                x = xb[bi % 2]
                mk.dma("sp", x[:, :, 0:w], xv[:, :, t0:t0 + w])
                mk.act(sq[:, :, 0:w], x[:, :, 0:w], AF.Square)
                ps = p.psum[bi % 2]
                for k in range(8):
                    mk.mm(ps[:, 0:w], lhsT=p.onesR[:], rhs=sq[:, k, 0:w], start=(k == 0), stop=(k == 7))
                mk.act(rstd[:, 0:w], ps[:, 0:w], AF.Sqrt, bias=EPS, scale=1.0 / D)
                mk.recip(rstd[:, 0:w], rstd[:, 0:w])
                for k in range(8):
                    t = tmp[k % 2]
                    mk.tt(t[:, 0:w], x[:, k, 0:w], rstd[:, 0:w], ALU.mult)
                    mk.act(hT[:, k, t0:t0 + w], t[:, 0:w], AF.Identity, bias=B[:, k, r:r + 1], scale=A[:, k, r:r + 1])

    def inproj(self, l, hT):
        p, mk = self, self.mk
        with mk.scope():
            wv = p.w_in[l, :, :].rearrange("(k q) n -> q k n", q=128)
            wst = [mk.sb("wst%d" % i, [128, 8, 512]) for i in range(2)]
            wr = [mk.sb("wr%d" % i, [128, 8, 512], F32R) for i in range(2)]
            ob = [mk.sb("ob%d" % i, [128, 512]) for i in range(3)]
            blocks = [("tok", 0, 512), ("tok", 512, 512), ("tok", 1024, 256)]
            blocks += [("feat", 1280 + 128 * i, 128) for i in range(6)]
            blocks += [("tok", 2048, 512), ("tok", 2560, 512), ("tok", 3072, 160)]
            oi = 0
            for bi, (kind, c0, cw) in enumerate(blocks):
                ws, w = wst[bi % 2], wr[bi % 2]
                mk.dma("sp" if bi % 2 == 0 else "pool", ws[:, :, 0:cw], wv[:, :, c0:c0 + cw])
                mk.copy(w[:, :, 0:cw], ws[:, :, 0:cw], en="pool")
                if kind == "tok":
                    for n in range(NT):
                        ps = p.psum[n % 4]
                        for k in range(8):
                            mk.mm(ps[:, 0:cw], lhsT=hT[:, k, n * 128:(n + 1) * 128], rhs=w[:, k, 0:cw],
                                  start=(k == 0), stop=(k == 7))
                        o = ob[oi % 3]
                        oi += 1
                        p.evac(o[:, 0:cw], ps[:, 0:cw])
                        mk.dma("sp", p.ptok[n * 128:(n + 1) * 128, c0:c0 + cw], o[:, 0:cw])
                else:
                    f0 = c0 - 1280
                    for ti, (t0, tw) in enumerate(TB):
                        ps = p.psum[4 + ti % 4]
                        for k in range(8):
                            mk.mm(ps[:, 0:tw], lhsT=w[:, k, 0:128], rhs=hT[:, k, t0:t0 + tw],
                                  start=(k == 0), stop=(k == 7))
                        o = ob[oi % 3]
                        oi += 1
                        p.evac(o[:, 0:tw], ps[:, 0:tw])
                        mk.dma("sp", p.uT[f0:f0 + 128, t0:t0 + tw], o[:, 0:tw])


CAP = 384
NSLOT = 32 * CAP
BIGIDX = 1.0e6


def _outproj(self, l):
    p, mk = self, self.mk
    mod = p.mod
    with mk.scope():
        wv = p.w_out[l, :, :].rearrange("(k q) n -> q k n", q=128)
        wst = mk.sb("wost", [128, 8, 512])
        wR = mk.sb("woR", [128, 8, 1024], F32R)
        for hf in range(2):
            mk.dma("sp", wst[:], wv[:, :, hf * 512:(hf + 1) * 512])
            mk.copy(wR[:, :, hf * 512:(hf + 1) * 512], wst[:], en="pool")
        cst = [mk.sb("cst%d" % i, [128, 8, 512]) for i in range(2)]
        cR = mk.sb("cR", [128, 8, 512], F32R)
        xb = [mk.sb("oxb%d" % i, [128, 8, 512]) for i in range(2)]
        cv = p.catT.ap().rearrange("(k q) t -> q k t", q=128)
        xv = p.xT.ap().rearrange("(k q) t -> q k t", q=128)
        for bi, (t0, w) in enumerate(TB):
            r = 0 if t0 < T_LAT else 1
            c, x = cst[bi % 2], xb[bi % 2]
            mk.dma("sp", c[:, :, 0:w], cv[:, :, t0:t0 + w])
            mk.dma("pool", x[:, :, 0:w], xv[:, :, t0:t0 + w])
            mk.copy(cR[:, :, 0:w], c[:, :, 0:w], en="act")
            for j in range(8):
                ps = p.psum[j % 4]
                for k in range(8):
                    mk.mm(ps[:, 0:w], lhsT=wR[:, k, j * 128:(j + 1) * 128], rhs=cR[:, k, 0:w],
                          start=(k == 0), stop=(k == 7))
                mk.stt(x[:, j, 0:w], ps[:, 0:w], mod[:, 16 + j, r:r + 1], x[:, j, 0:w], ALU.mult, ALU.add)
            mk.dma("sp", xv[:, :, t0:t0 + w], x[:, :, 0:w])


def _moe(self, l):
    p, mk = self, self.mk
    mod = p.mod
    with mk.scope():
        rt = mk.sb("rt", [128, NT, 8])
        gidx = mk.sb("gidx", [128, NT, 2], I32)
        with mk.scope():
            g2n = mk.sb("g2n", [128, 8])
            mk.dma("sp", g2n[:], p.n2g[l, :, :])
            A2 = mk.sb("A2", [128, 8, 2])
            for r in range(2):
                mk.stt(A2[:, :, r], mod[:, 32:40, r], 1.0, g2n[:], ALU.add, ALU.mult)
            B2 = mod[:, 24:32, :]
            wrt = mk.sb("wrt", [128, 8, 36])
            mk.dma("sp", wrt[:, :, 0:4], p.moe_wg[l, :, :].rearrange("(k q) n -> q k n", q=128))
            mk.dma("sp", wrt[:, :, 4:36], p.moe_we[l, :, :].rearrange("(k q) n -> q k n", q=128))
            bias = mk.sb("rbias", [128, 36])
            mk.dma("sp", bias[:, 0:4], p.moe_bg[l, :].partition_broadcast(128))
            mk.dma("sp", bias[:, 4:36], p.moe_be[l, :].partition_broadcast(128))
            lstrict = mk.sb("lstrict", [128, 128])
            mk.dma("sp", lstrict[:], p.c_lstrict[:, :])
            iotaE = mk.sb("iotaE", [128, 32])
            mk.dma("sp", iotaE[:], p.c_iotaE[:, :])
            xb = [mk.sb("mx%d" % i, [128, 8, 512]) for i in range(2)]
            sq = mk.sb("msq", [128, 8, 512], F32R)
            rstd = mk.sb("mrstd", [128, 512])
            hb = mk.sb("mhb", [128, 8, 512])
            htok = mk.sb("htok", [128, NT, D])
            lr = mk.sb("r_lr", [128, NT, 36])
            xv = p.xT.ap().rearrange("(k q) t -> q k t", q=128)
            for bi, (t0, w) in enumerate(TB):
                r = 0 if t0 < T_LAT else 1
                x = xb[bi % 2]
                mk.dma("sp", x[:, :, 0:w], xv[:, :, t0:t0 + w])
                mk.act(sq[:, :, 0:w], x[:, :, 0:w], AF.Square)
                ps = p.psum[0]
                for k in range(8):
                    mk.mm(ps[:, 0:w], lhsT=p.onesR[:], rhs=sq[:, k, 0:w], start=(k == 0), stop=(k == 7))
                mk.act(rstd[:, 0:w], ps[:, 0:w], AF.Sqrt, bias=EPS, scale=1.0 / D)
                mk.recip(rstd[:, 0:w], rstd[:, 0:w])
                for k in range(8):
                    mk.tt(hb[:, k, 0:w], x[:, k, 0:w], rstd[:, 0:w], ALU.mult)
                    mk.act(hb[:, k, 0:w], hb[:, k, 0:w], AF.Identity, bias=B2[:, k, r:r + 1], scale=A2[:, k, r:r + 1])
                for j in range(w // 128):
                    n = t0 // 128 + j
                    sl = slice(j * 128, (j + 1) * 128)
                    pl = p.psum[1]
                    for k in range(8):
                        mk.mm(pl[:, 0:36], lhsT=hb[:, k, sl], rhs=wrt[:, k, :], start=(k == 0), stop=(k == 7))
                    mk.tt(lr[:, n, :], pl[:, 0:36], bias[:], ALU.add)
                    for k in range(8):
                        pt = p.psum[2 + k % 4]
                        mk.tr(pt[:, 0:128], hb[:, k, sl], p.identS[:])
                        p.evac(htok[:, n, k * 128:(k + 1) * 128], pt[:, 0:128])
            R = lambda nm, shp: mk.sb("r_" + nm, [128] + shp)
            gmax, gsum, psel = R("gmax", [NT]), R("gsum", [NT]), R("psel", [NT])
            eg, ohg, pen = R("eg", [NT, 4]), R("ohg", [NT, 4]), R("pen", [NT, 4])
            lem, lem2 = R("lem", [NT, 32]), R("lem2", [NT, 32])
            m1, m2, dm, ee, w1 = R("m1", [NT]), R("m2", [NT]), R("dm", [NT]), R("e", [NT]), R("w1", [NT])
            oh1, oh2, oh, cnt, t32 = R("oh1", [NT, 32]), R("oh2", [NT, 32]), R("oh", [NT, 32]), R("cnt", [NT, 32]), R("t32", [NT, 32])
            s_, f__, gs = R("s", [NT, 2]), R("f", [NT, 2]), R("gs", [NT, 2])
            bc3 = lambda a, k: a[:].unsqueeze(2).to_broadcast([128, NT, k])
            lg4 = lr[:, :, 0:4]
            mk.reduce(gmax[:], lg4, ALU.max)
            mk.tt(eg[:], lg4, bc3(gmax, 4), ALU.subtract)
            mk.act(eg[:], eg[:], AF.Exp)
            mk.reduce(gsum[:], eg[:], ALU.add)
            mk.recip(psel[:], gsum[:])
            mk.tt(ohg[:], lg4, bc3(gmax, 4), ALU.is_equal)
            mk.tsc(pen[:], ohg[:], 1.0e9, ALU.mult, -1.0e9, ALU.add)
            mk.tt(lem[:].rearrange("q n (g e) -> q n g e", g=4), lr[:, :, 4:36].rearrange("q n (g e) -> q n g e", g=4),
                  pen[:].unsqueeze(3).to_broadcast([128, NT, 4, 8]), ALU.add)
            mk.reduce(m1[:], lem[:], ALU.max)
            mk.tt(oh1[:], lem[:], bc3(m1, 32), ALU.is_equal)
            mk.stt(lem2[:], oh1[:], -2.0e9, lem[:], ALU.mult, ALU.add)
            mk.reduce(m2[:], lem2[:], ALU.max)
            mk.tt(oh2[:], lem2[:], bc3(m2, 32), ALU.is_equal)
            mk.tt(oh[:], oh1[:], oh2[:], ALU.add)
            mk.tt(dm[:], m2[:], m1[:], ALU.subtract)
            mk.act(ee[:], dm[:], AF.Exp)
            mk.tsc(w1[:], ee[:], 1.0, ALU.add)
            mk.recip(w1[:], w1[:])
            mk.tt(rt[:, :, 0], w1[:], psel[:], ALU.mult)
            mk.tt(rt[:, :, 1], rt[:, :, 0], ee[:], ALU.mult)
            pcs = [p.psum[6], p.psum[7]]
            for n in range(NT):
                pc = pcs[n // 9][:, (n % 9) * 32:(n % 9 + 1) * 32]
                mk.mm(pc, lhsT=lstrict[:], rhs=oh[:, n, :], start=True, stop=(n == 0))
                for m_ in range(n):
                    mk.mm(pc, lhsT=p.onesF[:], rhs=oh[:, m_, :], start=False, stop=(m_ == n - 1))
            pos = R("pos", [NT, 32])
            for hf in range(2):
                mk.copy(pos[:, hf * 9:(hf + 1) * 9, :].rearrange("q n e -> q (n e)"), pcs[hf][:, 0:288])
            mk.tt(cnt[:], pos[:], iotaE[:].unsqueeze(1).to_broadcast([128, NT, 32]), ALU.add)
            for kk, ohk in enumerate((oh1, oh2)):
                mk.tt(t32[:], ohk[:], cnt[:], ALU.mult)
                mk.reduce(s_[:, :, kk], t32[:], ALU.add)
                mk.tt(t32[:], ohk[:], pos[:], ALU.mult)
                mk.reduce(f__[:, :, kk], t32[:], ALU.add)
            mk.tsc(f__[:], f__[:], float(CAP) - 0.5, ALU.is_gt, BIGIDX, ALU.mult)
            mk.tt(gs[:], s_[:], f__[:], ALU.add)
            sidx = mk.sb("sidx", [128, NT, 2], I32)
            mk.copy(sidx[:], gs[:])
            mk.tsc(gs[:], gs[:], float(NSLOT), ALU.min)
            mk.copy(gidx[:], gs[:])
            for n in range(NT):
                for kk in range(2):
                    mk.dma("pool", p.Xb[:, :], htok[:, n, :],
                           fn=lambda e, kk=kk, n=n: e.indirect_dma_start(
                               out=p.Xb[:, :], out_offset=bass.IndirectOffsetOnAxis(ap=sidx[:, n, kk:kk + 1], axis=0),
                               in_=htok[:, n, :], in_offset=None, bounds_check=p.breg, oob_is_err=False),
                           extra_reads=[sidx])
        if getattr(p, 'moe_stop', 3) < 2:
            return
        with mk.scope():
            W = [{"g": mk.sb("WgR%d" % i, [128, 8, 512], F32R), "u": mk.sb("WuR%d" % i, [128, 8, 512], F32R),
                  "d": mk.sb("WdR%d" % i, [128, 4, 1024], F32R)} for i in range(2)]
            NS = CAP // 128
            xtok = [mk.sb("extok%d" % i, [128, D]) for i in range(2 * NS)]
            xbT = [mk.sb("xbT%d" % i, [128, 8, CAP], F32R) for i in range(2)]
            hidT = mk.sb("hidT", [128, 4, CAP], F32R)
            silT = [mk.sb("silT%d" % i, [128, 512]) for i in range(2)]
            yt = [mk.sb("eyt%d" % i, [128, D]) for i in range(3)]
            yi = 0

            def load_w(e):
                w = W[e % 2]
                mk.dma("pool", w["g"][:], p.moe_w_gate[l, e, :, :].rearrange("(k q) n -> q k n", q=128))
                mk.dma("pool", w["u"][:], p.moe_w_up[l, e, :, :].rearrange("(k q) n -> q k n", q=128))
                mk.dma("pool", w["d"][:], p.moe_w_down[l, e, :, :].rearrange("(k q) n -> q k n", q=128))

            def load_x(e):
                for si_ in range(NS):
                    r0 = e * CAP + si_ * 128
                    mk.dma("sp", xtok[(e % 2) * NS + si_][:], p.Xb[r0:r0 + 128, :])

            load_w(0)
            load_x(0)
            for e in range(32):
                if e + 1 < 32:
                    load_w(e + 1)
                    load_x(e + 1)
                w = W[e % 2]
                xT_ = xbT[e % 2]
                for si_ in range(NS):
                    xt = xtok[(e % 2) * NS + si_]
                    for k in range(8):
                        pt = p.psum[k % 4]
                        mk.tr(pt[:, 0:128], xt[:, k * 128:(k + 1) * 128], p.identS[:])
                        p.evac(xT_[:, k, si_ * 128:(si_ + 1) * 128], pt[:, 0:128])
                for si_ in range(NS):
                    pg, pu = p.psum[4 + (si_ % 2) * 2], p.psum[5 + (si_ % 2) * 2]
                    for k in range(8):
                        mk.mm(pg[:, 0:512], lhsT=xT_[:, k, si_ * 128:(si_ + 1) * 128], rhs=w["g"][:, k, :], start=(k == 0), stop=(k == 7))
                    for k in range(8):
                        mk.mm(pu[:, 0:512], lhsT=xT_[:, k, si_ * 128:(si_ + 1) * 128], rhs=w["u"][:, k, :], start=(k == 0), stop=(k == 7))
                    sl_ = silT[si_ % 2]
                    mk.act(sl_[:], pg[:, 0:512], AF.Silu)
                    mk.tt(sl_[:], sl_[:], pu[:, 0:512], ALU.mult)
                    for f in range(4):
                        pt = p.psum[f % 4]
                        mk.tr(pt[:, 0:128], sl_[:, f * 128:(f + 1) * 128], p.identS[:])
                        p.evac(hidT[:, f, si_ * 128:(si_ + 1) * 128], pt[:, 0:128])
                for si_ in range(NS):
                    y = yt[yi % 3]
                    yi += 1
                    for hf in range(2):
                        ps = p.psum[hf]
                        for f in range(4):
                            mk.mm(ps[:, 0:512], lhsT=hidT[:, f, si_ * 128:(si_ + 1) * 128], rhs=w["d"][:, f, hf * 512:(hf + 1) * 512],
                                  start=(f == 0), stop=(f == 3))
                        p.evac(y[:, hf * 512:(hf + 1) * 512], ps[:, 0:512])
                    r0 = e * CAP + si_ * 128
                    mk.dma("act", p.Yb[r0:r0 + 128, :], y[:])
        if getattr(p, 'moe_stop', 3) < 3:
            return
        with mk.scope():
            y1 = [mk.sb("cy1%d" % i, [128, D]) for i in range(2)]
            y2 = [mk.sb("cy2%d" % i, [128, D]) for i in range(2)]
            xb = [mk.sb("cxb%d" % i, [128, 8, 512]) for i in range(2)]
            xv = p.xT.ap().rearrange("(k q) t -> q k t", q=128)
            for bi, (t0, w) in enumerate(TB):
                r = 0 if t0 < T_LAT else 1
                x = xb[bi % 2]
                mk.dma("sp", x[:, :, 0:w], xv[:, :, t0:t0 + w])
                for j in range(w // 128):
                    n = t0 // 128 + j
                    a, b = y1[n % 2], y2[n % 2]
                    for kk, dst in enumerate((a, b)):
                        mk.dma("pool", dst[:], p.Yb[:, :],
                               fn=lambda e, kk=kk, dst=dst, n=n: e.indirect_dma_start(
                                   out=dst[:], out_offset=None, in_=p.Yb[:, :],
                                   in_offset=bass.IndirectOffsetOnAxis(ap=gidx[:, n, kk:kk + 1], axis=0)),
                               extra_reads=[gidx])
                    mk.tsc(a[:], a[:], rt[:, n, 0:1], ALU.mult)
                    mk.stt(a[:], b[:], rt[:, n, 1:2], a[:], ALU.mult, ALU.add)
                    for k in range(8):
                        pt = p.psum[k % 4]
                        mk.tr(pt[:, 0:128], a[:, k * 128:(k + 1) * 128], p.identS[:])
                        mk.stt(x[:, k, j * 128:(j + 1) * 128], pt[:, 0:128], mod[:, 40 + k, r:r + 1],
                               x[:, k, j * 128:(j + 1) * 128], ALU.mult, ALU.add)
                mk.dma("sp", xv[:, :, t0:t0 + w], x[:, :, 0:w])


def _moe_init(self):
    p, mk = self, self.mk
    with mk.scope():
        z = mk.sb("zeros", [128, D])
        mk.memset(z[:], 0.0)
        for i in range((NSLOT + 128) // 128):
            mk.dma("sp" if i % 2 == 0 else "pool", p.Xb[i * 128:(i + 1) * 128, :], z[:])
        for i in range((NSLOT + 128) // 128):
            mk.dma("sp" if i % 2 == 0 else "pool", p.Yb[i * 128:(i + 1) * 128, :], z[:])


Prog.outproj = _outproj
Prog.moe = _moe
Prog.moe_init = _moe_init

NA_SCALE = 64 ** -0.5
MLA_SCALE = 96 ** -0.5


def _headnorm(self, out, x, nh, hd, gain_bc, sq, ss, np_=128):
    mk = self.mk
    xv = x.rearrange("q (h d) -> q h d", h=nh)
    sqv = sq.rearrange("q (h d) -> q h d", h=nh)
    mk.tt(sq, x, x, ALU.mult)
    mk.reduce(ss, sqv, ALU.add)
    mk.act(ss, ss, AF.Sqrt, bias=EPS, scale=1.0 / hd)
    mk.recip(ss, ss)
    mk.tt(sqv, xv, ss.unsqueeze(2).to_broadcast([np_, nh, hd]), ALU.mult)
    mk.tt(out, sq, gain_bc, ALU.mult)


def _transposes_out(self, src_all, row0):
    p, mk = self, self.mk
    with mk.scope():
        ob = [mk.sb("tob%d" % i, [128, 512]) for i in range(2)]
        oi = 0
        for c in range(2):
            for bi, (t0, w) in enumerate(TB):
                o = ob[oi % 2]
                oi += 1
                for j in range(w // 128):
                    n = t0 // 128 + j
                    pt = p.psum[(n + c) % 4]
                    mk.tr(pt[:, 0:128], src_all[:, n, c * 128:(c + 1) * 128], p.identS[:])
                    p.evac(o[:, j * 128:(j + 1) * 128], pt[:, 0:128])
                mk.dma("sp", p.catT[row0 + c * 128:row0 + (c + 1) * 128, t0:t0 + w], o[:, 0:w])


def _na(self, l):
    p, mk = self, self.mk
    with mk.scope():
        qT = mk.sb("naqT", [128, 2, T_ALL], F32R)
        kT = mk.sb("nakT", [128, 2, T_ALL], F32R)
        V = mk.sb("naV", [128, NT, 4, 66], F32R)
        out_all = mk.sb("naout", [128, NT, 256])
        with mk.scope():
            gq = mk.sb("nagq", [128, 4, 64])
            gk = mk.sb("nagk", [128, 4, 64])
            for h in range(4):
                mk.dma("sp", gq[:, h, :], p.na_q_g[l, :].partition_broadcast(128))
                mk.dma("sp", gk[:, h, :], p.na_k_g[l, :].partition_broadcast(128))
            ones65 = mk.sb("ones65", [128, 4, 2])
            mk.memset(ones65[:], 0.0)
            mk.memset(ones65[:, :, 0:1], 1.0)
            tin = [mk.sb("natin%d" % i, [128, 768]) for i in range(2)]
            sq = mk.sb("nasq", [128, 256])
            ss = mk.sb("nass", [128, 4])
            qn = mk.sb("naqn", [128, 256])
            for n in range(NT):
                t = tin[n % 2]
                mk.dma("sp", t[:], p.ptok[n * 128:(n + 1) * 128, 2048:2816])
                for (src, g, dstT) in ((t[:, 0:256], gq, qT), (t[:, 256:512], gk, kT)):
                    p.headnorm(qn[:], src, 4, 64, g[:].rearrange("q h d -> q (h d)"), sq[:], ss[:])
                    for c in range(2):
                        pt = p.psum[c]
                        mk.tr(pt[:, 0:128], qn[:, c * 128:(c + 1) * 128], p.identS[:])
                        p.evac(dstT[:, c, n * 128:(n + 1) * 128], pt[:, 0:128])
                mk.copy(V[:, n, :, 0:64], t[:, 512:768].rearrange("q (h d) -> q h d", h=4), en="pool")
                mk.copy(V[:, n, :, 64:66], ones65[:], en="pool")
        with mk.scope():
            bias = [mk.sb("nabias%d" % i, [128, 5, 5, 128]) for i in range(2)]
            PT = [mk.sb("naPT%d" % i, [128, 7, 128], F32R) for i in range(2)]
            tmpb = [mk.sb("natmp%d" % i, [128, 5, 128]) for i in range(2)]
            rec = mk.sb("narec", [128, 1])
            PTc = mk.sb("naPTc", [128, 2, 256], F32R)
            it = 0
            for h in range(4):
                hb, hc = (h % 2) * 64, h // 2
                bs = bias[h % 2]
                mk.dma("sp", bs[:], p.na_biasT[l, h, :, :, :, :])
                for pr in range(16):
                    pat = 0 if pr == 0 else 1 if pr == 1 else 3 if pr == 14 else 4 if pr == 15 else 2
                    rs0 = min(max(2 * pr - 4, 0), 24)
                    ws = min((rs0 // 2) * 2, 22)
                    kt0 = ws // 2
                    q0 = pr * 128
                    pa, pb = p.psum[(it % 2) * 2], p.psum[(it % 2) * 2 + 1]
                    P_, tb = PT[it % 2], tmpb[it % 2]
                    for kt in range(4):
                        mk.mm(pa[:, kt * 128:(kt + 1) * 128], lhsT=kT[hb:hb + 64, hc, (kt0 + kt) * 128:(kt0 + kt + 1) * 128],
                              rhs=qT[hb:hb + 64, hc, q0:q0 + 128])
                    mk.mm(pb[:, 0:128], lhsT=kT[hb:hb + 64, hc, (kt0 + 4) * 128:(kt0 + 5) * 128], rhs=qT[hb:hb + 64, hc, q0:q0 + 128])
                    for j in range(2):
                        mk.mm(pb[:, 128 + j * 128:256 + j * 128], lhsT=kT[hb:hb + 64, hc, T_LAT + j * 128:T_LAT + (j + 1) * 128],
                              rhs=qT[hb:hb + 64, hc, q0:q0 + 128])
                    mk.stt(tb[:, 0:4, :], pa[:, 0:512].rearrange("q (a b) -> q a b", a=4), NA_SCALE, bs[:, pat, 0:4, :], ALU.mult, ALU.add)
                    mk.stt(tb[:, 4, :], pb[:, 0:128], NA_SCALE, bs[:, pat, 4, :], ALU.mult, ALU.add)
                    mk.act(P_[:, 0:5, :], tb[:], AF.Exp)
                    mk.act(P_[:, 5:7, :], pb[:, 128:384].rearrange("q (a b) -> q a b", a=2), AF.Exp, scale=NA_SCALE)
                    po = p.psum[4 + it % 2]
                    for kt in range(7):
                        vt = kt0 + kt if kt < 5 else 16 + (kt - 5)
                        mk.mm(po[:, 0:66], lhsT=P_[:, kt, :], rhs=V[:, vt, h, :], start=(kt == 0), stop=(kt == 6))
                    mk.recip(rec[:], po[:, 64:65])
                    mk.tsc(out_all[:, pr, h * 64:(h + 1) * 64], po[:, 0:64], rec[:, 0:1], ALU.mult)
                    it += 1
                pc_ = p.psum[6]
                for j in range(2):
                    mk.mm(pc_[:, j * 256:(j + 1) * 256], lhsT=kT[hb:hb + 64, hc, T_LAT + j * 128:T_LAT + (j + 1) * 128],
                          rhs=qT[hb:hb + 64, hc, T_LAT:T_ALL])
                mk.act(PTc[:], pc_[:, 0:512].rearrange("q (a b) -> q a b", a=2), AF.Exp, scale=NA_SCALE)
                for qi in range(2):
                    po = p.psum[7]
                    for j in range(2):
                        mk.mm(po[:, 0:66], lhsT=PTc[:, j, qi * 128:(qi + 1) * 128], rhs=V[:, 16 + j, h, :], start=(j == 0), stop=(j == 1))
                    mk.recip(rec[:], po[:, 64:65])
                    mk.tsc(out_all[:, 16 + qi, h * 64:(h + 1) * 64], po[:, 0:64], rec[:, 0:1], ALU.mult)
        p.transposes_out(out_all, 512)


def _mla(self, l):
    p, mk = self, self.mk
    with mk.scope():
        qT = mk.sb("mlqT", [96, 4, T_ALL], F32R)
        kT = mk.sb("mlkT", [96, 4, T_ALL], F32R)
        V = mk.sb("mlV", [128, NT, 4, 66], F32R)
        out_all = mk.sb("mlout", [128, NT, 256])
        with mk.scope():
            cqT = mk.sb("cqT", [128, 2, T_ALL], F32R)
            ckvT = mk.sb("ckvT", [128, T_ALL], F32R)
            gqa = mk.sb("gqa", [128, 256])
            gkva = mk.sb("gkva", [128, 128])
            mk.dma("sp", gqa[:], p.mla_q_a_g[l, :].partition_broadcast(128))
            mk.dma("sp", gkva[:], p.mla_kv_a_g[l, :].partition_broadcast(128))
            gq = mk.sb("mgq", [128, 4, 96])
            gk = mk.sb("mgk", [128, 4, 96])
            for h in range(4):
                mk.dma("sp", gq[:, h, :], p.mla_q_g[l, :].partition_broadcast(128))
                mk.dma("sp", gk[:, h, :], p.mla_k_g[l, :].partition_broadcast(128))
            ones65 = mk.sb("mones65", [128, 4, 2])
            mk.memset(ones65[:], 0.0)
            mk.memset(ones65[:, :, 0:1], 1.0)
            wst = mk.sb("mwst", [128, 768])
            wuq = mk.sb("wuq", [128, 2, 384], F32R)
            wukv = mk.sb("wukv", [128, 512], F32R)
            mk.dma("sp", wst[:].rearrange("q (k n) -> q k n", k=2), p.mla_w_uq[l, :, :].rearrange("(k q) n -> q k n", q=128))
            mk.copy(wuq[:], wst[:].rearrange("q (k n) -> q k n", k=2), en="pool")
            mk.dma("sp", wst[:, 0:512], p.mla_w_ukv[l, :, :])
            mk.copy(wukv[:], wst[:, 0:512], en="pool")
            cosT = mk.sb("ropec", [128, 16, 32])
            sinT = mk.sb("ropes", [128, 16, 32])
            mk.dma("sp", cosT[:], p.c_ropecos[:, :, :])
            mk.dma("sp", sinT[:], p.c_ropesin[:, :, :])
            tin = [mk.sb("mltin%d" % i, [128, 416]) for i in range(2)]
            sq = mk.sb("mlsq", [128, 384])
            ss = mk.sb("mlss", [128, 4])
            nrm = mk.sb("mlnrm", [128, 384])
            for n in range(NT):
                t = tin[n % 2]
                mk.dma("sp", t[:], p.ptok[n * 128:(n + 1) * 128, 2816:3232])
                p.headnorm(nrm[:, 0:256], t[:, 0:256], 1, 256, gqa[:], sq[:, 0:256], ss[:, 0:1])
                for c in range(2):
                    pt = p.psum[c]
                    mk.tr(pt[:, 0:128], nrm[:, c * 128:(c + 1) * 128], p.identS[:])
                    p.evac(cqT[:, c, n * 128:(n + 1) * 128], pt[:, 0:128])
                p.headnorm(nrm[:, 256:384], t[:, 256:384], 1, 128, gkva[:], sq[:, 0:128], ss[:, 0:1])
                pt = p.psum[2]
                mk.tr(pt[:, 0:128], nrm[:, 256:384], p.identS[:])
                p.evac(ckvT[:, n * 128:(n + 1) * 128], pt[:, 0:128])
            qk = [mk.sb("mlqk%d" % i, [128, 384]) for i in range(2)]
            sw = mk.sb("mlsw", [128, 4, 32])
            for n in range(NT):
                t = tin[n % 2]
                mk.dma("sp", t[:, 384:416], p.ptok[n * 128:(n + 1) * 128, 3200:3232])
                pq, pkv = p.psum[3], p.psum[4]
                for c in range(2):
                    mk.mm(pq[:, 0:384], lhsT=cqT[:, c, n * 128:(n + 1) * 128], rhs=wuq[:, c, :], start=(c == 0), stop=(c == 1))
                mk.mm(pkv[:, 0:512], lhsT=ckvT[:, n * 128:(n + 1) * 128], rhs=wukv[:], start=True, stop=True)
                kvv = pkv[:, 0:512].rearrange("q (h d) -> q h d", h=4)
                mk.copy(V[:, n, :, 0:64], kvv[:, :, 64:128], en="act")
                mk.copy(V[:, n, :, 64:66], ones65[:], en="pool")
                for which in range(2):
                    raw = qk[which]
                    rv = raw[:].rearrange("q (h d) -> q h d", h=4)
                    if which == 0:
                        mk.copy(raw[:], pq[:, 0:384])
                        g, dstT = gq, qT
                    else:
                        mk.copy(rv[:, :, 0:64], kvv[:, :, 0:64])
                        mk.copy(rv[:, :, 64:96], t[:, 384:416].unsqueeze(1).to_broadcast([128, 4, 32]), en="pool")
                        g, dstT = gk, kT
                    p.headnorm(nrm[:], raw[:], 4, 96, g[:].rearrange("q h d -> q (h d)"), sq[:], ss[:])
                    nv = nrm[:].rearrange("q (h d) -> q h d", h=4)
                    if n < 16:
                        for s0 in (64, 80):
                            o0 = s0 - 64
                            mk.copy(sw[:, :, o0:o0 + 8], nv[:, :, s0 + 8:s0 + 16])
                            mk.copy(sw[:, :, o0 + 8:o0 + 16], nv[:, :, s0:s0 + 8])
                        mk.tt(sw[:], sw[:], sinT[:, n, :].unsqueeze(1).to_broadcast([128, 4, 32]), ALU.mult)
                        mk.tt(nv[:, :, 64:96], nv[:, :, 64:96], cosT[:, n, :].unsqueeze(1).to_broadcast([128, 4, 32]), ALU.mult)
                        mk.tt(nv[:, :, 64:96], nv[:, :, 64:96], sw[:], ALU.add)
                    for h in range(4):
                        pt = p.psum[5 + h % 2]
                        mk.tr(pt[0:96, 0:128], nrm[:, h * 96:(h + 1) * 96], p.identS[:])
                        p.evac(dstT[:, h, n * 128:(n + 1) * 128], pt[0:96, 0:128])
        with mk.scope():
            PT = mk.sb("mlPT", [128, NT, 512], F32R)
            rec = mk.sb("mlrec", [128, 1])
            for h in range(4):
                for bi, (t0, w) in enumerate(TB):
                    kts = list(range(NT)) if t0 < T_LAT else [16, 17]
                    for i, kt in enumerate(kts):
                        ps = p.psum[i % 4]
                        mk.mm(ps[:, 0:w], lhsT=kT[:, h, kt * 128:(kt + 1) * 128], rhs=qT[:, h, t0:t0 + w])
                        mk.act(PT[:, i, 0:w], ps[:, 0:w], AF.Exp, scale=MLA_SCALE)
                    for j in range(w // 128):
                        n = t0 // 128 + j
                        po = p.psum[4 + j % 2]
                        for i, kt in enumerate(kts):
                            mk.mm(po[:, 0:66], lhsT=PT[:, i, j * 128:(j + 1) * 128], rhs=V[:, kt, h, :],
                                  start=(i == 0), stop=(i == len(kts) - 1))
                        mk.recip(rec[:], po[:, 64:65])
                        mk.tsc(out_all[:, n, h * 64:(h + 1) * 64], po[:, 0:64], rec[:, 0:1], ALU.mult)
        p.transposes_out(out_all, 768)


Prog.headnorm = _headnorm
Prog.transposes_out = _transposes_out
Prog.na = _na
Prog.mla = _mla


def _hgrn_gen(self, l):
    p, mk = self, self.mk
    if True:
        lb = mk.sb("lb", [64, 512])
        oml = mk.sb("oml", [64, 512])
        if True:
            lg = mk.sb("lblg", [64, 4, 512])
            mk.dma("sp", lg[:], p.hgrn_lb.ap().rearrange("l a b -> l (a b)").partition_broadcast(64))
            mk.act(lg[:], lg[:], AF.Exp)
            tot = mk.sb("lbtot", [64, 512])
            mk.tt(tot[:], lg[:, 0, :], lg[:, 1, :], ALU.add)
            mk.tt(tot[:], tot[:], lg[:, 2, :], ALU.add)
            mk.tt(tot[:], tot[:], lg[:, 3, :], ALU.add)
            mk.recip(tot[:], tot[:])
            mk.memset(lb[:], 0.0)
            for ll in range(1, l + 1):
                mk.tt(lb[:], lb[:], lg[:, ll, :], ALU.add)
            mk.tt(lb[:], lb[:], tot[:], ALU.mult)
            mk.tsc(oml[:], lb[:], -1.0, ALU.mult, 1.0, ALU.add)
        gn = mk.sb("hgn", [64, 4, 64])
        for h in range(4):
            mk.dma("sp", gn[:, h, :], p.hgrn_norm_g[l, :].partition_broadcast(64))
        tri = [mk.sb("tri%d" % i, [64, 64]) for i in range(2)]
        mk.dma("sp", tri[0][:], p.c_trif[:, :])
        mk.dma("sp", tri[1][:], p.c_trib[:, :])
        ones1 = mk.sb("hones1", [64, 1])
        mk.memset(ones1[:], 1.0)
        S = mk.sb("hS", [64, 4, 64])
        tin = [mk.sb("htin%d" % i, [64, 1280]) for i in range(2)]
        NB = 2
        f_ = [mk.sb("hf%d" % i, [64, 256]) for i in range(NB)]
        lf = [mk.sb("hlf%d" % i, [64, 256]) for i in range(NB)]
        kk = [mk.sb("hkk%d" % i, [64, 256]) for i in range(NB)]
        bc = [mk.sb("hbc%d" % i, [64, 256]) for i in range(NB)]
        eb = [mk.sb("heb%d" % i, [64, 256]) for i in range(NB)]
        qe = [mk.sb("hqe%d" % i, [64, 256]) for i in range(NB)]
        ke = [mk.sb("hke%d" % i, [64, 256]) for i in range(NB)]
        qeT = [mk.sb("hqeT%d" % i, [64, 4, 64]) for i in range(NB)]
        keT = [mk.sb("hkeT%d" % i, [64, 4, 64]) for i in range(NB)]
        ebl = [mk.sb("hebl%d" % i, [64, 4]) for i in range(NB)]
        ATm = [mk.sb("hAT%d" % i, [64, 4, 64]) for i in range(NB)]
        osb = [mk.sb("hosb%d" % i, [64, 256]) for i in range(NB)]
        of_ = [mk.sb("hof%d" % i, [64, 256]) for i in range(NB)]
        sq = mk.sb("hsq", [64, 256])
        ss = mk.sb("hss", [64, 4])
        sg = mk.sb("hsg", [64, 256])
        oT = [mk.sb("hoT%d" % i, [128, 2, 64]) for i in range(NB)]
        tmpS = mk.sb("htmpS", [64, 4, 64])
        it = 0
        for d in range(2):
            mk.memset(S[:], 0.0)
            if d == 0:
                order = [(T_LAT + 64 * c) for c in range(4)] + [64 * c for c in range(32)]
            else:
                order = [(T_LAT + 64 * c) for c in reversed(range(4))] + [64 * c for c in reversed(range(32))]
            for tok0 in order:
                i = it % NB
                it += 1
                t = tin[i]
                mk.dma("sp", t[:], p.ptok[tok0:tok0 + 64, 0:1280])
                z = t[:, 256 + 256 * d:512 + 256 * d]
                mk.act(f_[i][:], z, AF.Sigmoid)
                mk.tt(f_[i][:], f_[i][:], oml[:, d * 256:(d + 1) * 256], ALU.mult)
                mk.tt(f_[i][:], f_[i][:], lb[:, d * 256:(d + 1) * 256], ALU.add)
                mk.act(lf[i][:], f_[i][:], AF.Ln)
                mk.tsc(kk[i][:], f_[i][:], -1.0, ALU.mult, 1.0, ALU.add)
                pb = p.psum[0]
                mk.mm(pb[0:64, 0:256], lhsT=tri[d][:], rhs=lf[i][:])
                mk.tsc(bc[i][:], pb[0:64, 0:256], -80.0, ALU.max)
                pl = p.psum[1]
                for h in range(4):
                    mk.mm(pl[0:64, h:h + 1], lhsT=lf[i][:, h * 64:(h + 1) * 64], rhs=ones1[:])
                mk.tsc(ebl[i][:], pl[0:64, 0:4], -80.0, ALU.max)
                mk.act(ebl[i][:], ebl[i][:], AF.Exp)
                mk.act(eb[i][:], bc[i][:], AF.Exp)
                mk.tt(qe[i][:], t[:, 0:256], eb[i][:], ALU.mult)
                mk.act(eb[i][:], bc[i][:], AF.Exp, scale=-1.0)
                mk.tt(ke[i][:], kk[i][:], eb[i][:], ALU.mult)
                pq, pk = p.psum[2], p.psum[3]
                for h in range(4):
                    mk.tr(pq[0:64, h * 64:(h + 1) * 64], qe[i][:, h * 64:(h + 1) * 64], p.identS[0:64, 0:64])
                    mk.tr(pk[0:64, h * 64:(h + 1) * 64], ke[i][:, h * 64:(h + 1) * 64], p.identS[0:64, 0:64])
                mk.copy(qeT[i][:].rearrange("q h t -> q (h t)"), pq[0:64, 0:256])
                mk.copy(keT[i][:].rearrange("q h t -> q (h t)"), pk[0:64, 0:256], en="act")
                pa = p.psum[4]
                for h in range(4):
                    mk.mm(pa[0:64, h * 64:(h + 1) * 64], lhsT=keT[i][:, h, :], rhs=qeT[i][:, h, :])
                mk.tt(ATm[i][:], pa[0:64, 0:256].rearrange("q (h t) -> q h t", h=4),
                      tri[d][:].unsqueeze(1).to_broadcast([64, 4, 64]), ALU.mult)
                po = p.psum[5]
                v = t[:, 768:1024]
                for h in range(4):
                    mk.mm(po[0:64, h * 64:(h + 1) * 64], lhsT=ATm[i][:, h, :], rhs=v[:, h * 64:(h + 1) * 64], start=True, stop=False)
                    mk.mm(po[0:64, h * 64:(h + 1) * 64], lhsT=qeT[i][:, h, :], rhs=S[:, h, :], start=False, stop=True)
                pd = p.psum[6]
                for h in range(4):
                    mk.mm(pd[0:64, h * 64:(h + 1) * 64], lhsT=ke[i][:, h * 64:(h + 1) * 64], rhs=v[:, h * 64:(h + 1) * 64])
                mk.tt(tmpS[:], S[:], pd[0:64, 0:256].rearrange("q (h t) -> q h t", h=4), ALU.add)
                mk.tt(S[:], tmpS[:], ebl[i][:].unsqueeze(2).to_broadcast([64, 4, 64]), ALU.mult)
                if d == 0:
                    mk.copy(osb[i][:], po[0:64, 0:256], en="act")
                    mk.dma("pool", p.hof[tok0:tok0 + 64, :], osb[i][:])
                else:
                    mk.dma("pool", of_[i][:], p.hof[tok0:tok0 + 64, :])
                    mk.tt(osb[i][:], po[0:64, 0:256], of_[i][:], ALU.add)
                    p.headnorm(osb[i][:], osb[i][:], 4, 64, gn[:].rearrange("q h d -> q (h d)"), sq[:], ss[:], np_=64)
                    mk.act(sg[:], t[:, 1024:1280], AF.Silu)
                    mk.tt(osb[i][:], osb[i][:], sg[:], ALU.mult)
                    pt = p.psum[7]
                    for c in range(2):
                        mk.tr(pt[:, c * 64:(c + 1) * 64], osb[i][:, c * 128:(c + 1) * 128], p.identS[0:64, 0:64])
                    mk.copy(oT[i][:].rearrange("q c t -> q (c t)"), pt[:, 0:128])
                    mk.dma("sp", p.catT[0:256, tok0:tok0 + 64].rearrange("(c q) t -> q c t", q=128), oT[i][:])
                yield


def _hgrn(self, l):
    p, mk = self, self.mk
    G = 4
    with mk.scope():
        lb = mk.sb("lb", [64, 512])
        oml = mk.sb("oml", [64, 512])
        with mk.scope():
            lg = mk.sb("lblg", [64, 4, 512])
            mk.dma("sp", lg[:], p.hgrn_lb.ap().rearrange("l a b -> l (a b)").partition_broadcast(64))
            mk.act(lg[:], lg[:], AF.Exp)
            tot = mk.sb("lbtot", [64, 512])
            mk.tt(tot[:], lg[:, 0, :], lg[:, 1, :], ALU.add)
            mk.tt(tot[:], tot[:], lg[:, 2, :], ALU.add)
            mk.tt(tot[:], tot[:], lg[:, 3, :], ALU.add)
            mk.recip(tot[:], tot[:])
            mk.memset(lb[:], 0.0)
            for ll in range(1, l + 1):
                mk.tt(lb[:], lb[:], lg[:, ll, :], ALU.add)
            mk.tt(lb[:], lb[:], tot[:], ALU.mult)
            mk.tsc(oml[:], lb[:], -1.0, ALU.mult, 1.0, ALU.add)
        gn = mk.sb("hgn", [64, G * 4, 64])
        for h in range(G * 4):
            mk.dma("sp", gn[:, h, :], p.hgrn_norm_g[l, :].partition_broadcast(64))
        tri = [mk.sb("tri%d" % i, [64, 64]) for i in range(2)]
        mk.dma("sp", tri[0][:], p.c_trif[:, :])
        mk.dma("sp", tri[1][:], p.c_trib[:, :])
        ones1 = mk.sb("hones1", [64, 1])
        mk.memset(ones1[:], 1.0)
        S = mk.sb("hS", [64, 4, 64])
        tmpS = mk.sb("htmpS", [64, 4, 64])
        NB = 2
        mkt = lambda nm, shp: [mk.sb("h%s%d" % (nm, i), shp) for i in range(NB)]
        tin = mkt("tin", [64, G, 1280])
        f_ = mkt("f", [64, G, 256]); lf = mkt("lf", [64, G, 256]); kk = mkt("kk", [64, G, 256])
        bc = mkt("bc", [64, G, 256]); eb = mkt("eb", [64, G, 256]); qe = mkt("qe", [64, G, 256]); ke = mkt("ke", [64, G, 256])
        qeT = mkt("qeT", [64, G, 4, 64]); keT = mkt("keT", [64, G, 4, 64]); ATm = mkt("ATm", [64, G, 4, 64])
        ebl = mkt("ebl", [64, G, 4]); osb = mkt("osb", [64, G, 256]); of_ = mkt("of", [64, G, 256])
        sq = mk.sb("hsq", [64, G * 256]); ss = mk.sb("hss", [64, G * 4]); sg = mk.sb("hsg", [64, G, 256])
        oT = mkt("oT", [128, 2, G * 64])
        batches = [T_LAT] + [G * 64 * b for b in range(8)]
        it = 0
        for d in range(2):
            mk.memset(S[:], 0.0)
            blist = batches if d == 0 else [T_LAT] + [G * 64 * b for b in reversed(range(8))]
            for tok0 in blist:
                i = it % NB
                it += 1
                t = tin[i]
                ncol = 1024 if d == 0 else 1280
                mk.dma("sp", t[:, :, 0:ncol], p.ptok[tok0:tok0 + G * 64, 0:ncol].rearrange("(g q) n -> q g n", q=64))
                z = t[:, :, 256 + 256 * d:512 + 256 * d]
                lbd = lb[:, d * 256:(d + 1) * 256].unsqueeze(1).to_broadcast([64, G, 256])
                omd = oml[:, d * 256:(d + 1) * 256].unsqueeze(1).to_broadcast([64, G, 256])
                mk.act(f_[i][:], z, AF.Sigmoid)
                mk.tt(f_[i][:], f_[i][:], omd, ALU.mult)
                mk.tt(f_[i][:], f_[i][:], lbd, ALU.add)
                mk.act(lf[i][:], f_[i][:], AF.Ln)
                mk.tsc(kk[i][:], f_[i][:], -1.0, ALU.mult, 1.0, ALU.add)
                for g2 in range(G // 2):
                    pb = p.psum[g2]
                    mk.mm(pb[0:64, 0:512], lhsT=tri[d][:], rhs=lf[i][:, 2 * g2:2 * g2 + 2, :].rearrange("q g k -> q (g k)"))
                    mk.tsc(bc[i][:, 2 * g2:2 * g2 + 2, :].rearrange("q g k -> q (g k)"), pb[0:64, 0:512], -80.0, ALU.max)
                pl = p.psum[2]
                for g in range(G):
                    for h in range(4):
                        mk.mm(pl[0:64, g * 4 + h:g * 4 + h + 1], lhsT=lf[i][:, g, h * 64:(h + 1) * 64], rhs=ones1[:])
                mk.tsc(ebl[i][:].rearrange("q g h -> q (g h)"), pl[0:64, 0:G * 4], -80.0, ALU.max)
                mk.act(ebl[i][:], ebl[i][:], AF.Exp)
                mk.act(eb[i][:], bc[i][:], AF.Exp)
                mk.tt(qe[i][:], t[:, :, 0:256], eb[i][:], ALU.mult)
                mk.act(eb[i][:], bc[i][:], AF.Exp, scale=-1.0)
                mk.tt(ke[i][:], kk[i][:], eb[i][:], ALU.mult)
                for g in range(G):
                    pq, pk = p.psum[3], p.psum[4]
                    for h in range(4):
                        mk.tr(pq[0:64, h * 64:(h + 1) * 64], qe[i][:, g, h * 64:(h + 1) * 64], p.identS[0:64, 0:64])
                        mk.tr(pk[0:64, h * 64:(h + 1) * 64], ke[i][:, g, h * 64:(h + 1) * 64], p.identS[0:64, 0:64])
                    mk.copy(qeT[i][:, g, :, :].rearrange("q h t -> q (h t)"), pq[0:64, 0:256])
                    mk.copy(keT[i][:, g, :, :].rearrange("q h t -> q (h t)"), pk[0:64, 0:256], en="act")
                    pa = p.psum[5]
                    for h in range(4):
                        mk.mm(pa[0:64, h * 64:(h + 1) * 64], lhsT=keT[i][:, g, h, :], rhs=qeT[i][:, g, h, :])
                    mk.tt(ATm[i][:, g, :, :], pa[0:64, 0:256].rearrange("q (h t) -> q h t", h=4),
                          tri[d][:].unsqueeze(1).to_broadcast([64, 4, 64]), ALU.mult)
                gorder = range(G) if d == 0 else reversed(range(G))
                for g in gorder:
                    v = t[:, g, 768:1024]
                    po = p.psum[6]
                    for h in range(4):
                        mk.mm(po[0:64, h * 64:(h + 1) * 64], lhsT=ATm[i][:, g, h, :], rhs=v[:, h * 64:(h + 1) * 64], start=True, stop=False)
                        mk.mm(po[0:64, h * 64:(h + 1) * 64], lhsT=qeT[i][:, g, h, :], rhs=S[:, h, :], start=False, stop=True)
                    pd = p.psum[7]
                    for h in range(4):
                        mk.mm(pd[0:64, h * 64:(h + 1) * 64], lhsT=ke[i][:, g, h * 64:(h + 1) * 64], rhs=v[:, h * 64:(h + 1) * 64])
                    mk.tt(tmpS[:], S[:], pd[0:64, 0:256].rearrange("q (h t) -> q h t", h=4), ALU.add)
                    mk.tt(S[:], tmpS[:], ebl[i][:, g, :].unsqueeze(2).to_broadcast([64, 4, 64]), ALU.mult)
                    mk.copy(osb[i][:, g, :], po[0:64, 0:256], en="act")
                hv = p.hof[tok0:tok0 + G * 64, :].rearrange("(g q) n -> q g n", q=64)
                if d == 0:
                    mk.dma("sp", hv, osb[i][:])
                else:
                    mk.dma("sp", of_[i][:], hv)
                    o2 = osb[i][:].rearrange("q g n -> q (g n)")
                    mk.tt(o2, o2, of_[i][:].rearrange("q g n -> q (g n)"), ALU.add)
                    p.headnorm(o2, o2, G * 4, 64, gn[:].rearrange("q h d -> q (h d)"), sq[:], ss[:], np_=64)
                    mk.act(sg[:], t[:, :, 1024:1280], AF.Silu)
                    mk.tt(osb[i][:], osb[i][:], sg[:], ALU.mult)
                    for c in range(2):
                        pt = p.psum[c]
                        for g in range(G):
                            mk.tr(pt[:, g * 64:(g + 1) * 64], osb[i][:, g, c * 128:(c + 1) * 128], p.identS[0:64, 0:64])
                        mk.copy(oT[i][:, c, :], pt[:, 0:G * 64], en="dve" if c == 0 else "act")
                    mk.dma("sp", p.catT[0:256, tok0:tok0 + G * 64].rearrange("(c q) t -> q c t", q=128), oT[i][:])


Prog.hgrn_gen = _hgrn_gen
Prog.hgrn = _hgrn


def _hy_sin(self, out, ps, b_col, f_col, w, tmp):
    mk = self.mk
    arg, s4, s2 = tmp
    mk.tsc(arg[:, 0:w], ps, b_col, ALU.add, f_col, ALU.mult)
    mk.act(s4[:, 0:w], arg[:, 0:w], AF.Sin, scale=0.25)
    mk.act(s2[:, 0:w], arg[:, 0:w], AF.Sin, scale=0.5)
    mk.tt(s4[:, 0:w], s4[:, 0:w], s4[:, 0:w], ALU.mult)
    mk.tsc(s4[:, 0:w], s4[:, 0:w], -2.0, ALU.mult, 1.0, ALU.add)
    mk.stt(out, s2[:, 0:w], 2.0, s4[:, 0:w], ALU.mult, ALU.mult)


def _hy_filters(self, l):
    p, mk = self, self.mk
    with mk.scope():
        w1 = mk.sb("hyw1", [33, 64])
        w2 = mk.sb("hyw2", [64, 64])
        w3 = mk.sb("hyw3", [64, 1024])
        mk.dma("sp", w1[:], p.hy_w1[l, :, :])
        mk.dma("sp", w2[:], p.hy_w2[l, :, :])
        mk.dma("sp", w3[:], p.hy_w3[l, :, :])
        cols = mk.sb("hycols", [64, 4])
        mk.dma("sp", cols[:, 0:1], p.hy_b1[l, :].rearrange("(q o) -> q o", o=1))
        mk.dma("sp", cols[:, 1:2], p.hy_freq[l, :].rearrange("(q o) -> q o", o=1))
        mk.dma("sp", cols[:, 2:3], p.hy_b2[l, :].rearrange("(q o) -> q o", o=1))
        b3 = mk.sb("hyb3", [128, 8])
        ndec = mk.sb("hyndec", [128, 8])
        mk.dma("sp", b3[:], p.hy_b3T[l, :, :])
        mk.dma("sp", ndec[:], p.hy_decT[l, :, :])
        mk.tsc(ndec[:], ndec[:], -1.0, ALU.mult)
        feats = mk.sb("hyfeats", [33, 512])
        tun = mk.sb("hytun", [128, 512])
        tmp = [mk.sb("hytmp%d" % i, [64, 512]) for i in range(3)]
        h1 = mk.sb("hyh1", [64, 512])
        h2 = mk.sb("hyh2", [64, 512])
        E = mk.sb("hyE", [128, 512])
        ssq = mk.sb("hyssq", [128, 8, 4])
        junk = mk.sb("hyjunk", [128, 2048])
        inv = mk.sb("hyinv", [128, 4])
        for (L, featsD, tunD, Kd, KW) in ((2048, p.c_feats, p.c_tun, p.Kd, 4096), (256, p.c_featsc, p.c_tunc, p.Kdc, 512)):
            with mk.scope():
                FT = mk.sb("hyFT", [128, 8, L])
                nblk = (L + 511) // 512
                for tdir in range(2):
                    for bi in range(nblk):
                        w = min(512, L)
                        t0 = bi * 512
                        mk.dma("sp", feats[:, 0:w], featsD[tdir, :, t0:t0 + w])
                        mk.dma("sp", tun[:, 0:w], tunD[tdir, t0:t0 + w].partition_broadcast(128))
                        ps = p.psum[0]
                        mk.mm(ps[0:64, 0:w], lhsT=w1[:], rhs=feats[:, 0:w])
                        p.hy_sin(h1[:, 0:w], ps[0:64, 0:w], cols[:, 0:1], cols[:, 1:2], w, tmp)
                        ps = p.psum[1]
                        mk.mm(ps[0:64, 0:w], lhsT=w2[:], rhs=h1[:, 0:w])
                        p.hy_sin(h2[:, 0:w], ps[0:64, 0:w], cols[:, 2:3], cols[:, 1:2], w, tmp)
                        for j in (0, 1, 4, 5):
                            jj = j + 2 * tdir
                            ps = p.psum[2 + jj % 4]
                            mk.mm(ps[:, 0:w], lhsT=w3[:, jj * 128:(jj + 1) * 128], rhs=h2[:, 0:w])
                            mk.act(E[:, 0:w], tun[:, 0:w], AF.Exp, scale=ndec[:, jj:jj + 1])
                            mk.stt(FT[:, jj, t0:t0 + w], ps[:, 0:w], b3[:, jj:jj + 1], E[:, 0:w], ALU.add, ALU.mult)
                for jj in range(8):
                    tdir = (jj // 2) % 2
                    n = L if tdir == 0 else L - 1
                    mk.act(junk[:, 0:n], FT[:, jj, 0:n], AF.Square, accum_out=ssq[:, jj, 0:1])
                for j in (0, 1, 4, 5):
                    col = inv[:, 0:1]
                    mk.tt(col, ssq[:, j, 0:1], ssq[:, j + 2, 0:1], ALU.add)
                    mk.act(col, col, AF.Sqrt, bias=EPS, scale=1.0)
                    mk.recip(col, col)
                    o, ch = j // 4, j % 2
                    r0 = o * 256 + ch * 128
                    mk.tsc(FT[:, j, :], FT[:, j, :], col, ALU.mult)
                    mk.tsc(FT[:, j + 2, :], FT[:, j + 2, :], col, ALU.mult)
                    mk.dma("sp", Kd[r0:r0 + 128, L - 1:2 * L - 1], FT[:, j, :])
                    mk.dma("sp", Kd[r0:r0 + 128, 0:L - 1], FT[:, j + 2, 0:L - 1])


def _hyena(self, l):
    p, mk = self, self.mk
    p.hy_filters(l)
    with mk.scope():
        sw = mk.sb("hysw", [128, 6, 3])
        sbias = mk.sb("hysb", [128, 6])
        mk.dma("sp", sw[:], p.hy_swT[l, :, :, :])
        mk.dma("sp", sbias[:], p.hy_sbT[l, :, :])
        db = mk.sb("hydb", [128, 2, 256])
        mk.dma("sp", db[:], p.hy_bias[l, :, :].partition_broadcast(128))
        Ex = mk.sb("hyEx", [128, 128])
        mk.dma("sp", Ex[:], p.c_exch[:, :])
        uf = [mk.sb("hyuf%d" % i, [128, T_ALL]) for i in range(2)]
        acc = mk.sb("hyacc", [128, T_ALL])
        tk = [mk.sb("hytk%d" % i, [128, NT, 128]) for i in range(3)]
        z1 = mk.sb("hyz1", [128, NT, 128])
        zr = mk.sb("hyzr", [128, NT, 128])
        R = [mk.sb("hyR%d" % i, [128, 3968]) for i in range(3)]
        Rc = [mk.sb("hyRc%d" % i, [128, 384]) for i in range(3)]
        tmpg = [mk.sb("hytg%d" % i, [128, NT]) for i in range(2)]
        ob = [mk.sb("hyob%d" % i, [128, 512]) for i in range(2)]
        ci = 0
        for ch in range(2):
            for part in range(3):
                jt = part * 2 + ch
                u = uf[part % 2]
                mk.dma("sp", u[:], p.uT[jt * 128:(jt + 1) * 128, :])
                mk.tsc(acc[:], u[:], sw[:, jt, 1:2], ALU.mult, sbias[:, jt:jt + 1], ALU.add)
                for (a, b) in ((0, T_LAT), (T_LAT, T_ALL)):
                    mk.stt(acc[:, a + 1:b], u[:, a:b - 1], sw[:, jt, 0:1], acc[:, a + 1:b], ALU.mult, ALU.add)
                    mk.stt(acc[:, a:b - 1], u[:, a + 1:b], sw[:, jt, 2:3], acc[:, a:b - 1], ALU.mult, ALU.add)
                for n in range(NT):
                    pt = p.psum[n % 4]
                    mk.tr(pt[:, 0:128], acc[:, n * 128:(n + 1) * 128], p.identS[:])
                    p.evac(tk[part][:, n, :], pt[:, 0:128])
            for o in range(2):
                z = tk[0] if o == 0 else z1
                gate = tk[1] if o == 0 else tk[2]
                zo = z1 if o == 0 else tk[0]
                for n in range(NT):
                    pt = p.psum[n % 4]
                    mk.mm(pt[:, 0:128], lhsT=Ex[:], rhs=z[:, n, :])
                    p.evac(zr[:, n, :], pt[:, 0:128])
                for c in range(128):
                    row = o * 256 + ch * 128 + c
                    r, rc = R[ci % 3], Rc[ci % 3]
                    mk.dma("sp" if ci % 2 == 0 else "act", r[:],
                           bass.AP(tensor=p.Kd.ap().tensor, offset=row * 4096, ap=[[1, 128], [1, 3968]]))
                    mk.dma("pool", rc[:], bass.AP(tensor=p.Kdc.ap().tensor, offset=row * 512, ap=[[1, 128], [1, 384]]))
                    ps = p.psum[4 + ci % 4]
                    ci += 1
                    Ds = [0] + [d for k in range(1, 16) for d in (k, -k)]
                    for di, Dd in enumerate(Ds):
                        i0, i1 = max(0, Dd), min(16, 16 + Dd)
                        mk.mm(ps[:, i0:i1], lhsT=r[:, 128 * (Dd + 15):128 * (Dd + 16)], rhs=zr[:, i0 - Dd:i1 - Dd, c],
                              start=(di == 0), stop=(di == len(Ds) - 1))
                    for di, Dd in enumerate((0, 1, -1)):
                        i0, i1 = max(0, Dd), min(2, 2 + Dd)
                        mk.mm(ps[:, 16 + i0:16 + i1], lhsT=rc[:, 128 * (Dd + 1):128 * (Dd + 2)], rhs=zr[:, 16 + i0 - Dd:16 + i1 - Dd, c],
                              start=(di == 0), stop=(di == 2))
                    tg = tmpg[c % 2]
                    cc = ch * 128 + c
                    mk.stt(tg[:], z[:, :, c], db[:, o, cc:cc + 1], ps[:, 0:NT], ALU.mult, ALU.add)
                    mk.tt(zo[:, :, c], tg[:], gate[:, :, c], ALU.mult)
            oi = 0
            for bi, (t0, w) in enumerate(TB):
                o_ = ob[oi % 2]
                oi += 1
                for j in range(w // 128):
                    n = t0 // 128 + j
                    pt = p.psum[n % 4]
                    mk.tr(pt[:, 0:128], tk[0][:, n, :], p.identS[:])
                    p.evac(o_[:, j * 128:(j + 1) * 128], pt[:, 0:128])
                mk.dma("sp", p.catT[256 + ch * 128:256 + (ch + 1) * 128, t0:t0 + w], o_[:, 0:w])


Prog.hy_sin = _hy_sin
Prog.hy_filters = _hy_filters
Prog.hyena = _hyena


def _hy_spectrum(self, l):
    p, mk = self, self.mk
    with mk.scope():
        w1 = mk.sb("hyw1", [33, 64])
        w2 = mk.sb("hyw2", [64, 64])
        w3 = mk.sb("hyw3", [64, 1024])
        mk.dma("sp", w1[:], p.hy_w1[l, :, :])
        mk.dma("sp", w2[:], p.hy_w2[l, :, :])
        mk.dma("sp", w3[:], p.hy_w3[l, :, :])
        cols = mk.sb("hycols", [64, 4])
        mk.dma("sp", cols[:, 0:1], p.hy_b1[l, :].rearrange("(q o) -> q o", o=1))
        mk.dma("sp", cols[:, 1:2], p.hy_freq[l, :].rearrange("(q o) -> q o", o=1))
        mk.dma("sp", cols[:, 2:3], p.hy_b2[l, :].rearrange("(q o) -> q o", o=1))
        b3 = mk.sb("hyb3", [128, 1024])
        dec = mk.sb("hydec", [128, 1024])
        mk.dma("sp", b3[:], p.hy_b3[l, :].partition_broadcast(128))
        mk.dma("sp", dec[:], p.hy_decay[l, :, :, :].rearrange("a b c -> (a b c)").partition_broadcast(128))
        feats = mk.sb("hyfeats", [33, 512])
        tmp = [mk.sb("hytmp%d" % i, [64, 512]) for i in range(3)]
        h1 = mk.sb("hyh1", [64, 512])
        h2 = mk.sb("hyh2", [64, 512])
        E = mk.sb("hyE", [128, 1024])
        tot = mk.sb("hytot", [128, 512])
        for (L, featsD, ntunD, dftB, Ksp, nrm) in ((2048, p.c_feats, p.c_ntun, p.c_dftB, p.Ksp, 2.0 / 4096),
                                                 (256, p.c_featsc, p.c_ntunc, p.c_dftBc, p.Kspc, 2.0 / 512)):
            nt = L // 128
            with mk.scope():
                filtR = mk.sb("hyfilt", [128, nt, 1024], F32R)
                filt = filtR[:].bitcast(F32)
                ntun = mk.sb("hyntun", [128, nt])
                mk.dma("sp", ntun[:], ntunD[:, :])
                sq = mk.sb("hysq", [128, 1024], F32R)
                for bi in range((L + 511) // 512):
                    w = min(512, L)
                    t0 = bi * 512
                    mk.dma("sp", feats[:, 0:w], featsD[0, :, t0:t0 + w])
                    ps = p.psum[0]
                    mk.mm(ps[0:64, 0:w], lhsT=w1[:], rhs=feats[:, 0:w])
                    p.hy_sin(h1[:, 0:w], ps[0:64, 0:w], cols[:, 0:1], cols[:, 1:2], w, tmp)
                    ps = p.psum[1]
                    mk.mm(ps[0:64, 0:w], lhsT=w2[:], rhs=h1[:, 0:w])
                    p.hy_sin(h2[:, 0:w], ps[0:64, 0:w], cols[:, 2:3], cols[:, 1:2], w, tmp)
                    for j in range(w // 128):
                        n = t0 // 128 + j
                        p.tick()
                        mk.act(E[:], dec[:], AF.Exp, scale=ntun[:, n:n + 1])
                        if n == 0:
                            mk.memset(E[:].rearrange("q (o d c) -> q o d c", o=2, d=2)[0:1, :, 1, :], 0.0)
                        for hf in range(2):
                            ps = p.psum[2 + hf]
                            mk.mm(ps[:, 0:512], lhsT=h2[:, j * 128:(j + 1) * 128], rhs=w3[:, hf * 512:(hf + 1) * 512])
                            mk.tt(filtR[:, n, hf * 512:(hf + 1) * 512], ps[:, 0:512], b3[:, hf * 512:(hf + 1) * 512], ALU.add)
                        mk.tt(filtR[:, n, :], filt[:, n, :], E[:], ALU.mult, en="pool")
                pss = [p.psum[4], p.psum[5]]
                for n in range(nt):
                    mk.act(sq[:], filt[:, n, :], AF.Square)
                    for hf in range(2):
                        mk.mm(pss[hf][:, 0:512], lhsT=p.onesR[:], rhs=sq[:, hf * 512:(hf + 1) * 512], start=(n == 0), stop=(n == nt - 1))
                for o in range(2):
                    mk.copy(tot[:, o * 256:(o + 1) * 256], pss[o][:, 256:512], en="act")
                    mk.tt(tot[:, o * 256:(o + 1) * 256], tot[:, o * 256:(o + 1) * 256], pss[o][:, 0:256], ALU.add)
                mk.act(tot[:], tot[:], AF.Sqrt, bias=EPS, scale=1.0)
                mk.recip(tot[:], tot[:])
                mk.tsc(tot[:], tot[:], nrm, ALU.mult)
                totv = tot[:].rearrange("q (o c) -> q o c", o=2).unsqueeze(2).to_broadcast([128, 2, 2, 256])
                for n in range(nt):
                    mk.tt(filtR[:, n, :].rearrange("q (o d c) -> q o d c", o=2, d=2),
                          filt[:, n, :].rearrange("q (o d c) -> q o d c", o=2, d=2), totv, ALU.mult,
                          en="dve" if n % 2 == 0 else "pool")
                for n in range(nt):
                    fv4 = filt[:, n, :].rearrange("q (o d c) -> q o d c", o=2, d=2)
                    fr4 = filtR[:, n, :].rearrange("q (o d c) -> q o d c", o=2, d=2)
                    eng = "dve" if n % 2 == 0 else "pool"
                    mk.tt(fr4[:, :, 1, :], fv4[:, :, 0, :], fv4[:, :, 1, :], ALU.subtract, en=eng)
                    if eng == "dve":
                        mk.stt(fr4[:, :, 0, :], fv4[:, :, 0, :], 2.0, fv4[:, :, 1, :], ALU.mult, ALU.subtract)
                    else:
                        mk.tsc(fr4[:, :, 0, :], fv4[:, :, 0, :], 2.0, ALU.mult, en="pool")
                        mk.tt(fr4[:, :, 0, :], fv4[:, :, 0, :], fv4[:, :, 1, :], ALU.subtract, en="pool")
                blk = [mk.sb("hyblk%d" % i, [128, 2, nt, 128], F32R) for i in range(2)]
                ko = [mk.sb("hyko%d" % i, [128, 2, 2, 256]) for i in range(2)]
                for k in range(nt):
                    p.tick()
                    bk = blk[k % 2]
                    mk.dma("pool", bk[:], dftB[k, :, 0:2, :, :], max_dma_last_dim=8192)
                    kk_ = ko[k % 2]
                    for cs in range(2):
                        ps = p.psum[cs]
                        for n in range(nt):
                            rhs = filtR[:, n, :].rearrange("q (o d c) -> q o d c", o=2, d=2)[:, :, cs, :]
                            mk.mm(ps[:, 0:512].rearrange("q (o c) -> q o c", o=2), lhsT=bk[:, 1 - cs, n, :], rhs=rhs,
                                  start=(n == 0), stop=(n == nt - 1))
                        mk.copy(kk_[:, :, cs, :], ps[:, 0:512].rearrange("q (o c) -> q o c", o=2), en="dve" if cs == 0 else "act")
                    if k == 0:
                        ps = p.psum[2]
                        for n in range(nt):
                            rhs = filtR[:, n, :].rearrange("q (o d c) -> q o d c", o=2, d=2)[:, :, 0, :]
                            mk.mm(ps[:, 0:512].rearrange("q (o c) -> q o c", o=2), lhsT=bk[:, 0, n, :], rhs=rhs,
                                  start=(n == 0), stop=(n == nt - 1))
                        mk.copy(kk_[0:1, :, 1, :], ps[0:1, 0:512].rearrange("q (o c) -> q o c", o=2))
                        mk.tsc(kk_[0:1, :, :, :], kk_[0:1, :, :, :], 0.5, ALU.mult)
                    mk.dma("sp", Ksp[:, :, k * 128:(k + 1) * 128, :].rearrange("o s q c -> q o s c"), kk_[:])


def _hy_conv(self, zR, o, nt, tile0, dftB, Ksp, gtok_o, db_o, zout, blkname):
    p, mk = self, self.mk
    zF = zR[:].bitcast(F32)
    with mk.scope():
        blk = [mk.sb(blkname + "%d" % i, [128, 2, nt, 128], F32R) for i in range(2)]
        kc = [mk.sb("hykc%d" % i, [128, 2, 256]) for i in range(2)]
        YR = mk.sb("hyYR", [128, 2, nt, 256], F32R)
        t1 = [mk.sb("hyt1%d" % i, [128, 256]) for i in range(2)]
        t2 = [mk.sb("hyt2%d" % i, [128, 256]) for i in range(2)]
        t3 = [mk.sb("hyt3%d" % i, [128, 256]) for i in range(2)]
        t4 = [mk.sb("hyt4%d" % i, [128, 256]) for i in range(2)]
        gt = [mk.sb("hygt%d" % i, [128, 256]) for i in range(2)]
        dz = [mk.sb("hydz%d" % i, [128, 256]) for i in range(2)]
        bi = 0
        for k in range(nt):
            p.tick()
            bk = blk[bi % 2]
            bi += 1
            mk.dma("pool", bk[:], dftB[k, :, 0:2, :, :], max_dma_last_dim=8192)
            kk_ = kc[k % 2]
            mk.dma("sp", kk_[:], Ksp[o, :, k * 128:(k + 1) * 128, :].rearrange("s q c -> q s c"))
            psA, psB = p.psum[(k % 2) * 2], p.psum[(k % 2) * 2 + 1]
            for cs, ps in ((0, psA), (1, psB)):
                for n in range(nt):
                    mk.mm(ps[:, 0:256], lhsT=bk[:, 1 - cs, n, :], rhs=zR[:, tile0 + n, :], start=(n == 0), stop=(n == nt - 1))
            a1, a2, b1_, b2_ = t1[k % 2], t2[k % 2], t3[k % 2], t4[k % 2]
            mk.tt(a1[:], psA[:, 0:256], kk_[:, 0, :], ALU.mult)
            mk.tt(a2[:], psB[:, 0:256], kk_[:, 1, :], ALU.mult)
            mk.tt(YR[:, 0, k, :], a1[:], a2[:], ALU.subtract, en="pool")
            mk.tt(b1_[:], psA[:, 0:256], kk_[:, 1, :], ALU.mult)
            mk.tt(b2_[:], psB[:, 0:256], kk_[:, 0, :], ALU.mult)
            mk.tt(YR[:, 1, k, :], b1_[:], b2_[:], ALU.add, en="pool")
            if k == 0:
                mk.copy(YR[0:1, 0, 0, :], a1[0:1, :])
                mk.copy(YR[0:1, 1, 0, :], a2[0:1, :])
        for a in range(nt):
            p.tick()
            bk = blk[bi % 2]
            bi += 1
            mk.dma("pool", bk[:], dftB[a, :, 1:3, :, :], max_dma_last_dim=8192)
            n = tile0 + a
            g = gt[a % 2]
            mk.dma("sp", g[:], gtok_o[n * 128:(n + 1) * 128, :])
            ps = p.psum[4 + a % 2]
            for cs in range(2):
                for b in range(nt):
                    mk.mm(ps[:, 0:256], lhsT=bk[:, cs, b, :], rhs=YR[:, cs, b, :], start=(cs == 0 and b == 0),
                          stop=(cs == 1 and b == nt - 1))
            d_ = dz[a % 2]
            mk.tt(d_[:], zF[:, n, :], db_o, ALU.mult, en="pool")
            mk.tt(d_[:], d_[:], ps[:, 0:256], ALU.add)
            mk.tt(zout[:, n, :], d_[:], g[:], ALU.mult, en="pool")


def _hyena2(self, l):
    p, mk = self, self.mk
    with mk.scope():
        if p.bg_hgrn:
            p._bg = p.hgrn_gen(l)
            p.tick()
        p.hyena_body(l)
        while getattr(p, "_bg", None) is not None:
            p.tick()


def _hyena_body(self, l):
    p, mk = self, self.mk
    p.hy_spectrum(l)
    with mk.scope():
        z0 = mk.sb("hyz0", [128, NT, 256], F32R)
        z1 = mk.sb("hyz1", [128, NT, 256], F32R)
        db = mk.sb("hydb", [128, 2, 256])
        mk.dma("sp", db[:], p.hy_bias[l, :, :].partition_broadcast(128))
        with mk.scope():
            sw = mk.sb("hysw", [128, 6, 3])
            sbias = mk.sb("hysb", [128, 6])
            mk.dma("sp", sw[:], p.hy_swT[l, :, :, :])
            mk.dma("sp", sbias[:], p.hy_sbT[l, :, :])
            uf = [mk.sb("hyuf%d" % i, [128, T_ALL]) for i in range(2)]
            acc = [mk.sb("hyacc%d" % i, [128, T_ALL]) for i in range(2)]
            gs = [mk.sb("hygs%d" % i, [128, 128]) for i in range(3)]
            gi = 0
            for jt in range(6):
                part, ch = jt // 2, jt % 2
                u, ac = uf[jt % 2], acc[jt % 2]
                mk.dma("sp", u[:], p.uT[jt * 128:(jt + 1) * 128, :])
                mk.tsc(ac[:], u[:], sw[:, jt, 1:2], ALU.mult, sbias[:, jt:jt + 1], ALU.add)
                for (a, b) in ((0, T_LAT), (T_LAT, T_ALL)):
                    mk.stt(ac[:, a + 1:b], u[:, a:b - 1], sw[:, jt, 0:1], ac[:, a + 1:b], ALU.mult, ALU.add)
                    mk.stt(ac[:, a:b - 1], u[:, a + 1:b], sw[:, jt, 2:3], ac[:, a:b - 1], ALU.mult, ALU.add)
                for n in range(NT):
                    pt = p.psum[n % 4]
                    mk.tr(pt[:, 0:128], ac[:, n * 128:(n + 1) * 128], p.identS[:])
                    if part == 0:
                        p.evac(z0[:, n, ch * 128:(ch + 1) * 128], pt[:, 0:128])
                    else:
                        g_ = gs[gi % 3]
                        gi += 1
                        p.evac(g_[:], pt[:, 0:128])
                        mk.dma("sp", p.gtok[part - 1, n * 128:(n + 1) * 128, ch * 128:(ch + 1) * 128], g_[:])
        for o in range(2):
            zin = z0 if o == 0 else z1
            zo = z1 if o == 0 else z0
            p.hy_conv(zin, o, 16, 0, p.c_dftB, p.Ksp, p.gtok[o, :, :], db[:, o, :], zo, "hyblkL")
            p.hy_conv(zin, o, 2, 16, p.c_dftBc, p.Kspc, p.gtok[o, :, :], db[:, o, :], zo, "hyblkC")
        p.transposes_out(z0[:].bitcast(F32), 256)


Prog.hyena_body = _hyena_body


def _tick(self, n=1):
    g = getattr(self, "_bg", None)
    if g is None:
        return
    for _ in range(n):
        try:
            next(g)
        except StopIteration:
            self._bg = None
            return


Prog.tick = _tick
Prog.hy_spectrum = _hy_spectrum
Prog.hy_conv = _hy_conv
Prog.hyena = _hyena2


def _consts():
    f = np.float32
    c = {}
    s = np.arange(128)
    c["c_lstrict"] = (s[:, None] < s[None, :]).astype(f)
    c["c_iotaE"] = np.tile((np.arange(32) * CAP).astype(f)[None, :], (128, 1))
    s = np.arange(64)
    c["c_trif"] = (s[:, None] <= s[None, :]).astype(f)
    c["c_trib"] = (s[:, None] >= s[None, :]).astype(f)
    c["c_exch"] = np.eye(128, dtype=f)[::-1].copy()
    c["ident"] = np.eye(128, dtype=f)
    t = np.arange(T_LAT)
    row = (t // 64).astype(f)
    col = (t % 64).astype(f)
    inv = (f(10000.0) ** (-np.arange(0, 16, 2, dtype=f) / f(16))).astype(f)
    ar = row[:, None] * inv[None, :]
    ac = col[:, None] * inv[None, :]
    cos = np.concatenate([np.cos(ar), np.cos(ar), np.cos(ac), np.cos(ac)], axis=1).astype(f)
    sin = np.concatenate([-np.sin(ar), np.sin(ar), -np.sin(ac), np.sin(ac)], axis=1).astype(f)
    c["c_ropecos"] = np.ascontiguousarray(cos.reshape(16, 128, 32).transpose(1, 0, 2))
    c["c_ropesin"] = np.ascontiguousarray(sin.reshape(16, 128, 32).transpose(1, 0, 2))
    for nm, tn, L in (("c_feats", "c_ntun", 2048), ("c_featsc", "c_ntunc", 256)):
        tt = np.arange(L, dtype=f)
        tu = np.linspace(0.0, 1.0, L, dtype=f)
        bands = np.linspace(1e-4, 15, 16, dtype=f)
        ang = (f(2.0 * np.pi / L) * tt[:, None] * bands[None, :]).astype(f)
        feats = np.concatenate([tu[:, None], np.cos(ang), -np.sin(ang)], axis=-1).astype(f)
        fT = feats.T
        c[nm] = np.ascontiguousarray(np.stack([fT, fT[:, ::-1]], axis=0))
        c[tn] = np.ascontiguousarray(-tu.reshape(L // 128, 128).T)
        N = 2 * L
        idx = np.arange(L, dtype=np.int64)
        prod = (idx[:, None] * idx[None, :]) % N
        ang64 = prod.astype(np.float64) * (2.0 * np.pi / N)
        Cm = np.cos(ang64)
        Sm = np.sin(ang64)
        sgn = np.where(idx % 2 == 0, 1.0, -1.0)
        Sm[:, 0] = sgn
        M = np.stack([Sm, Cm, Sm.T], axis=0).astype(f)
        nt = L // 128
        Mb = M.reshape(3, nt, 128, nt, 128).transpose(3, 2, 0, 1, 4)
        c["c_dftB" if L == 2048 else "c_dftBc"] = np.ascontiguousarray(Mb)
    return c


def _na_bias_table(rpb):
    Ld, H = rpb.shape[0], rpb.shape[1]
    out = np.full((Ld, H, 5, 5, 128, 128), -30000.0, np.float32)
    pat_pr = {0: 0, 1: 1, 2: 2, 3: 14, 4: 15}
    kp = np.arange(128)
    q = np.arange(128)
    for pat, pr in pat_pr.items():
        rs0 = min(max(2 * pr - 4, 0), 24)
        ws = min((rs0 // 2) * 2, 22)
        for kt in range(5):
            krow = ws + (kt * 128 + kp) // 64
            kcol = (kt * 128 + kp) % 64
            qrow = 2 * pr + q // 64
            qcol = q % 64
            rs = np.clip(qrow - 4, 0, 24)
            cs = np.clip(qcol - 8, 0, 48)
            ok = (krow[:, None] >= rs[None, :]) & (krow[:, None] < rs[None, :] + 8) & \
                 (kcol[:, None] >= cs[None, :]) & (kcol[:, None] < cs[None, :] + 16)
            dr = np.clip(krow[:, None] - qrow[None, :] + 7, 0, 14)
            dc = np.clip(kcol[:, None] - qcol[None, :] + 15, 0, 30)
            g = rpb[:, :, dr, dc]
            out[:, :, pat, kt] = np.where(ok[None, None], g, np.float32(-30000.0))
    return np.ascontiguousarray(out.transpose(0, 1, 4, 2, 3, 5))


def host_shared(inp):
    f = np.float32
    m = dict(_consts())
    Ld = DEPTH
    m["w_ada"] = inp["w_ada"]
    m["b_adaT"] = np.ascontiguousarray(inp["b_ada"].reshape(Ld, 48, 128).transpose(0, 2, 1))
    m["n1g"] = np.ascontiguousarray(inp["norm1_g"].reshape(Ld, 8, 128).transpose(0, 2, 1))
    m["n2g"] = np.ascontiguousarray(inp["norm2_g"].reshape(Ld, 8, 128).transpose(0, 2, 1))
    m["w_in"] = inp["w_in"]
    m["w_out"] = inp["w_out"]
    m["hgrn_lb"] = inp["hgrn_lb_logits"]
    m["hgrn_norm_g"] = inp["hgrn_norm_g"]
    m["hy_swT"] = np.ascontiguousarray(inp["hy_short_w"].reshape(Ld, 3, 6, 128).transpose(0, 3, 2, 1))
    m["hy_sbT"] = np.ascontiguousarray(inp["hy_short_b"].reshape(Ld, 6, 128).transpose(0, 2, 1))
    for k in ("hy_w1", "hy_b1", "hy_freq", "hy_w2", "hy_b2", "hy_w3", "hy_bias", "hy_b3", "hy_decay", "na_q_g", "na_k_g", "mla_q_a_g",
              "mla_kv_a_g", "mla_w_uq", "mla_w_ukv", "mla_q_g", "mla_k_g", "moe_wg", "moe_bg", "moe_we", "moe_be",
              "moe_w_gate", "moe_w_up", "moe_w_down"):
        m[k] = inp[k]
    m["na_biasT"] = _na_bias_table(inp["na_rpb"])
    return m


def host_inputs(inp, b, shared):
    m = dict(shared)
    m["xin"] = np.ascontiguousarray(np.concatenate([inp["x"][b], inp["ctx"][b]], axis=0))
    m["cvecT"] = np.ascontiguousarray(np.stack([inp["c"][b], inp["c_ctx"]], axis=1))
    return m


def run_prog(inp, cores, extra=None, **kw):
    from concourse.bass_utils import run_bass_kernel_spmd
    prog = Prog(**kw)
    prog.build()
    shared = host_shared(inp)
    in_maps = []
    for b in cores:
        m = host_inputs(inp, b, shared)
        if extra:
            m.update(extra)
        in_maps.append({k: np.ascontiguousarray(m[k], dtype=np.float32) for k in prog.inputs})
    res = run_bass_kernel_spmd(prog.nc, in_maps, core_ids=list(range(len(cores))))
    return prog, res


def kernel(**inp):
    inp = {k: np.asarray(v) for k, v in inp.items()}
    prog, res = run_prog(inp, list(range(8)))
    out = np.stack([res.results[b]["xout"] for b in range(8)], axis=0)
    return out.astype(np.float32)
```

```python
import contextlib
import numpy as np
import concourse.bass as bass
import concourse.mybir as mybir

F32 = mybir.dt.float32
F32R = mybir.dt.float32r
I32 = mybir.dt.int32
U32 = mybir.dt.uint32
ALU = mybir.AluOpType
AF = mybir.ActivationFunctionType
AX = mybir.AxisListType

SEM_ROT = 30000


class MK:
    def __init__(self, nc):
        self.nc = nc
        self.root = contextlib.ExitStack()
        self.stack = [self.root]
        self.eng = {"pe": nc.tensor, "dve": nc.vector, "act": nc.scalar,
                    "pool": nc.gpsimd, "sp": nc.sync}
        self.esem = {}
        self.ecnt = {}
        self.nsem = 0
        for k in self.eng:
            self.esem[k] = self._newsem("e_" + k)
            self.ecnt[k] = 0
        self.waited = {}
        self.ts = {}
        self.uid = 0
        self.dq = {}
        for q, n in (("sp", 10), ("pool", 6), ("act", 4)):
            self.dq[q] = {"sems": [[self._newsem("d_%s%d" % (q, i)), 0] for i in range(n)], "i": 0}
        self.ninstr = 0

    def _newsem(self, name):
        self.nsem += 1
        return self.root.enter_context(self.nc.semaphore("%s_%d" % (name, self.nsem)))

    def _nm(self, name):
        self.uid += 1
        return "%s_%d" % (name, self.uid)

    def sb(self, name, shape, dtype=F32):
        return self.stack[-1].enter_context(self.nc.sbuf_tensor(self._nm(name), list(shape), dtype))

    def ps(self, name, shape, dtype=F32):
        return self.stack[-1].enter_context(self.nc.psum_tensor(self._nm(name), list(shape), dtype))

    def dram(self, name, shape, dtype=F32, kind="Internal"):
        return self.nc.dram_tensor(name, list(shape), dtype, kind=kind)

    @contextlib.contextmanager
    def scope(self):
        es = contextlib.ExitStack()
        self.stack.append(es)
        try:
            yield
        finally:
            self.barrier()
            self.stack.pop()
            es.close()

    def _wait(self, en, ev):
        if ev is None:
            return
        sem, val = ev
        if en == "pe" and sem.name.startswith("e_pe"):
            return
        key = (en, sem.name if hasattr(sem, "name") else id(sem))
        if self.waited.get(key, 0) >= val:
            return
        self.waited[key] = val
        self.eng[en].wait_ge(sem, val)

    def _tn(self, x):
        if isinstance(x, str):
            return x
        if hasattr(x, "tensor"):
            return x.tensor.name
        return x.name

    def _region(self, x):
        if isinstance(x, str) or not hasattr(x, "tensor"):
            return (self._tn(x), None)
        t = x.tensor
        name = t.name
        try:
            apl = [(int(a[0]), int(a[1])) for a in x.ap]
            off = int(x.offset)
            if any(st < 0 for st, _ in apl):
                return (name, None)
            if type(t).__name__ == "PSumTensorHandle":
                return (name, None)
            if type(t).__name__ == "DRamTensorHandle":
                ext = sum((n - 1) * st for st, n in apl)
                return (name, (0, 0, off, off + ext))
            row = 1
            for d in list(t.shape)[1:]:
                row *= int(d)
            pst, pn = apl[0]
            if pst != row:
                return (name, None)
            p0 = off // row
            f0 = off % row
            ext = sum((n - 1) * st for st, n in apl[1:])
            return (name, (p0, p0 + pn - 1, f0, f0 + ext))
        except Exception:
            return (name, None)

    @staticmethod
    def _ovl(a, b):
        if a is None or b is None:
            return True
        return not (a[1] < b[0] or b[1] < a[0] or a[3] < b[2] or b[3] < a[2])

    @staticmethod
    def _contains(a, b):
        if a is None:
            return True
        if b is None:
            return False
        return a[0] <= b[0] and a[1] >= b[1] and a[2] <= b[2] and a[3] >= b[3]

    def _deps(self, en, reads, writes):
        for r in reads:
            name, box = self._region(r)
            st = self.ts.get(name)
            if st is not None:
                for b, ev in st["w"]:
                    if self._ovl(box, b):
                        self._wait(en, ev)
        for w in writes:
            name, box = self._region(w)
            st = self.ts.get(name)
            if st is not None:
                for b, ev in st["w"]:
                    if self._ovl(box, b):
                        self._wait(en, ev)
                for b, ev in st["r"]:
                    if self._ovl(box, b):
                        self._wait(en, ev)

    @staticmethod
    def _compress(lst):
        d = {}
        for b, (s, v) in lst:
            k = id(s)
            if k not in d or d[k][1] < v:
                d[k] = (s, v)
        return [(None, ev) for ev in d.values()]

    def _record(self, ev, reads, writes):
        for r in reads:
            name, box = self._region(r)
            st = self.ts.setdefault(name, {"w": [], "r": []})
            st["r"].append((box, ev))
            if len(st["r"]) > 40:
                st["r"] = self._compress(st["r"])
        for w in writes:
            name, box = self._region(w)
            st = self.ts.setdefault(name, {"w": [], "r": []})
            st["w"] = [(b, e) for b, e in st["w"] if not self._contains(box, b)]
            st["r"] = [(b, e) for b, e in st["r"] if not self._contains(box, b)]
            st["w"].append((box, ev))
            if len(st["w"]) > 40:
                st["w"] = self._compress(st["w"])

    def op(self, en, fn, reads=(), writes=()):
        reads = [r for r in reads if r is not None and not isinstance(r, (int, float))]
        writes = [w for w in writes if w is not None]
        self._deps(en, reads, writes)
        ins = fn(self.eng[en])
        if self.ecnt[en] >= SEM_ROT:
            self.esem[en] = self._newsem("e_" + en)
            self.ecnt[en] = 0
        self.ecnt[en] += 1
        ins.then_inc(self.esem[en], 1)
        ev = (self.esem[en], self.ecnt[en])
        self._record(ev, reads, writes)
        self.ninstr += 1
        return ev

    def dma(self, q, out, in_, fn=None, extra_reads=(), **kw):
        dq = self.dq[q]
        slot = dq["sems"][dq["i"] % len(dq["sems"])]
        dq["i"] += 1
        sem, cnt = slot
        if cnt > 0:
            self._wait(q, (sem, cnt))
        reads = [in_] + list(extra_reads)
        writes = [out]
        self._deps(q, reads, writes)
        if fn is None:
            ins = self.eng[q].dma_start(out=out, in_=in_, **kw)
        else:
            ins = fn(self.eng[q])
        slot[1] = cnt + 16
        ins.then_inc(sem, 16)
        ev = (sem, slot[1])
        self._record(ev, reads, writes)
        self.ninstr += 1
        return ev

    def barrier(self):
        evs = [(self.esem[k], self.ecnt[k]) for k in self.eng if self.ecnt[k] > 0]
        for q in self.dq.values():
            for sem, cnt in q["sems"]:
                if cnt > 0:
                    evs.append((sem, cnt))
        for en in self.eng:
            for ev in evs:
                self._wait(en, ev)
        self.ts = {}

    def finish(self):
        self.barrier()
        self.root.close()

    def mm(self, out, lhsT, rhs, start=True, stop=True):
        return self.op("pe", lambda e: e.matmul(out, lhsT=lhsT, rhs=rhs, start=start, stop=stop),
                       reads=[lhsT, rhs], writes=[out])

    def tr(self, out, in_, ident):
        return self.op("pe", lambda e: e.transpose(out, in_, ident), reads=[in_, ident], writes=[out])

    def act(self, out, in_, func, bias=None, scale=1.0, accum_out=None, en="act"):
        kw = {}
        if bias is not None:
            kw["bias"] = bias
        if accum_out is not None:
            kw["accum_out"] = accum_out
        rd = [in_]
        if isinstance(bias, bass.AP):
            rd.append(bias)
        if isinstance(scale, bass.AP):
            rd.append(scale)
        return self.op("act", lambda e: e.activation(out=out, in_=in_, func=func, scale=scale, **kw),
                       reads=rd, writes=[out, accum_out])

    def tt(self, out, in0, in1, op, en="dve"):
        return self.op(en, lambda e: e.tensor_tensor(out=out, in0=in0, in1=in1, op=op),
                       reads=[in0, in1], writes=[out])

    def tsc(self, out, in0, s1, op0, s2=None, op1=None, accum_out=None, en="dve"):
        kw = {}
        if op1 is not None:
            kw["op1"] = op1
        if accum_out is not None:
            kw["accum_out"] = accum_out
        rd = [in0] + [s for s in (s1, s2) if isinstance(s, bass.AP)]
        return self.op(en, lambda e: e.tensor_scalar(out=out, in0=in0, scalar1=s1, scalar2=s2, op0=op0, **kw),
                       reads=rd, writes=[out, accum_out])

    def stt(self, out, in0, scalar, in1, op0, op1):
        rd = [in0, in1] + ([scalar] if isinstance(scalar, bass.AP) else [])
        return self.op("dve", lambda e: e.scalar_tensor_tensor(out=out, in0=in0, scalar=scalar, in1=in1,
                                                               op0=op0, op1=op1),
                       reads=rd, writes=[out])

    def copy(self, out, in_, en="dve"):
        if en == "act":
            return self.op("act", lambda e: e.copy(out=out, in_=in_), reads=[in_], writes=[out])
        return self.op(en, lambda e: e.tensor_copy(out=out, in_=in_), reads=[in_], writes=[out])

    def memset(self, out, val, en="dve"):
        return self.op(en, lambda e: e.memset(out, val), reads=[], writes=[out])

    def recip(self, out, in_):
        return self.op("dve", lambda e: e.reciprocal(out=out, in_=in_), reads=[in_], writes=[out])

    def reduce(self, out, in_, op, axis=AX.X):
        return self.op("dve", lambda e: e.tensor_reduce(out=out, in_=in_, op=op, axis=axis),
                       reads=[in_], writes=[out])


T_LAT = 2048
T_CTX = 256
T_ALL = 2304
NT = 18
D = 1024
D_IN = 3232
DEPTH = 4
TB = [(0, 512), (512, 512), (1024, 512), (1536, 512), (2048, 256)]
EPS = 1e-6


class Prog:
    def __init__(self, debug=(), depth=DEPTH, stop=None, phases=("front", "hgrn", "hyena", "na", "mla", "outproj", "moe")):
        self.phases = phases
        self.nc = bass.Bass("TRN2", target_bir_lowering=False)
        self.mk = MK(self.nc)
        self.debug = set(debug)
        self.depth = depth
        self.stop = stop
        self.inputs = {}
        self.outs = []
        self._evi = 0

    def din(self, name, shape, dtype=F32):
        t = self.nc.dram_tensor(name, list(shape), dtype, kind="ExternalInput")
        self.inputs[name] = (tuple(shape), dtype)
        return t

    def dscr(self, name, shape, dtype=F32):
        if ("in:" + name) in self.debug:
            return self.din(name, shape, dtype)
        if name in self.debug:
            self.outs.append(name)
            return self.nc.dram_tensor(name, list(shape), dtype, kind="ExternalOutput")
        return self.nc.dram_tensor(name, list(shape), dtype, kind="Internal")

    def dout(self, name, shape, dtype=F32):
        self.outs.append(name)
        return self.nc.dram_tensor(name, list(shape), dtype, kind="ExternalOutput")

    def evac(self, out, in_):
        self._evi += 1
        if self._evi % 2:
            return self.mk.copy(out, in_, en="dve")
        return self.mk.copy(out, in_, en="act")

    def declare(self):
        p = self
        L = DEPTH
        p.xin = p.din("xin", [T_ALL, D])
        p.cvecT = p.din("cvecT", [D, 2])
        p.w_ada = p.din("w_ada", [L, D, 6 * D])
        p.b_adaT = p.din("b_adaT", [L, 128, 48])
        p.n1g = p.din("n1g", [L, 128, 8])
        p.n2g = p.din("n2g", [L, 128, 8])
        p.w_in = p.din("w_in", [L, D, D_IN])
        p.w_out = p.din("w_out", [L, D, D])
        p.ident = p.din("ident", [128, 128])
        p.xT = p.dscr("xT", [D, T_ALL])
        p.ptok = p.dscr("ptok", [T_ALL, D_IN])
        p.uT = p.dscr("uT", [768, T_ALL])
        p.catT = p.dscr("catT", [D, T_ALL])
        p.h2T = p.dscr("h2T", [D, T_ALL])
        p.hof = p.dscr("hof", [T_ALL, 256])
        p.Xb = p.dscr("Xb", [NSLOT + 128, D])
        p.Yb = p.dscr("Yb", [NSLOT + 128, D])
        p.Ksp = p.dscr("Ksp", [2, 2, 2048, 256])
        p.Kspc = p.dscr("Kspc", [2, 2, 256, 256])
        p.gch = p.dscr("gch", [2, 256, T_ALL])
        p.xout = p.dout("xout", [T_LAT, D])
        for nm, shp in (("hgrn_lb", [L, 2, 256]), ("hgrn_norm_g", [L, 64]), ("hy_swT", [L, 128, 6, 3]), ("hy_sbT", [L, 128, 6]),
                        ("hy_w1", [L, 33, 64]), ("hy_b1", [L, 64]), ("hy_freq", [L, 64]), ("hy_w2", [L, 64, 64]),
                        ("hy_b2", [L, 64]), ("hy_w3", [L, 64, 1024]), ("hy_b3", [L, 1024]), ("hy_decay", [L, 2, 2, 256]),
                        ("hy_bias", [L, 2, 256]), ("na_biasT", [L, 4, 128, 5, 5, 128]), ("na_q_g", [L, 64]), ("na_k_g", [L, 64]),
                        ("mla_q_a_g", [L, 256]), ("mla_kv_a_g", [L, 128]), ("mla_w_uq", [L, 256, 384]), ("mla_w_ukv", [L, 128, 512]),
                        ("mla_q_g", [L, 96]), ("mla_k_g", [L, 96]), ("moe_wg", [L, D, 4]), ("moe_bg", [L, 4]),
                        ("moe_we", [L, D, 32]), ("moe_be", [L, 32]), ("moe_w_gate", [L, 32, D, 512]),
                        ("moe_w_up", [L, 32, D, 512]), ("moe_w_down", [L, 32, 512, D]),
                        ("c_lstrict", [128, 128]), ("c_iotaE", [128, 32]), ("c_trif", [64, 64]), ("c_trib", [64, 64]),
                        ("c_exch", [128, 128]), ("c_ropecos", [128, 16, 32]), ("c_ropesin", [128, 16, 32]),
                        ("c_feats", [2, 33, 2048]), ("c_ntun", [128, 16]), ("c_featsc", [2, 33, 256]), ("c_ntunc", [128, 2]),
                        ("c_dftB", [16, 128, 3, 16, 128]), ("c_dftBc", [2, 128, 3, 2, 128]), ("c_dftN", [2, 2048, 2048]), ("c_dftNc", [2, 256, 256]), ("hy_biasT", [L, 128, 2, 2])):
            setattr(p, nm, p.din(nm, shp))

    def build(self):
        p, mk = self, self.mk
        p.declare()
        p.identS = mk.sb("ident", [128, 128])
        mk.dma("sp", p.identS[:], p.ident[:, :])
        p.onesF = mk.sb("onesF", [128, 128])
        mk.memset(p.onesF[:], 1.0)
        p.onesR = mk.sb("onesR", [128, 128], F32R)
        mk.copy(p.onesR[:], p.onesF[:])
        p.psum = [mk.ps("ps%d" % i, [128, 512]) for i in range(8)]
        p.breg = p.nc.gpsimd.to_reg(NSLOT - 1)
        p.load_x()
        phases = p.phases
        if "moe" in phases:
            p.moe_init()
        for l in range(p.depth):
            if "front" in phases:
                p.front(l)
            else:
                p.mod = p.modulation(l)
            p.bg_hgrn = False
            for ph in ("hgrn", "hyena", "na", "mla", "outproj", "moe"):
                if ph == "hgrn" and p.bg_hgrn:
                    continue
                if ph in phases:
                    getattr(p, ph)(l)
        p.store_x()
        mk.finish()

    def load_x(self):
        p, mk = self, self.mk
        with mk.scope():
            xt = [mk.sb("xt%d" % i, [128, D]) for i in range(2)]
            xTt = mk.sb("xTt", [128, 8, 512])
            xTv = p.xT.ap().rearrange("(k q) t -> q k t", q=128)
            for (t0, w) in TB:
                for j in range(w // 128):
                    n = t0 // 128 + j
                    xb = xt[n % 2]
                    mk.dma("sp", xb[:], p.xin[n * 128:(n + 1) * 128, :])
                    for k in range(8):
                        ps = p.psum[k % 8]
                        mk.tr(ps[:, 0:128], xb[:, k * 128:(k + 1) * 128], p.identS[:])
                        p.evac(xTt[:, k, j * 128:(j + 1) * 128], ps[:, 0:128])
                mk.dma("sp", xTv[:, :, t0:t0 + w], xTt[:, :, 0:w])

    def store_x(self):
        p, mk = self, self.mk
        with mk.scope():
            xTt = mk.sb("xTt", [128, 8, 512])
            xo = [mk.sb("xo%d" % i, [128, D]) for i in range(2)]
            xTv = p.xT.ap().rearrange("(k q) t -> q k t", q=128)
            for (t0, w) in TB[:4]:
                mk.dma("sp", xTt[:, :, 0:w], xTv[:, :, t0:t0 + w])
                for j in range(w // 128):
                    n = t0 // 128 + j
                    xb = xo[n % 2]
                    for k in range(8):
                        ps = p.psum[k % 8]
                        mk.tr(ps[:, 0:128], xTt[:, k, j * 128:(j + 1) * 128], p.identS[:])
                        p.evac(xb[:, k * 128:(k + 1) * 128], ps[:, 0:128])
                    mk.dma("sp", p.xout[n * 128:(n + 1) * 128, :], xb[:])

    def modulation(self, l):
        p, mk = self, self.mk
        mod = mk.sb("mod%d" % l, [128, 48, 2])
        with mk.scope():
            cT = mk.sb("cT", [128, 8, 2])
            mk.dma("sp", cT[:], p.cvecT.ap().rearrange("(k q) r -> q k r", q=128))
            scT = mk.sb("scT", [128, 8, 2])
            mk.act(scT[:], cT[:], AF.Silu)
            bT = mk.sb("bT", [128, 48])
            mk.dma("sp", bT[:], p.b_adaT[l, :, :])
            wv = p.w_ada[l, :, :].rearrange("(k q) n -> q k n", q=128)
            wb = [mk.sb("wada%d" % i, [128, 8, 512]) for i in range(2)]
            ps = p.psum[0]
            psv = ps[:, 0:96].rearrange("q (j r) -> q j r", r=2)
            for jb in range(12):
                w = wb[jb % 2]
                mk.dma("sp" if jb % 2 == 0 else "pool", w[:], wv[:, :, jb * 512:(jb + 1) * 512])
                for jj in range(4):
                    j = jb * 4 + jj
                    for k in range(8):
                        mk.mm(psv[:, j, :], lhsT=w[:, k, jj * 128:(jj + 1) * 128], rhs=scT[:, k, :],
                              start=(k == 0), stop=(k == 7))
            for r in range(2):
                mk.tt(mod[:, :, r], psv[:, :, r], bT[:], ALU.add)
        return mod

    def front(self, l):
        p, mk = self, self.mk
        mod = p.modulation(l)
        p.mod = mod
        with mk.scope():
            g1n = mk.sb("g1n", [128, 8])
            mk.dma("sp", g1n[:], p.n1g[l, :, :])
            A1 = mk.sb("A1", [128, 8, 2])
            for r in range(2):
                mk.stt(A1[:, :, r], mod[:, 8:16, r], 1.0, g1n[:], ALU.add, ALU.mult)
            hT = mk.sb("hT", [128, 8, T_ALL], F32R)
            p.norm_mod(p.xT, A1, mod[:, 0:8, :], hT)
            p.inproj(l, hT)

    def norm_mod(self, src, A, B, hT, dst_dram=None):
        p, mk = self, self.mk
        with mk.scope():
            xb = [mk.sb("nx%d" % i, [128, 8, 512]) for i in range(2)]
            sq = mk.sb("nsq", [128, 8, 512], F32R)
            rstd = mk.sb("rstd", [128, 512])
            tmp = [mk.sb("ntmp%d" % i, [128, 512]) for i in range(2)]
            xv = src.ap().rearrange("(k q) t -> q k t", q=128)
            for bi, (t0, w) in enumerate(TB):
                r = 0 if t0 < T_LAT else 1
                x = xb[bi % 2]
                mk.dma("sp", x[:, :, 0:w], xv[:, :, t0:t0 + w])
                mk.act(sq[:, :, 0:w], x[:, :, 0:w], AF.Square)
                ps = p.psum[bi % 2]
                for k in range(8):
                    mk.mm(ps[:, 0:w], lhsT=p.onesR[:], rhs=sq[:, k, 0:w], start=(k == 0), stop=(k == 7))
                mk.act(rstd[:, 0:w], ps[:, 0:w], AF.Sqrt, bias=EPS, scale=1.0 / D)
                mk.recip(rstd[:, 0:w], rstd[:, 0:w])
                for k in range(8):
                    t = tmp[k % 2]
                    mk.tt(t[:, 0:w], x[:, k, 0:w], rstd[:, 0:w], ALU.mult)
                    mk.act(hT[:, k, t0:t0 + w], t[:, 0:w], AF.Identity, bias=B[:, k, r:r + 1], scale=A[:, k, r:r + 1])

    def inproj(self, l, hT):
        p, mk = self, self.mk
        with mk.scope():
            wv = p.w_in[l, :, :].rearrange("(k q) n -> q k n", q=128)
            wst = [mk.sb("wst%d" % i, [128, 8, 512]) for i in range(2)]
            wr = [mk.sb("wr%d" % i, [128, 8, 512], F32R) for i in range(2)]
            ob = [mk.sb("ob%d" % i, [128, 512]) for i in range(3)]
            blocks = [("tok", 0, 512), ("tok", 512, 512), ("tok", 1024, 256)]
            blocks += [("feat", 1280 + 128 * i, 128) for i in range(6)]
            blocks += [("tok", 2048, 512), ("tok", 2560, 512), ("tok", 3072, 160)]
            oi = 0
            for bi, (kind, c0, cw) in enumerate(blocks):
                ws, w = wst[bi % 2], wr[bi % 2]
                mk.dma("sp" if bi % 2 == 0 else "pool", ws[:, :, 0:cw], wv[:, :, c0:c0 + cw])
                mk.copy(w[:, :, 0:cw], ws[:, :, 0:cw], en="pool")
                if kind == "tok":
                    for n in range(NT):
                        ps = p.psum[n % 4]
                        for k in range(8):
                            mk.mm(ps[:, 0:cw], lhsT=hT[:, k, n * 128:(n + 1) * 128], rhs=w[:, k, 0:cw],
                                  start=(k == 0), stop=(k == 7))
                        o = ob[oi % 3]
                        oi += 1
                        p.evac(o[:, 0:cw], ps[:, 0:cw])
                        mk.dma("sp", p.ptok[n * 128:(n + 1) * 128, c0:c0 + cw], o[:, 0:cw])
                else:
                    f0 = c0 - 1280
                    for ti, (t0, tw) in enumerate(TB):
                        ps = p.psum[4 + ti % 4]
                        for k in range(8):
                            mk.mm(ps[:, 0:tw], lhsT=w[:, k, 0:128], rhs=hT[:, k, t0:t0 + tw],
                                  start=(k == 0), stop=(k == 7))
                        o = ob[oi % 3]
                        oi += 1
                        p.evac(o[:, 0:tw], ps[:, 0:tw])
                        mk.dma("sp", p.uT[f0:f0 + 128, t0:t0 + tw], o[:, 0:tw])


CAP = 384
NSLOT = 32 * CAP
BIGIDX = 1.0e6


def _outproj(self, l):
    p, mk = self, self.mk
    mod = p.mod
    with mk.scope():
        wv = p.w_out[l, :, :].rearrange("(k q) n -> q k n", q=128)
        wst = mk.sb("wost", [128, 8, 512])
        wR = mk.sb("woR", [128, 8, 1024], F32R)
        for hf in range(2):
            mk.dma("sp", wst[:], wv[:, :, hf * 512:(hf + 1) * 512])
            mk.copy(wR[:, :, hf * 512:(hf + 1) * 512], wst[:], en="pool")
        cst = [mk.sb("cst%d" % i, [128, 8, 512]) for i in range(2)]
        cR = mk.sb("cR", [128, 8, 512], F32R)
        xb = [mk.sb("oxb%d" % i, [128, 8, 512]) for i in range(2)]
        cv = p.catT.ap().rearrange("(k q) t -> q k t", q=128)
        xv = p.xT.ap().rearrange("(k q) t -> q k t", q=128)
        for bi, (t0, w) in enumerate(TB):
            r = 0 if t0 < T_LAT else 1
            c, x = cst[bi % 2], xb[bi % 2]
            mk.dma("sp", c[:, :, 0:w], cv[:, :, t0:t0 + w])
            mk.dma("pool", x[:, :, 0:w], xv[:, :, t0:t0 + w])
            mk.copy(cR[:, :, 0:w], c[:, :, 0:w], en="act")
            for j in range(8):
                ps = p.psum[j % 4]
                for k in range(8):
                    mk.mm(ps[:, 0:w], lhsT=wR[:, k, j * 128:(j + 1) * 128], rhs=cR[:, k, 0:w],
                          start=(k == 0), stop=(k == 7))
                mk.stt(x[:, j, 0:w], ps[:, 0:w], mod[:, 16 + j, r:r + 1], x[:, j, 0:w], ALU.mult, ALU.add)
            mk.dma("sp", xv[:, :, t0:t0 + w], x[:, :, 0:w])


def _moe(self, l):
    p, mk = self, self.mk
    mod = p.mod
    with mk.scope():
        rt = mk.sb("rt", [128, NT, 8])
        gidx = mk.sb("gidx", [128, NT, 2], I32)
        with mk.scope():
            g2n = mk.sb("g2n", [128, 8])
            mk.dma("sp", g2n[:], p.n2g[l, :, :])
            A2 = mk.sb("A2", [128, 8, 2])
            for r in range(2):
                mk.stt(A2[:, :, r], mod[:, 32:40, r], 1.0, g2n[:], ALU.add, ALU.mult)
            B2 = mod[:, 24:32, :]
            wrt = mk.sb("wrt", [128, 8, 36])
            mk.dma("sp", wrt[:, :, 0:4], p.moe_wg[l, :, :].rearrange("(k q) n -> q k n", q=128))
            mk.dma("sp", wrt[:, :, 4:36], p.moe_we[l, :, :].rearrange("(k q) n -> q k n", q=128))
            bias = mk.sb("rbias", [128, 36])
            mk.dma("sp", bias[:, 0:4], p.moe_bg[l, :].partition_broadcast(128))
            mk.dma("sp", bias[:, 4:36], p.moe_be[l, :].partition_broadcast(128))
            lstrict = mk.sb("lstrict", [128, 128])
            mk.dma("sp", lstrict[:], p.c_lstrict[:, :])
            iotaE = mk.sb("iotaE", [128, 32])
            mk.dma("sp", iotaE[:], p.c_iotaE[:, :])
            xb = [mk.sb("mx%d" % i, [128, 8, 512]) for i in range(2)]
            sq = mk.sb("msq", [128, 8, 512], F32R)
            rstd = mk.sb("mrstd", [128, 512])
            hb = mk.sb("mhb", [128, 8, 512])
            htok = mk.sb("htok", [128, NT, D])
            lr = mk.sb("r_lr", [128, NT, 36])
            xv = p.xT.ap().rearrange("(k q) t -> q k t", q=128)
            for bi, (t0, w) in enumerate(TB):
                r = 0 if t0 < T_LAT else 1
                x = xb[bi % 2]
                mk.dma("sp", x[:, :, 0:w], xv[:, :, t0:t0 + w])
                mk.act(sq[:, :, 0:w], x[:, :, 0:w], AF.Square)
                ps = p.psum[0]
                for k in range(8):
                    mk.mm(ps[:, 0:w], lhsT=p.onesR[:], rhs=sq[:, k, 0:w], start=(k == 0), stop=(k == 7))
                mk.act(rstd[:, 0:w], ps[:, 0:w], AF.Sqrt, bias=EPS, scale=1.0 / D)
                mk.recip(rstd[:, 0:w], rstd[:, 0:w])
                for k in range(8):
                    mk.tt(hb[:, k, 0:w], x[:, k, 0:w], rstd[:, 0:w], ALU.mult)
                    mk.act(hb[:, k, 0:w], hb[:, k, 0:w], AF.Identity, bias=B2[:, k, r:r + 1], scale=A2[:, k, r:r + 1])
                for j in range(w // 128):
                    n = t0 // 128 + j
                    sl = slice(j * 128, (j + 1) * 128)
                    pl = p.psum[1]
                    for k in range(8):
                        mk.mm(pl[:, 0:36], lhsT=hb[:, k, sl], rhs=wrt[:, k, :], start=(k == 0), stop=(k == 7))
                    mk.tt(lr[:, n, :], pl[:, 0:36], bias[:], ALU.add)
                    for k in range(8):
                        pt = p.psum[2 + k % 4]
                        mk.tr(pt[:, 0:128], hb[:, k, sl], p.identS[:])
                        p.evac(htok[:, n, k * 128:(k + 1) * 128], pt[:, 0:128])
            R = lambda nm, shp: mk.sb("r_" + nm, [128] + shp)
            gmax, gsum, psel = R("gmax", [NT]), R("gsum", [NT]), R("psel", [NT])
            eg, ohg, pen = R("eg", [NT, 4]), R("ohg", [NT, 4]), R("pen", [NT, 4])
            lem, lem2 = R("lem", [NT, 32]), R("lem2", [NT, 32])
            m1, m2, dm, ee, w1 = R("m1", [NT]), R("m2", [NT]), R("dm", [NT]), R("e", [NT]), R("w1", [NT])
            oh1, oh2, oh, cnt, t32 = R("oh1", [NT, 32]), R("oh2", [NT, 32]), R("oh", [NT, 32]), R("cnt", [NT, 32]), R("t32", [NT, 32])
            s_, f__, gs = R("s", [NT, 2]), R("f", [NT, 2]), R("gs", [NT, 2])
            bc3 = lambda a, k: a[:].unsqueeze(2).to_broadcast([128, NT, k])
            lg4 = lr[:, :, 0:4]
            mk.reduce(gmax[:], lg4, ALU.max)
            mk.tt(eg[:], lg4, bc3(gmax, 4), ALU.subtract)
            mk.act(eg[:], eg[:], AF.Exp)
            mk.reduce(gsum[:], eg[:], ALU.add)
            mk.recip(psel[:], gsum[:])
            mk.tt(ohg[:], lg4, bc3(gmax, 4), ALU.is_equal)
            mk.tsc(pen[:], ohg[:], 1.0e9, ALU.mult, -1.0e9, ALU.add)
            mk.tt(lem[:].rearrange("q n (g e) -> q n g e", g=4), lr[:, :, 4:36].rearrange("q n (g e) -> q n g e", g=4),
                  pen[:].unsqueeze(3).to_broadcast([128, NT, 4, 8]), ALU.add)
            mk.reduce(m1[:], lem[:], ALU.max)
            mk.tt(oh1[:], lem[:], bc3(m1, 32), ALU.is_equal)
            mk.stt(lem2[:], oh1[:], -2.0e9, lem[:], ALU.mult, ALU.add)
            mk.reduce(m2[:], lem2[:], ALU.max)
            mk.tt(oh2[:], lem2[:], bc3(m2, 32), ALU.is_equal)
            mk.tt(oh[:], oh1[:], oh2[:], ALU.add)
            mk.tt(dm[:], m2[:], m1[:], ALU.subtract)
            mk.act(ee[:], dm[:], AF.Exp)
            mk.tsc(w1[:], ee[:], 1.0, ALU.add)
            mk.recip(w1[:], w1[:])
            mk.tt(rt[:, :, 0], w1[:], psel[:], ALU.mult)
            mk.tt(rt[:, :, 1], rt[:, :, 0], ee[:], ALU.mult)
            pcs = [p.psum[6], p.psum[7]]
            for n in range(NT):
                pc = pcs[n // 9][:, (n % 9) * 32:(n % 9 + 1) * 32]
                mk.mm(pc, lhsT=lstrict[:], rhs=oh[:, n, :], start=True, stop=(n == 0))
                for m_ in range(n):
                    mk.mm(pc, lhsT=p.onesF[:], rhs=oh[:, m_, :], start=False, stop=(m_ == n - 1))
            pos = R("pos", [NT, 32])
            for hf in range(2):
                mk.copy(pos[:, hf * 9:(hf + 1) * 9, :].rearrange("q n e -> q (n e)"), pcs[hf][:, 0:288])
            mk.tt(cnt[:], pos[:], iotaE[:].unsqueeze(1).to_broadcast([128, NT, 32]), ALU.add)
            for kk, ohk in enumerate((oh1, oh2)):
                mk.tt(t32[:], ohk[:], cnt[:], ALU.mult)
                mk.reduce(s_[:, :, kk], t32[:], ALU.add)
                mk.tt(t32[:], ohk[:], pos[:], ALU.mult)
                mk.reduce(f__[:, :, kk], t32[:], ALU.add)
            mk.tsc(f__[:], f__[:], float(CAP) - 0.5, ALU.is_gt, BIGIDX, ALU.mult)
            mk.tt(gs[:], s_[:], f__[:], ALU.add)
            sidx = mk.sb("sidx", [128, NT, 2], I32)
            mk.copy(sidx[:], gs[:])
            mk.tsc(gs[:], gs[:], float(NSLOT), ALU.min)
            mk.copy(gidx[:], gs[:])
            for n in range(NT):
                for kk in range(2):
                    mk.dma("pool", p.Xb[:, :], htok[:, n, :],
                           fn=lambda e, kk=kk, n=n: e.indirect_dma_start(
                               out=p.Xb[:, :], out_offset=bass.IndirectOffsetOnAxis(ap=sidx[:, n, kk:kk + 1], axis=0),
                               in_=htok[:, n, :], in_offset=None, bounds_check=p.breg, oob_is_err=False),
                           extra_reads=[sidx])
        if getattr(p, 'moe_stop', 3) < 2:
            return
        with mk.scope():
            W = [{"g": mk.sb("WgR%d" % i, [128, 8, 512], F32R), "u": mk.sb("WuR%d" % i, [128, 8, 512], F32R),
                  "d": mk.sb("WdR%d" % i, [128, 4, 1024], F32R)} for i in range(2)]
            NS = CAP // 128
            xtok = [mk.sb("extok%d" % i, [128, D]) for i in range(2 * NS)]
            xbT = [mk.sb("xbT%d" % i, [128, 8, CAP], F32R) for i in range(2)]
            hidT = mk.sb("hidT", [128, 4, CAP], F32R)
            sil = [mk.sb("sil%d" % i, [128, CAP]) for i in range(2)]
            yt = [mk.sb("eyt%d" % i, [128, D]) for i in range(3)]
            yi = 0

            def load_w(e):
                w = W[e % 2]
                mk.dma("pool", w["g"][:], p.moe_w_gate[l, e, :, :].rearrange("(k q) n -> q k n", q=128))
                mk.dma("pool", w["u"][:], p.moe_w_up[l, e, :, :].rearrange("(k q) n -> q k n", q=128))
                mk.dma("pool", w["d"][:], p.moe_w_down[l, e, :, :].rearrange("(k q) n -> q k n", q=128))

            def load_x(e):
                for si_ in range(NS):
                    r0 = e * CAP + si_ * 128
                    mk.dma("sp", xtok[(e % 2) * NS + si_][:], p.Xb[r0:r0 + 128, :])

            load_w(0)
            load_x(0)
            for e in range(32):
                if e + 1 < 32:
                    load_w(e + 1)
                    load_x(e + 1)
                w = W[e % 2]
                xT_ = xbT[e % 2]
                for si_ in range(NS):
                    xt = xtok[(e % 2) * NS + si_]
                    for k in range(8):
                        pt = p.psum[k % 4]
                        mk.tr(pt[:, 0:128], xt[:, k * 128:(k + 1) * 128], p.identS[:])
                        p.evac(xT_[:, k, si_ * 128:(si_ + 1) * 128], pt[:, 0:128])
                for f in range(4):
                    pg, pu = p.psum[4 + (f % 2) * 2], p.psum[5 + (f % 2) * 2]
                    for k in range(8):
                        mk.mm(pg[:, 0:CAP], lhsT=w["g"][:, k, f * 128:(f + 1) * 128], rhs=xT_[:, k, :], start=(k == 0), stop=(k == 7))
                    for k in range(8):
                        mk.mm(pu[:, 0:CAP], lhsT=w["u"][:, k, f * 128:(f + 1) * 128], rhs=xT_[:, k, :], start=(k == 0), stop=(k == 7))
                    sl_ = sil[f % 2]
                    mk.act(sl_[:], pg[:, 0:CAP], AF.Silu)
                    mk.tt(hidT[:, f, :], sl_[:], pu[:, 0:CAP], ALU.mult)
                for si_ in range(NS):
                    y = yt[yi % 3]
                    yi += 1
                    for hf in range(2):
                        ps = p.psum[hf]
                        for f in range(4):
                            mk.mm(ps[:, 0:512], lhsT=hidT[:, f, si_ * 128:(si_ + 1) * 128], rhs=w["d"][:, f, hf * 512:(hf + 1) * 512],
                                  start=(f == 0), stop=(f == 3))
                        p.evac(y[:, hf * 512:(hf + 1) * 512], ps[:, 0:512])
                    r0 = e * CAP + si_ * 128
                    mk.dma("act", p.Yb[r0:r0 + 128, :], y[:])
        if getattr(p, 'moe_stop', 3) < 3:
            return
        with mk.scope():
            y1 = [mk.sb("cy1%d" % i, [128, D]) for i in range(2)]
            y2 = [mk.sb("cy2%d" % i, [128, D]) for i in range(2)]
            xb = [mk.sb("cxb%d" % i, [128, 8, 512]) for i in range(2)]
            xv = p.xT.ap().rearrange("(k q) t -> q k t", q=128)
            for bi, (t0, w) in enumerate(TB):
                r = 0 if t0 < T_LAT else 1
                x = xb[bi % 2]
                mk.dma("sp", x[:, :, 0:w], xv[:, :, t0:t0 + w])
                for j in range(w // 128):
                    n = t0 // 128 + j
                    a, b = y1[n % 2], y2[n % 2]
                    for kk, dst in enumerate((a, b)):
                        mk.dma("pool", dst[:], p.Yb[:, :],
                               fn=lambda e, kk=kk, dst=dst, n=n: e.indirect_dma_start(
                                   out=dst[:], out_offset=None, in_=p.Yb[:, :],
                                   in_offset=bass.IndirectOffsetOnAxis(ap=gidx[:, n, kk:kk + 1], axis=0)),
                               extra_reads=[gidx])
                    mk.tsc(a[:], a[:], rt[:, n, 0:1], ALU.mult)
                    mk.stt(a[:], b[:], rt[:, n, 1:2], a[:], ALU.mult, ALU.add)
                    for k in range(8):
                        pt = p.psum[k % 4]
                        mk.tr(pt[:, 0:128], a[:, k * 128:(k + 1) * 128], p.identS[:])
                        mk.stt(x[:, k, j * 128:(j + 1) * 128], pt[:, 0:128], mod[:, 40 + k, r:r + 1],
                               x[:, k, j * 128:(j + 1) * 128], ALU.mult, ALU.add)
                mk.dma("sp", xv[:, :, t0:t0 + w], x[:, :, 0:w])


def _moe_init(self):
    p, mk = self, self.mk
    with mk.scope():
        z = mk.sb("zeros", [128, D])
        mk.memset(z[:], 0.0)
        for i in range((NSLOT + 128) // 128):
            mk.dma("sp" if i % 2 == 0 else "pool", p.Xb[i * 128:(i + 1) * 128, :], z[:])
        for i in range((NSLOT + 128) // 128):
            mk.dma("sp" if i % 2 == 0 else "pool", p.Yb[i * 128:(i + 1) * 128, :], z[:])


Prog.outproj = _outproj
Prog.moe = _moe
Prog.moe_init = _moe_init

NA_SCALE = 64 ** -0.5
MLA_SCALE = 96 ** -0.5


def _headnorm(self, out, x, nh, hd, gain_bc, sq, ss, np_=128):
    mk = self.mk
    xv = x.rearrange("q (h d) -> q h d", h=nh)
    sqv = sq.rearrange("q (h d) -> q h d", h=nh)
    mk.tt(sq, x, x, ALU.mult)
    mk.reduce(ss, sqv, ALU.add)
    mk.act(ss, ss, AF.Sqrt, bias=EPS, scale=1.0 / hd)
    mk.recip(ss, ss)
    mk.tt(sqv, xv, ss.unsqueeze(2).to_broadcast([np_, nh, hd]), ALU.mult)
    mk.tt(out, sq, gain_bc, ALU.mult)


def _transposes_out(self, src_all, row0):
    p, mk = self, self.mk
    with mk.scope():
        ob = [mk.sb("tob%d" % i, [128, 512]) for i in range(2)]
        oi = 0
        for c in range(2):
            for bi, (t0, w) in enumerate(TB):
                o = ob[oi % 2]
                oi += 1
                for j in range(w // 128):
                    n = t0 // 128 + j
                    pt = p.psum[(n + c) % 4]
                    mk.tr(pt[:, 0:128], src_all[:, n, c * 128:(c + 1) * 128], p.identS[:])
                    p.evac(o[:, j * 128:(j + 1) * 128], pt[:, 0:128])
                mk.dma("sp", p.catT[row0 + c * 128:row0 + (c + 1) * 128, t0:t0 + w], o[:, 0:w])


def _na(self, l):
    p, mk = self, self.mk
    with mk.scope():
        qT = mk.sb("naqT", [128, 2, T_ALL], F32R)
        kT = mk.sb("nakT", [128, 2, T_ALL], F32R)
        V = mk.sb("naV", [128, NT, 4, 66], F32R)
        out_all = mk.sb("naout", [128, NT, 256])
        with mk.scope():
            gq = mk.sb("nagq", [128, 4, 64])
            gk = mk.sb("nagk", [128, 4, 64])
            for h in range(4):
                mk.dma("sp", gq[:, h, :], p.na_q_g[l, :].partition_broadcast(128))
                mk.dma("sp", gk[:, h, :], p.na_k_g[l, :].partition_broadcast(128))
            ones65 = mk.sb("ones65", [128, 4, 2])
            mk.memset(ones65[:], 0.0)
            mk.memset(ones65[:, :, 0:1], 1.0)
            tin = [mk.sb("natin%d" % i, [128, 768]) for i in range(2)]
            sq = mk.sb("nasq", [128, 256])
            ss = mk.sb("nass", [128, 4])
            qn = mk.sb("naqn", [128, 256])
            for n in range(NT):
                t = tin[n % 2]
                mk.dma("sp", t[:], p.ptok[n * 128:(n + 1) * 128, 2048:2816])
                for (src, g, dstT) in ((t[:, 0:256], gq, qT), (t[:, 256:512], gk, kT)):
                    p.headnorm(qn[:], src, 4, 64, g[:].rearrange("q h d -> q (h d)"), sq[:], ss[:])
                    for c in range(2):
                        pt = p.psum[c]
                        mk.tr(pt[:, 0:128], qn[:, c * 128:(c + 1) * 128], p.identS[:])
                        p.evac(dstT[:, c, n * 128:(n + 1) * 128], pt[:, 0:128])
                mk.copy(V[:, n, :, 0:64], t[:, 512:768].rearrange("q (h d) -> q h d", h=4), en="pool")
                mk.copy(V[:, n, :, 64:66], ones65[:], en="pool")
        with mk.scope():
            bias = [mk.sb("nabias%d" % i, [128, 5, 5, 128]) for i in range(2)]
            PT = [mk.sb("naPT%d" % i, [128, 7, 128], F32R) for i in range(2)]
            tmpb = [mk.sb("natmp%d" % i, [128, 5, 128]) for i in range(2)]
            rec = mk.sb("narec", [128, 1])
            PTc = mk.sb("naPTc", [128, 2, 256], F32R)
            it = 0
            for h in range(4):
                hb, hc = (h % 2) * 64, h // 2
                bs = bias[h % 2]
                mk.dma("sp", bs[:], p.na_biasT[l, h, :, :, :, :])
                for pr in range(16):
                    pat = 0 if pr == 0 else 1 if pr == 1 else 3 if pr == 14 else 4 if pr == 15 else 2
                    rs0 = min(max(2 * pr - 4, 0), 24)
                    ws = min((rs0 // 2) * 2, 22)
                    kt0 = ws // 2
                    q0 = pr * 128
                    pa, pb = p.psum[(it % 2) * 2], p.psum[(it % 2) * 2 + 1]
                    P_, tb = PT[it % 2], tmpb[it % 2]
                    for kt in range(4):
                        mk.mm(pa[:, kt * 128:(kt + 1) * 128], lhsT=kT[hb:hb + 64, hc, (kt0 + kt) * 128:(kt0 + kt + 1) * 128],
                              rhs=qT[hb:hb + 64, hc, q0:q0 + 128])
                    mk.mm(pb[:, 0:128], lhsT=kT[hb:hb + 64, hc, (kt0 + 4) * 128:(kt0 + 5) * 128], rhs=qT[hb:hb + 64, hc, q0:q0 + 128])
                    for j in range(2):
                        mk.mm(pb[:, 128 + j * 128:256 + j * 128], lhsT=kT[hb:hb + 64, hc, T_LAT + j * 128:T_LAT + (j + 1) * 128],
                              rhs=qT[hb:hb + 64, hc, q0:q0 + 128])
                    mk.stt(tb[:, 0:4, :], pa[:, 0:512].rearrange("q (a b) -> q a b", a=4), NA_SCALE, bs[:, pat, 0:4, :], ALU.mult, ALU.add)
                    mk.stt(tb[:, 4, :], pb[:, 0:128], NA_SCALE, bs[:, pat, 4, :], ALU.mult, ALU.add)
                    mk.act(P_[:, 0:5, :], tb[:], AF.Exp)
                    mk.act(P_[:, 5:7, :], pb[:, 128:384].rearrange("q (a b) -> q a b", a=2), AF.Exp, scale=NA_SCALE)
                    po = p.psum[4 + it % 2]
                    for kt in range(7):
                        vt = kt0 + kt if kt < 5 else 16 + (kt - 5)
                        mk.mm(po[:, 0:66], lhsT=P_[:, kt, :], rhs=V[:, vt, h, :], start=(kt == 0), stop=(kt == 6))
                    mk.recip(rec[:], po[:, 64:65])
                    mk.tsc(out_all[:, pr, h * 64:(h + 1) * 64], po[:, 0:64], rec[:, 0:1], ALU.mult)
                    it += 1
                pc_ = p.psum[6]
                for j in range(2):
                    mk.mm(pc_[:, j * 256:(j + 1) * 256], lhsT=kT[hb:hb + 64, hc, T_LAT + j * 128:T_LAT + (j + 1) * 128],
                          rhs=qT[hb:hb + 64, hc, T_LAT:T_ALL])
                mk.act(PTc[:], pc_[:, 0:512].rearrange("q (a b) -> q a b", a=2), AF.Exp, scale=NA_SCALE)
                for qi in range(2):
                    po = p.psum[7]
                    for j in range(2):
                        mk.mm(po[:, 0:66], lhsT=PTc[:, j, qi * 128:(qi + 1) * 128], rhs=V[:, 16 + j, h, :], start=(j == 0), stop=(j == 1))
                    mk.recip(rec[:], po[:, 64:65])
                    mk.tsc(out_all[:, 16 + qi, h * 64:(h + 1) * 64], po[:, 0:64], rec[:, 0:1], ALU.mult)
        p.transposes_out(out_all, 512)


def _mla(self, l):
    p, mk = self, self.mk
    with mk.scope():
        qT = mk.sb("mlqT", [96, 4, T_ALL], F32R)
        kT = mk.sb("mlkT", [96, 4, T_ALL], F32R)
        V = mk.sb("mlV", [128, NT, 4, 66], F32R)
        out_all = mk.sb("mlout", [128, NT, 256])
        with mk.scope():
            cqT = mk.sb("cqT", [128, 2, T_ALL], F32R)
            ckvT = mk.sb("ckvT", [128, T_ALL], F32R)
            gqa = mk.sb("gqa", [128, 256])
            gkva = mk.sb("gkva", [128, 128])
            mk.dma("sp", gqa[:], p.mla_q_a_g[l, :].partition_broadcast(128))
            mk.dma("sp", gkva[:], p.mla_kv_a_g[l, :].partition_broadcast(128))
            gq = mk.sb("mgq", [128, 4, 96])
            gk = mk.sb("mgk", [128, 4, 96])
            for h in range(4):
                mk.dma("sp", gq[:, h, :], p.mla_q_g[l, :].partition_broadcast(128))
                mk.dma("sp", gk[:, h, :], p.mla_k_g[l, :].partition_broadcast(128))
            ones65 = mk.sb("mones65", [128, 4, 2])
            mk.memset(ones65[:], 0.0)
            mk.memset(ones65[:, :, 0:1], 1.0)
            wst = mk.sb("mwst", [128, 768])
            wuq = mk.sb("wuq", [128, 2, 384], F32R)
            wukv = mk.sb("wukv", [128, 512], F32R)
            mk.dma("sp", wst[:].rearrange("q (k n) -> q k n", k=2), p.mla_w_uq[l, :, :].rearrange("(k q) n -> q k n", q=128))
            mk.copy(wuq[:], wst[:].rearrange("q (k n) -> q k n", k=2), en="pool")
            mk.dma("sp", wst[:, 0:512], p.mla_w_ukv[l, :, :])
            mk.copy(wukv[:], wst[:, 0:512], en="pool")
            cosT = mk.sb("ropec", [128, 16, 32])
            sinT = mk.sb("ropes", [128, 16, 32])
            mk.dma("sp", cosT[:], p.c_ropecos[:, :, :])
            mk.dma("sp", sinT[:], p.c_ropesin[:, :, :])
            tin = [mk.sb("mltin%d" % i, [128, 416]) for i in range(2)]
            sq = mk.sb("mlsq", [128, 384])
            ss = mk.sb("mlss", [128, 4])
            nrm = mk.sb("mlnrm", [128, 384])
            for n in range(NT):
                t = tin[n % 2]
                mk.dma("sp", t[:], p.ptok[n * 128:(n + 1) * 128, 2816:3232])
                p.headnorm(nrm[:, 0:256], t[:, 0:256], 1, 256, gqa[:], sq[:, 0:256], ss[:, 0:1])
                for c in range(2):
                    pt = p.psum[c]
                    mk.tr(pt[:, 0:128], nrm[:, c * 128:(c + 1) * 128], p.identS[:])
                    p.evac(cqT[:, c, n * 128:(n + 1) * 128], pt[:, 0:128])
                p.headnorm(nrm[:, 256:384], t[:, 256:384], 1, 128, gkva[:], sq[:, 0:128], ss[:, 0:1])
                pt = p.psum[2]
                mk.tr(pt[:, 0:128], nrm[:, 256:384], p.identS[:])
                p.evac(ckvT[:, n * 128:(n + 1) * 128], pt[:, 0:128])
            qk = [mk.sb("mlqk%d" % i, [128, 384]) for i in range(2)]
            sw = mk.sb("mlsw", [128, 4, 32])
            for n in range(NT):
                t = tin[n % 2]
                mk.dma("sp", t[:, 384:416], p.ptok[n * 128:(n + 1) * 128, 3200:3232])
                pq, pkv = p.psum[3], p.psum[4]
                for c in range(2):
                    mk.mm(pq[:, 0:384], lhsT=cqT[:, c, n * 128:(n + 1) * 128], rhs=wuq[:, c, :], start=(c == 0), stop=(c == 1))
                mk.mm(pkv[:, 0:512], lhsT=ckvT[:, n * 128:(n + 1) * 128], rhs=wukv[:], start=True, stop=True)
                kvv = pkv[:, 0:512].rearrange("q (h d) -> q h d", h=4)
                mk.copy(V[:, n, :, 0:64], kvv[:, :, 64:128], en="act")
                mk.copy(V[:, n, :, 64:66], ones65[:], en="pool")
                for which in range(2):
                    raw = qk[which]
                    rv = raw[:].rearrange("q (h d) -> q h d", h=4)
                    if which == 0:
                        mk.copy(raw[:], pq[:, 0:384])
                        g, dstT = gq, qT
                    else:
                        mk.copy(rv[:, :, 0:64], kvv[:, :, 0:64])
                        mk.copy(rv[:, :, 64:96], t[:, 384:416].unsqueeze(1).to_broadcast([128, 4, 32]), en="pool")
                        g, dstT = gk, kT
                    p.headnorm(nrm[:], raw[:], 4, 96, g[:].rearrange("q h d -> q (h d)"), sq[:], ss[:])
                    nv = nrm[:].rearrange("q (h d) -> q h d", h=4)
                    if n < 16:
                        for s0 in (64, 80):
                            o0 = s0 - 64
                            mk.copy(sw[:, :, o0:o0 + 8], nv[:, :, s0 + 8:s0 + 16])
                            mk.copy(sw[:, :, o0 + 8:o0 + 16], nv[:, :, s0:s0 + 8])
                        mk.tt(sw[:], sw[:], sinT[:, n, :].unsqueeze(1).to_broadcast([128, 4, 32]), ALU.mult)
                        mk.tt(nv[:, :, 64:96], nv[:, :, 64:96], cosT[:, n, :].unsqueeze(1).to_broadcast([128, 4, 32]), ALU.mult)
                        mk.tt(nv[:, :, 64:96], nv[:, :, 64:96], sw[:], ALU.add)
                    for h in range(4):
                        pt = p.psum[5 + h % 2]
                        mk.tr(pt[0:96, 0:128], nrm[:, h * 96:(h + 1) * 96], p.identS[:])
                        p.evac(dstT[:, h, n * 128:(n + 1) * 128], pt[0:96, 0:128])
        with mk.scope():
            PT = mk.sb("mlPT", [128, NT, 512], F32R)
            rec = mk.sb("mlrec", [128, 1])
            for h in range(4):
                for bi, (t0, w) in enumerate(TB):
                    kts = list(range(NT)) if t0 < T_LAT else [16, 17]
                    for i, kt in enumerate(kts):
                        ps = p.psum[i % 4]
                        mk.mm(ps[:, 0:w], lhsT=kT[:, h, kt * 128:(kt + 1) * 128], rhs=qT[:, h, t0:t0 + w])
                        mk.act(PT[:, i, 0:w], ps[:, 0:w], AF.Exp, scale=MLA_SCALE)
                    for j in range(w // 128):
                        n = t0 // 128 + j
                        po = p.psum[4 + j % 2]
                        for i, kt in enumerate(kts):
                            mk.mm(po[:, 0:66], lhsT=PT[:, i, j * 128:(j + 1) * 128], rhs=V[:, kt, h, :],
                                  start=(i == 0), stop=(i == len(kts) - 1))
                        mk.recip(rec[:], po[:, 64:65])
                        mk.tsc(out_all[:, n, h * 64:(h + 1) * 64], po[:, 0:64], rec[:, 0:1], ALU.mult)
        p.transposes_out(out_all, 768)


Prog.headnorm = _headnorm
Prog.transposes_out = _transposes_out
Prog.na = _na
Prog.mla = _mla


def _hgrn_gen(self, l):
    p, mk = self, self.mk
    if True:
        lb = mk.sb("lb", [64, 512])
        oml = mk.sb("oml", [64, 512])
        if True:
            lg = mk.sb("lblg", [64, 4, 512])
            mk.dma("sp", lg[:], p.hgrn_lb.ap().rearrange("l a b -> l (a b)").partition_broadcast(64))
            mk.act(lg[:], lg[:], AF.Exp)
            tot = mk.sb("lbtot", [64, 512])
            mk.tt(tot[:], lg[:, 0, :], lg[:, 1, :], ALU.add)
            mk.tt(tot[:], tot[:], lg[:, 2, :], ALU.add)
            mk.tt(tot[:], tot[:], lg[:, 3, :], ALU.add)
            mk.recip(tot[:], tot[:])
            mk.memset(lb[:], 0.0)
            for ll in range(1, l + 1):
                mk.tt(lb[:], lb[:], lg[:, ll, :], ALU.add)
            mk.tt(lb[:], lb[:], tot[:], ALU.mult)
            mk.tsc(oml[:], lb[:], -1.0, ALU.mult, 1.0, ALU.add)
        gn = mk.sb("hgn", [64, 4, 64])
        for h in range(4):
            mk.dma("sp", gn[:, h, :], p.hgrn_norm_g[l, :].partition_broadcast(64))
        tri = [mk.sb("tri%d" % i, [64, 64]) for i in range(2)]
        mk.dma("sp", tri[0][:], p.c_trif[:, :])
        mk.dma("sp", tri[1][:], p.c_trib[:, :])
        ones1 = mk.sb("hones1", [64, 1])
        mk.memset(ones1[:], 1.0)
        S = mk.sb("hS", [64, 4, 64])
        tin = [mk.sb("htin%d" % i, [64, 1280]) for i in range(2)]
        NB = 2
        f_ = [mk.sb("hf%d" % i, [64, 256]) for i in range(NB)]
        lf = [mk.sb("hlf%d" % i, [64, 256]) for i in range(NB)]
        kk = [mk.sb("hkk%d" % i, [64, 256]) for i in range(NB)]
        bc = [mk.sb("hbc%d" % i, [64, 256]) for i in range(NB)]
        eb = [mk.sb("heb%d" % i, [64, 256]) for i in range(NB)]
        qe = [mk.sb("hqe%d" % i, [64, 256]) for i in range(NB)]
        ke = [mk.sb("hke%d" % i, [64, 256]) for i in range(NB)]
        qeT = [mk.sb("hqeT%d" % i, [64, 4, 64]) for i in range(NB)]
        keT = [mk.sb("hkeT%d" % i, [64, 4, 64]) for i in range(NB)]
        ebl = [mk.sb("hebl%d" % i, [64, 4]) for i in range(NB)]
        ATm = [mk.sb("hAT%d" % i, [64, 4, 64]) for i in range(NB)]
        osb = [mk.sb("hosb%d" % i, [64, 256]) for i in range(NB)]
        of_ = [mk.sb("hof%d" % i, [64, 256]) for i in range(NB)]
        sq = mk.sb("hsq", [64, 256])
        ss = mk.sb("hss", [64, 4])
        sg = mk.sb("hsg", [64, 256])
        oT = [mk.sb("hoT%d" % i, [128, 2, 64]) for i in range(NB)]
        tmpS = mk.sb("htmpS", [64, 4, 64])
        it = 0
        for d in range(2):
            mk.memset(S[:], 0.0)
            if d == 0:
                order = [(T_LAT + 64 * c) for c in range(4)] + [64 * c for c in range(32)]
            else:
                order = [(T_LAT + 64 * c) for c in reversed(range(4))] + [64 * c for c in reversed(range(32))]
            for tok0 in order:
                i = it % NB
                it += 1
                t = tin[i]
                mk.dma("sp", t[:], p.ptok[tok0:tok0 + 64, 0:1280])
                z = t[:, 256 + 256 * d:512 + 256 * d]
                mk.act(f_[i][:], z, AF.Sigmoid)
                mk.tt(f_[i][:], f_[i][:], oml[:, d * 256:(d + 1) * 256], ALU.mult)
                mk.tt(f_[i][:], f_[i][:], lb[:, d * 256:(d + 1) * 256], ALU.add)
                mk.act(lf[i][:], f_[i][:], AF.Ln)
                mk.tsc(kk[i][:], f_[i][:], -1.0, ALU.mult, 1.0, ALU.add)
                pb = p.psum[0]
                mk.mm(pb[0:64, 0:256], lhsT=tri[d][:], rhs=lf[i][:])
                mk.tsc(bc[i][:], pb[0:64, 0:256], -80.0, ALU.max)
                pl = p.psum[1]
                for h in range(4):
                    mk.mm(pl[0:64, h:h + 1], lhsT=lf[i][:, h * 64:(h + 1) * 64], rhs=ones1[:])
                mk.tsc(ebl[i][:], pl[0:64, 0:4], -80.0, ALU.max)
                mk.act(ebl[i][:], ebl[i][:], AF.Exp)
                mk.act(eb[i][:], bc[i][:], AF.Exp)
                mk.tt(qe[i][:], t[:, 0:256], eb[i][:], ALU.mult)
                mk.act(eb[i][:], bc[i][:], AF.Exp, scale=-1.0)
                mk.tt(ke[i][:], kk[i][:], eb[i][:], ALU.mult)
                pq, pk = p.psum[2], p.psum[3]
                for h in range(4):
                    mk.tr(pq[0:64, h * 64:(h + 1) * 64], qe[i][:, h * 64:(h + 1) * 64], p.identS[0:64, 0:64])
                    mk.tr(pk[0:64, h * 64:(h + 1) * 64], ke[i][:, h * 64:(h + 1) * 64], p.identS[0:64, 0:64])
                mk.copy(qeT[i][:].rearrange("q h t -> q (h t)"), pq[0:64, 0:256])
                mk.copy(keT[i][:].rearrange("q h t -> q (h t)"), pk[0:64, 0:256], en="act")
                pa = p.psum[4]
                for h in range(4):
                    mk.mm(pa[0:64, h * 64:(h + 1) * 64], lhsT=keT[i][:, h, :], rhs=qeT[i][:, h, :])
                mk.tt(ATm[i][:], pa[0:64, 0:256].rearrange("q (h t) -> q h t", h=4),
                      tri[d][:].unsqueeze(1).to_broadcast([64, 4, 64]), ALU.mult)
                po = p.psum[5]
                v = t[:, 768:1024]
                for h in range(4):
                    mk.mm(po[0:64, h * 64:(h + 1) * 64], lhsT=ATm[i][:, h, :], rhs=v[:, h * 64:(h + 1) * 64], start=True, stop=False)
                    mk.mm(po[0:64, h * 64:(h + 1) * 64], lhsT=qeT[i][:, h, :], rhs=S[:, h, :], start=False, stop=True)
                pd = p.psum[6]
                for h in range(4):
                    mk.mm(pd[0:64, h * 64:(h + 1) * 64], lhsT=ke[i][:, h * 64:(h + 1) * 64], rhs=v[:, h * 64:(h + 1) * 64])
                mk.tt(tmpS[:], S[:], pd[0:64, 0:256].rearrange("q (h t) -> q h t", h=4), ALU.add)
                mk.tt(S[:], tmpS[:], ebl[i][:].unsqueeze(2).to_broadcast([64, 4, 64]), ALU.mult)
                if d == 0:
                    mk.copy(osb[i][:], po[0:64, 0:256], en="act")
                    mk.dma("pool", p.hof[tok0:tok0 + 64, :], osb[i][:])
                else:
                    mk.dma("pool", of_[i][:], p.hof[tok0:tok0 + 64, :])
                    mk.tt(osb[i][:], po[0:64, 0:256], of_[i][:], ALU.add)
                    p.headnorm(osb[i][:], osb[i][:], 4, 64, gn[:].rearrange("q h d -> q (h d)"), sq[:], ss[:], np_=64)
                    mk.act(sg[:], t[:, 1024:1280], AF.Silu)
                    mk.tt(osb[i][:], osb[i][:], sg[:], ALU.mult)
                    pt = p.psum[7]
                    for c in range(2):
                        mk.tr(pt[:, c * 64:(c + 1) * 64], osb[i][:, c * 128:(c + 1) * 128], p.identS[0:64, 0:64])
                    mk.copy(oT[i][:].rearrange("q c t -> q (c t)"), pt[:, 0:128])
                    mk.dma("sp", p.catT[0:256, tok0:tok0 + 64].rearrange("(c q) t -> q c t", q=128), oT[i][:])
                yield


def _hgrn(self, l):
    p, mk = self, self.mk
    G = 4
    with mk.scope():
        lb = mk.sb("lb", [64, 512])
        oml = mk.sb("oml", [64, 512])
        with mk.scope():
            lg = mk.sb("lblg", [64, 4, 512])
            mk.dma("sp", lg[:], p.hgrn_lb.ap().rearrange("l a b -> l (a b)").partition_broadcast(64))
            mk.act(lg[:], lg[:], AF.Exp)
            tot = mk.sb("lbtot", [64, 512])
            mk.tt(tot[:], lg[:, 0, :], lg[:, 1, :], ALU.add)
            mk.tt(tot[:], tot[:], lg[:, 2, :], ALU.add)
            mk.tt(tot[:], tot[:], lg[:, 3, :], ALU.add)
            mk.recip(tot[:], tot[:])
            mk.memset(lb[:], 0.0)
            for ll in range(1, l + 1):
                mk.tt(lb[:], lb[:], lg[:, ll, :], ALU.add)
            mk.tt(lb[:], lb[:], tot[:], ALU.mult)
            mk.tsc(oml[:], lb[:], -1.0, ALU.mult, 1.0, ALU.add)
        gn = mk.sb("hgn", [64, G * 4, 64])
        for h in range(G * 4):
            mk.dma("sp", gn[:, h, :], p.hgrn_norm_g[l, :].partition_broadcast(64))
        tri = [mk.sb("tri%d" % i, [64, 64]) for i in range(2)]
        mk.dma("sp", tri[0][:], p.c_trif[:, :])
        mk.dma("sp", tri[1][:], p.c_trib[:, :])
        ones1 = mk.sb("hones1", [64, 1])
        mk.memset(ones1[:], 1.0)
        S = mk.sb("hS", [64, 4, 64])
        tmpS = mk.sb("htmpS", [64, 4, 64])
        NB = 2
        mkt = lambda nm, shp: [mk.sb("h%s%d" % (nm, i), shp) for i in range(NB)]
        tin = mkt("tin", [64, G, 1280])
        f_ = mkt("f", [64, G, 256]); lf = mkt("lf", [64, G, 256]); kk = mkt("kk", [64, G, 256])
        bc = mkt("bc", [64, G, 256]); eb = mkt("eb", [64, G, 256]); qe = mkt("qe", [64, G, 256]); ke = mkt("ke", [64, G, 256])
        qeT = mkt("qeT", [64, G, 4, 64]); keT = mkt("keT", [64, G, 4, 64]); ATm = mkt("ATm", [64, G, 4, 64])
        ebl = mkt("ebl", [64, G, 4]); osb = mkt("osb", [64, G, 256]); of_ = mkt("of", [64, G, 256])
        sq = mk.sb("hsq", [64, G * 256]); ss = mk.sb("hss", [64, G * 4]); sg = mk.sb("hsg", [64, G, 256])
        oT = mkt("oT", [128, 2, G * 64])
        batches = [T_LAT] + [G * 64 * b for b in range(8)]
        it = 0
        for d in range(2):
            mk.memset(S[:], 0.0)
            blist = batches if d == 0 else [T_LAT] + [G * 64 * b for b in reversed(range(8))]
            for tok0 in blist:
                i = it % NB
                it += 1
                t = tin[i]
                ncol = 1024 if d == 0 else 1280
                mk.dma("sp", t[:, :, 0:ncol], p.ptok[tok0:tok0 + G * 64, 0:ncol].rearrange("(g q) n -> q g n", q=64))
                z = t[:, :, 256 + 256 * d:512 + 256 * d]
                lbd = lb[:, d * 256:(d + 1) * 256].unsqueeze(1).to_broadcast([64, G, 256])
                omd = oml[:, d * 256:(d + 1) * 256].unsqueeze(1).to_broadcast([64, G, 256])
                mk.act(f_[i][:], z, AF.Sigmoid)
                mk.tt(f_[i][:], f_[i][:], omd, ALU.mult)
                mk.tt(f_[i][:], f_[i][:], lbd, ALU.add)
                mk.act(lf[i][:], f_[i][:], AF.Ln)
                mk.tsc(kk[i][:], f_[i][:], -1.0, ALU.mult, 1.0, ALU.add)
                for g2 in range(G // 2):
                    pb = p.psum[g2]
                    mk.mm(pb[0:64, 0:512], lhsT=tri[d][:], rhs=lf[i][:, 2 * g2:2 * g2 + 2, :].rearrange("q g k -> q (g k)"))
                    mk.tsc(bc[i][:, 2 * g2:2 * g2 + 2, :].rearrange("q g k -> q (g k)"), pb[0:64, 0:512], -80.0, ALU.max)
                pl = p.psum[2]
                for g in range(G):
                    for h in range(4):
                        mk.mm(pl[0:64, g * 4 + h:g * 4 + h + 1], lhsT=lf[i][:, g, h * 64:(h + 1) * 64], rhs=ones1[:])
                mk.tsc(ebl[i][:].rearrange("q g h -> q (g h)"), pl[0:64, 0:G * 4], -80.0, ALU.max)
                mk.act(ebl[i][:], ebl[i][:], AF.Exp)
                mk.act(eb[i][:], bc[i][:], AF.Exp)
                mk.tt(qe[i][:], t[:, :, 0:256], eb[i][:], ALU.mult)
                mk.act(eb[i][:], bc[i][:], AF.Exp, scale=-1.0)
                mk.tt(ke[i][:], kk[i][:], eb[i][:], ALU.mult)
                for g in range(G):
                    pq, pk = p.psum[3], p.psum[4]
                    for h in range(4):
                        mk.tr(pq[0:64, h * 64:(h + 1) * 64], qe[i][:, g, h * 64:(h + 1) * 64], p.identS[0:64, 0:64])
                        mk.tr(pk[0:64, h * 64:(h + 1) * 64], ke[i][:, g, h * 64:(h + 1) * 64], p.identS[0:64, 0:64])
                    mk.copy(qeT[i][:, g, :, :].rearrange("q h t -> q (h t)"), pq[0:64, 0:256])
                    mk.copy(keT[i][:, g, :, :].rearrange("q h t -> q (h t)"), pk[0:64, 0:256], en="act")
                    pa = p.psum[5]
                    for h in range(4):
                        mk.mm(pa[0:64, h * 64:(h + 1) * 64], lhsT=keT[i][:, g, h, :], rhs=qeT[i][:, g, h, :])
                    mk.tt(ATm[i][:, g, :, :], pa[0:64, 0:256].rearrange("q (h t) -> q h t", h=4),
                          tri[d][:].unsqueeze(1).to_broadcast([64, 4, 64]), ALU.mult)
                gorder = range(G) if d == 0 else reversed(range(G))
                for g in gorder:
                    v = t[:, g, 768:1024]
                    po = p.psum[6]
                    for h in range(4):
                        mk.mm(po[0:64, h * 64:(h + 1) * 64], lhsT=ATm[i][:, g, h, :], rhs=v[:, h * 64:(h + 1) * 64], start=True, stop=False)
                        mk.mm(po[0:64, h * 64:(h + 1) * 64], lhsT=qeT[i][:, g, h, :], rhs=S[:, h, :], start=False, stop=True)
                    pd = p.psum[7]
                    for h in range(4):
                        mk.mm(pd[0:64, h * 64:(h + 1) * 64], lhsT=ke[i][:, g, h * 64:(h + 1) * 64], rhs=v[:, h * 64:(h + 1) * 64])
                    mk.tt(tmpS[:], S[:], pd[0:64, 0:256].rearrange("q (h t) -> q h t", h=4), ALU.add)
                    mk.tt(S[:], tmpS[:], ebl[i][:, g, :].unsqueeze(2).to_broadcast([64, 4, 64]), ALU.mult)
                    mk.copy(osb[i][:, g, :], po[0:64, 0:256], en="act")
                hv = p.hof[tok0:tok0 + G * 64, :].rearrange("(g q) n -> q g n", q=64)
                if d == 0:
                    mk.dma("sp", hv, osb[i][:])
                else:
                    mk.dma("sp", of_[i][:], hv)
                    o2 = osb[i][:].rearrange("q g n -> q (g n)")
                    mk.tt(o2, o2, of_[i][:].rearrange("q g n -> q (g n)"), ALU.add)
                    p.headnorm(o2, o2, G * 4, 64, gn[:].rearrange("q h d -> q (h d)"), sq[:], ss[:], np_=64)
                    mk.act(sg[:], t[:, :, 1024:1280], AF.Silu)
                    mk.tt(osb[i][:], osb[i][:], sg[:], ALU.mult)
                    for c in range(2):
                        pt = p.psum[c]
                        for g in range(G):
                            mk.tr(pt[:, g * 64:(g + 1) * 64], osb[i][:, g, c * 128:(c + 1) * 128], p.identS[0:64, 0:64])
                        mk.copy(oT[i][:, c, :], pt[:, 0:G * 64], en="dve" if c == 0 else "act")
                    mk.dma("sp", p.catT[0:256, tok0:tok0 + G * 64].rearrange("(c q) t -> q c t", q=128), oT[i][:])


Prog.hgrn_gen = _hgrn_gen
Prog.hgrn = _hgrn


def _hy_sin(self, out, ps, b_col, f_col, w, tmp):
    mk = self.mk
    arg, s4, s2 = tmp
    mk.tsc(arg[:, 0:w], ps, b_col, ALU.add, f_col, ALU.mult)
    mk.act(s4[:, 0:w], arg[:, 0:w], AF.Sin, scale=0.25)
    mk.act(s2[:, 0:w], arg[:, 0:w], AF.Sin, scale=0.5)
    mk.tt(s4[:, 0:w], s4[:, 0:w], s4[:, 0:w], ALU.mult)
    mk.tsc(s4[:, 0:w], s4[:, 0:w], -2.0, ALU.mult, 1.0, ALU.add)
    mk.stt(out, s2[:, 0:w], 2.0, s4[:, 0:w], ALU.mult, ALU.mult)


def _hy_filters(self, l):
    p, mk = self, self.mk
    with mk.scope():
        w1 = mk.sb("hyw1", [33, 64])
        w2 = mk.sb("hyw2", [64, 64])
        w3 = mk.sb("hyw3", [64, 1024])
        mk.dma("sp", w1[:], p.hy_w1[l, :, :])
        mk.dma("sp", w2[:], p.hy_w2[l, :, :])
        mk.dma("sp", w3[:], p.hy_w3[l, :, :])
        cols = mk.sb("hycols", [64, 4])
        mk.dma("sp", cols[:, 0:1], p.hy_b1[l, :].rearrange("(q o) -> q o", o=1))
        mk.dma("sp", cols[:, 1:2], p.hy_freq[l, :].rearrange("(q o) -> q o", o=1))
        mk.dma("sp", cols[:, 2:3], p.hy_b2[l, :].rearrange("(q o) -> q o", o=1))
        b3 = mk.sb("hyb3", [128, 8])
        ndec = mk.sb("hyndec", [128, 8])
        mk.dma("sp", b3[:], p.hy_b3T[l, :, :])
        mk.dma("sp", ndec[:], p.hy_decT[l, :, :])
        mk.tsc(ndec[:], ndec[:], -1.0, ALU.mult)
        feats = mk.sb("hyfeats", [33, 512])
        tun = mk.sb("hytun", [128, 512])
        tmp = [mk.sb("hytmp%d" % i, [64, 512]) for i in range(3)]
        h1 = mk.sb("hyh1", [64, 512])
        h2 = mk.sb("hyh2", [64, 512])
        E = mk.sb("hyE", [128, 512])
        ssq = mk.sb("hyssq", [128, 8, 4])
        junk = mk.sb("hyjunk", [128, 2048])
        inv = mk.sb("hyinv", [128, 4])
        for (L, featsD, tunD, Kd, KW) in ((2048, p.c_feats, p.c_tun, p.Kd, 4096), (256, p.c_featsc, p.c_tunc, p.Kdc, 512)):
            with mk.scope():
                FT = mk.sb("hyFT", [128, 8, L])
                nblk = (L + 511) // 512
                for tdir in range(2):
                    for bi in range(nblk):
                        w = min(512, L)
                        t0 = bi * 512
                        mk.dma("sp", feats[:, 0:w], featsD[tdir, :, t0:t0 + w])
                        mk.dma("sp", tun[:, 0:w], tunD[tdir, t0:t0 + w].partition_broadcast(128))
                        ps = p.psum[0]
                        mk.mm(ps[0:64, 0:w], lhsT=w1[:], rhs=feats[:, 0:w])
                        p.hy_sin(h1[:, 0:w], ps[0:64, 0:w], cols[:, 0:1], cols[:, 1:2], w, tmp)
                        ps = p.psum[1]
                        mk.mm(ps[0:64, 0:w], lhsT=w2[:], rhs=h1[:, 0:w])
                        p.hy_sin(h2[:, 0:w], ps[0:64, 0:w], cols[:, 2:3], cols[:, 1:2], w, tmp)
                        for j in (0, 1, 4, 5):
                            jj = j + 2 * tdir
                            ps = p.psum[2 + jj % 4]
                            mk.mm(ps[:, 0:w], lhsT=w3[:, jj * 128:(jj + 1) * 128], rhs=h2[:, 0:w])
                            mk.act(E[:, 0:w], tun[:, 0:w], AF.Exp, scale=ndec[:, jj:jj + 1])
                            mk.stt(FT[:, jj, t0:t0 + w], ps[:, 0:w], b3[:, jj:jj + 1], E[:, 0:w], ALU.add, ALU.mult)
                for jj in range(8):
                    tdir = (jj // 2) % 2
                    n = L if tdir == 0 else L - 1
                    mk.act(junk[:, 0:n], FT[:, jj, 0:n], AF.Square, accum_out=ssq[:, jj, 0:1])
                for j in (0, 1, 4, 5):
                    col = inv[:, 0:1]
                    mk.tt(col, ssq[:, j, 0:1], ssq[:, j + 2, 0:1], ALU.add)
                    mk.act(col, col, AF.Sqrt, bias=EPS, scale=1.0)
                    mk.recip(col, col)
                    o, ch = j // 4, j % 2
                    r0 = o * 256 + ch * 128
                    mk.tsc(FT[:, j, :], FT[:, j, :], col, ALU.mult)
                    mk.tsc(FT[:, j + 2, :], FT[:, j + 2, :], col, ALU.mult)
                    mk.dma("sp", Kd[r0:r0 + 128, L - 1:2 * L - 1], FT[:, j, :])
                    mk.dma("sp", Kd[r0:r0 + 128, 0:L - 1], FT[:, j + 2, 0:L - 1])


def _hyena(self, l):
    p, mk = self, self.mk
    p.hy_filters(l)
    with mk.scope():
        sw = mk.sb("hysw", [128, 6, 3])
        sbias = mk.sb("hysb", [128, 6])
        mk.dma("sp", sw[:], p.hy_swT[l, :, :, :])
        mk.dma("sp", sbias[:], p.hy_sbT[l, :, :])
        db = mk.sb("hydb", [128, 2, 256])
        mk.dma("sp", db[:], p.hy_bias[l, :, :].partition_broadcast(128))
        Ex = mk.sb("hyEx", [128, 128])
        mk.dma("sp", Ex[:], p.c_exch[:, :])
        uf = [mk.sb("hyuf%d" % i, [128, T_ALL]) for i in range(2)]
        acc = mk.sb("hyacc", [128, T_ALL])
        tk = [mk.sb("hytk%d" % i, [128, NT, 128]) for i in range(3)]
        z1 = mk.sb("hyz1", [128, NT, 128])
        zr = mk.sb("hyzr", [128, NT, 128])
        R = [mk.sb("hyR%d" % i, [128, 3968]) for i in range(3)]
        Rc = [mk.sb("hyRc%d" % i, [128, 384]) for i in range(3)]
        tmpg = [mk.sb("hytg%d" % i, [128, NT]) for i in range(2)]
        ob = [mk.sb("hyob%d" % i, [128, 512]) for i in range(2)]
        ci = 0
        for ch in range(2):
            for part in range(3):
                jt = part * 2 + ch
                u = uf[part % 2]
                mk.dma("sp", u[:], p.uT[jt * 128:(jt + 1) * 128, :])
                mk.tsc(acc[:], u[:], sw[:, jt, 1:2], ALU.mult, sbias[:, jt:jt + 1], ALU.add)
                for (a, b) in ((0, T_LAT), (T_LAT, T_ALL)):
                    mk.stt(acc[:, a + 1:b], u[:, a:b - 1], sw[:, jt, 0:1], acc[:, a + 1:b], ALU.mult, ALU.add)
                    mk.stt(acc[:, a:b - 1], u[:, a + 1:b], sw[:, jt, 2:3], acc[:, a:b - 1], ALU.mult, ALU.add)
                for n in range(NT):
                    pt = p.psum[n % 4]
                    mk.tr(pt[:, 0:128], acc[:, n * 128:(n + 1) * 128], p.identS[:])
                    p.evac(tk[part][:, n, :], pt[:, 0:128])
            for o in range(2):
                z = tk[0] if o == 0 else z1
                gate = tk[1] if o == 0 else tk[2]
                zo = z1 if o == 0 else tk[0]
                for n in range(NT):
                    pt = p.psum[n % 4]
                    mk.mm(pt[:, 0:128], lhsT=Ex[:], rhs=z[:, n, :])
                    p.evac(zr[:, n, :], pt[:, 0:128])
                for c in range(128):
                    row = o * 256 + ch * 128 + c
                    r, rc = R[ci % 3], Rc[ci % 3]
                    mk.dma("sp" if ci % 2 == 0 else "act", r[:],
                           bass.AP(tensor=p.Kd.ap().tensor, offset=row * 4096, ap=[[1, 128], [1, 3968]]))
                    mk.dma("pool", rc[:], bass.AP(tensor=p.Kdc.ap().tensor, offset=row * 512, ap=[[1, 128], [1, 384]]))
                    ps = p.psum[4 + ci % 4]
                    ci += 1
                    Ds = [0] + [d for k in range(1, 16) for d in (k, -k)]
                    for di, Dd in enumerate(Ds):
                        i0, i1 = max(0, Dd), min(16, 16 + Dd)
                        mk.mm(ps[:, i0:i1], lhsT=r[:, 128 * (Dd + 15):128 * (Dd + 16)], rhs=zr[:, i0 - Dd:i1 - Dd, c],
                              start=(di == 0), stop=(di == len(Ds) - 1))
                    for di, Dd in enumerate((0, 1, -1)):
                        i0, i1 = max(0, Dd), min(2, 2 + Dd)
                        mk.mm(ps[:, 16 + i0:16 + i1], lhsT=rc[:, 128 * (Dd + 1):128 * (Dd + 2)], rhs=zr[:, 16 + i0 - Dd:16 + i1 - Dd, c],
                              start=(di == 0), stop=(di == 2))
                    tg = tmpg[c % 2]
                    cc = ch * 128 + c
                    mk.stt(tg[:], z[:, :, c], db[:, o, cc:cc + 1], ps[:, 0:NT], ALU.mult, ALU.add)
                    mk.tt(zo[:, :, c], tg[:], gate[:, :, c], ALU.mult)
            oi = 0
            for bi, (t0, w) in enumerate(TB):
                o_ = ob[oi % 2]
                oi += 1
                for j in range(w // 128):
                    n = t0 // 128 + j
                    pt = p.psum[n % 4]
                    mk.tr(pt[:, 0:128], tk[0][:, n, :], p.identS[:])
                    p.evac(o_[:, j * 128:(j + 1) * 128], pt[:, 0:128])
                mk.dma("sp", p.catT[256 + ch * 128:256 + (ch + 1) * 128, t0:t0 + w], o_[:, 0:w])


Prog.hy_sin = _hy_sin
Prog.hy_filters = _hy_filters
Prog.hyena = _hyena


def _hy_spectrum(self, l):
    p, mk = self, self.mk
    with mk.scope():
        w1 = mk.sb("hyw1", [33, 64])
        w2 = mk.sb("hyw2", [64, 64])
        w3 = mk.sb("hyw3", [64, 1024])
        mk.dma("sp", w1[:], p.hy_w1[l, :, :])
        mk.dma("sp", w2[:], p.hy_w2[l, :, :])
        mk.dma("sp", w3[:], p.hy_w3[l, :, :])
        cols = mk.sb("hycols", [64, 4])
        mk.dma("sp", cols[:, 0:1], p.hy_b1[l, :].rearrange("(q o) -> q o", o=1))
        mk.dma("sp", cols[:, 1:2], p.hy_freq[l, :].rearrange("(q o) -> q o", o=1))
        mk.dma("sp", cols[:, 2:3], p.hy_b2[l, :].rearrange("(q o) -> q o", o=1))
        b3 = mk.sb("hyb3", [128, 1024])
        dec = mk.sb("hydec", [128, 1024])
        mk.dma("sp", b3[:], p.hy_b3[l, :].partition_broadcast(128))
        mk.dma("sp", dec[:], p.hy_decay[l, :, :, :].rearrange("a b c -> (a b c)").partition_broadcast(128))
        feats = mk.sb("hyfeats", [33, 512])
        tmp = [mk.sb("hytmp%d" % i, [64, 512]) for i in range(3)]
        h1 = mk.sb("hyh1", [64, 512])
        h2 = mk.sb("hyh2", [64, 512])
        E = mk.sb("hyE", [128, 1024])
        tot = mk.sb("hytot", [128, 512])
        for (L, featsD, ntunD, dftB, Ksp, nrm) in ((2048, p.c_feats, p.c_ntun, p.c_dftB, p.Ksp, 2.0 / 4096),
                                                 (256, p.c_featsc, p.c_ntunc, p.c_dftBc, p.Kspc, 2.0 / 512)):
            nt = L // 128
            with mk.scope():
                filtR = mk.sb("hyfilt", [128, nt, 1024], F32R)
                filt = filtR[:].bitcast(F32)
                ntun = mk.sb("hyntun", [128, nt])
                mk.dma("sp", ntun[:], ntunD[:, :])
                sq = mk.sb("hysq", [128, 1024], F32R)
                for bi in range((L + 511) // 512):
                    w = min(512, L)
                    t0 = bi * 512
                    mk.dma("sp", feats[:, 0:w], featsD[0, :, t0:t0 + w])
                    ps = p.psum[0]
                    mk.mm(ps[0:64, 0:w], lhsT=w1[:], rhs=feats[:, 0:w])
                    p.hy_sin(h1[:, 0:w], ps[0:64, 0:w], cols[:, 0:1], cols[:, 1:2], w, tmp)
                    ps = p.psum[1]
                    mk.mm(ps[0:64, 0:w], lhsT=w2[:], rhs=h1[:, 0:w])
                    p.hy_sin(h2[:, 0:w], ps[0:64, 0:w], cols[:, 2:3], cols[:, 1:2], w, tmp)
                    for j in range(w // 128):
                        n = t0 // 128 + j
                        p.tick()
                        mk.act(E[:], dec[:], AF.Exp, scale=ntun[:, n:n + 1])
                        if n == 0:
                            mk.memset(E[:].rearrange("q (o d c) -> q o d c", o=2, d=2)[0:1, :, 1, :], 0.0)
                        for hf in range(2):
                            ps = p.psum[2 + hf]
                            mk.mm(ps[:, 0:512], lhsT=h2[:, j * 128:(j + 1) * 128], rhs=w3[:, hf * 512:(hf + 1) * 512])
                            mk.tt(filtR[:, n, hf * 512:(hf + 1) * 512], ps[:, 0:512], b3[:, hf * 512:(hf + 1) * 512], ALU.add)
                        mk.tt(filtR[:, n, :], filt[:, n, :], E[:], ALU.mult, en="pool")
                pss = [p.psum[4], p.psum[5]]
                for n in range(nt):
                    mk.act(sq[:], filt[:, n, :], AF.Square)
                    for hf in range(2):
                        mk.mm(pss[hf][:, 0:512], lhsT=p.onesR[:], rhs=sq[:, hf * 512:(hf + 1) * 512], start=(n == 0), stop=(n == nt - 1))
                for o in range(2):
                    mk.copy(tot[:, o * 256:(o + 1) * 256], pss[o][:, 256:512], en="act")
                    mk.tt(tot[:, o * 256:(o + 1) * 256], tot[:, o * 256:(o + 1) * 256], pss[o][:, 0:256], ALU.add)
                mk.act(tot[:], tot[:], AF.Sqrt, bias=EPS, scale=1.0)
                mk.recip(tot[:], tot[:])
                mk.tsc(tot[:], tot[:], nrm, ALU.mult)
                totv = tot[:].rearrange("q (o c) -> q o c", o=2).unsqueeze(2).to_broadcast([128, 2, 2, 256])
                for n in range(nt):
                    mk.tt(filtR[:, n, :].rearrange("q (o d c) -> q o d c", o=2, d=2),
                          filt[:, n, :].rearrange("q (o d c) -> q o d c", o=2, d=2), totv, ALU.mult,
                          en="dve" if n % 2 == 0 else "pool")
                for n in range(nt):
                    fv4 = filt[:, n, :].rearrange("q (o d c) -> q o d c", o=2, d=2)
                    fr4 = filtR[:, n, :].rearrange("q (o d c) -> q o d c", o=2, d=2)
                    eng = "dve" if n % 2 == 0 else "pool"
                    mk.tt(fr4[:, :, 1, :], fv4[:, :, 0, :], fv4[:, :, 1, :], ALU.subtract, en=eng)
                    if eng == "dve":
                        mk.stt(fr4[:, :, 0, :], fv4[:, :, 0, :], 2.0, fv4[:, :, 1, :], ALU.mult, ALU.subtract)
                    else:
                        mk.tsc(fr4[:, :, 0, :], fv4[:, :, 0, :], 2.0, ALU.mult, en="pool")
                        mk.tt(fr4[:, :, 0, :], fv4[:, :, 0, :], fv4[:, :, 1, :], ALU.subtract, en="pool")
                blk = [mk.sb("hyblk%d" % i, [128, 2, nt, 128], F32R) for i in range(2)]
                ko = [mk.sb("hyko%d" % i, [128, 2, 2, 256]) for i in range(2)]
                for k in range(nt):
                    p.tick()
                    bk = blk[k % 2]
                    mk.dma("pool", bk[:], dftB[k, :, 0:2, :, :], max_dma_last_dim=8192)
                    kk_ = ko[k % 2]
                    for cs in range(2):
                        ps = p.psum[cs]
                        for n in range(nt):
                            rhs = filtR[:, n, :].rearrange("q (o d c) -> q o d c", o=2, d=2)[:, :, cs, :]
                            mk.mm(ps[:, 0:512].rearrange("q (o c) -> q o c", o=2), lhsT=bk[:, 1 - cs, n, :], rhs=rhs,
                                  start=(n == 0), stop=(n == nt - 1))
                        mk.copy(kk_[:, :, cs, :], ps[:, 0:512].rearrange("q (o c) -> q o c", o=2), en="dve" if cs == 0 else "act")
                    if k == 0:
                        ps = p.psum[2]
                        for n in range(nt):
                            rhs = filtR[:, n, :].rearrange("q (o d c) -> q o d c", o=2, d=2)[:, :, 0, :]
                            mk.mm(ps[:, 0:512].rearrange("q (o c) -> q o c", o=2), lhsT=bk[:, 0, n, :], rhs=rhs,
                                  start=(n == 0), stop=(n == nt - 1))
                        mk.copy(kk_[0:1, :, 1, :], ps[0:1, 0:512].rearrange("q (o c) -> q o c", o=2))
                        mk.tsc(kk_[0:1, :, :, :], kk_[0:1, :, :, :], 0.5, ALU.mult)
                    mk.dma("sp", Ksp[:, :, k * 128:(k + 1) * 128, :].rearrange("o s q c -> q o s c"), kk_[:])


def _hy_conv(self, ztok, zch, o, nt, tile0, dftB, dftN, Ksp, gch_o, dbc, zout_ch, blkname):
    p, mk = self, self.mk
    W = nt * 128
    wb = min(512, W)
    ntb = W // wb
    with mk.scope():
        blk = [mk.sb(blkname + "%d" % i, [128, 2, nt, 128], F32R) for i in range(2)]
        rowt = [mk.sb(blkname + "r%d" % i, [128, W], F32R) for i in range(2)]
        kc = [mk.sb("hykc%d" % i, [128, 2, 256]) for i in range(2)]
        YR = mk.sb("hyYR", [128, 2, nt, 256], F32R)
        t1 = [mk.sb("hyt1%d" % i, [128, 256]) for i in range(2)]
        t2 = [mk.sb("hyt2%d" % i, [128, 256]) for i in range(2)]
        t3 = [mk.sb("hyt3%d" % i, [128, 256]) for i in range(2)]
        t4 = [mk.sb("hyt4%d" % i, [128, 256]) for i in range(2)]
        gt = [mk.sb("hygt%d" % i, [128, wb]) for i in range(2)]
        dz = [mk.sb("hydz%d" % i, [128, wb]) for i in range(2)]
        for k in range(nt):
            p.tick()
            bk = blk[k % 2]
            mk.dma("pool", bk[:], dftB[k, :, 0:2, :, :], max_dma_last_dim=8192)
            kk_ = kc[k % 2]
            mk.dma("sp", kk_[:], Ksp[o, :, k * 128:(k + 1) * 128, :].rearrange("s q c -> q s c"))
            psA, psB = p.psum[(k % 2) * 2], p.psum[(k % 2) * 2 + 1]
            for cs, ps in ((0, psA), (1, psB)):
                for n in range(nt):
                    mk.mm(ps[:, 0:256], lhsT=bk[:, 1 - cs, n, :], rhs=ztok[:, tile0 + n, :], start=(n == 0), stop=(n == nt - 1))
            a1, a2, b1_, b2_ = t1[k % 2], t2[k % 2], t3[k % 2], t4[k % 2]
            mk.tt(a1[:], psA[:, 0:256], kk_[:, 0, :], ALU.mult)
            mk.tt(a2[:], psB[:, 0:256], kk_[:, 1, :], ALU.mult)
            mk.tt(YR[:, 0, k, :], a1[:], a2[:], ALU.subtract, en="pool")
            mk.tt(b1_[:], psA[:, 0:256], kk_[:, 1, :], ALU.mult)
            mk.tt(b2_[:], psB[:, 0:256], kk_[:, 0, :], ALU.mult)
            mk.tt(YR[:, 1, k, :], b1_[:], b2_[:], ALU.add, en="pool")
            if k == 0:
                mk.copy(YR[0:1, 0, 0, :], a1[0:1, :])
                mk.copy(YR[0:1, 1, 0, :], a2[0:1, :])
        ri = 0
        for cs in range(2):
            for b in range(nt):
                p.tick()
                rt_ = rowt[ri % 2]
                ri += 1
                mk.dma("pool", rt_[:], dftN[cs, b * 128:(b + 1) * 128, 0:W], max_dma_last_dim=8192)
                for ct in range(2):
                    for tb in range(ntb):
                        ps = p.psum[ct * ntb + tb]
                        mk.mm(ps[:, 0:wb], lhsT=YR[:, cs, b, ct * 128:(ct + 1) * 128], rhs=rt_[:, tb * wb:(tb + 1) * wb],
                              start=(cs == 0 and b == 0), stop=(cs == 1 and b == nt - 1))
        gi = 0
        for ct in range(2):
            for tb in range(ntb):
                ps = p.psum[ct * ntb + tb]
                c0 = tile0 * 128 + tb * wb
                g, d_ = gt[gi % 2], dz[gi % 2]
                gi += 1
                mk.dma("sp", g[:], gch_o[ct * 128:(ct + 1) * 128, c0:c0 + wb])
                mk.stt(d_[:], zch[:, ct, c0:c0 + wb], dbc[:, o, ct:ct + 1], ps[:, 0:wb], ALU.mult, ALU.add)
                mk.tt(zout_ch[:, ct, c0:c0 + wb], d_[:], g[:], ALU.mult, en="pool")


def _hyena2(self, l):
    p, mk = self, self.mk
    with mk.scope():
        if p.bg_hgrn:
            p._bg = p.hgrn_gen(l)
            p.tick()
        p.hyena_body(l)
        while getattr(p, "_bg", None) is not None:
            p.tick()


def _hyena_body(self, l):
    p, mk = self, self.mk
    p.hy_spectrum(l)
    with mk.scope():
        ztok = mk.sb("hyztok", [128, NT, 256], F32R)
        zcA = mk.sb("hyzcA", [128, 2, T_ALL])
        zcB = mk.sb("hyzcB", [128, 2, T_ALL])
        dbc = mk.sb("hydbc", [128, 2, 2])
        mk.dma("sp", dbc[:], p.hy_biasT[l, :, :, :])
        with mk.scope():
            sw = mk.sb("hysw", [128, 6, 3])
            sbias = mk.sb("hysb", [128, 6])
            mk.dma("sp", sw[:], p.hy_swT[l, :, :, :])
            mk.dma("sp", sbias[:], p.hy_sbT[l, :, :])
            uf = [mk.sb("hyuf%d" % i, [128, T_ALL]) for i in range(2)]
            acc = [mk.sb("hyacc%d" % i, [128, T_ALL]) for i in range(2)]
            for jt in range(6):
                part, ch = jt // 2, jt % 2
                u = uf[jt % 2]
                ac = zcA[:, ch, :] if part == 0 else acc[jt % 2][:]
                mk.dma("sp", u[:], p.uT[jt * 128:(jt + 1) * 128, :])
                mk.tsc(ac, u[:], sw[:, jt, 1:2], ALU.mult, sbias[:, jt:jt + 1], ALU.add)
                for (a, b) in ((0, T_LAT), (T_LAT, T_ALL)):
                    mk.stt(ac[:, a + 1:b], u[:, a:b - 1], sw[:, jt, 0:1], ac[:, a + 1:b], ALU.mult, ALU.add)
                    mk.stt(ac[:, a:b - 1], u[:, a + 1:b], sw[:, jt, 2:3], ac[:, a:b - 1], ALU.mult, ALU.add)
                if part == 0:
                    for n in range(NT):
                        pt = p.psum[n % 4]
                        mk.tr(pt[:, 0:128], ac[:, n * 128:(n + 1) * 128], p.identS[:])
                        p.evac(ztok[:, n, ch * 128:(ch + 1) * 128], pt[:, 0:128])
                else:
                    mk.dma("act", p.gch[part - 1, ch * 128:(ch + 1) * 128, :], ac)
        for o in range(2):
            zin_ch = zcA if o == 0 else zcB
            zo_ch = zcB if o == 0 else zcA
            p.hy_conv(ztok, zin_ch, o, 16, 0, p.c_dftB, p.c_dftN, p.Ksp, p.gch[o, :, :], dbc, zo_ch, "hyblkL")
            p.hy_conv(ztok, zin_ch, o, 2, 16, p.c_dftBc, p.c_dftNc, p.Kspc, p.gch[o, :, :], dbc, zo_ch, "hyblkC")
            if o == 0:
                for ct in range(2):
                    for n in range(NT):
                        pt = p.psum[4 + n % 4]
                        mk.tr(pt[:, 0:128], zcB[:, ct, n * 128:(n + 1) * 128], p.identS[:])
                        p.evac(ztok[:, n, ct * 128:(ct + 1) * 128], pt[:, 0:128])
        for ct in range(2):
            mk.dma("sp", p.catT[256 + ct * 128:256 + (ct + 1) * 128, :], zcA[:, ct, :])


Prog.hyena_body = _hyena_body


def _tick(self, n=1):
    g = getattr(self, "_bg", None)
    if g is None:
        return
    for _ in range(n):
        try:
            next(g)
        except StopIteration:
            self._bg = None
            return


Prog.tick = _tick
Prog.hy_spectrum = _hy_spectrum
Prog.hy_conv = _hy_conv
Prog.hyena = _hyena2


def _consts():
    f = np.float32
    c = {}
    s = np.arange(128)
    c["c_lstrict"] = (s[:, None] < s[None, :]).astype(f)
    c["c_iotaE"] = np.tile((np.arange(32) * CAP).astype(f)[None, :], (128, 1))
    s = np.arange(64)
    c["c_trif"] = (s[:, None] <= s[None, :]).astype(f)
    c["c_trib"] = (s[:, None] >= s[None, :]).astype(f)
    c["c_exch"] = np.eye(128, dtype=f)[::-1].copy()
    c["ident"] = np.eye(128, dtype=f)
    t = np.arange(T_LAT)
    row = (t // 64).astype(f)
    col = (t % 64).astype(f)
    inv = (f(10000.0) ** (-np.arange(0, 16, 2, dtype=f) / f(16))).astype(f)
    ar = row[:, None] * inv[None, :]
    ac = col[:, None] * inv[None, :]
    cos = np.concatenate([np.cos(ar), np.cos(ar), np.cos(ac), np.cos(ac)], axis=1).astype(f)
    sin = np.concatenate([-np.sin(ar), np.sin(ar), -np.sin(ac), np.sin(ac)], axis=1).astype(f)
    c["c_ropecos"] = np.ascontiguousarray(cos.reshape(16, 128, 32).transpose(1, 0, 2))
    c["c_ropesin"] = np.ascontiguousarray(sin.reshape(16, 128, 32).transpose(1, 0, 2))
    for nm, tn, L in (("c_feats", "c_ntun", 2048), ("c_featsc", "c_ntunc", 256)):
        tt = np.arange(L, dtype=f)
        tu = np.linspace(0.0, 1.0, L, dtype=f)
        bands = np.linspace(1e-4, 15, 16, dtype=f)
        ang = (f(2.0 * np.pi / L) * tt[:, None] * bands[None, :]).astype(f)
        feats = np.concatenate([tu[:, None], np.cos(ang), -np.sin(ang)], axis=-1).astype(f)
        fT = feats.T
        c[nm] = np.ascontiguousarray(np.stack([fT, fT[:, ::-1]], axis=0))
        c[tn] = np.ascontiguousarray(-tu.reshape(L // 128, 128).T)
        N = 2 * L
        idx = np.arange(L, dtype=np.int64)
        prod = (idx[:, None] * idx[None, :]) % N
        ang64 = prod.astype(np.float64) * (2.0 * np.pi / N)
        Cm = np.cos(ang64)
        Sm = np.sin(ang64)
        sgn = np.where(idx % 2 == 0, 1.0, -1.0)
        Sm[:, 0] = sgn
        M = np.stack([Sm, Cm, Sm.T], axis=0).astype(f)
        nt = L // 128
        Mb = M.reshape(3, nt, 128, nt, 128).transpose(3, 2, 0, 1, 4)
        c["c_dftB" if L == 2048 else "c_dftBc"] = np.ascontiguousarray(Mb)
        c["c_dftN" if L == 2048 else "c_dftNc"] = np.ascontiguousarray(M[1:3])
    return c


def _na_bias_table(rpb):
    Ld, H = rpb.shape[0], rpb.shape[1]
    out = np.full((Ld, H, 5, 5, 128, 128), -30000.0, np.float32)
    pat_pr = {0: 0, 1: 1, 2: 2, 3: 14, 4: 15}
    kp = np.arange(128)
    q = np.arange(128)
    for pat, pr in pat_pr.items():
        rs0 = min(max(2 * pr - 4, 0), 24)
        ws = min((rs0 // 2) * 2, 22)
        for kt in range(5):
            krow = ws + (kt * 128 + kp) // 64
            kcol = (kt * 128 + kp) % 64
            qrow = 2 * pr + q // 64
            qcol = q % 64
            rs = np.clip(qrow - 4, 0, 24)
            cs = np.clip(qcol - 8, 0, 48)
            ok = (krow[:, None] >= rs[None, :]) & (krow[:, None] < rs[None, :] + 8) & \
                 (kcol[:, None] >= cs[None, :]) & (kcol[:, None] < cs[None, :] + 16)
            dr = np.clip(krow[:, None] - qrow[None, :] + 7, 0, 14)
            dc = np.clip(kcol[:, None] - qcol[None, :] + 15, 0, 30)
            g = rpb[:, :, dr, dc]
            out[:, :, pat, kt] = np.where(ok[None, None], g, np.float32(-30000.0))
    return np.ascontiguousarray(out.transpose(0, 1, 4, 2, 3, 5))


def host_shared(inp):
    f = np.float32
    m = dict(_consts())
    Ld = DEPTH
    m["w_ada"] = inp["w_ada"]
    m["b_adaT"] = np.ascontiguousarray(inp["b_ada"].reshape(Ld, 48, 128).transpose(0, 2, 1))
    m["n1g"] = np.ascontiguousarray(inp["norm1_g"].reshape(Ld, 8, 128).transpose(0, 2, 1))
    m["n2g"] = np.ascontiguousarray(inp["norm2_g"].reshape(Ld, 8, 128).transpose(0, 2, 1))
    m["w_in"] = inp["w_in"]
    m["w_out"] = inp["w_out"]
    m["hgrn_lb"] = inp["hgrn_lb_logits"]
    m["hgrn_norm_g"] = inp["hgrn_norm_g"]
    m["hy_swT"] = np.ascontiguousarray(inp["hy_short_w"].reshape(Ld, 3, 6, 128).transpose(0, 3, 2, 1))
    m["hy_sbT"] = np.ascontiguousarray(inp["hy_short_b"].reshape(Ld, 6, 128).transpose(0, 2, 1))
    for k in ("hy_w1", "hy_b1", "hy_freq", "hy_w2", "hy_b2", "hy_w3", "hy_bias", "hy_b3", "hy_decay", "na_q_g", "na_k_g", "mla_q_a_g",
              "mla_kv_a_g", "mla_w_uq", "mla_w_ukv", "mla_q_g", "mla_k_g", "moe_wg", "moe_bg", "moe_we", "moe_be",
              "moe_w_gate", "moe_w_up", "moe_w_down"):
        m[k] = inp[k]
    m["na_biasT"] = _na_bias_table(inp["na_rpb"])
    m["hy_biasT"] = np.ascontiguousarray(inp["hy_bias"].reshape(Ld, 2, 2, 128).transpose(0, 3, 1, 2))
    return m


def host_inputs(inp, b, shared):
    m = dict(shared)
    m["xin"] = np.ascontiguousarray(np.concatenate([inp["x"][b], inp["ctx"][b]], axis=0))
    m["cvecT"] = np.ascontiguousarray(np.stack([inp["c"][b], inp["c_ctx"]], axis=1))
    return m


def run_prog(inp, cores, extra=None, **kw):
    from concourse.bass_utils import run_bass_kernel_spmd
    prog = Prog(**kw)
    prog.build()
    shared = host_shared(inp)
    in_maps = []
    for b in cores:
        m = host_inputs(inp, b, shared)
        if extra:
            m.update(extra)
        in_maps.append({k: np.ascontiguousarray(m[k], dtype=np.float32) for k in prog.inputs})
    res = run_bass_kernel_spmd(prog.nc, in_maps, core_ids=list(range(len(cores))))
    return prog, res


def kernel(**inp):
    inp = {k: np.asarray(v) for k, v in inp.items()}
    prog, res = run_prog(inp, list(range(8)))
    out = np.stack([res.results[b]["xout"] for b in range(8)], axis=0)
    return out.astype(np.float32)
```

```python
import contextlib
import numpy as np
import concourse.bass as bass
import concourse.mybir as mybir

F32 = mybir.dt.float32
F32R = mybir.dt.float32r
I32 = mybir.dt.int32
U32 = mybir.dt.uint32
ALU = mybir.AluOpType
AF = mybir.ActivationFunctionType
AX = mybir.AxisListType

SEM_ROT = 30000


class MK:
    def __init__(self, nc):
        self.nc = nc
        self.root = contextlib.ExitStack()
        self.stack = [self.root]
        self.eng = {"pe": nc.tensor, "dve": nc.vector, "act": nc.scalar,
                    "pool": nc.gpsimd, "sp": nc.sync}
        self.esem = {}
        self.ecnt = {}
        self.nsem = 0
        for k in self.eng:
            self.esem[k] = self._newsem("e_" + k)
            self.ecnt[k] = 0
        self.waited = {}
        self.ts = {}
        self.uid = 0
        self.dq = {}
        for q, n in (("sp", 10), ("pool", 6), ("act", 4)):
            self.dq[q] = {"sems": [[self._newsem("d_%s%d" % (q, i)), 0] for i in range(n)], "i": 0}
        self.ninstr = 0

    def _newsem(self, name):
        self.nsem += 1
        return self.root.enter_context(self.nc.semaphore("%s_%d" % (name, self.nsem)))

    def _nm(self, name):
        self.uid += 1
        return "%s_%d" % (name, self.uid)

    def sb(self, name, shape, dtype=F32):
        return self.stack[-1].enter_context(self.nc.sbuf_tensor(self._nm(name), list(shape), dtype))

    def ps(self, name, shape, dtype=F32):
        return self.stack[-1].enter_context(self.nc.psum_tensor(self._nm(name), list(shape), dtype))

    def dram(self, name, shape, dtype=F32, kind="Internal"):
        return self.nc.dram_tensor(name, list(shape), dtype, kind=kind)

    @contextlib.contextmanager
    def scope(self):
        es = contextlib.ExitStack()
        self.stack.append(es)
        try:
            yield
        finally:
            self.barrier()
            self.stack.pop()
            es.close()

    def _wait(self, en, ev):
        if ev is None:
            return
        sem, val = ev
        if en == "pe" and sem.name.startswith("e_pe"):
            return
        key = (en, sem.name if hasattr(sem, "name") else id(sem))
        if self.waited.get(key, 0) >= val:
            return
        self.waited[key] = val
        self.eng[en].wait_ge(sem, val)

    def _tn(self, x):
        if isinstance(x, str):
            return x
        if hasattr(x, "tensor"):
            return x.tensor.name
        return x.name

    def _region(self, x):
        if isinstance(x, str) or not hasattr(x, "tensor"):
            return (self._tn(x), None)
        t = x.tensor
        name = t.name
        try:
            apl = [(int(a[0]), int(a[1])) for a in x.ap]
            off = int(x.offset)
            if any(st < 0 for st, _ in apl):
                return (name, None)
            if type(t).__name__ == "PSumTensorHandle":
                return (name, None)
            if type(t).__name__ == "DRamTensorHandle":
                ext = sum((n - 1) * st for st, n in apl)
                return (name, (0, 0, off, off + ext))
            row = 1
            for d in list(t.shape)[1:]:
                row *= int(d)
            pst, pn = apl[0]
            if pst != row:
                return (name, None)
            p0 = off // row
            f0 = off % row
            ext = sum((n - 1) * st for st, n in apl[1:])
            return (name, (p0, p0 + pn - 1, f0, f0 + ext))
        except Exception:
            return (name, None)

    @staticmethod
    def _ovl(a, b):
        if a is None or b is None:
            return True
        return not (a[1] < b[0] or b[1] < a[0] or a[3] < b[2] or b[3] < a[2])

    @staticmethod
    def _contains(a, b):
        if a is None:
            return True
        if b is None:
            return False
        return a[0] <= b[0] and a[1] >= b[1] and a[2] <= b[2] and a[3] >= b[3]

    def _deps(self, en, reads, writes):
        for r in reads:
            name, box = self._region(r)
            st = self.ts.get(name)
            if st is not None:
                for b, ev in st["w"]:
                    if self._ovl(box, b):
                        self._wait(en, ev)
        for w in writes:
            name, box = self._region(w)
            st = self.ts.get(name)
            if st is not None:
                for b, ev in st["w"]:
                    if self._ovl(box, b):
                        self._wait(en, ev)
                for b, ev in st["r"]:
                    if self._ovl(box, b):
                        self._wait(en, ev)

    @staticmethod
    def _compress(lst):
        d = {}
        for b, (s, v) in lst:
            k = id(s)
            if k not in d or d[k][1] < v:
                d[k] = (s, v)
        return [(None, ev) for ev in d.values()]

    def _record(self, ev, reads, writes):
        for r in reads:
            name, box = self._region(r)
            st = self.ts.setdefault(name, {"w": [], "r": []})
            st["r"].append((box, ev))
            if len(st["r"]) > 40:
                st["r"] = self._compress(st["r"])
        for w in writes:
            name, box = self._region(w)
            st = self.ts.setdefault(name, {"w": [], "r": []})
            st["w"] = [(b, e) for b, e in st["w"] if not self._contains(box, b)]
            st["r"] = [(b, e) for b, e in st["r"] if not self._contains(box, b)]
            st["w"].append((box, ev))
            if len(st["w"]) > 40:
                st["w"] = self._compress(st["w"])

    def op(self, en, fn, reads=(), writes=()):
        reads = [r for r in reads if r is not None and not isinstance(r, (int, float))]
        writes = [w for w in writes if w is not None]
        self._deps(en, reads, writes)
        ins = fn(self.eng[en])
        if self.ecnt[en] >= SEM_ROT:
            self.esem[en] = self._newsem("e_" + en)
            self.ecnt[en] = 0
        self.ecnt[en] += 1
        ins.then_inc(self.esem[en], 1)
        ev = (self.esem[en], self.ecnt[en])
        self._record(ev, reads, writes)
        self.ninstr += 1
        return ev

    def dma(self, q, out, in_, fn=None, extra_reads=(), **kw):
        dq = self.dq[q]
        slot = dq["sems"][dq["i"] % len(dq["sems"])]
        dq["i"] += 1
        sem, cnt = slot
        if cnt > 0:
            self._wait(q, (sem, cnt))
        reads = [in_] + list(extra_reads)
        writes = [out]
        self._deps(q, reads, writes)
        if fn is None:
            ins = self.eng[q].dma_start(out=out, in_=in_, **kw)
        else:
            ins = fn(self.eng[q])
        slot[1] = cnt + 16
        ins.then_inc(sem, 16)
        ev = (sem, slot[1])
        self._record(ev, reads, writes)
        self.ninstr += 1
        return ev

    def barrier(self):
        evs = [(self.esem[k], self.ecnt[k]) for k in self.eng if self.ecnt[k] > 0]
        for q in self.dq.values():
            for sem, cnt in q["sems"]:
                if cnt > 0:
                    evs.append((sem, cnt))
        for en in self.eng:
            for ev in evs:
                self._wait(en, ev)
        self.ts = {}

    def finish(self):
        self.barrier()
        self.root.close()

    def mm(self, out, lhsT, rhs, start=True, stop=True):
        return self.op("pe", lambda e: e.matmul(out, lhsT=lhsT, rhs=rhs, start=start, stop=stop),
                       reads=[lhsT, rhs], writes=[out])

    def tr(self, out, in_, ident):
        return self.op("pe", lambda e: e.transpose(out, in_, ident), reads=[in_, ident], writes=[out])

    def act(self, out, in_, func, bias=None, scale=1.0, accum_out=None, en="act"):
        kw = {}
        if bias is not None:
            kw["bias"] = bias
        if accum_out is not None:
            kw["accum_out"] = accum_out
        rd = [in_]
        if isinstance(bias, bass.AP):
            rd.append(bias)
        if isinstance(scale, bass.AP):
            rd.append(scale)
        return self.op("act", lambda e: e.activation(out=out, in_=in_, func=func, scale=scale, **kw),
                       reads=rd, writes=[out, accum_out])

    def tt(self, out, in0, in1, op, en="dve"):
        return self.op(en, lambda e: e.tensor_tensor(out=out, in0=in0, in1=in1, op=op),
                       reads=[in0, in1], writes=[out])

    def tsc(self, out, in0, s1, op0, s2=None, op1=None, accum_out=None, en="dve"):
        kw = {}
        if op1 is not None:
            kw["op1"] = op1
        if accum_out is not None:
            kw["accum_out"] = accum_out
        rd = [in0] + [s for s in (s1, s2) if isinstance(s, bass.AP)]
        return self.op(en, lambda e: e.tensor_scalar(out=out, in0=in0, scalar1=s1, scalar2=s2, op0=op0, **kw),
                       reads=rd, writes=[out, accum_out])

    def stt(self, out, in0, scalar, in1, op0, op1):
        rd = [in0, in1] + ([scalar] if isinstance(scalar, bass.AP) else [])
        return self.op("dve", lambda e: e.scalar_tensor_tensor(out=out, in0=in0, scalar=scalar, in1=in1,
                                                               op0=op0, op1=op1),
                       reads=rd, writes=[out])

    def copy(self, out, in_, en="dve"):
        if en == "act":
            return self.op("act", lambda e: e.copy(out=out, in_=in_), reads=[in_], writes=[out])
        return self.op(en, lambda e: e.tensor_copy(out=out, in_=in_), reads=[in_], writes=[out])

    def memset(self, out, val, en="dve"):
        return self.op(en, lambda e: e.memset(out, val), reads=[], writes=[out])

    def recip(self, out, in_):
        return self.op("dve", lambda e: e.reciprocal(out=out, in_=in_), reads=[in_], writes=[out])

    def reduce(self, out, in_, op, axis=AX.X):
        return self.op("dve", lambda e: e.tensor_reduce(out=out, in_=in_, op=op, axis=axis),
                       reads=[in_], writes=[out])


T_LAT = 2048
T_CTX = 256
T_ALL = 2304
NT = 18
D = 1024
D_IN = 3232
DEPTH = 4
TB = [(0, 512), (512, 512), (1024, 512), (1536, 512), (2048, 256)]
EPS = 1e-6


class Prog:
    def __init__(self, debug=(), depth=DEPTH, stop=None, phases=("front", "hgrn", "hyena", "na", "mla", "outproj", "moe")):
        self.phases = phases
        self.nc = bass.Bass("TRN2", target_bir_lowering=False)
        self.mk = MK(self.nc)
        self.debug = set(debug)
        self.depth = depth
        self.stop = stop
        self.inputs = {}
        self.outs = []
        self._evi = 0

    def din(self, name, shape, dtype=F32):
        t = self.nc.dram_tensor(name, list(shape), dtype, kind="ExternalInput")
        self.inputs[name] = (tuple(shape), dtype)
        return t

    def dscr(self, name, shape, dtype=F32):
        if ("in:" + name) in self.debug:
            return self.din(name, shape, dtype)
        if name in self.debug:
            self.outs.append(name)
            return self.nc.dram_tensor(name, list(shape), dtype, kind="ExternalOutput")
        return self.nc.dram_tensor(name, list(shape), dtype, kind="Internal")

    def dout(self, name, shape, dtype=F32):
        self.outs.append(name)
        return self.nc.dram_tensor(name, list(shape), dtype, kind="ExternalOutput")

    def evac(self, out, in_):
        self._evi += 1
        if self._evi % 2:
            return self.mk.copy(out, in_, en="dve")
        return self.mk.copy(out, in_, en="act")

    def declare(self):
        p = self
        L = DEPTH
        p.xin = p.din("xin", [T_ALL, D])
        p.cvecT = p.din("cvecT", [D, 2])
        p.w_ada = p.din("w_ada", [L, D, 6 * D])
        p.b_adaT = p.din("b_adaT", [L, 128, 48])
        p.n1g = p.din("n1g", [L, 128, 8])
        p.n2g = p.din("n2g", [L, 128, 8])
        p.w_in = p.din("w_in", [L, D, D_IN])
        p.w_out = p.din("w_out", [L, D, D])
        p.ident = p.din("ident", [128, 128])
        p.xT = p.dscr("xT", [D, T_ALL])
        p.ptok = p.dscr("ptok", [T_ALL, D_IN])
        p.uT = p.dscr("uT", [768, T_ALL])
        p.catT = p.dscr("catT", [D, T_ALL])
        p.h2T = p.dscr("h2T", [D, T_ALL])
        p.hof = p.dscr("hof", [T_ALL, 256])
        p.Xb = p.dscr("Xb", [NSLOT + 128, D])
        p.Yb = p.dscr("Yb", [NSLOT + 128, D])
        p.Ksp = p.dscr("Ksp", [2, 2, 2048, 256])
        p.Kspc = p.dscr("Kspc", [2, 2, 256, 256])
        p.gch = p.dscr("gch", [2, 256, T_ALL])
        p.xout = p.dout("xout", [T_LAT, D])
        for nm, shp in (("hgrn_lb", [L, 2, 256]), ("hgrn_norm_g", [L, 64]), ("hy_swT", [L, 128, 6, 3]), ("hy_sbT", [L, 128, 6]),
                        ("hy_w1", [L, 33, 64]), ("hy_b1", [L, 64]), ("hy_freq", [L, 64]), ("hy_w2", [L, 64, 64]),
                        ("hy_b2", [L, 64]), ("hy_w3", [L, 64, 1024]), ("hy_b3", [L, 1024]), ("hy_decay", [L, 2, 2, 256]),
                        ("hy_bias", [L, 2, 256]), ("na_biasT", [L, 4, 128, 5, 5, 128]), ("na_q_g", [L, 64]), ("na_k_g", [L, 64]),
                        ("mla_q_a_g", [L, 256]), ("mla_kv_a_g", [L, 128]), ("mla_w_uq", [L, 256, 384]), ("mla_w_ukv", [L, 128, 512]),
                        ("mla_q_g", [L, 96]), ("mla_k_g", [L, 96]), ("moe_wg", [L, D, 4]), ("moe_bg", [L, 4]),
                        ("moe_we", [L, D, 32]), ("moe_be", [L, 32]), ("moe_w_gate", [L, 32, D, 512]),
                        ("moe_w_up", [L, 32, D, 512]), ("moe_w_down", [L, 32, 512, D]),
                        ("c_lstrict", [128, 128]), ("c_iotaE", [128, 32]), ("c_trif", [64, 64]), ("c_trib", [64, 64]),
                        ("c_exch", [128, 128]), ("c_ropecos", [128, 16, 32]), ("c_ropesin", [128, 16, 32]),
                        ("c_feats", [2, 33, 2048]), ("c_ntun", [128, 16]), ("c_featsc", [2, 33, 256]), ("c_ntunc", [128, 2]),
                        ("c_dftB", [16, 128, 3, 16, 128]), ("c_dftBc", [2, 128, 3, 2, 128]), ("c_dftN", [2, 2048, 2048]), ("c_dftNc", [2, 256, 256]), ("hy_biasT", [L, 128, 2, 2])):
            setattr(p, nm, p.din(nm, shp))

    def build(self):
        p, mk = self, self.mk
        p.declare()
        p.identS = mk.sb("ident", [128, 128])
        mk.dma("sp", p.identS[:], p.ident[:, :])
        p.onesF = mk.sb("onesF", [128, 128])
        mk.memset(p.onesF[:], 1.0)
        p.onesR = mk.sb("onesR", [128, 128], F32R)
        mk.copy(p.onesR[:], p.onesF[:])
        p.psum = [mk.ps("ps%d" % i, [128, 512]) for i in range(8)]
        p.breg = p.nc.gpsimd.to_reg(NSLOT - 1)
        p.load_x()
        phases = p.phases
        if "moe" in phases:
            p.moe_init()
        for l in range(p.depth):
            if "front" in phases:
                p.front(l)
            else:
                p.mod = p.modulation(l)
            p.bg_hgrn = False
            for ph in ("hgrn", "hyena", "na", "mla", "outproj", "moe"):
                if ph == "hgrn" and p.bg_hgrn:
                    continue
                if ph in phases:
                    getattr(p, ph)(l)
        p.store_x()
        mk.finish()

    def load_x(self):
        p, mk = self, self.mk
        with mk.scope():
            xt = [mk.sb("xt%d" % i, [128, D]) for i in range(2)]
            xTt = mk.sb("xTt", [128, 8, 512])
            xTv = p.xT.ap().rearrange("(k q) t -> q k t", q=128)
            for (t0, w) in TB:
                for j in range(w // 128):
                    n = t0 // 128 + j
                    xb = xt[n % 2]
                    mk.dma("sp", xb[:], p.xin[n * 128:(n + 1) * 128, :])
                    for k in range(8):
                        ps = p.psum[k % 8]
                        mk.tr(ps[:, 0:128], xb[:, k * 128:(k + 1) * 128], p.identS[:])
                        p.evac(xTt[:, k, j * 128:(j + 1) * 128], ps[:, 0:128])
                mk.dma("sp", xTv[:, :, t0:t0 + w], xTt[:, :, 0:w])

    def store_x(self):
        p, mk = self, self.mk
        with mk.scope():
            xTt = mk.sb("xTt", [128, 8, 512])
            xo = [mk.sb("xo%d" % i, [128, D]) for i in range(2)]
            xTv = p.xT.ap().rearrange("(k q) t -> q k t", q=128)
            for (t0, w) in TB[:4]:
                mk.dma("sp", xTt[:, :, 0:w], xTv[:, :, t0:t0 + w])
                for j in range(w // 128):
                    n = t0 // 128 + j
                    xb = xo[n % 2]
                    for k in range(8):
                        ps = p.psum[k % 8]
                        mk.tr(ps[:, 0:128], xTt[:, k, j * 128:(j + 1) * 128], p.identS[:])
                        p.evac(xb[:, k * 128:(k + 1) * 128], ps[:, 0:128])
                    mk.dma("sp", p.xout[n * 128:(n + 1) * 128, :], xb[:])

    def modulation(self, l):
        p, mk = self, self.mk
        mod = mk.sb("mod%d" % l, [128, 48, 2])
        with mk.scope():
            cT = mk.sb("cT", [128, 8, 2])
            mk.dma("sp", cT[:], p.cvecT.ap().rearrange("(k q) r -> q k r", q=128))
            scT = mk.sb("scT", [128, 8, 2])
            mk.act(scT[:], cT[:], AF.Silu)
            bT = mk.sb("bT", [128, 48])
            mk.dma("sp", bT[:], p.b_adaT[l, :, :])
            wv = p.w_ada[l, :, :].rearrange("(k q) n -> q k n", q=128)
            wb = [mk.sb("wada%d" % i, [128, 8, 512]) for i in range(2)]
            ps = p.psum[0]
            psv = ps[:, 0:96].rearrange("q (j r) -> q j r", r=2)
            for jb in range(12):
                w = wb[jb % 2]
                mk.dma("sp" if jb % 2 == 0 else "pool", w[:], wv[:, :, jb * 512:(jb + 1) * 512])
                for jj in range(4):
                    j = jb * 4 + jj
                    for k in range(8):
                        mk.mm(psv[:, j, :], lhsT=w[:, k, jj * 128:(jj + 1) * 128], rhs=scT[:, k, :],
                              start=(k == 0), stop=(k == 7))
            for r in range(2):
                mk.tt(mod[:, :, r], psv[:, :, r], bT[:], ALU.add)
        return mod

    def front(self, l):
        p, mk = self, self.mk
        mod = p.modulation(l)
        p.mod = mod
        with mk.scope():
            g1n = mk.sb("g1n", [128, 8])
            mk.dma("sp", g1n[:], p.n1g[l, :, :])
            A1 = mk.sb("A1", [128, 8, 2])
            for r in range(2):
                mk.stt(A1[:, :, r], mod[:, 8:16, r], 1.0, g1n[:], ALU.add, ALU.mult)
            hT = mk.sb("hT", [128, 8, T_ALL], F32R)
            p.norm_mod(p.xT, A1, mod[:, 0:8, :], hT)
            p.inproj(l, hT)

    def norm_mod(self, src, A, B, hT, dst_dram=None):
        p, mk = self, self.mk
        with mk.scope():
            xb = [mk.sb("nx%d" % i, [128, 8, 512]) for i in range(2)]
            sq = mk.sb("nsq", [128, 8, 512], F32R)
            rstd = mk.sb("rstd", [128, 512])
            tmp = [mk.sb("ntmp%d" % i, [128, 512]) for i in range(2)]
            xv = src.ap().rearrange("(k q) t -> q k t", q=128)
            for bi, (t0, w) in enumerate(TB):
                r = 0 if t0 < T_LAT else 1
                x = xb[bi % 2]
                mk.dma("sp", x[:, :, 0:w], xv[:, :, t0:t0 + w])
                mk.act(sq[:, :, 0:w], x[:, :, 0:w], AF.Square)
                ps = p.psum[bi % 2]
                for k in range(8):
                    mk.mm(ps[:, 0:w], lhsT=p.onesR[:], rhs=sq[:, k, 0:w], start=(k == 0), stop=(k == 7))
                mk.act(rstd[:, 0:w], ps[:, 0:w], AF.Sqrt, bias=EPS, scale=1.0 / D)
                mk.recip(rstd[:, 0:w], rstd[:, 0:w])
                for k in range(8):
                    t = tmp[k % 2]
                    mk.tt(t[:, 0:w], x[:, k, 0:w], rstd[:, 0:w], ALU.mult)
                    mk.act(hT[:, k, t0:t0 + w], t[:, 0:w], AF.Identity, bias=B[:, k, r:r + 1], scale=A[:, k, r:r + 1])

    def inproj(self, l, hT):
        p, mk = self, self.mk
        with mk.scope():
            wv = p.w_in[l, :, :].rearrange("(k q) n -> q k n", q=128)
            wr = [mk.sb("wr%d" % i, [128, 8, 512], F32R) for i in range(3)]
            ob = [mk.sb("ob%d" % i, [128, 512]) for i in range(3)]
            blocks = [("tok", 0, 512), ("tok", 512, 512), ("tok", 1024, 256)]
            blocks += [("feat", 1280 + 128 * i, 128) for i in range(6)]
            blocks += [("tok", 2048, 512), ("tok", 2560, 512), ("tok", 3072, 160)]
            oi = 0
            for bi, (kind, c0, cw) in enumerate(blocks):
                w = wr[bi % 3]
                mk.dma("pool", w[:, :, 0:cw], wv[:, :, c0:c0 + cw])
                if kind == "tok":
                    for n in range(NT):
                        ps = p.psum[n % 4]
                        for k in range(8):
                            mk.mm(ps[:, 0:cw], lhsT=hT[:, k, n * 128:(n + 1) * 128], rhs=w[:, k, 0:cw],
                                  start=(k == 0), stop=(k == 7))
                        o = ob[oi % 3]
                        oi += 1
                        p.evac(o[:, 0:cw], ps[:, 0:cw])
                        mk.dma("sp", p.ptok[n * 128:(n + 1) * 128, c0:c0 + cw], o[:, 0:cw])
                else:
                    f0 = c0 - 1280
                    for ti, (t0, tw) in enumerate(TB):
                        ps = p.psum[4 + ti % 4]
                        for k in range(8):
                            mk.mm(ps[:, 0:tw], lhsT=w[:, k, 0:128], rhs=hT[:, k, t0:t0 + tw],
                                  start=(k == 0), stop=(k == 7))
                        o = ob[oi % 3]
                        oi += 1
                        p.evac(o[:, 0:tw], ps[:, 0:tw])
                        mk.dma("sp", p.uT[f0:f0 + 128, t0:t0 + tw], o[:, 0:tw])


CAP = 384
NSLOT = 32 * CAP
BIGIDX = 1.0e6


def _outproj(self, l):
    p, mk = self, self.mk
    mod = p.mod
    with mk.scope():
        wv = p.w_out[l, :, :].rearrange("(k q) n -> q k n", q=128)
        wR = mk.sb("woR", [128, 8, 1024], F32R)
        for hf in range(2):
            mk.dma("pool", wR[:, :, hf * 512:(hf + 1) * 512], wv[:, :, hf * 512:(hf + 1) * 512])
        cRs = [mk.sb("cR%d" % i, [128, 8, 512], F32R) for i in range(2)]
        xb = [mk.sb("oxb%d" % i, [128, 8, 512]) for i in range(2)]
        cv = p.catT.ap().rearrange("(k q) t -> q k t", q=128)
        xv = p.xT.ap().rearrange("(k q) t -> q k t", q=128)
        for bi, (t0, w) in enumerate(TB):
            r = 0 if t0 < T_LAT else 1
            cR, x = cRs[bi % 2], xb[bi % 2]
            mk.dma("pool", cR[:, :, 0:w], cv[:, :, t0:t0 + w])
            mk.dma("sp", x[:, :, 0:w], xv[:, :, t0:t0 + w])
            for j in range(8):
                ps = p.psum[j % 4]
                for k in range(8):
                    mk.mm(ps[:, 0:w], lhsT=wR[:, k, j * 128:(j + 1) * 128], rhs=cR[:, k, 0:w],
                          start=(k == 0), stop=(k == 7))
                mk.stt(x[:, j, 0:w], ps[:, 0:w], mod[:, 16 + j, r:r + 1], x[:, j, 0:w], ALU.mult, ALU.add)
            mk.dma("sp", xv[:, :, t0:t0 + w], x[:, :, 0:w])


def _moe(self, l):
    p, mk = self, self.mk
    mod = p.mod
    with mk.scope():
        rt = mk.sb("rt", [128, NT, 8])
        gidx = mk.sb("gidx", [128, NT, 2], I32)
        with mk.scope():
            g2n = mk.sb("g2n", [128, 8])
            mk.dma("sp", g2n[:], p.n2g[l, :, :])
            A2 = mk.sb("A2", [128, 8, 2])
            for r in range(2):
                mk.stt(A2[:, :, r], mod[:, 32:40, r], 1.0, g2n[:], ALU.add, ALU.mult)
            B2 = mod[:, 24:32, :]
            wrt = mk.sb("wrt", [128, 8, 36])
            mk.dma("sp", wrt[:, :, 0:4], p.moe_wg[l, :, :].rearrange("(k q) n -> q k n", q=128))
            mk.dma("sp", wrt[:, :, 4:36], p.moe_we[l, :, :].rearrange("(k q) n -> q k n", q=128))
            bias = mk.sb("rbias", [128, 36])
            mk.dma("sp", bias[:, 0:4], p.moe_bg[l, :].partition_broadcast(128))
            mk.dma("sp", bias[:, 4:36], p.moe_be[l, :].partition_broadcast(128))
            lstrict = mk.sb("lstrict", [128, 128])
            mk.dma("sp", lstrict[:], p.c_lstrict[:, :])
            iotaE = mk.sb("iotaE", [128, 32])
            mk.dma("sp", iotaE[:], p.c_iotaE[:, :])
            xb = [mk.sb("mx%d" % i, [128, 8, 512]) for i in range(2)]
            sq = mk.sb("msq", [128, 8, 512], F32R)
            rstd = mk.sb("mrstd", [128, 512])
            hb = mk.sb("mhb", [128, 8, 512])
            htok = mk.sb("htok", [128, NT, D])
            lr = mk.sb("r_lr", [128, NT, 36])
            xv = p.xT.ap().rearrange("(k q) t -> q k t", q=128)
            for bi, (t0, w) in enumerate(TB):
                r = 0 if t0 < T_LAT else 1
                x = xb[bi % 2]
                mk.dma("sp", x[:, :, 0:w], xv[:, :, t0:t0 + w])
                mk.act(sq[:, :, 0:w], x[:, :, 0:w], AF.Square)
                ps = p.psum[0]
                for k in range(8):
                    mk.mm(ps[:, 0:w], lhsT=p.onesR[:], rhs=sq[:, k, 0:w], start=(k == 0), stop=(k == 7))
                mk.act(rstd[:, 0:w], ps[:, 0:w], AF.Sqrt, bias=EPS, scale=1.0 / D)
                mk.recip(rstd[:, 0:w], rstd[:, 0:w])
                for k in range(8):
                    mk.tt(hb[:, k, 0:w], x[:, k, 0:w], rstd[:, 0:w], ALU.mult)
                    mk.act(hb[:, k, 0:w], hb[:, k, 0:w], AF.Identity, bias=B2[:, k, r:r + 1], scale=A2[:, k, r:r + 1])
                for j in range(w // 128):
                    n = t0 // 128 + j
                    sl = slice(j * 128, (j + 1) * 128)
                    pl = p.psum[1]
                    for k in range(8):
                        mk.mm(pl[:, 0:36], lhsT=hb[:, k, sl], rhs=wrt[:, k, :], start=(k == 0), stop=(k == 7))
                    mk.tt(lr[:, n, :], pl[:, 0:36], bias[:], ALU.add)
                    for k in range(8):
                        pt = p.psum[2 + k % 4]
                        mk.tr(pt[:, 0:128], hb[:, k, sl], p.identS[:])
                        p.evac(htok[:, n, k * 128:(k + 1) * 128], pt[:, 0:128])
            R = lambda nm, shp: mk.sb("r_" + nm, [128] + shp)
            gmax, gsum, psel = R("gmax", [NT]), R("gsum", [NT]), R("psel", [NT])
            eg, ohg, pen = R("eg", [NT, 4]), R("ohg", [NT, 4]), R("pen", [NT, 4])
            lem, lem2 = R("lem", [NT, 32]), R("lem2", [NT, 32])
            m1, m2, dm, ee, w1 = R("m1", [NT]), R("m2", [NT]), R("dm", [NT]), R("e", [NT]), R("w1", [NT])
            oh1, oh2, oh, cnt, t32 = R("oh1", [NT, 32]), R("oh2", [NT, 32]), R("oh", [NT, 32]), R("cnt", [NT, 32]), R("t32", [NT, 32])
            s_, f__, gs = R("s", [NT, 2]), R("f", [NT, 2]), R("gs", [NT, 2])
            bc3 = lambda a, k: a[:].unsqueeze(2).to_broadcast([128, NT, k])
            lg4 = lr[:, :, 0:4]
            mk.reduce(gmax[:], lg4, ALU.max)
            mk.tt(eg[:], lg4, bc3(gmax, 4), ALU.subtract)
            mk.act(eg[:], eg[:], AF.Exp)
            mk.reduce(gsum[:], eg[:], ALU.add)
            mk.recip(psel[:], gsum[:])
            mk.tt(ohg[:], lg4, bc3(gmax, 4), ALU.is_equal)
            mk.tsc(pen[:], ohg[:], 1.0e9, ALU.mult, -1.0e9, ALU.add)
            mk.tt(lem[:].rearrange("q n (g e) -> q n g e", g=4), lr[:, :, 4:36].rearrange("q n (g e) -> q n g e", g=4),
                  pen[:].unsqueeze(3).to_broadcast([128, NT, 4, 8]), ALU.add)
            mk.reduce(m1[:], lem[:], ALU.max)
            mk.tt(oh1[:], lem[:], bc3(m1, 32), ALU.is_equal)
            mk.stt(lem2[:], oh1[:], -2.0e9, lem[:], ALU.mult, ALU.add)
            mk.reduce(m2[:], lem2[:], ALU.max)
            mk.tt(oh2[:], lem2[:], bc3(m2, 32), ALU.is_equal)
            mk.tt(oh[:], oh1[:], oh2[:], ALU.add)
            mk.tt(dm[:], m2[:], m1[:], ALU.subtract)
            mk.act(ee[:], dm[:], AF.Exp)
            mk.tsc(w1[:], ee[:], 1.0, ALU.add)
            mk.recip(w1[:], w1[:])
            mk.tt(rt[:, :, 0], w1[:], psel[:], ALU.mult)
            mk.tt(rt[:, :, 1], rt[:, :, 0], ee[:], ALU.mult)
            pcs = [p.psum[6], p.psum[7]]
            for n in range(NT):
                pc = pcs[n // 9][:, (n % 9) * 32:(n % 9 + 1) * 32]
                mk.mm(pc, lhsT=lstrict[:], rhs=oh[:, n, :], start=True, stop=(n == 0))
                for m_ in range(n):
                    mk.mm(pc, lhsT=p.onesF[:], rhs=oh[:, m_, :], start=False, stop=(m_ == n - 1))
            pos = R("pos", [NT, 32])
            for hf in range(2):
                mk.copy(pos[:, hf * 9:(hf + 1) * 9, :].rearrange("q n e -> q (n e)"), pcs[hf][:, 0:288])
            mk.tt(cnt[:], pos[:], iotaE[:].unsqueeze(1).to_broadcast([128, NT, 32]), ALU.add)
            for kk, ohk in enumerate((oh1, oh2)):
                mk.tt(t32[:], ohk[:], cnt[:], ALU.mult)
                mk.reduce(s_[:, :, kk], t32[:], ALU.add)
                mk.tt(t32[:], ohk[:], pos[:], ALU.mult)
                mk.reduce(f__[:, :, kk], t32[:], ALU.add)
            mk.tsc(f__[:], f__[:], float(CAP) - 0.5, ALU.is_gt, BIGIDX, ALU.mult)
            mk.tt(gs[:], s_[:], f__[:], ALU.add)
            sidx = mk.sb("sidx", [128, NT, 2], I32)
            mk.copy(sidx[:], gs[:])
            mk.tsc(gs[:], gs[:], float(NSLOT), ALU.min)
            mk.copy(gidx[:], gs[:])
            for n in range(NT):
                for kk in range(2):
                    mk.dma("pool", p.Xb[:, :], htok[:, n, :],
                           fn=lambda e, kk=kk, n=n: e.indirect_dma_start(
                               out=p.Xb[:, :], out_offset=bass.IndirectOffsetOnAxis(ap=sidx[:, n, kk:kk + 1], axis=0),
                               in_=htok[:, n, :], in_offset=None, bounds_check=p.breg, oob_is_err=False),
                           extra_reads=[sidx])
        if getattr(p, 'moe_stop', 3) < 2:
            return
        with mk.scope():
            W = [{"g": mk.sb("WgR%d" % i, [128, 8, 512], F32R), "u": mk.sb("WuR%d" % i, [128, 8, 512], F32R),
                  "d": mk.sb("WdR%d" % i, [128, 4, 1024], F32R)} for i in range(2)]
            NS = CAP // 128
            xtok = [mk.sb("extok%d" % i, [128, D]) for i in range(2 * NS)]
            xbT = [mk.sb("xbT%d" % i, [128, 8, CAP], F32R) for i in range(2)]
            hidT = mk.sb("hidT", [128, 4, CAP], F32R)
            sil = [mk.sb("sil%d" % i, [128, CAP]) for i in range(2)]
            yt = [mk.sb("eyt%d" % i, [128, D]) for i in range(3)]
            yi = 0

            def load_w(e):
                w = W[e % 2]
                mk.dma("pool", w["g"][:], p.moe_w_gate[l, e, :, :].rearrange("(k q) n -> q k n", q=128))
                mk.dma("pool", w["u"][:], p.moe_w_up[l, e, :, :].rearrange("(k q) n -> q k n", q=128))
                mk.dma("pool", w["d"][:], p.moe_w_down[l, e, :, :].rearrange("(k q) n -> q k n", q=128))

            def load_x(e):
                for si_ in range(NS):
                    r0 = e * CAP + si_ * 128
                    mk.dma("sp", xtok[(e % 2) * NS + si_][:], p.Xb[r0:r0 + 128, :])

            load_w(0)
            load_x(0)
            for e in range(32):
                if e + 1 < 32:
                    load_w(e + 1)
                    load_x(e + 1)
                w = W[e % 2]
                xT_ = xbT[e % 2]
                for si_ in range(NS):
                    xt = xtok[(e % 2) * NS + si_]
                    for k in range(8):
                        pt = p.psum[k % 4]
                        mk.tr(pt[:, 0:128], xt[:, k * 128:(k + 1) * 128], p.identS[:])
                        p.evac(xT_[:, k, si_ * 128:(si_ + 1) * 128], pt[:, 0:128])
                for f in range(4):
                    pg, pu = p.psum[4 + (f % 2) * 2], p.psum[5 + (f % 2) * 2]
                    for k in range(8):
                        mk.mm(pg[:, 0:CAP], lhsT=w["g"][:, k, f * 128:(f + 1) * 128], rhs=xT_[:, k, :], start=(k == 0), stop=(k == 7))
                    for k in range(8):
                        mk.mm(pu[:, 0:CAP], lhsT=w["u"][:, k, f * 128:(f + 1) * 128], rhs=xT_[:, k, :], start=(k == 0), stop=(k == 7))
                    sl_ = sil[f % 2]
                    mk.act(sl_[:], pg[:, 0:CAP], AF.Silu)
                    mk.tt(hidT[:, f, :], sl_[:], pu[:, 0:CAP], ALU.mult)
                for si_ in range(NS):
                    y = yt[yi % 3]
                    yi += 1
                    for hf in range(2):
                        ps = p.psum[hf]
                        for f in range(4):
                            mk.mm(ps[:, 0:512], lhsT=hidT[:, f, si_ * 128:(si_ + 1) * 128], rhs=w["d"][:, f, hf * 512:(hf + 1) * 512],
                                  start=(f == 0), stop=(f == 3))
                        p.evac(y[:, hf * 512:(hf + 1) * 512], ps[:, 0:512])
                    r0 = e * CAP + si_ * 128
                    mk.dma("act", p.Yb[r0:r0 + 128, :], y[:])
        if getattr(p, 'moe_stop', 3) < 3:
            return
        with mk.scope():
            y1 = [mk.sb("cy1%d" % i, [128, D]) for i in range(2)]
            y2 = [mk.sb("cy2%d" % i, [128, D]) for i in range(2)]
            xb = [mk.sb("cxb%d" % i, [128, 8, 512]) for i in range(2)]
            xv = p.xT.ap().rearrange("(k q) t -> q k t", q=128)
            for bi, (t0, w) in enumerate(TB):
                r = 0 if t0 < T_LAT else 1
                x = xb[bi % 2]
                mk.dma("sp", x[:, :, 0:w], xv[:, :, t0:t0 + w])
                for j in range(w // 128):
                    n = t0 // 128 + j
                    a, b = y1[n % 2], y2[n % 2]
                    for kk, dst in enumerate((a, b)):
                        mk.dma("pool", dst[:], p.Yb[:, :],
                               fn=lambda e, kk=kk, dst=dst, n=n: e.indirect_dma_start(
                                   out=dst[:], out_offset=None, in_=p.Yb[:, :],
                                   in_offset=bass.IndirectOffsetOnAxis(ap=gidx[:, n, kk:kk + 1], axis=0)),
                               extra_reads=[gidx])
                    mk.tsc(a[:], a[:], rt[:, n, 0:1], ALU.mult)
                    mk.stt(a[:], b[:], rt[:, n, 1:2], a[:], ALU.mult, ALU.add)
                    for k in range(8):
                        pt = p.psum[k % 4]
                        mk.tr(pt[:, 0:128], a[:, k * 128:(k + 1) * 128], p.identS[:])
                        mk.stt(x[:, k, j * 128:(j + 1) * 128], pt[:, 0:128], mod[:, 40 + k, r:r + 1],
                               x[:, k, j * 128:(j + 1) * 128], ALU.mult, ALU.add)
                mk.dma("sp", xv[:, :, t0:t0 + w], x[:, :, 0:w])


def _moe_init(self):
    p, mk = self, self.mk
    with mk.scope():
        z = mk.sb("zeros", [128, D])
        mk.memset(z[:], 0.0)
        for i in range((NSLOT + 128) // 128):
            mk.dma("sp" if i % 2 == 0 else "pool", p.Xb[i * 128:(i + 1) * 128, :], z[:])
        for i in range((NSLOT + 128) // 128):
            mk.dma("sp" if i % 2 == 0 else "pool", p.Yb[i * 128:(i + 1) * 128, :], z[:])


Prog.outproj = _outproj
Prog.moe = _moe
Prog.moe_init = _moe_init

NA_SCALE = 64 ** -0.5
MLA_SCALE = 96 ** -0.5


def _headnorm(self, out, x, nh, hd, gain_bc, sq, ss, np_=128):
    mk = self.mk
    xv = x.rearrange("q (h d) -> q h d", h=nh)
    sqv = sq.rearrange("q (h d) -> q h d", h=nh)
    mk.tt(sq, x, x, ALU.mult)
    mk.reduce(ss, sqv, ALU.add)
    mk.act(ss, ss, AF.Sqrt, bias=EPS, scale=1.0 / hd)
    mk.recip(ss, ss)
    mk.tt(sqv, xv, ss.unsqueeze(2).to_broadcast([np_, nh, hd]), ALU.mult)
    mk.tt(out, sq, gain_bc, ALU.mult)


def _transposes_out(self, src_all, row0):
    p, mk = self, self.mk
    with mk.scope():
        ob = [mk.sb("tob%d" % i, [128, 512]) for i in range(2)]
        oi = 0
        for c in range(2):
            for bi, (t0, w) in enumerate(TB):
                o = ob[oi % 2]
                oi += 1
                for j in range(w // 128):
                    n = t0 // 128 + j
                    pt = p.psum[(n + c) % 4]
                    mk.tr(pt[:, 0:128], src_all[:, n, c * 128:(c + 1) * 128], p.identS[:])
                    p.evac(o[:, j * 128:(j + 1) * 128], pt[:, 0:128])
                mk.dma("sp", p.catT[row0 + c * 128:row0 + (c + 1) * 128, t0:t0 + w], o[:, 0:w])


def _na(self, l):
    p, mk = self, self.mk
    with mk.scope():
        qT = mk.sb("naqT", [128, 2, T_ALL], F32R)
        kT = mk.sb("nakT", [128, 2, T_ALL], F32R)
        V = mk.sb("naV", [128, NT, 4, 66], F32R)
        out_all = mk.sb("naout", [128, NT, 256])
        with mk.scope():
            gq = mk.sb("nagq", [128, 4, 64])
            gk = mk.sb("nagk", [128, 4, 64])
            for h in range(4):
                mk.dma("sp", gq[:, h, :], p.na_q_g[l, :].partition_broadcast(128))
                mk.dma("sp", gk[:, h, :], p.na_k_g[l, :].partition_broadcast(128))
            ones65 = mk.sb("ones65", [128, 4, 2])
            mk.memset(ones65[:], 0.0)
            mk.memset(ones65[:, :, 0:1], 1.0)
            tin = [mk.sb("natin%d" % i, [128, 768]) for i in range(2)]
            sq = mk.sb("nasq", [128, 256])
            ss = mk.sb("nass", [128, 4])
            qn = mk.sb("naqn", [128, 256])
            for n in range(NT):
                t = tin[n % 2]
                mk.dma("sp", t[:], p.ptok[n * 128:(n + 1) * 128, 2048:2816])
                for (src, g, dstT) in ((t[:, 0:256], gq, qT), (t[:, 256:512], gk, kT)):
                    p.headnorm(qn[:], src, 4, 64, g[:].rearrange("q h d -> q (h d)"), sq[:], ss[:])
                    for c in range(2):
                        pt = p.psum[c]
                        mk.tr(pt[:, 0:128], qn[:, c * 128:(c + 1) * 128], p.identS[:])
                        p.evac(dstT[:, c, n * 128:(n + 1) * 128], pt[:, 0:128])
                mk.copy(V[:, n, :, 0:64], t[:, 512:768].rearrange("q (h d) -> q h d", h=4), en="pool")
                mk.copy(V[:, n, :, 64:66], ones65[:], en="pool")
        with mk.scope():
            bias = [mk.sb("nabias%d" % i, [128, 5, 5, 128]) for i in range(2)]
            PT = [mk.sb("naPT%d" % i, [128, 7, 128], F32R) for i in range(2)]
            tmpb = [mk.sb("natmp%d" % i, [128, 5, 128]) for i in range(2)]
            rec = mk.sb("narec", [128, 1])
            PTc = mk.sb("naPTc", [128, 2, 256], F32R)
            it = 0
            for h in range(4):
                hb, hc = (h % 2) * 64, h // 2
                bs = bias[h % 2]
                mk.dma("sp", bs[:], p.na_biasT[l, h, :, :, :, :])
                for pr in range(16):
                    pat = 0 if pr == 0 else 1 if pr == 1 else 3 if pr == 14 else 4 if pr == 15 else 2
                    rs0 = min(max(2 * pr - 4, 0), 24)
                    ws = min((rs0 // 2) * 2, 22)
                    kt0 = ws // 2
                    q0 = pr * 128
                    pa, pb = p.psum[(it % 2) * 2], p.psum[(it % 2) * 2 + 1]
                    P_, tb = PT[it % 2], tmpb[it % 2]
                    for kt in range(4):
                        mk.mm(pa[:, kt * 128:(kt + 1) * 128], lhsT=kT[hb:hb + 64, hc, (kt0 + kt) * 128:(kt0 + kt + 1) * 128],
                              rhs=qT[hb:hb + 64, hc, q0:q0 + 128])
                    mk.mm(pb[:, 0:128], lhsT=kT[hb:hb + 64, hc, (kt0 + 4) * 128:(kt0 + 5) * 128], rhs=qT[hb:hb + 64, hc, q0:q0 + 128])
                    for j in range(2):
                        mk.mm(pb[:, 128 + j * 128:256 + j * 128], lhsT=kT[hb:hb + 64, hc, T_LAT + j * 128:T_LAT + (j + 1) * 128],
                              rhs=qT[hb:hb + 64, hc, q0:q0 + 128])
                    mk.stt(tb[:, 0:4, :], pa[:, 0:512].rearrange("q (a b) -> q a b", a=4), NA_SCALE, bs[:, pat, 0:4, :], ALU.mult, ALU.add)
                    mk.stt(tb[:, 4, :], pb[:, 0:128], NA_SCALE, bs[:, pat, 4, :], ALU.mult, ALU.add)
                    mk.act(P_[:, 0:5, :], tb[:], AF.Exp)
                    mk.act(P_[:, 5:7, :], pb[:, 128:384].rearrange("q (a b) -> q a b", a=2), AF.Exp, scale=NA_SCALE)
                    po = p.psum[4 + it % 2]
                    for kt in range(7):
                        vt = kt0 + kt if kt < 5 else 16 + (kt - 5)
                        mk.mm(po[:, 0:66], lhsT=P_[:, kt, :], rhs=V[:, vt, h, :], start=(kt == 0), stop=(kt == 6))
                    mk.recip(rec[:], po[:, 64:65])
                    mk.tsc(out_all[:, pr, h * 64:(h + 1) * 64], po[:, 0:64], rec[:, 0:1], ALU.mult)
                    it += 1
                pc_ = p.psum[6]
                for j in range(2):
                    mk.mm(pc_[:, j * 256:(j + 1) * 256], lhsT=kT[hb:hb + 64, hc, T_LAT + j * 128:T_LAT + (j + 1) * 128],
                          rhs=qT[hb:hb + 64, hc, T_LAT:T_ALL])
                mk.act(PTc[:], pc_[:, 0:512].rearrange("q (a b) -> q a b", a=2), AF.Exp, scale=NA_SCALE)
                for qi in range(2):
                    po = p.psum[7]
                    for j in range(2):
                        mk.mm(po[:, 0:66], lhsT=PTc[:, j, qi * 128:(qi + 1) * 128], rhs=V[:, 16 + j, h, :], start=(j == 0), stop=(j == 1))
                    mk.recip(rec[:], po[:, 64:65])
                    mk.tsc(out_all[:, 16 + qi, h * 64:(h + 1) * 64], po[:, 0:64], rec[:, 0:1], ALU.mult)
        p.transposes_out(out_all, 512)


def _mla(self, l):
    p, mk = self, self.mk
    with mk.scope():
        qT = mk.sb("mlqT", [96, 4, T_ALL], F32R)
        kT = mk.sb("mlkT", [96, 4, T_ALL], F32R)
        V = mk.sb("mlV", [128, NT, 4, 66], F32R)
        out_all = mk.sb("mlout", [128, NT, 256])
        with mk.scope():
            cqT = mk.sb("cqT", [128, 2, T_ALL], F32R)
            ckvT = mk.sb("ckvT", [128, T_ALL], F32R)
            gqa = mk.sb("gqa", [128, 256])
            gkva = mk.sb("gkva", [128, 128])
            mk.dma("sp", gqa[:], p.mla_q_a_g[l, :].partition_broadcast(128))
            mk.dma("sp", gkva[:], p.mla_kv_a_g[l, :].partition_broadcast(128))
            gq = mk.sb("mgq", [128, 4, 96])
            gk = mk.sb("mgk", [128, 4, 96])
            for h in range(4):
                mk.dma("sp", gq[:, h, :], p.mla_q_g[l, :].partition_broadcast(128))
                mk.dma("sp", gk[:, h, :], p.mla_k_g[l, :].partition_broadcast(128))
            ones65 = mk.sb("mones65", [128, 4, 2])
            mk.memset(ones65[:], 0.0)
            mk.memset(ones65[:, :, 0:1], 1.0)
            wst = mk.sb("mwst", [128, 768])
            wuq = mk.sb("wuq", [128, 2, 384], F32R)
            wukv = mk.sb("wukv", [128, 512], F32R)
            mk.dma("sp", wst[:].rearrange("q (k n) -> q k n", k=2), p.mla_w_uq[l, :, :].rearrange("(k q) n -> q k n", q=128))
            mk.copy(wuq[:], wst[:].rearrange("q (k n) -> q k n", k=2), en="pool")
            mk.dma("sp", wst[:, 0:512], p.mla_w_ukv[l, :, :])
            mk.copy(wukv[:], wst[:, 0:512], en="pool")
            cosT = mk.sb("ropec", [128, 16, 32])
            sinT = mk.sb("ropes", [128, 16, 32])
            mk.dma("sp", cosT[:], p.c_ropecos[:, :, :])
            mk.dma("sp", sinT[:], p.c_ropesin[:, :, :])
            tin = [mk.sb("mltin%d" % i, [128, 416]) for i in range(2)]
            sq = mk.sb("mlsq", [128, 384])
            ss = mk.sb("mlss", [128, 4])
            nrm = mk.sb("mlnrm", [128, 384])
            for n in range(NT):
                t = tin[n % 2]
                mk.dma("sp", t[:], p.ptok[n * 128:(n + 1) * 128, 2816:3232])
                p.headnorm(nrm[:, 0:256], t[:, 0:256], 1, 256, gqa[:], sq[:, 0:256], ss[:, 0:1])
                for c in range(2):
                    pt = p.psum[c]
                    mk.tr(pt[:, 0:128], nrm[:, c * 128:(c + 1) * 128], p.identS[:])
                    p.evac(cqT[:, c, n * 128:(n + 1) * 128], pt[:, 0:128])
                p.headnorm(nrm[:, 256:384], t[:, 256:384], 1, 128, gkva[:], sq[:, 0:128], ss[:, 0:1])
                pt = p.psum[2]
                mk.tr(pt[:, 0:128], nrm[:, 256:384], p.identS[:])
                p.evac(ckvT[:, n * 128:(n + 1) * 128], pt[:, 0:128])
            qk = [mk.sb("mlqk%d" % i, [128, 384]) for i in range(2)]
            sw = mk.sb("mlsw", [128, 4, 32])
            for n in range(NT):
                t = tin[n % 2]
                mk.dma("sp", t[:, 384:416], p.ptok[n * 128:(n + 1) * 128, 3200:3232])
                pq, pkv = p.psum[3], p.psum[4]
                for c in range(2):
                    mk.mm(pq[:, 0:384], lhsT=cqT[:, c, n * 128:(n + 1) * 128], rhs=wuq[:, c, :], start=(c == 0), stop=(c == 1))
                mk.mm(pkv[:, 0:512], lhsT=ckvT[:, n * 128:(n + 1) * 128], rhs=wukv[:], start=True, stop=True)
                kvv = pkv[:, 0:512].rearrange("q (h d) -> q h d", h=4)
                mk.copy(V[:, n, :, 0:64], kvv[:, :, 64:128], en="act")
                mk.copy(V[:, n, :, 64:66], ones65[:], en="pool")
                for which in range(2):
                    raw = qk[which]
                    rv = raw[:].rearrange("q (h d) -> q h d", h=4)
                    if which == 0:
                        mk.copy(raw[:], pq[:, 0:384])
                        g, dstT = gq, qT
                    else:
                        mk.copy(rv[:, :, 0:64], kvv[:, :, 0:64])
                        mk.copy(rv[:, :, 64:96], t[:, 384:416].unsqueeze(1).to_broadcast([128, 4, 32]), en="pool")
                        g, dstT = gk, kT
                    p.headnorm(nrm[:], raw[:], 4, 96, g[:].rearrange("q h d -> q (h d)"), sq[:], ss[:])
                    nv = nrm[:].rearrange("q (h d) -> q h d", h=4)
                    if n < 16:
                        for s0 in (64, 80):
                            o0 = s0 - 64
                            mk.copy(sw[:, :, o0:o0 + 8], nv[:, :, s0 + 8:s0 + 16])
                            mk.copy(sw[:, :, o0 + 8:o0 + 16], nv[:, :, s0:s0 + 8])
                        mk.tt(sw[:], sw[:], sinT[:, n, :].unsqueeze(1).to_broadcast([128, 4, 32]), ALU.mult)
                        mk.tt(nv[:, :, 64:96], nv[:, :, 64:96], cosT[:, n, :].unsqueeze(1).to_broadcast([128, 4, 32]), ALU.mult)
                        mk.tt(nv[:, :, 64:96], nv[:, :, 64:96], sw[:], ALU.add)
                    for h in range(4):
                        pt = p.psum[5 + h % 2]
                        mk.tr(pt[0:96, 0:128], nrm[:, h * 96:(h + 1) * 96], p.identS[:])
                        p.evac(dstT[:, h, n * 128:(n + 1) * 128], pt[0:96, 0:128])
        with mk.scope():
            PT = mk.sb("mlPT", [128, NT, 512], F32R)
            rec = mk.sb("mlrec", [128, 1])
            for h in range(4):
                for bi, (t0, w) in enumerate(TB):
                    kts = list(range(NT)) if t0 < T_LAT else [16, 17]
                    for i, kt in enumerate(kts):
                        ps = p.psum[i % 4]
                        mk.mm(ps[:, 0:w], lhsT=kT[:, h, kt * 128:(kt + 1) * 128], rhs=qT[:, h, t0:t0 + w])
                        mk.act(PT[:, i, 0:w], ps[:, 0:w], AF.Exp, scale=MLA_SCALE)
                    for j in range(w // 128):
                        n = t0 // 128 + j
                        po = p.psum[4 + j % 2]
                        for i, kt in enumerate(kts):
                            mk.mm(po[:, 0:66], lhsT=PT[:, i, j * 128:(j + 1) * 128], rhs=V[:, kt, h, :],
                                  start=(i == 0), stop=(i == len(kts) - 1))
                        mk.recip(rec[:], po[:, 64:65])
                        mk.tsc(out_all[:, n, h * 64:(h + 1) * 64], po[:, 0:64], rec[:, 0:1], ALU.mult)
        p.transposes_out(out_all, 768)


Prog.headnorm = _headnorm
Prog.transposes_out = _transposes_out
Prog.na = _na
Prog.mla = _mla


def _hgrn_gen(self, l):
    p, mk = self, self.mk
    if True:
        lb = mk.sb("lb", [64, 512])
        oml = mk.sb("oml", [64, 512])
        if True:
            lg = mk.sb("lblg", [64, 4, 512])
            mk.dma("sp", lg[:], p.hgrn_lb.ap().rearrange("l a b -> l (a b)").partition_broadcast(64))
            mk.act(lg[:], lg[:], AF.Exp)
            tot = mk.sb("lbtot", [64, 512])
            mk.tt(tot[:], lg[:, 0, :], lg[:, 1, :], ALU.add)
            mk.tt(tot[:], tot[:], lg[:, 2, :], ALU.add)
            mk.tt(tot[:], tot[:], lg[:, 3, :], ALU.add)
            mk.recip(tot[:], tot[:])
            mk.memset(lb[:], 0.0)
            for ll in range(1, l + 1):
                mk.tt(lb[:], lb[:], lg[:, ll, :], ALU.add)
            mk.tt(lb[:], lb[:], tot[:], ALU.mult)
            mk.tsc(oml[:], lb[:], -1.0, ALU.mult, 1.0, ALU.add)
        gn = mk.sb("hgn", [64, 4, 64])
        for h in range(4):
            mk.dma("sp", gn[:, h, :], p.hgrn_norm_g[l, :].partition_broadcast(64))
        tri = [mk.sb("tri%d" % i, [64, 64]) for i in range(2)]
        mk.dma("sp", tri[0][:], p.c_trif[:, :])
        mk.dma("sp", tri[1][:], p.c_trib[:, :])
        ones1 = mk.sb("hones1", [64, 1])
        mk.memset(ones1[:], 1.0)
        S = mk.sb("hS", [64, 4, 64])
        tin = [mk.sb("htin%d" % i, [64, 1280]) for i in range(2)]
        NB = 2
        f_ = [mk.sb("hf%d" % i, [64, 256]) for i in range(NB)]
        lf = [mk.sb("hlf%d" % i, [64, 256]) for i in range(NB)]
        kk = [mk.sb("hkk%d" % i, [64, 256]) for i in range(NB)]
        bc = [mk.sb("hbc%d" % i, [64, 256]) for i in range(NB)]
        eb = [mk.sb("heb%d" % i, [64, 256]) for i in range(NB)]
        qe = [mk.sb("hqe%d" % i, [64, 256]) for i in range(NB)]
        ke = [mk.sb("hke%d" % i, [64, 256]) for i in range(NB)]
        qeT = [mk.sb("hqeT%d" % i, [64, 4, 64]) for i in range(NB)]
        keT = [mk.sb("hkeT%d" % i, [64, 4, 64]) for i in range(NB)]
        ebl = [mk.sb("hebl%d" % i, [64, 4]) for i in range(NB)]
        ATm = [mk.sb("hAT%d" % i, [64, 4, 64]) for i in range(NB)]
        osb = [mk.sb("hosb%d" % i, [64, 256]) for i in range(NB)]
        of_ = [mk.sb("hof%d" % i, [64, 256]) for i in range(NB)]
        sq = mk.sb("hsq", [64, 256])
        ss = mk.sb("hss", [64, 4])
        sg = mk.sb("hsg", [64, 256])
        oT = [mk.sb("hoT%d" % i, [128, 2, 64]) for i in range(NB)]
        tmpS = mk.sb("htmpS", [64, 4, 64])
        it = 0
        for d in range(2):
            mk.memset(S[:], 0.0)
            if d == 0:
                order = [(T_LAT + 64 * c) for c in range(4)] + [64 * c for c in range(32)]
            else:
                order = [(T_LAT + 64 * c) for c in reversed(range(4))] + [64 * c for c in reversed(range(32))]
            for tok0 in order:
                i = it % NB
                it += 1
                t = tin[i]
                mk.dma("sp", t[:], p.ptok[tok0:tok0 + 64, 0:1280])
                z = t[:, 256 + 256 * d:512 + 256 * d]
                mk.act(f_[i][:], z, AF.Sigmoid)
                mk.tt(f_[i][:], f_[i][:], oml[:, d * 256:(d + 1) * 256], ALU.mult)
                mk.tt(f_[i][:], f_[i][:], lb[:, d * 256:(d + 1) * 256], ALU.add)
                mk.act(lf[i][:], f_[i][:], AF.Ln)
                mk.tsc(kk[i][:], f_[i][:], -1.0, ALU.mult, 1.0, ALU.add)
                pb = p.psum[0]
                mk.mm(pb[0:64, 0:256], lhsT=tri[d][:], rhs=lf[i][:])
                mk.tsc(bc[i][:], pb[0:64, 0:256], -80.0, ALU.max)
                pl = p.psum[1]
                for h in range(4):
                    mk.mm(pl[0:64, h:h + 1], lhsT=lf[i][:, h * 64:(h + 1) * 64], rhs=ones1[:])
                mk.tsc(ebl[i][:], pl[0:64, 0:4], -80.0, ALU.max)
                mk.act(ebl[i][:], ebl[i][:], AF.Exp)
                mk.act(eb[i][:], bc[i][:], AF.Exp)
                mk.tt(qe[i][:], t[:, 0:256], eb[i][:], ALU.mult)
                mk.act(eb[i][:], bc[i][:], AF.Exp, scale=-1.0)
                mk.tt(ke[i][:], kk[i][:], eb[i][:], ALU.mult)
                pq, pk = p.psum[2], p.psum[3]
                for h in range(4):
                    mk.tr(pq[0:64, h * 64:(h + 1) * 64], qe[i][:, h * 64:(h + 1) * 64], p.identS[0:64, 0:64])
                    mk.tr(pk[0:64, h * 64:(h + 1) * 64], ke[i][:, h * 64:(h + 1) * 64], p.identS[0:64, 0:64])
                mk.copy(qeT[i][:].rearrange("q h t -> q (h t)"), pq[0:64, 0:256])
                mk.copy(keT[i][:].rearrange("q h t -> q (h t)"), pk[0:64, 0:256], en="act")
                pa = p.psum[4]
                for h in range(4):
                    mk.mm(pa[0:64, h * 64:(h + 1) * 64], lhsT=keT[i][:, h, :], rhs=qeT[i][:, h, :])
                mk.tt(ATm[i][:], pa[0:64, 0:256].rearrange("q (h t) -> q h t", h=4),
                      tri[d][:].unsqueeze(1).to_broadcast([64, 4, 64]), ALU.mult)
                po = p.psum[5]
                v = t[:, 768:1024]
                for h in range(4):
                    mk.mm(po[0:64, h * 64:(h + 1) * 64], lhsT=ATm[i][:, h, :], rhs=v[:, h * 64:(h + 1) * 64], start=True, stop=False)
                    mk.mm(po[0:64, h * 64:(h + 1) * 64], lhsT=qeT[i][:, h, :], rhs=S[:, h, :], start=False, stop=True)
                pd = p.psum[6]
                for h in range(4):
                    mk.mm(pd[0:64, h * 64:(h + 1) * 64], lhsT=ke[i][:, h * 64:(h + 1) * 64], rhs=v[:, h * 64:(h + 1) * 64])
                mk.tt(tmpS[:], S[:], pd[0:64, 0:256].rearrange("q (h t) -> q h t", h=4), ALU.add)
                mk.tt(S[:], tmpS[:], ebl[i][:].unsqueeze(2).to_broadcast([64, 4, 64]), ALU.mult)
                if d == 0:
                    mk.copy(osb[i][:], po[0:64, 0:256], en="act")
                    mk.dma("pool", p.hof[tok0:tok0 + 64, :], osb[i][:])
                else:
                    mk.dma("pool", of_[i][:], p.hof[tok0:tok0 + 64, :])
                    mk.tt(osb[i][:], po[0:64, 0:256], of_[i][:], ALU.add)
                    p.headnorm(osb[i][:], osb[i][:], 4, 64, gn[:].rearrange("q h d -> q (h d)"), sq[:], ss[:], np_=64)
                    mk.act(sg[:], t[:, 1024:1280], AF.Silu)
                    mk.tt(osb[i][:], osb[i][:], sg[:], ALU.mult)
                    pt = p.psum[7]
                    for c in range(2):
                        mk.tr(pt[:, c * 64:(c + 1) * 64], osb[i][:, c * 128:(c + 1) * 128], p.identS[0:64, 0:64])
                    mk.copy(oT[i][:].rearrange("q c t -> q (c t)"), pt[:, 0:128])
                    mk.dma("sp", p.catT[0:256, tok0:tok0 + 64].rearrange("(c q) t -> q c t", q=128), oT[i][:])
                yield


def _hgrn(self, l):
    p, mk = self, self.mk
    G = 4
    with mk.scope():
        lb = mk.sb("lb", [64, 512])
        oml = mk.sb("oml", [64, 512])
        with mk.scope():
            lg = mk.sb("lblg", [64, 4, 512])
            mk.dma("sp", lg[:], p.hgrn_lb.ap().rearrange("l a b -> l (a b)").partition_broadcast(64))
            mk.act(lg[:], lg[:], AF.Exp)
            tot = mk.sb("lbtot", [64, 512])
            mk.tt(tot[:], lg[:, 0, :], lg[:, 1, :], ALU.add)
            mk.tt(tot[:], tot[:], lg[:, 2, :], ALU.add)
            mk.tt(tot[:], tot[:], lg[:, 3, :], ALU.add)
            mk.recip(tot[:], tot[:])
            mk.memset(lb[:], 0.0)
            for ll in range(1, l + 1):
                mk.tt(lb[:], lb[:], lg[:, ll, :], ALU.add)
            mk.tt(lb[:], lb[:], tot[:], ALU.mult)
            mk.tsc(oml[:], lb[:], -1.0, ALU.mult, 1.0, ALU.add)
        gn = mk.sb("hgn", [64, G * 4, 64])
        for h in range(G * 4):
            mk.dma("sp", gn[:, h, :], p.hgrn_norm_g[l, :].partition_broadcast(64))
        tri = [mk.sb("tri%d" % i, [64, 64]) for i in range(2)]
        mk.dma("sp", tri[0][:], p.c_trif[:, :])
        mk.dma("sp", tri[1][:], p.c_trib[:, :])
        ones1 = mk.sb("hones1", [64, 1])
        mk.memset(ones1[:], 1.0)
        S = mk.sb("hS", [64, 4, 64])
        tmpS = mk.sb("htmpS", [64, 4, 64])
        NB = 2
        mkt = lambda nm, shp: [mk.sb("h%s%d" % (nm, i), shp) for i in range(NB)]
        tin = mkt("tin", [64, G, 1280])
        f_ = mkt("f", [64, G, 256]); lf = mkt("lf", [64, G, 256]); kk = mkt("kk", [64, G, 256])
        bc = mkt("bc", [64, G, 256]); eb = mkt("eb", [64, G, 256]); qe = mkt("qe", [64, G, 256]); ke = mkt("ke", [64, G, 256])
        qeT = mkt("qeT", [64, G, 4, 64]); keT = mkt("keT", [64, G, 4, 64]); ATm = mkt("ATm", [64, G, 4, 64])
        ebl = mkt("ebl", [64, G, 4]); osb = mkt("osb", [64, G, 256]); of_ = mkt("of", [64, G, 256])
        sq = mk.sb("hsq", [64, G * 256]); ss = mk.sb("hss", [64, G * 4]); sg = mk.sb("hsg", [64, G, 256])
        oT = mkt("oT", [128, 2, G * 64])
        batches = [T_LAT] + [G * 64 * b for b in range(8)]
        it = 0
        for d in range(2):
            mk.memset(S[:], 0.0)
            blist = batches if d == 0 else [T_LAT] + [G * 64 * b for b in reversed(range(8))]
            for tok0 in blist:
                i = it % NB
                it += 1
                t = tin[i]
                ncol = 1024 if d == 0 else 1280
                mk.dma("sp", t[:, :, 0:ncol], p.ptok[tok0:tok0 + G * 64, 0:ncol].rearrange("(g q) n -> q g n", q=64))
                z = t[:, :, 256 + 256 * d:512 + 256 * d]
                lbd = lb[:, d * 256:(d + 1) * 256].unsqueeze(1).to_broadcast([64, G, 256])
                omd = oml[:, d * 256:(d + 1) * 256].unsqueeze(1).to_broadcast([64, G, 256])
                mk.act(f_[i][:], z, AF.Sigmoid)
                mk.tt(f_[i][:], f_[i][:], omd, ALU.mult)
                mk.tt(f_[i][:], f_[i][:], lbd, ALU.add)
                mk.act(lf[i][:], f_[i][:], AF.Ln)
                mk.tsc(kk[i][:], f_[i][:], -1.0, ALU.mult, 1.0, ALU.add)
                for g2 in range(G // 2):
                    pb = p.psum[g2]
                    mk.mm(pb[0:64, 0:512], lhsT=tri[d][:], rhs=lf[i][:, 2 * g2:2 * g2 + 2, :].rearrange("q g k -> q (g k)"))
                    mk.tsc(bc[i][:, 2 * g2:2 * g2 + 2, :].rearrange("q g k -> q (g k)"), pb[0:64, 0:512], -80.0, ALU.max)
                pl = p.psum[2]
                for g in range(G):
                    for h in range(4):
                        mk.mm(pl[0:64, g * 4 + h:g * 4 + h + 1], lhsT=lf[i][:, g, h * 64:(h + 1) * 64], rhs=ones1[:])
                mk.tsc(ebl[i][:].rearrange("q g h -> q (g h)"), pl[0:64, 0:G * 4], -80.0, ALU.max)
                mk.act(ebl[i][:], ebl[i][:], AF.Exp)
                mk.act(eb[i][:], bc[i][:], AF.Exp)
                mk.tt(qe[i][:], t[:, :, 0:256], eb[i][:], ALU.mult)
                mk.act(eb[i][:], bc[i][:], AF.Exp, scale=-1.0)
                mk.tt(ke[i][:], kk[i][:], eb[i][:], ALU.mult)
                for g in range(G):
                    pq, pk = p.psum[3], p.psum[4]
                    for h in range(4):
                        mk.tr(pq[0:64, h * 64:(h + 1) * 64], qe[i][:, g, h * 64:(h + 1) * 64], p.identS[0:64, 0:64])
                        mk.tr(pk[0:64, h * 64:(h + 1) * 64], ke[i][:, g, h * 64:(h + 1) * 64], p.identS[0:64, 0:64])
                    mk.copy(qeT[i][:, g, :, :].rearrange("q h t -> q (h t)"), pq[0:64, 0:256])
                    mk.copy(keT[i][:, g, :, :].rearrange("q h t -> q (h t)"), pk[0:64, 0:256], en="act")
                    pa = p.psum[5]
                    for h in range(4):
                        mk.mm(pa[0:64, h * 64:(h + 1) * 64], lhsT=keT[i][:, g, h, :], rhs=qeT[i][:, g, h, :])
                    mk.tt(ATm[i][:, g, :, :], pa[0:64, 0:256].rearrange("q (h t) -> q h t", h=4),
                          tri[d][:].unsqueeze(1).to_broadcast([64, 4, 64]), ALU.mult)
                gorder = range(G) if d == 0 else reversed(range(G))
                for g in gorder:
                    v = t[:, g, 768:1024]
                    po = p.psum[6]
                    for h in range(4):
                        mk.mm(po[0:64, h * 64:(h + 1) * 64], lhsT=ATm[i][:, g, h, :], rhs=v[:, h * 64:(h + 1) * 64], start=True, stop=False)
                        mk.mm(po[0:64, h * 64:(h + 1) * 64], lhsT=qeT[i][:, g, h, :], rhs=S[:, h, :], start=False, stop=True)
                    pd = p.psum[7]
                    for h in range(4):
                        mk.mm(pd[0:64, h * 64:(h + 1) * 64], lhsT=ke[i][:, g, h * 64:(h + 1) * 64], rhs=v[:, h * 64:(h + 1) * 64])
                    mk.tt(tmpS[:], S[:], pd[0:64, 0:256].rearrange("q (h t) -> q h t", h=4), ALU.add)
                    mk.tt(S[:], tmpS[:], ebl[i][:, g, :].unsqueeze(2).to_broadcast([64, 4, 64]), ALU.mult)
                    mk.copy(osb[i][:, g, :], po[0:64, 0:256], en="act")
                hv = p.hof[tok0:tok0 + G * 64, :].rearrange("(g q) n -> q g n", q=64)
                if d == 0:
                    mk.dma("sp", hv, osb[i][:])
                else:
                    mk.dma("sp", of_[i][:], hv)
                    o2 = osb[i][:].rearrange("q g n -> q (g n)")
                    mk.tt(o2, o2, of_[i][:].rearrange("q g n -> q (g n)"), ALU.add)
                    p.headnorm(o2, o2, G * 4, 64, gn[:].rearrange("q h d -> q (h d)"), sq[:], ss[:], np_=64)
                    mk.act(sg[:], t[:, :, 1024:1280], AF.Silu)
                    mk.tt(osb[i][:], osb[i][:], sg[:], ALU.mult)
                    for c in range(2):
                        pt = p.psum[c]
                        for g in range(G):
                            mk.tr(pt[:, g * 64:(g + 1) * 64], osb[i][:, g, c * 128:(c + 1) * 128], p.identS[0:64, 0:64])
                        mk.copy(oT[i][:, c, :], pt[:, 0:G * 64], en="dve" if c == 0 else "act")
                    mk.dma("sp", p.catT[0:256, tok0:tok0 + G * 64].rearrange("(c q) t -> q c t", q=128), oT[i][:])


Prog.hgrn_gen = _hgrn_gen
Prog.hgrn = _hgrn


def _hy_sin(self, out, ps, b_col, f_col, w, tmp):
    mk = self.mk
    arg, s4, s2 = tmp
    mk.tsc(arg[:, 0:w], ps, b_col, ALU.add, f_col, ALU.mult)
    mk.act(s4[:, 0:w], arg[:, 0:w], AF.Sin, scale=0.25)
    mk.act(s2[:, 0:w], arg[:, 0:w], AF.Sin, scale=0.5)
    mk.tt(s4[:, 0:w], s4[:, 0:w], s4[:, 0:w], ALU.mult)
    mk.tsc(s4[:, 0:w], s4[:, 0:w], -2.0, ALU.mult, 1.0, ALU.add)
    mk.stt(out, s2[:, 0:w], 2.0, s4[:, 0:w], ALU.mult, ALU.mult)


def _hy_filters(self, l):
    p, mk = self, self.mk
    with mk.scope():
        w1 = mk.sb("hyw1", [33, 64])
        w2 = mk.sb("hyw2", [64, 64])
        w3 = mk.sb("hyw3", [64, 1024])
        mk.dma("sp", w1[:], p.hy_w1[l, :, :])
        mk.dma("sp", w2[:], p.hy_w2[l, :, :])
        mk.dma("sp", w3[:], p.hy_w3[l, :, :])
        cols = mk.sb("hycols", [64, 4])
        mk.dma("sp", cols[:, 0:1], p.hy_b1[l, :].rearrange("(q o) -> q o", o=1))
        mk.dma("sp", cols[:, 1:2], p.hy_freq[l, :].rearrange("(q o) -> q o", o=1))
        mk.dma("sp", cols[:, 2:3], p.hy_b2[l, :].rearrange("(q o) -> q o", o=1))
        b3 = mk.sb("hyb3", [128, 8])
        ndec = mk.sb("hyndec", [128, 8])
        mk.dma("sp", b3[:], p.hy_b3T[l, :, :])
        mk.dma("sp", ndec[:], p.hy_decT[l, :, :])
        mk.tsc(ndec[:], ndec[:], -1.0, ALU.mult)
        feats = mk.sb("hyfeats", [33, 512])
        tun = mk.sb("hytun", [128, 512])
        tmp = [mk.sb("hytmp%d" % i, [64, 512]) for i in range(3)]
        h1 = mk.sb("hyh1", [64, 512])
        h2 = mk.sb("hyh2", [64, 512])
        E = mk.sb("hyE", [128, 512])
        ssq = mk.sb("hyssq", [128, 8, 4])
        junk = mk.sb("hyjunk", [128, 2048])
        inv = mk.sb("hyinv", [128, 4])
        for (L, featsD, tunD, Kd, KW) in ((2048, p.c_feats, p.c_tun, p.Kd, 4096), (256, p.c_featsc, p.c_tunc, p.Kdc, 512)):
            with mk.scope():
                FT = mk.sb("hyFT", [128, 8, L])
                nblk = (L + 511) // 512
                for tdir in range(2):
                    for bi in range(nblk):
                        w = min(512, L)
                        t0 = bi * 512
                        mk.dma("sp", feats[:, 0:w], featsD[tdir, :, t0:t0 + w])
                        mk.dma("sp", tun[:, 0:w], tunD[tdir, t0:t0 + w].partition_broadcast(128))
                        ps = p.psum[0]
                        mk.mm(ps[0:64, 0:w], lhsT=w1[:], rhs=feats[:, 0:w])
                        p.hy_sin(h1[:, 0:w], ps[0:64, 0:w], cols[:, 0:1], cols[:, 1:2], w, tmp)
                        ps = p.psum[1]
                        mk.mm(ps[0:64, 0:w], lhsT=w2[:], rhs=h1[:, 0:w])
                        p.hy_sin(h2[:, 0:w], ps[0:64, 0:w], cols[:, 2:3], cols[:, 1:2], w, tmp)
                        for j in (0, 1, 4, 5):
                            jj = j + 2 * tdir
                            ps = p.psum[2 + jj % 4]
                            mk.mm(ps[:, 0:w], lhsT=w3[:, jj * 128:(jj + 1) * 128], rhs=h2[:, 0:w])
                            mk.act(E[:, 0:w], tun[:, 0:w], AF.Exp, scale=ndec[:, jj:jj + 1])
                            mk.stt(FT[:, jj, t0:t0 + w], ps[:, 0:w], b3[:, jj:jj + 1], E[:, 0:w], ALU.add, ALU.mult)
                for jj in range(8):
                    tdir = (jj // 2) % 2
                    n = L if tdir == 0 else L - 1
                    mk.act(junk[:, 0:n], FT[:, jj, 0:n], AF.Square, accum_out=ssq[:, jj, 0:1])
                for j in (0, 1, 4, 5):
                    col = inv[:, 0:1]
                    mk.tt(col, ssq[:, j, 0:1], ssq[:, j + 2, 0:1], ALU.add)
                    mk.act(col, col, AF.Sqrt, bias=EPS, scale=1.0)
                    mk.recip(col, col)
                    o, ch = j // 4, j % 2
                    r0 = o * 256 + ch * 128
                    mk.tsc(FT[:, j, :], FT[:, j, :], col, ALU.mult)
                    mk.tsc(FT[:, j + 2, :], FT[:, j + 2, :], col, ALU.mult)
                    mk.dma("sp", Kd[r0:r0 + 128, L - 1:2 * L - 1], FT[:, j, :])
                    mk.dma("sp", Kd[r0:r0 + 128, 0:L - 1], FT[:, j + 2, 0:L - 1])


def _hyena(self, l):
    p, mk = self, self.mk
    p.hy_filters(l)
    with mk.scope():
        sw = mk.sb("hysw", [128, 6, 3])
        sbias = mk.sb("hysb", [128, 6])
        mk.dma("sp", sw[:], p.hy_swT[l, :, :, :])
        mk.dma("sp", sbias[:], p.hy_sbT[l, :, :])
        db = mk.sb("hydb", [128, 2, 256])
        mk.dma("sp", db[:], p.hy_bias[l, :, :].partition_broadcast(128))
        Ex = mk.sb("hyEx", [128, 128])
        mk.dma("sp", Ex[:], p.c_exch[:, :])
        uf = [mk.sb("hyuf%d" % i, [128, T_ALL]) for i in range(2)]
        acc = mk.sb("hyacc", [128, T_ALL])
        tk = [mk.sb("hytk%d" % i, [128, NT, 128]) for i in range(3)]
        z1 = mk.sb("hyz1", [128, NT, 128])
        zr = mk.sb("hyzr", [128, NT, 128])
        R = [mk.sb("hyR%d" % i, [128, 3968]) for i in range(3)]
        Rc = [mk.sb("hyRc%d" % i, [128, 384]) for i in range(3)]
        tmpg = [mk.sb("hytg%d" % i, [128, NT]) for i in range(2)]
        ob = [mk.sb("hyob%d" % i, [128, 512]) for i in range(2)]
        ci = 0
        for ch in range(2):
            for part in range(3):
                jt = part * 2 + ch
                u = uf[part % 2]
                mk.dma("sp", u[:], p.uT[jt * 128:(jt + 1) * 128, :])
                mk.tsc(acc[:], u[:], sw[:, jt, 1:2], ALU.mult, sbias[:, jt:jt + 1], ALU.add)
                for (a, b) in ((0, T_LAT), (T_LAT, T_ALL)):
                    mk.stt(acc[:, a + 1:b], u[:, a:b - 1], sw[:, jt, 0:1], acc[:, a + 1:b], ALU.mult, ALU.add)
                    mk.stt(acc[:, a:b - 1], u[:, a + 1:b], sw[:, jt, 2:3], acc[:, a:b - 1], ALU.mult, ALU.add)
                for n in range(NT):
                    pt = p.psum[n % 4]
                    mk.tr(pt[:, 0:128], acc[:, n * 128:(n + 1) * 128], p.identS[:])
                    p.evac(tk[part][:, n, :], pt[:, 0:128])
            for o in range(2):
                z = tk[0] if o == 0 else z1
                gate = tk[1] if o == 0 else tk[2]
                zo = z1 if o == 0 else tk[0]
                for n in range(NT):
                    pt = p.psum[n % 4]
                    mk.mm(pt[:, 0:128], lhsT=Ex[:], rhs=z[:, n, :])
                    p.evac(zr[:, n, :], pt[:, 0:128])
                for c in range(128):
                    row = o * 256 + ch * 128 + c
                    r, rc = R[ci % 3], Rc[ci % 3]
                    mk.dma("sp" if ci % 2 == 0 else "act", r[:],
                           bass.AP(tensor=p.Kd.ap().tensor, offset=row * 4096, ap=[[1, 128], [1, 3968]]))
                    mk.dma("pool", rc[:], bass.AP(tensor=p.Kdc.ap().tensor, offset=row * 512, ap=[[1, 128], [1, 384]]))
                    ps = p.psum[4 + ci % 4]
                    ci += 1
                    Ds = [0] + [d for k in range(1, 16) for d in (k, -k)]
                    for di, Dd in enumerate(Ds):
                        i0, i1 = max(0, Dd), min(16, 16 + Dd)
                        mk.mm(ps[:, i0:i1], lhsT=r[:, 128 * (Dd + 15):128 * (Dd + 16)], rhs=zr[:, i0 - Dd:i1 - Dd, c],
                              start=(di == 0), stop=(di == len(Ds) - 1))
                    for di, Dd in enumerate((0, 1, -1)):
                        i0, i1 = max(0, Dd), min(2, 2 + Dd)
                        mk.mm(ps[:, 16 + i0:16 + i1], lhsT=rc[:, 128 * (Dd + 1):128 * (Dd + 2)], rhs=zr[:, 16 + i0 - Dd:16 + i1 - Dd, c],
                              start=(di == 0), stop=(di == 2))
                    tg = tmpg[c % 2]
                    cc = ch * 128 + c
                    mk.stt(tg[:], z[:, :, c], db[:, o, cc:cc + 1], ps[:, 0:NT], ALU.mult, ALU.add)
                    mk.tt(zo[:, :, c], tg[:], gate[:, :, c], ALU.mult)
            oi = 0
            for bi, (t0, w) in enumerate(TB):
                o_ = ob[oi % 2]
                oi += 1
                for j in range(w // 128):
                    n = t0 // 128 + j
                    pt = p.psum[n % 4]
                    mk.tr(pt[:, 0:128], tk[0][:, n, :], p.identS[:])
                    p.evac(o_[:, j * 128:(j + 1) * 128], pt[:, 0:128])
                mk.dma("sp", p.catT[256 + ch * 128:256 + (ch + 1) * 128, t0:t0 + w], o_[:, 0:w])


Prog.hy_sin = _hy_sin
Prog.hy_filters = _hy_filters
Prog.hyena = _hyena


def _hy_spectrum(self, l):
    p, mk = self, self.mk
    with mk.scope():
        w1 = mk.sb("hyw1", [33, 64])
        w2 = mk.sb("hyw2", [64, 64])
        w3 = mk.sb("hyw3", [64, 1024])
        mk.dma("sp", w1[:], p.hy_w1[l, :, :])
        mk.dma("sp", w2[:], p.hy_w2[l, :, :])
        mk.dma("sp", w3[:], p.hy_w3[l, :, :])
        cols = mk.sb("hycols", [64, 4])
        mk.dma("sp", cols[:, 0:1], p.hy_b1[l, :].rearrange("(q o) -> q o", o=1))
        mk.dma("sp", cols[:, 1:2], p.hy_freq[l, :].rearrange("(q o) -> q o", o=1))
        mk.dma("sp", cols[:, 2:3], p.hy_b2[l, :].rearrange("(q o) -> q o", o=1))
        b3 = mk.sb("hyb3", [128, 1024])
        dec = mk.sb("hydec", [128, 1024])
        mk.dma("sp", b3[:], p.hy_b3[l, :].partition_broadcast(128))
        mk.dma("sp", dec[:], p.hy_decay[l, :, :, :].rearrange("a b c -> (a b c)").partition_broadcast(128))
        feats = mk.sb("hyfeats", [33, 512])
        tmp = [mk.sb("hytmp%d" % i, [64, 512]) for i in range(3)]
        h1 = mk.sb("hyh1", [64, 512])
        h2 = mk.sb("hyh2", [64, 512])
        E = mk.sb("hyE", [128, 1024])
        tot = mk.sb("hytot", [128, 512])
        for (L, featsD, ntunD, dftB, Ksp, nrm) in ((2048, p.c_feats, p.c_ntun, p.c_dftB, p.Ksp, 2.0 / 4096),
                                                 (256, p.c_featsc, p.c_ntunc, p.c_dftBc, p.Kspc, 2.0 / 512)):
            nt = L // 128
            with mk.scope():
                filtR = mk.sb("hyfilt", [128, nt, 1024], F32R)
                filt = filtR[:].bitcast(F32)
                ntun = mk.sb("hyntun", [128, nt])
                mk.dma("sp", ntun[:], ntunD[:, :])
                sq = mk.sb("hysq", [128, 1024], F32R)
                for bi in range((L + 511) // 512):
                    w = min(512, L)
                    t0 = bi * 512
                    mk.dma("sp", feats[:, 0:w], featsD[0, :, t0:t0 + w])
                    ps = p.psum[0]
                    mk.mm(ps[0:64, 0:w], lhsT=w1[:], rhs=feats[:, 0:w])
                    p.hy_sin(h1[:, 0:w], ps[0:64, 0:w], cols[:, 0:1], cols[:, 1:2], w, tmp)
                    ps = p.psum[1]
                    mk.mm(ps[0:64, 0:w], lhsT=w2[:], rhs=h1[:, 0:w])
                    p.hy_sin(h2[:, 0:w], ps[0:64, 0:w], cols[:, 2:3], cols[:, 1:2], w, tmp)
                    for j in range(w // 128):
                        n = t0 // 128 + j
                        p.tick()
                        mk.act(E[:], dec[:], AF.Exp, scale=ntun[:, n:n + 1])
                        if n == 0:
                            mk.memset(E[:].rearrange("q (o d c) -> q o d c", o=2, d=2)[0:1, :, 1, :], 0.0)
                        for hf in range(2):
                            ps = p.psum[2 + hf]
                            mk.mm(ps[:, 0:512], lhsT=h2[:, j * 128:(j + 1) * 128], rhs=w3[:, hf * 512:(hf + 1) * 512])
                            mk.tt(filtR[:, n, hf * 512:(hf + 1) * 512], ps[:, 0:512], b3[:, hf * 512:(hf + 1) * 512], ALU.add)
                        mk.tt(filtR[:, n, :], filt[:, n, :], E[:], ALU.mult, en="pool")
                pss = [p.psum[4], p.psum[5]]
                for n in range(nt):
                    mk.act(sq[:], filt[:, n, :], AF.Square)
                    for hf in range(2):
                        mk.mm(pss[hf][:, 0:512], lhsT=p.onesR[:], rhs=sq[:, hf * 512:(hf + 1) * 512], start=(n == 0), stop=(n == nt - 1))
                for o in range(2):
                    mk.copy(tot[:, o * 256:(o + 1) * 256], pss[o][:, 256:512], en="act")
                    mk.tt(tot[:, o * 256:(o + 1) * 256], tot[:, o * 256:(o + 1) * 256], pss[o][:, 0:256], ALU.add)
                mk.act(tot[:], tot[:], AF.Sqrt, bias=EPS, scale=1.0)
                mk.recip(tot[:], tot[:])
                mk.tsc(tot[:], tot[:], nrm, ALU.mult)
                totv = tot[:].rearrange("q (o c) -> q o c", o=2).unsqueeze(2).to_broadcast([128, 2, 2, 256])
                for n in range(nt):
                    mk.tt(filtR[:, n, :].rearrange("q (o d c) -> q o d c", o=2, d=2),
                          filt[:, n, :].rearrange("q (o d c) -> q o d c", o=2, d=2), totv, ALU.mult,
                          en="dve" if n % 2 == 0 else "pool")
                for n in range(nt):
                    fv4 = filt[:, n, :].rearrange("q (o d c) -> q o d c", o=2, d=2)
                    fr4 = filtR[:, n, :].rearrange("q (o d c) -> q o d c", o=2, d=2)
                    eng = "dve" if n % 2 == 0 else "pool"
                    mk.tt(fr4[:, :, 1, :], fv4[:, :, 0, :], fv4[:, :, 1, :], ALU.subtract, en=eng)
                    if eng == "dve":
                        mk.stt(fr4[:, :, 0, :], fv4[:, :, 0, :], 2.0, fv4[:, :, 1, :], ALU.mult, ALU.subtract)
                    else:
                        mk.tsc(fr4[:, :, 0, :], fv4[:, :, 0, :], 2.0, ALU.mult, en="pool")
                        mk.tt(fr4[:, :, 0, :], fv4[:, :, 0, :], fv4[:, :, 1, :], ALU.subtract, en="pool")
                blk = [mk.sb("hyblk%d" % i, [128, 2, nt, 128], F32R) for i in range(2)]
                ko = [mk.sb("hyko%d" % i, [128, 2, 2, 256]) for i in range(2)]
                for k in range(nt):
                    p.tick()
                    bk = blk[k % 2]
                    mk.dma("pool", bk[:], dftB[k, :, 0:2, :, :], max_dma_last_dim=8192)
                    kk_ = ko[k % 2]
                    for cs in range(2):
                        ps = p.psum[cs]
                        for n in range(nt):
                            rhs = filtR[:, n, :].rearrange("q (o d c) -> q o d c", o=2, d=2)[:, :, cs, :]
                            mk.mm(ps[:, 0:512].rearrange("q (o c) -> q o c", o=2), lhsT=bk[:, 1 - cs, n, :], rhs=rhs,
                                  start=(n == 0), stop=(n == nt - 1))
                        mk.copy(kk_[:, :, cs, :], ps[:, 0:512].rearrange("q (o c) -> q o c", o=2), en="dve" if cs == 0 else "act")
                    if k == 0:
                        ps = p.psum[2]
                        for n in range(nt):
                            rhs = filtR[:, n, :].rearrange("q (o d c) -> q o d c", o=2, d=2)[:, :, 0, :]
                            mk.mm(ps[:, 0:512].rearrange("q (o c) -> q o c", o=2), lhsT=bk[:, 0, n, :], rhs=rhs,
                                  start=(n == 0), stop=(n == nt - 1))
                        mk.copy(kk_[0:1, :, 1, :], ps[0:1, 0:512].rearrange("q (o c) -> q o c", o=2))
                        mk.tsc(kk_[0:1, :, :, :], kk_[0:1, :, :, :], 0.5, ALU.mult)
                    mk.dma("sp", Ksp[:, :, k * 128:(k + 1) * 128, :].rearrange("o s q c -> q o s c"), kk_[:])


def _hy_conv(self, ztok, zch, o, nt, tile0, dftB, dftN, Ksp, gch_o, dbc, zout_ch, blkname):
    p, mk = self, self.mk
    W = nt * 128
    wb = min(512, W)
    ntb = W // wb
    with mk.scope():
        blk = [mk.sb(blkname + "%d" % i, [128, 2, nt, 128], F32R) for i in range(2)]
        rowt = [mk.sb(blkname + "r%d" % i, [128, W], F32R) for i in range(2)]
        kc = [mk.sb("hykc%d" % i, [128, 2, 256]) for i in range(2)]
        YR = mk.sb("hyYR", [128, 2, nt, 256], F32R)
        t1 = [mk.sb("hyt1%d" % i, [128, 256]) for i in range(2)]
        t2 = [mk.sb("hyt2%d" % i, [128, 256]) for i in range(2)]
        t3 = [mk.sb("hyt3%d" % i, [128, 256]) for i in range(2)]
        t4 = [mk.sb("hyt4%d" % i, [128, 256]) for i in range(2)]
        gt = [mk.sb("hygt%d" % i, [128, wb]) for i in range(2)]
        dz = [mk.sb("hydz%d" % i, [128, wb]) for i in range(2)]
        for k in range(nt):
            p.tick()
            bk = blk[k % 2]
            mk.dma("pool", bk[:], dftB[k, :, 0:2, :, :], max_dma_last_dim=8192)
            kk_ = kc[k % 2]
            mk.dma("sp", kk_[:], Ksp[o, :, k * 128:(k + 1) * 128, :].rearrange("s q c -> q s c"))
            psA, psB = p.psum[(k % 2) * 2], p.psum[(k % 2) * 2 + 1]
            for cs, ps in ((0, psA), (1, psB)):
                for n in range(nt):
                    mk.mm(ps[:, 0:256], lhsT=bk[:, 1 - cs, n, :], rhs=ztok[:, tile0 + n, :], start=(n == 0), stop=(n == nt - 1))
            a1, a2, b1_, b2_ = t1[k % 2], t2[k % 2], t3[k % 2], t4[k % 2]
            mk.tt(a1[:], psA[:, 0:256], kk_[:, 0, :], ALU.mult)
            mk.tt(a2[:], psB[:, 0:256], kk_[:, 1, :], ALU.mult)
            mk.tt(YR[:, 0, k, :], a1[:], a2[:], ALU.subtract, en="pool")
            mk.tt(b1_[:], psA[:, 0:256], kk_[:, 1, :], ALU.mult)
            mk.tt(b2_[:], psB[:, 0:256], kk_[:, 0, :], ALU.mult)
            mk.tt(YR[:, 1, k, :], b1_[:], b2_[:], ALU.add, en="pool")
            if k == 0:
                mk.copy(YR[0:1, 0, 0, :], a1[0:1, :])
                mk.copy(YR[0:1, 1, 0, :], a2[0:1, :])
        ri = 0
        for cs in range(2):
            for b in range(nt):
                p.tick()
                rt_ = rowt[ri % 2]
                ri += 1
                mk.dma("pool", rt_[:], dftN[cs, b * 128:(b + 1) * 128, 0:W], max_dma_last_dim=8192)
                for ct in range(2):
                    for tb in range(ntb):
                        ps = p.psum[ct * ntb + tb]
                        mk.mm(ps[:, 0:wb], lhsT=YR[:, cs, b, ct * 128:(ct + 1) * 128], rhs=rt_[:, tb * wb:(tb + 1) * wb],
                              start=(cs == 0 and b == 0), stop=(cs == 1 and b == nt - 1))
        gi = 0
        for ct in range(2):
            for tb in range(ntb):
                ps = p.psum[ct * ntb + tb]
                c0 = tile0 * 128 + tb * wb
                g, d_ = gt[gi % 2], dz[gi % 2]
                gi += 1
                mk.dma("sp", g[:], gch_o[ct * 128:(ct + 1) * 128, c0:c0 + wb])
                mk.stt(d_[:], zch[:, ct, c0:c0 + wb], dbc[:, o, ct:ct + 1], ps[:, 0:wb], ALU.mult, ALU.add)
                mk.tt(zout_ch[:, ct, c0:c0 + wb], d_[:], g[:], ALU.mult, en="pool")


def _hyena2(self, l):
    p, mk = self, self.mk
    with mk.scope():
        if p.bg_hgrn:
            p._bg = p.hgrn_gen(l)
            p.tick()
        p.hyena_body(l)
        while getattr(p, "_bg", None) is not None:
            p.tick()


def _hyena_body(self, l):
    p, mk = self, self.mk
    p.hy_spectrum(l)
    with mk.scope():
        ztok = mk.sb("hyztok", [128, NT, 256], F32R)
        zcA = mk.sb("hyzcA", [128, 2, T_ALL])
        zcB = mk.sb("hyzcB", [128, 2, T_ALL])
        dbc = mk.sb("hydbc", [128, 2, 2])
        mk.dma("sp", dbc[:], p.hy_biasT[l, :, :, :])
        with mk.scope():
            sw = mk.sb("hysw", [128, 6, 3])
            sbias = mk.sb("hysb", [128, 6])
            mk.dma("sp", sw[:], p.hy_swT[l, :, :, :])
            mk.dma("sp", sbias[:], p.hy_sbT[l, :, :])
            uf = [mk.sb("hyuf%d" % i, [128, T_ALL]) for i in range(2)]
            acc = [mk.sb("hyacc%d" % i, [128, T_ALL]) for i in range(2)]
            for jt in range(6):
                part, ch = jt // 2, jt % 2
                u = uf[jt % 2]
                ac = zcA[:, ch, :] if part == 0 else acc[jt % 2][:]
                mk.dma("sp", u[:], p.uT[jt * 128:(jt + 1) * 128, :])
                mk.tsc(ac, u[:], sw[:, jt, 1:2], ALU.mult, sbias[:, jt:jt + 1], ALU.add)
                for (a, b) in ((0, T_LAT), (T_LAT, T_ALL)):
                    mk.stt(ac[:, a + 1:b], u[:, a:b - 1], sw[:, jt, 0:1], ac[:, a + 1:b], ALU.mult, ALU.add)
                    mk.stt(ac[:, a:b - 1], u[:, a + 1:b], sw[:, jt, 2:3], ac[:, a:b - 1], ALU.mult, ALU.add)
                if part == 0:
                    for n in range(NT):
                        pt = p.psum[n % 4]
                        mk.tr(pt[:, 0:128], ac[:, n * 128:(n + 1) * 128], p.identS[:])
                        p.evac(ztok[:, n, ch * 128:(ch + 1) * 128], pt[:, 0:128])
                else:
                    mk.dma("act", p.gch[part - 1, ch * 128:(ch + 1) * 128, :], ac)
        for o in range(2):
            zin_ch = zcA if o == 0 else zcB
            zo_ch = zcB if o == 0 else zcA
            p.hy_conv(ztok, zin_ch, o, 16, 0, p.c_dftB, p.c_dftN, p.Ksp, p.gch[o, :, :], dbc, zo_ch, "hyblkL")
            p.hy_conv(ztok, zin_ch, o, 2, 16, p.c_dftBc, p.c_dftNc, p.Kspc, p.gch[o, :, :], dbc, zo_ch, "hyblkC")
            if o == 0:
                for ct in range(2):
                    for n in range(NT):
                        pt = p.psum[4 + n % 4]
                        mk.tr(pt[:, 0:128], zcB[:, ct, n * 128:(n + 1) * 128], p.identS[:])
                        p.evac(ztok[:, n, ct * 128:(ct + 1) * 128], pt[:, 0:128])
        for ct in range(2):
            mk.dma("sp", p.catT[256 + ct * 128:256 + (ct + 1) * 128, :], zcA[:, ct, :])


Prog.hyena_body = _hyena_body


def _tick(self, n=1):
    g = getattr(self, "_bg", None)
    if g is None:
        return
    for _ in range(n):
        try:
            next(g)
        except StopIteration:
            self._bg = None
            return


Prog.tick = _tick
Prog.hy_spectrum = _hy_spectrum
Prog.hy_conv = _hy_conv
Prog.hyena = _hyena2


def _consts():
    f = np.float32
    c = {}
    s = np.arange(128)
    c["c_lstrict"] = (s[:, None] < s[None, :]).astype(f)
    c["c_iotaE"] = np.tile((np.arange(32) * CAP).astype(f)[None, :], (128, 1))
    s = np.arange(64)
    c["c_trif"] = (s[:, None] <= s[None, :]).astype(f)
    c["c_trib"] = (s[:, None] >= s[None, :]).astype(f)
    c["c_exch"] = np.eye(128, dtype=f)[::-1].copy()
    c["ident"] = np.eye(128, dtype=f)
    t = np.arange(T_LAT)
    row = (t // 64).astype(f)
    col = (t % 64).astype(f)
    inv = (f(10000.0) ** (-np.arange(0, 16, 2, dtype=f) / f(16))).astype(f)
    ar = row[:, None] * inv[None, :]
    ac = col[:, None] * inv[None, :]
    cos = np.concatenate([np.cos(ar), np.cos(ar), np.cos(ac), np.cos(ac)], axis=1).astype(f)
    sin = np.concatenate([-np.sin(ar), np.sin(ar), -np.sin(ac), np.sin(ac)], axis=1).astype(f)
    c["c_ropecos"] = np.ascontiguousarray(cos.reshape(16, 128, 32).transpose(1, 0, 2))
    c["c_ropesin"] = np.ascontiguousarray(sin.reshape(16, 128, 32).transpose(1, 0, 2))
    for nm, tn, L in (("c_feats", "c_ntun", 2048), ("c_featsc", "c_ntunc", 256)):
        tt = np.arange(L, dtype=f)
        tu = np.linspace(0.0, 1.0, L, dtype=f)
        bands = np.linspace(1e-4, 15, 16, dtype=f)
        ang = (f(2.0 * np.pi / L) * tt[:, None] * bands[None, :]).astype(f)
        feats = np.concatenate([tu[:, None], np.cos(ang), -np.sin(ang)], axis=-1).astype(f)
        fT = feats.T
        c[nm] = np.ascontiguousarray(np.stack([fT, fT[:, ::-1]], axis=0))
        c[tn] = np.ascontiguousarray(-tu.reshape(L // 128, 128).T)
        N = 2 * L
        idx = np.arange(L, dtype=np.int64)
        prod = (idx[:, None] * idx[None, :]) % N
        ang64 = prod.astype(np.float64) * (2.0 * np.pi / N)
        Cm = np.cos(ang64)
        Sm = np.sin(ang64)
        sgn = np.where(idx % 2 == 0, 1.0, -1.0)
        Sm[:, 0] = sgn
        M = np.stack([Sm, Cm, Sm.T], axis=0).astype(f)
        nt = L // 128
        Mb = M.reshape(3, nt, 128, nt, 128).transpose(3, 2, 0, 1, 4)
        c["c_dftB" if L == 2048 else "c_dftBc"] = np.ascontiguousarray(Mb)
        c["c_dftN" if L == 2048 else "c_dftNc"] = np.ascontiguousarray(M[1:3])
    return c


def _na_bias_table(rpb):
    Ld, H = rpb.shape[0], rpb.shape[1]
    out = np.full((Ld, H, 5, 5, 128, 128), -30000.0, np.float32)
    pat_pr = {0: 0, 1: 1, 2: 2, 3: 14, 4: 15}
    kp = np.arange(128)
    q = np.arange(128)
    for pat, pr in pat_pr.items():
        rs0 = min(max(2 * pr - 4, 0), 24)
        ws = min((rs0 // 2) * 2, 22)
        for kt in range(5):
            krow = ws + (kt * 128 + kp) // 64
            kcol = (kt * 128 + kp) % 64
            qrow = 2 * pr + q // 64
            qcol = q % 64
            rs = np.clip(qrow - 4, 0, 24)
            cs = np.clip(qcol - 8, 0, 48)
            ok = (krow[:, None] >= rs[None, :]) & (krow[:, None] < rs[None, :] + 8) & \
                 (kcol[:, None] >= cs[None, :]) & (kcol[:, None] < cs[None, :] + 16)
            dr = np.clip(krow[:, None] - qrow[None, :] + 7, 0, 14)
            dc = np.clip(kcol[:, None] - qcol[None, :] + 15, 0, 30)
            g = rpb[:, :, dr, dc]
            out[:, :, pat, kt] = np.where(ok[None, None], g, np.float32(-30000.0))
    return np.ascontiguousarray(out.transpose(0, 1, 4, 2, 3, 5))


def host_shared(inp):
    f = np.float32
    m = dict(_consts())
    Ld = DEPTH
    m["w_ada"] = inp["w_ada"]
    m["b_adaT"] = np.ascontiguousarray(inp["b_ada"].reshape(Ld, 48, 128).transpose(0, 2, 1))
    m["n1g"] = np.ascontiguousarray(inp["norm1_g"].reshape(Ld, 8, 128).transpose(0, 2, 1))
    m["n2g"] = np.ascontiguousarray(inp["norm2_g"].reshape(Ld, 8, 128).transpose(0, 2, 1))
    m["w_in"] = inp["w_in"]
    m["w_out"] = inp["w_out"]
    m["hgrn_lb"] = inp["hgrn_lb_logits"]
    m["hgrn_norm_g"] = inp["hgrn_norm_g"]
    m["hy_swT"] = np.ascontiguousarray(inp["hy_short_w"].reshape(Ld, 3, 6, 128).transpose(0, 3, 2, 1))
    m["hy_sbT"] = np.ascontiguousarray(inp["hy_short_b"].reshape(Ld, 6, 128).transpose(0, 2, 1))
    for k in ("hy_w1", "hy_b1", "hy_freq", "hy_w2", "hy_b2", "hy_w3", "hy_bias", "hy_b3", "hy_decay", "na_q_g", "na_k_g", "mla_q_a_g",
              "mla_kv_a_g", "mla_w_uq", "mla_w_ukv", "mla_q_g", "mla_k_g", "moe_wg", "moe_bg", "moe_we", "moe_be",
              "moe_w_gate", "moe_w_up", "moe_w_down"):
        m[k] = inp[k]
    m["na_biasT"] = _na_bias_table(inp["na_rpb"])
    m["hy_biasT"] = np.ascontiguousarray(inp["hy_bias"].reshape(Ld, 2, 2, 128).transpose(0, 3, 1, 2))
    return m


def host_inputs(inp, b, shared):
    m = dict(shared)
    m["xin"] = np.ascontiguousarray(np.concatenate([inp["x"][b], inp["ctx"][b]], axis=0))
    m["cvecT"] = np.ascontiguousarray(np.stack([inp["c"][b], inp["c_ctx"]], axis=1))
    return m


def run_prog(inp, cores, extra=None, **kw):
    from concourse.bass_utils import run_bass_kernel_spmd
    prog = Prog(**kw)
    prog.build()
    shared = host_shared(inp)
    in_maps = []
    for b in cores:
        m = host_inputs(inp, b, shared)
        if extra:
            m.update(extra)
        in_maps.append({k: np.ascontiguousarray(m[k], dtype=np.float32) for k in prog.inputs})
    res = run_bass_kernel_spmd(prog.nc, in_maps, core_ids=list(range(len(cores))))
    return prog, res


def kernel(**inp):
    inp = {k: np.asarray(v) for k, v in inp.items()}
    prog, res = run_prog(inp, list(range(8)))
    out = np.stack([res.results[b]["xout"] for b in range(8)], axis=0)
    return out.astype(np.float32)
```

```python
import contextlib
import numpy as np
import concourse.bass as bass
import concourse.mybir as mybir

F32 = mybir.dt.float32
F32R = mybir.dt.float32r
I32 = mybir.dt.int32
U32 = mybir.dt.uint32
ALU = mybir.AluOpType
AF = mybir.ActivationFunctionType
AX = mybir.AxisListType

SEM_ROT = 30000


class MK:
    def __init__(self, nc):
        self.nc = nc
        self.root = contextlib.ExitStack()
        self.stack = [self.root]
        self.eng = {"pe": nc.tensor, "dve": nc.vector, "act": nc.scalar,
                    "pool": nc.gpsimd, "sp": nc.sync}
        self.esem = {}
        self.ecnt = {}
        self.nsem = 0
        for k in self.eng:
            self.esem[k] = self._newsem("e_" + k)
            self.ecnt[k] = 0
        self.waited = {}
        self.ts = {}
        self.uid = 0
        self.dq = {}
        for q, n in (("sp", 10), ("pool", 6), ("act", 4)):
            self.dq[q] = {"sems": [[self._newsem("d_%s%d" % (q, i)), 0] for i in range(n)], "i": 0}
        self.ninstr = 0

    def _newsem(self, name):
        self.nsem += 1
        return self.root.enter_context(self.nc.semaphore("%s_%d" % (name, self.nsem)))

    def _nm(self, name):
        self.uid += 1
        return "%s_%d" % (name, self.uid)

    def sb(self, name, shape, dtype=F32):
        return self.stack[-1].enter_context(self.nc.sbuf_tensor(self._nm(name), list(shape), dtype))

    def ps(self, name, shape, dtype=F32):
        return self.stack[-1].enter_context(self.nc.psum_tensor(self._nm(name), list(shape), dtype))

    def dram(self, name, shape, dtype=F32, kind="Internal"):
        return self.nc.dram_tensor(name, list(shape), dtype, kind=kind)

    @contextlib.contextmanager
    def scope(self):
        es = contextlib.ExitStack()
        self.stack.append(es)
        try:
            yield
        finally:
            self.barrier()
            self.stack.pop()
            es.close()

    def _wait(self, en, ev):
        if ev is None:
            return
        sem, val = ev
        if en == "pe" and sem.name.startswith("e_pe"):
            return
        key = (en, sem.name if hasattr(sem, "name") else id(sem))
        if self.waited.get(key, 0) >= val:
            return
        self.waited[key] = val
        self.eng[en].wait_ge(sem, val)

    def _tn(self, x):
        if isinstance(x, str):
            return x
        if hasattr(x, "tensor"):
            return x.tensor.name
        return x.name

    def _region(self, x):
        if isinstance(x, str) or not hasattr(x, "tensor"):
            return (self._tn(x), None)
        t = x.tensor
        name = t.name
        try:
            apl = [(int(a[0]), int(a[1])) for a in x.ap]
            off = int(x.offset)
            if any(st < 0 for st, _ in apl):
                return (name, None)
            if type(t).__name__ == "PSumTensorHandle":
                return (name, None)
            if type(t).__name__ == "DRamTensorHandle":
                ext = sum((n - 1) * st for st, n in apl)
                return (name, (0, 0, off, off + ext))
            row = 1
            for d in list(t.shape)[1:]:
                row *= int(d)
            pst, pn = apl[0]
            if pst != row:
                return (name, None)
            p0 = off // row
            f0 = off % row
            ext = sum((n - 1) * st for st, n in apl[1:])
            return (name, (p0, p0 + pn - 1, f0, f0 + ext))
        except Exception:
            return (name, None)

    @staticmethod
    def _ovl(a, b):
        if a is None or b is None:
            return True
        return not (a[1] < b[0] or b[1] < a[0] or a[3] < b[2] or b[3] < a[2])

    @staticmethod
    def _contains(a, b):
        if a is None:
            return True
        if b is None:
            return False
        return a[0] <= b[0] and a[1] >= b[1] and a[2] <= b[2] and a[3] >= b[3]

    def _deps(self, en, reads, writes):
        for r in reads:
            name, box = self._region(r)
            st = self.ts.get(name)
            if st is not None:
                for b, ev in st["w"]:
                    if self._ovl(box, b):
                        self._wait(en, ev)
        for w in writes:
            name, box = self._region(w)
            st = self.ts.get(name)
            if st is not None:
                for b, ev in st["w"]:
                    if self._ovl(box, b):
                        self._wait(en, ev)
                for b, ev in st["r"]:
                    if self._ovl(box, b):
                        self._wait(en, ev)

    @staticmethod
    def _compress(lst):
        d = {}
        for b, (s, v) in lst:
            k = id(s)
            if k not in d or d[k][1] < v:
                d[k] = (s, v)
        return [(None, ev) for ev in d.values()]

    def _record(self, ev, reads, writes):
        for r in reads:
            name, box = self._region(r)
            st = self.ts.setdefault(name, {"w": [], "r": []})
            st["r"].append((box, ev))
            if len(st["r"]) > 40:
                st["r"] = self._compress(st["r"])
        for w in writes:
            name, box = self._region(w)
            st = self.ts.setdefault(name, {"w": [], "r": []})
            st["w"] = [(b, e) for b, e in st["w"] if not self._contains(box, b)]
            st["r"] = [(b, e) for b, e in st["r"] if not self._contains(box, b)]
            st["w"].append((box, ev))
            if len(st["w"]) > 40:
                st["w"] = self._compress(st["w"])

    def op(self, en, fn, reads=(), writes=()):
        reads = [r for r in reads if r is not None and not isinstance(r, (int, float))]
        writes = [w for w in writes if w is not None]
        self._deps(en, reads, writes)
        ins = fn(self.eng[en])
        if self.ecnt[en] >= SEM_ROT:
            self.esem[en] = self._newsem("e_" + en)
            self.ecnt[en] = 0
        self.ecnt[en] += 1
        ins.then_inc(self.esem[en], 1)
        ev = (self.esem[en], self.ecnt[en])
        self._record(ev, reads, writes)
        self.ninstr += 1
        return ev

    def dma(self, q, out, in_, fn=None, extra_reads=(), **kw):
        dq = self.dq[q]
        slot = dq["sems"][dq["i"] % len(dq["sems"])]
        dq["i"] += 1
        sem, cnt = slot
        if cnt > 0:
            self._wait(q, (sem, cnt))
        reads = [in_] + list(extra_reads)
        writes = [out]
        self._deps(q, reads, writes)
        if fn is None:
            ins = self.eng[q].dma_start(out=out, in_=in_, **kw)
        else:
            ins = fn(self.eng[q])
        slot[1] = cnt + 16
        ins.then_inc(sem, 16)
        ev = (sem, slot[1])
        self._record(ev, reads, writes)
        self.ninstr += 1
        return ev

    def barrier(self):
        evs = [(self.esem[k], self.ecnt[k]) for k in self.eng if self.ecnt[k] > 0]
        for q in self.dq.values():
            for sem, cnt in q["sems"]:
                if cnt > 0:
                    evs.append((sem, cnt))
        for en in self.eng:
            for ev in evs:
                self._wait(en, ev)
        self.ts = {}

    def finish(self):
        self.barrier()
        self.root.close()

    def mm(self, out, lhsT, rhs, start=True, stop=True):
        return self.op("pe", lambda e: e.matmul(out, lhsT=lhsT, rhs=rhs, start=start, stop=stop),
                       reads=[lhsT, rhs], writes=[out])

    def tr(self, out, in_, ident):
        return self.op("pe", lambda e: e.transpose(out, in_, ident), reads=[in_, ident], writes=[out])

    def act(self, out, in_, func, bias=None, scale=1.0, accum_out=None, en="act"):
        kw = {}
        if bias is not None:
            kw["bias"] = bias
        if accum_out is not None:
            kw["accum_out"] = accum_out
        rd = [in_]
        if isinstance(bias, bass.AP):
            rd.append(bias)
        if isinstance(scale, bass.AP):
            rd.append(scale)
        return self.op("act", lambda e: e.activation(out=out, in_=in_, func=func, scale=scale, **kw),
                       reads=rd, writes=[out, accum_out])

    def tt(self, out, in0, in1, op, en="dve"):
        return self.op(en, lambda e: e.tensor_tensor(out=out, in0=in0, in1=in1, op=op),
                       reads=[in0, in1], writes=[out])

    def tsc(self, out, in0, s1, op0, s2=None, op1=None, accum_out=None, en="dve"):
        kw = {}
        if op1 is not None:
            kw["op1"] = op1
        if accum_out is not None:
            kw["accum_out"] = accum_out
        rd = [in0] + [s for s in (s1, s2) if isinstance(s, bass.AP)]
        return self.op(en, lambda e: e.tensor_scalar(out=out, in0=in0, scalar1=s1, scalar2=s2, op0=op0, **kw),
                       reads=rd, writes=[out, accum_out])

    def stt(self, out, in0, scalar, in1, op0, op1):
        rd = [in0, in1] + ([scalar] if isinstance(scalar, bass.AP) else [])
        return self.op("dve", lambda e: e.scalar_tensor_tensor(out=out, in0=in0, scalar=scalar, in1=in1,
                                                               op0=op0, op1=op1),
                       reads=rd, writes=[out])

    def copy(self, out, in_, en="dve"):
        if en == "act":
            return self.op("act", lambda e: e.copy(out=out, in_=in_), reads=[in_], writes=[out])
        return self.op(en, lambda e: e.tensor_copy(out=out, in_=in_), reads=[in_], writes=[out])

    def memset(self, out, val, en="dve"):
        return self.op(en, lambda e: e.memset(out, val), reads=[], writes=[out])

    def recip(self, out, in_):
        return self.op("dve", lambda e: e.reciprocal(out=out, in_=in_), reads=[in_], writes=[out])

    def reduce(self, out, in_, op, axis=AX.X):
        return self.op("dve", lambda e: e.tensor_reduce(out=out, in_=in_, op=op, axis=axis),
                       reads=[in_], writes=[out])


T_LAT = 2048
T_CTX = 256
T_ALL = 2304
NT = 18
D = 1024
D_IN = 3232
DEPTH = 4
TB = [(0, 512), (512, 512), (1024, 512), (1536, 512), (2048, 256)]
EPS = 1e-6


class Prog:
    def __init__(self, debug=(), depth=DEPTH, stop=None, phases=("front", "hgrn", "hyena", "na", "mla", "outproj", "moe")):
        self.phases = phases
        self.nc = bass.Bass("TRN2", target_bir_lowering=False)
        self.mk = MK(self.nc)
        self.debug = set(debug)
        self.depth = depth
        self.stop = stop
        self.inputs = {}
        self.outs = []
        self._evi = 0

    def din(self, name, shape, dtype=F32):
        t = self.nc.dram_tensor(name, list(shape), dtype, kind="ExternalInput")
        self.inputs[name] = (tuple(shape), dtype)
        return t

    def dscr(self, name, shape, dtype=F32):
        if ("in:" + name) in self.debug:
            return self.din(name, shape, dtype)
        if name in self.debug:
            self.outs.append(name)
            return self.nc.dram_tensor(name, list(shape), dtype, kind="ExternalOutput")
        return self.nc.dram_tensor(name, list(shape), dtype, kind="Internal")

    def dout(self, name, shape, dtype=F32):
        self.outs.append(name)
        return self.nc.dram_tensor(name, list(shape), dtype, kind="ExternalOutput")

    def evac(self, out, in_):
        self._evi += 1
        if self._evi % 2:
            return self.mk.copy(out, in_, en="dve")
        return self.mk.copy(out, in_, en="act")

    def declare(self):
        p = self
        L = DEPTH
        p.xin = p.din("xin", [T_ALL, D])
        p.cvecT = p.din("cvecT", [D, 2])
        p.w_ada = p.din("w_ada", [L, D, 6 * D])
        p.b_adaT = p.din("b_adaT", [L, 128, 48])
        p.n1g = p.din("n1g", [L, 128, 8])
        p.n2g = p.din("n2g", [L, 128, 8])
        p.w_in = p.din("w_in", [L, D, D_IN])
        p.w_out = p.din("w_out", [L, D, D])
        p.ident = p.din("ident", [128, 128])
        p.xT = p.dscr("xT", [D, T_ALL])
        p.ptok = p.dscr("ptok", [T_ALL, D_IN])
        p.uT = p.dscr("uT", [768, T_ALL])
        p.catT = p.dscr("catT", [D, T_ALL])
        p.h2T = p.dscr("h2T", [D, T_ALL])
        p.hof = p.dscr("hof", [T_ALL, 256])
        p.Xb = p.dscr("Xb", [NSLOT + 128, D])
        p.Yb = p.dscr("Yb", [NSLOT + 128, D])
        p.Ksp = p.dscr("Ksp", [2, 2, 2048, 256])
        p.Kspc = p.dscr("Kspc", [2, 2, 256, 256])
        p.gch = p.dscr("gch", [2, 256, T_ALL])
        p.xout = p.dout("xout", [T_LAT, D])
        for nm, shp in (("hgrn_lb", [L, 2, 256]), ("hgrn_norm_g", [L, 64]), ("hy_swT", [L, 128, 6, 3]), ("hy_sbT", [L, 128, 6]),
                        ("hy_w1", [L, 33, 64]), ("hy_b1", [L, 64]), ("hy_freq", [L, 64]), ("hy_w2", [L, 64, 64]),
                        ("hy_b2", [L, 64]), ("hy_w3", [L, 64, 1024]), ("hy_b3", [L, 1024]), ("hy_decay", [L, 2, 2, 256]),
                        ("hy_bias", [L, 2, 256]), ("na_biasT", [L, 4, 128, 5, 5, 128]), ("na_q_g", [L, 64]), ("na_k_g", [L, 64]),
                        ("mla_q_a_g", [L, 256]), ("mla_kv_a_g", [L, 128]), ("mla_w_uq", [L, 256, 384]), ("mla_w_ukv", [L, 128, 512]),
                        ("mla_q_g", [L, 96]), ("mla_k_g", [L, 96]), ("moe_wg", [L, D, 4]), ("moe_bg", [L, 4]),
                        ("moe_we", [L, D, 32]), ("moe_be", [L, 32]), ("moe_w_gate", [L, 32, D, 512]),
                        ("moe_w_up", [L, 32, D, 512]), ("moe_w_down", [L, 32, 512, D]),
                        ("c_lstrict", [128, 128]), ("c_iotaE", [128, 32]), ("c_trif", [64, 64]), ("c_trib", [64, 64]),
                        ("c_exch", [128, 128]), ("c_ropecos", [128, 16, 32]), ("c_ropesin", [128, 16, 32]),
                        ("c_feats", [2, 33, 2048]), ("c_ntun", [128, 16]), ("c_featsc", [2, 33, 256]), ("c_ntunc", [128, 2]),
                        ("c_dftB", [16, 128, 3, 16, 128]), ("c_dftBc", [2, 128, 3, 2, 128]), ("c_dftN", [2, 2048, 2048]), ("c_dftNc", [2, 256, 256]), ("hy_biasT", [L, 128, 2, 2])):
            setattr(p, nm, p.din(nm, shp))

    def build(self):
        p, mk = self, self.mk
        p.declare()
        p.identS = mk.sb("ident", [128, 128])
        mk.dma("sp", p.identS[:], p.ident[:, :])
        p.onesF = mk.sb("onesF", [128, 128])
        mk.memset(p.onesF[:], 1.0)
        p.onesR = mk.sb("onesR", [128, 128], F32R)
        mk.copy(p.onesR[:], p.onesF[:])
        p.psum = [mk.ps("ps%d" % i, [128, 512]) for i in range(8)]
        p.breg = p.nc.gpsimd.to_reg(NSLOT - 1)
        p.load_x()
        phases = p.phases
        if "moe" in phases:
            p.moe_init()
        for l in range(p.depth):
            if "front" in phases:
                p.front(l)
            else:
                p.mod = p.modulation(l)
            p.bg_hgrn = False
            for ph in ("hgrn", "hyena", "na", "mla", "outproj", "moe"):
                if ph == "hgrn" and p.bg_hgrn:
                    continue
                if ph in phases:
                    getattr(p, ph)(l)
        p.store_x()
        mk.finish()

    def load_x(self):
        p, mk = self, self.mk
        with mk.scope():
            xt = [mk.sb("xt%d" % i, [128, D]) for i in range(2)]
            xTt = mk.sb("xTt", [128, 8, 512])
            xTv = p.xT.ap().rearrange("(k q) t -> q k t", q=128)
            for (t0, w) in TB:
                for j in range(w // 128):
                    n = t0 // 128 + j
                    xb = xt[n % 2]
                    mk.dma("sp", xb[:], p.xin[n * 128:(n + 1) * 128, :])
                    for k in range(8):
                        ps = p.psum[k % 8]
                        mk.tr(ps[:, 0:128], xb[:, k * 128:(k + 1) * 128], p.identS[:])
                        p.evac(xTt[:, k, j * 128:(j + 1) * 128], ps[:, 0:128])
                mk.dma("sp", xTv[:, :, t0:t0 + w], xTt[:, :, 0:w])

    def store_x(self):
        p, mk = self, self.mk
        with mk.scope():
            xTt = mk.sb("xTt", [128, 8, 512])
            xo = [mk.sb("xo%d" % i, [128, D]) for i in range(2)]
            xTv = p.xT.ap().rearrange("(k q) t -> q k t", q=128)
            for (t0, w) in TB[:4]:
                mk.dma("sp", xTt[:, :, 0:w], xTv[:, :, t0:t0 + w])
                for j in range(w // 128):
                    n = t0 // 128 + j
                    xb = xo[n % 2]
                    for k in range(8):
                        ps = p.psum[k % 8]
                        mk.tr(ps[:, 0:128], xTt[:, k, j * 128:(j + 1) * 128], p.identS[:])
                        p.evac(xb[:, k * 128:(k + 1) * 128], ps[:, 0:128])
                    mk.dma("sp", p.xout[n * 128:(n + 1) * 128, :], xb[:])

    def modulation(self, l):
        p, mk = self, self.mk
        mod = mk.sb("mod%d" % l, [128, 48, 2])
        with mk.scope():
            cT = mk.sb("cT", [128, 8, 2])
            mk.dma("sp", cT[:], p.cvecT.ap().rearrange("(k q) r -> q k r", q=128))
            scT = mk.sb("scT", [128, 8, 2])
            mk.act(scT[:], cT[:], AF.Silu)
            bT = mk.sb("bT", [128, 48])
            mk.dma("sp", bT[:], p.b_adaT[l, :, :])
            wv = p.w_ada[l, :, :].rearrange("(k q) n -> q k n", q=128)
            wb = [mk.sb("wada%d" % i, [128, 8, 512]) for i in range(2)]
            ps = p.psum[0]
            psv = ps[:, 0:96].rearrange("q (j r) -> q j r", r=2)
            for jb in range(12):
                w = wb[jb % 2]
                mk.dma("sp" if jb % 2 == 0 else "pool", w[:], wv[:, :, jb * 512:(jb + 1) * 512])
                for jj in range(4):
                    j = jb * 4 + jj
                    for k in range(8):
                        mk.mm(psv[:, j, :], lhsT=w[:, k, jj * 128:(jj + 1) * 128], rhs=scT[:, k, :],
                              start=(k == 0), stop=(k == 7))
            for r in range(2):
                mk.tt(mod[:, :, r], psv[:, :, r], bT[:], ALU.add)
        return mod

    def front(self, l):
        p, mk = self, self.mk
        mod = p.modulation(l)
        p.mod = mod
        with mk.scope():
            g1n = mk.sb("g1n", [128, 8])
            mk.dma("sp", g1n[:], p.n1g[l, :, :])
            A1 = mk.sb("A1", [128, 8, 2])
            for r in range(2):
                mk.stt(A1[:, :, r], mod[:, 8:16, r], 1.0, g1n[:], ALU.add, ALU.mult)
            hT = mk.sb("hT", [128, 8, T_ALL], F32R)
            p.norm_mod(p.xT, A1, mod[:, 0:8, :], hT)
            p.inproj(l, hT)

    def norm_mod(self, src, A, B, hT, dst_dram=None):
        p, mk = self, self.mk
        with mk.scope():
            xb = [mk.sb("nx%d" % i, [128, 8, 512]) for i in range(2)]
            sq = mk.sb("nsq", [128, 8, 512], F32R)
            rstd = mk.sb("rstd", [128, 512])
            tmp = [mk.sb("ntmp%d" % i, [128, 512]) for i in range(2)]
            xv = src.ap().rearrange("(k q) t -> q k t", q=128)
            for bi, (t0, w) in enumerate(TB):
                r = 0 if t0 < T_LAT else 1
                x = xb[bi % 2]
                mk.dma("sp", x[:, :, 0:w], xv[:, :, t0:t0 + w])
                mk.act(sq[:, :, 0:w], x[:, :, 0:w], AF.Square)
                ps = p.psum[bi % 2]
                for k in range(8):
                    mk.mm(ps[:, 0:w], lhsT=p.onesR[:], rhs=sq[:, k, 0:w], start=(k == 0), stop=(k == 7))
                mk.act(rstd[:, 0:w], ps[:, 0:w], AF.Sqrt, bias=EPS, scale=1.0 / D)
                mk.recip(rstd[:, 0:w], rstd[:, 0:w])
                for k in range(8):
                    t = tmp[k % 2]
                    mk.tt(t[:, 0:w], x[:, k, 0:w], rstd[:, 0:w], ALU.mult)
                    mk.act(hT[:, k, t0:t0 + w], t[:, 0:w], AF.Identity, bias=B[:, k, r:r + 1], scale=A[:, k, r:r + 1])

    def inproj(self, l, hT):
        p, mk = self, self.mk
        with mk.scope():
            wv = p.w_in[l, :, :].rearrange("(k q) n -> q k n", q=128)
            wr = [mk.sb("wr%d" % i, [128, 8, 512], F32R) for i in range(3)]
            ob = [mk.sb("ob%d" % i, [128, 512]) for i in range(3)]
            blocks = [("tok", 0, 512), ("tok", 512, 512), ("tok", 1024, 256)]
            blocks += [("feat", 1280 + 128 * i, 128) for i in range(6)]
            blocks += [("tok", 2048, 512), ("tok", 2560, 512), ("tok", 3072, 160)]
            oi = 0
            for bi, (kind, c0, cw) in enumerate(blocks):
                w = wr[bi % 3]
                mk.dma("pool", w[:, :, 0:cw], wv[:, :, c0:c0 + cw])
                if kind == "tok":
                    for n in range(NT):
                        ps = p.psum[n % 4]
                        for k in range(8):
                            mk.mm(ps[:, 0:cw], lhsT=hT[:, k, n * 128:(n + 1) * 128], rhs=w[:, k, 0:cw],
                                  start=(k == 0), stop=(k == 7))
                        o = ob[oi % 3]
                        oi += 1
                        p.evac(o[:, 0:cw], ps[:, 0:cw])
                        mk.dma("sp", p.ptok[n * 128:(n + 1) * 128, c0:c0 + cw], o[:, 0:cw])
                else:
                    f0 = c0 - 1280
                    for ti, (t0, tw) in enumerate(TB):
                        ps = p.psum[4 + ti % 4]
                        for k in range(8):
                            mk.mm(ps[:, 0:tw], lhsT=w[:, k, 0:128], rhs=hT[:, k, t0:t0 + tw],
                                  start=(k == 0), stop=(k == 7))
                        o = ob[oi % 3]
                        oi += 1
                        p.evac(o[:, 0:tw], ps[:, 0:tw])
                        mk.dma("sp", p.uT[f0:f0 + 128, t0:t0 + tw], o[:, 0:tw])


CAP = 384
NSLOT = 32 * CAP
BIGIDX = 1.0e6


def _outproj(self, l):
    p, mk = self, self.mk
    mod = p.mod
    with mk.scope():
        wv = p.w_out[l, :, :].rearrange("(k q) n -> q k n", q=128)
        wR = mk.sb("woR", [128, 8, 1024], F32R)
        for hf in range(2):
            mk.dma("pool", wR[:, :, hf * 512:(hf + 1) * 512], wv[:, :, hf * 512:(hf + 1) * 512])
        cRs = [mk.sb("cR%d" % i, [128, 8, 512], F32R) for i in range(2)]
        xb = [mk.sb("oxb%d" % i, [128, 8, 512]) for i in range(2)]
        cv = p.catT.ap().rearrange("(k q) t -> q k t", q=128)
        xv = p.xT.ap().rearrange("(k q) t -> q k t", q=128)
        for bi, (t0, w) in enumerate(TB):
            r = 0 if t0 < T_LAT else 1
            cR, x = cRs[bi % 2], xb[bi % 2]
            mk.dma("pool", cR[:, :, 0:w], cv[:, :, t0:t0 + w])
            mk.dma("sp", x[:, :, 0:w], xv[:, :, t0:t0 + w])
            for j in range(8):
                ps = p.psum[j % 4]
                for k in range(8):
                    mk.mm(ps[:, 0:w], lhsT=wR[:, k, j * 128:(j + 1) * 128], rhs=cR[:, k, 0:w],
                          start=(k == 0), stop=(k == 7))
                mk.stt(x[:, j, 0:w], ps[:, 0:w], mod[:, 16 + j, r:r + 1], x[:, j, 0:w], ALU.mult, ALU.add)
            mk.dma("sp", xv[:, :, t0:t0 + w], x[:, :, 0:w])


def _moe(self, l):
    p, mk = self, self.mk
    mod = p.mod
    with mk.scope():
        rt = mk.sb("rt", [128, NT, 8])
        gidx = mk.sb("gidx", [128, NT, 2], I32)
        with mk.scope():
            g2n = mk.sb("g2n", [128, 8])
            mk.dma("sp", g2n[:], p.n2g[l, :, :])
            A2 = mk.sb("A2", [128, 8, 2])
            for r in range(2):
                mk.stt(A2[:, :, r], mod[:, 32:40, r], 1.0, g2n[:], ALU.add, ALU.mult)
            B2 = mod[:, 24:32, :]
            wrt = mk.sb("wrt", [128, 8, 36])
            mk.dma("sp", wrt[:, :, 0:4], p.moe_wg[l, :, :].rearrange("(k q) n -> q k n", q=128))
            mk.dma("sp", wrt[:, :, 4:36], p.moe_we[l, :, :].rearrange("(k q) n -> q k n", q=128))
            bias = mk.sb("rbias", [128, 36])
            mk.dma("sp", bias[:, 0:4], p.moe_bg[l, :].partition_broadcast(128))
            mk.dma("sp", bias[:, 4:36], p.moe_be[l, :].partition_broadcast(128))
            lstrict = mk.sb("lstrict", [128, 128])
            mk.dma("sp", lstrict[:], p.c_lstrict[:, :])
            iotaE = mk.sb("iotaE", [128, 32])
            mk.dma("sp", iotaE[:], p.c_iotaE[:, :])
            xb = [mk.sb("mx%d" % i, [128, 8, 512]) for i in range(2)]
            sq = mk.sb("msq", [128, 8, 512], F32R)
            rstd = mk.sb("mrstd", [128, 512])
            hb = mk.sb("mhb", [128, 8, 512])
            htok = mk.sb("htok", [128, NT, D])
            lr = mk.sb("r_lr", [128, NT, 36])
            xv = p.xT.ap().rearrange("(k q) t -> q k t", q=128)
            for bi, (t0, w) in enumerate(TB):
                r = 0 if t0 < T_LAT else 1
                x = xb[bi % 2]
                mk.dma("sp", x[:, :, 0:w], xv[:, :, t0:t0 + w])
                mk.act(sq[:, :, 0:w], x[:, :, 0:w], AF.Square)
                ps = p.psum[0]
                for k in range(8):
                    mk.mm(ps[:, 0:w], lhsT=p.onesR[:], rhs=sq[:, k, 0:w], start=(k == 0), stop=(k == 7))
                mk.act(rstd[:, 0:w], ps[:, 0:w], AF.Sqrt, bias=EPS, scale=1.0 / D)
                mk.recip(rstd[:, 0:w], rstd[:, 0:w])
                for k in range(8):
                    mk.tt(hb[:, k, 0:w], x[:, k, 0:w], rstd[:, 0:w], ALU.mult)
                    mk.act(hb[:, k, 0:w], hb[:, k, 0:w], AF.Identity, bias=B2[:, k, r:r + 1], scale=A2[:, k, r:r + 1])
                for j in range(w // 128):
                    n = t0 // 128 + j
                    sl = slice(j * 128, (j + 1) * 128)
                    pl = p.psum[1]
                    for k in range(8):
                        mk.mm(pl[:, 0:36], lhsT=hb[:, k, sl], rhs=wrt[:, k, :], start=(k == 0), stop=(k == 7))
                    mk.tt(lr[:, n, :], pl[:, 0:36], bias[:], ALU.add)
                    for k in range(8):
                        pt = p.psum[2 + k % 4]
                        mk.tr(pt[:, 0:128], hb[:, k, sl], p.identS[:])
                        p.evac(htok[:, n, k * 128:(k + 1) * 128], pt[:, 0:128])
            R = lambda nm, shp: mk.sb("r_" + nm, [128] + shp)
            gmax, gsum, psel = R("gmax", [NT]), R("gsum", [NT]), R("psel", [NT])
            eg, ohg, pen = R("eg", [NT, 4]), R("ohg", [NT, 4]), R("pen", [NT, 4])
            lem, lem2 = R("lem", [NT, 32]), R("lem2", [NT, 32])
            m1, m2, dm, ee, w1 = R("m1", [NT]), R("m2", [NT]), R("dm", [NT]), R("e", [NT]), R("w1", [NT])
            oh1, oh2, oh, cnt, t32 = R("oh1", [NT, 32]), R("oh2", [NT, 32]), R("oh", [NT, 32]), R("cnt", [NT, 32]), R("t32", [NT, 32])
            s_, f__, gs = R("s", [NT, 2]), R("f", [NT, 2]), R("gs", [NT, 2])
            bc3 = lambda a, k: a[:].unsqueeze(2).to_broadcast([128, NT, k])
            lg4 = lr[:, :, 0:4]
            mk.reduce(gmax[:], lg4, ALU.max)
            mk.tt(eg[:], lg4, bc3(gmax, 4), ALU.subtract)
            mk.act(eg[:], eg[:], AF.Exp)
            mk.reduce(gsum[:], eg[:], ALU.add)
            mk.recip(psel[:], gsum[:])
            mk.tt(ohg[:], lg4, bc3(gmax, 4), ALU.is_equal)
            mk.tsc(pen[:], ohg[:], 1.0e9, ALU.mult, -1.0e9, ALU.add)
            mk.tt(lem[:].rearrange("q n (g e) -> q n g e", g=4), lr[:, :, 4:36].rearrange("q n (g e) -> q n g e", g=4),
                  pen[:].unsqueeze(3).to_broadcast([128, NT, 4, 8]), ALU.add)
            mk.reduce(m1[:], lem[:], ALU.max)
            mk.tt(oh1[:], lem[:], bc3(m1, 32), ALU.is_equal)
            mk.stt(lem2[:], oh1[:], -2.0e9, lem[:], ALU.mult, ALU.add)
            mk.reduce(m2[:], lem2[:], ALU.max)
            mk.tt(oh2[:], lem2[:], bc3(m2, 32), ALU.is_equal)
            mk.tt(oh[:], oh1[:], oh2[:], ALU.add)
            mk.tt(dm[:], m2[:], m1[:], ALU.subtract)
            mk.act(ee[:], dm[:], AF.Exp)
            mk.tsc(w1[:], ee[:], 1.0, ALU.add)
            mk.recip(w1[:], w1[:])
            mk.tt(rt[:, :, 0], w1[:], psel[:], ALU.mult)
            mk.tt(rt[:, :, 1], rt[:, :, 0], ee[:], ALU.mult)
            pcs = [p.psum[6], p.psum[7]]
            for n in range(NT):
                pc = pcs[n // 9][:, (n % 9) * 32:(n % 9 + 1) * 32]
                mk.mm(pc, lhsT=lstrict[:], rhs=oh[:, n, :], start=True, stop=(n == 0))
                for m_ in range(n):
                    mk.mm(pc, lhsT=p.onesF[:], rhs=oh[:, m_, :], start=False, stop=(m_ == n - 1))
            pos = R("pos", [NT, 32])
            for hf in range(2):
                mk.copy(pos[:, hf * 9:(hf + 1) * 9, :].rearrange("q n e -> q (n e)"), pcs[hf][:, 0:288])
            mk.tt(cnt[:], pos[:], iotaE[:].unsqueeze(1).to_broadcast([128, NT, 32]), ALU.add)
            for kk, ohk in enumerate((oh1, oh2)):
                mk.tt(t32[:], ohk[:], cnt[:], ALU.mult)
                mk.reduce(s_[:, :, kk], t32[:], ALU.add)
                mk.tt(t32[:], ohk[:], pos[:], ALU.mult)
                mk.reduce(f__[:, :, kk], t32[:], ALU.add)
            mk.tsc(f__[:], f__[:], float(CAP) - 0.5, ALU.is_gt, BIGIDX, ALU.mult)
            mk.tt(gs[:], s_[:], f__[:], ALU.add)
            sidx = mk.sb("sidx", [128, NT, 2], I32)
            mk.copy(sidx[:], gs[:])
            mk.tsc(gs[:], gs[:], float(NSLOT), ALU.min)
            mk.copy(gidx[:], gs[:])
            for n in range(NT):
                for kk in range(2):
                    mk.dma("pool", p.Xb[:, :], htok[:, n, :],
                           fn=lambda e, kk=kk, n=n: e.indirect_dma_start(
                               out=p.Xb[:, :], out_offset=bass.IndirectOffsetOnAxis(ap=sidx[:, n, kk:kk + 1], axis=0),
                               in_=htok[:, n, :], in_offset=None, bounds_check=p.breg, oob_is_err=False),
                           extra_reads=[sidx])
        if getattr(p, 'moe_stop', 3) < 2:
            return
        with mk.scope():
            W = [{"g": mk.sb("WgR%d" % i, [128, 8, 512], F32R), "u": mk.sb("WuR%d" % i, [128, 8, 512], F32R),
                  "d": mk.sb("WdR%d" % i, [128, 4, 1024], F32R)} for i in range(2)]
            NS = CAP // 128
            xtok = [mk.sb("extok%d" % i, [128, D]) for i in range(2 * NS)]
            xbT = [mk.sb("xbT%d" % i, [128, 8, CAP], F32R) for i in range(2)]
            hidT = mk.sb("hidT", [128, 4, CAP], F32R)
            sil = [mk.sb("sil%d" % i, [128, CAP]) for i in range(2)]
            yt = [mk.sb("eyt%d" % i, [128, D]) for i in range(3)]
            yi = 0

            def load_w(e):
                w = W[e % 2]
                mk.dma("pool", w["g"][:], p.moe_w_gate[l, e, :, :].rearrange("(k q) n -> q k n", q=128))
                mk.dma("pool", w["u"][:], p.moe_w_up[l, e, :, :].rearrange("(k q) n -> q k n", q=128))
                mk.dma("pool", w["d"][:], p.moe_w_down[l, e, :, :].rearrange("(k q) n -> q k n", q=128))

            def load_x(e):
                for si_ in range(NS):
                    r0 = e * CAP + si_ * 128
                    mk.dma("sp", xtok[(e % 2) * NS + si_][:], p.Xb[r0:r0 + 128, :])

            load_w(0)
            load_x(0)
            for e in range(32):
                if e + 1 < 32:
                    load_w(e + 1)
                    load_x(e + 1)
                w = W[e % 2]
                xT_ = xbT[e % 2]
                for si_ in range(NS):
                    xt = xtok[(e % 2) * NS + si_]
                    for k in range(8):
                        pt = p.psum[k % 4]
                        mk.tr(pt[:, 0:128], xt[:, k * 128:(k + 1) * 128], p.identS[:])
                        p.evac(xT_[:, k, si_ * 128:(si_ + 1) * 128], pt[:, 0:128])
                for f in range(4):
                    pg, pu = p.psum[4 + (f % 2) * 2], p.psum[5 + (f % 2) * 2]
                    for k in range(8):
                        mk.mm(pg[:, 0:CAP], lhsT=w["g"][:, k, f * 128:(f + 1) * 128], rhs=xT_[:, k, :], start=(k == 0), stop=(k == 7))
                    for k in range(8):
                        mk.mm(pu[:, 0:CAP], lhsT=w["u"][:, k, f * 128:(f + 1) * 128], rhs=xT_[:, k, :], start=(k == 0), stop=(k == 7))
                    sl_ = sil[f % 2]
                    mk.act(sl_[:], pg[:, 0:CAP], AF.Silu)
                    mk.tt(hidT[:, f, :], sl_[:], pu[:, 0:CAP], ALU.mult)
                for si_ in range(NS):
                    y = yt[yi % 3]
                    yi += 1
                    for hf in range(2):
                        ps = p.psum[hf]
                        for f in range(4):
                            mk.mm(ps[:, 0:512], lhsT=hidT[:, f, si_ * 128:(si_ + 1) * 128], rhs=w["d"][:, f, hf * 512:(hf + 1) * 512],
                                  start=(f == 0), stop=(f == 3))
                        p.evac(y[:, hf * 512:(hf + 1) * 512], ps[:, 0:512])
                    r0 = e * CAP + si_ * 128
                    mk.dma("act", p.Yb[r0:r0 + 128, :], y[:])
        if getattr(p, 'moe_stop', 3) < 3:
            return
        with mk.scope():
            y1 = [mk.sb("cy1%d" % i, [128, D]) for i in range(2)]
            y2 = [mk.sb("cy2%d" % i, [128, D]) for i in range(2)]
            xb = [mk.sb("cxb%d" % i, [128, 8, 512]) for i in range(2)]
            xv = p.xT.ap().rearrange("(k q) t -> q k t", q=128)
            for bi, (t0, w) in enumerate(TB):
                r = 0 if t0 < T_LAT else 1
                x = xb[bi % 2]
                mk.dma("sp", x[:, :, 0:w], xv[:, :, t0:t0 + w])
                for j in range(w // 128):
                    n = t0 // 128 + j
                    a, b = y1[n % 2], y2[n % 2]
                    for kk, dst in enumerate((a, b)):
                        mk.dma("pool", dst[:], p.Yb[:, :],
                               fn=lambda e, kk=kk, dst=dst, n=n: e.indirect_dma_start(
                                   out=dst[:], out_offset=None, in_=p.Yb[:, :],
                                   in_offset=bass.IndirectOffsetOnAxis(ap=gidx[:, n, kk:kk + 1], axis=0)),
                               extra_reads=[gidx])
                    mk.tsc(a[:], a[:], rt[:, n, 0:1], ALU.mult)
                    mk.stt(a[:], b[:], rt[:, n, 1:2], a[:], ALU.mult, ALU.add)
                    for k in range(8):
                        pt = p.psum[k % 4]
                        mk.tr(pt[:, 0:128], a[:, k * 128:(k + 1) * 128], p.identS[:])
                        mk.stt(x[:, k, j * 128:(j + 1) * 128], pt[:, 0:128], mod[:, 40 + k, r:r + 1],
                               x[:, k, j * 128:(j + 1) * 128], ALU.mult, ALU.add)
                mk.dma("sp", xv[:, :, t0:t0 + w], x[:, :, 0:w])


def _moe_init(self):
    p, mk = self, self.mk
    with mk.scope():
        z = mk.sb("zeros", [128, D])
        mk.memset(z[:], 0.0)
        for i in range((NSLOT + 128) // 128):
            mk.dma("sp" if i % 2 == 0 else "pool", p.Xb[i * 128:(i + 1) * 128, :], z[:])
        mk.dma("sp", p.Yb[NSLOT:NSLOT + 128, :], z[:])


Prog.outproj = _outproj
Prog.moe = _moe
Prog.moe_init = _moe_init

NA_SCALE = 64 ** -0.5
MLA_SCALE = 96 ** -0.5


def _headnorm(self, out, x, nh, hd, gain_bc, sq, ss, np_=128):
    mk = self.mk
    xv = x.rearrange("q (h d) -> q h d", h=nh)
    sqv = sq.rearrange("q (h d) -> q h d", h=nh)
    mk.tt(sq, x, x, ALU.mult)
    mk.reduce(ss, sqv, ALU.add)
    mk.act(ss, ss, AF.Sqrt, bias=EPS, scale=1.0 / hd)
    mk.recip(ss, ss)
    mk.tt(sqv, xv, ss.unsqueeze(2).to_broadcast([np_, nh, hd]), ALU.mult)
    mk.tt(out, sq, gain_bc, ALU.mult)


def _transposes_out(self, src_all, row0):
    p, mk = self, self.mk
    with mk.scope():
        ob = [mk.sb("tob%d" % i, [128, 512]) for i in range(2)]
        oi = 0
        for c in range(2):
            for bi, (t0, w) in enumerate(TB):
                o = ob[oi % 2]
                oi += 1
                for j in range(w // 128):
                    n = t0 // 128 + j
                    pt = p.psum[(n + c) % 4]
                    mk.tr(pt[:, 0:128], src_all[:, n, c * 128:(c + 1) * 128], p.identS[:])
                    p.evac(o[:, j * 128:(j + 1) * 128], pt[:, 0:128])
                mk.dma("sp", p.catT[row0 + c * 128:row0 + (c + 1) * 128, t0:t0 + w], o[:, 0:w])


def _na(self, l):
    p, mk = self, self.mk
    with mk.scope():
        qT = mk.sb("naqT", [128, 2, T_ALL], F32R)
        kT = mk.sb("nakT", [128, 2, T_ALL], F32R)
        V = mk.sb("naV", [128, NT, 4, 66], F32R)
        out_all = mk.sb("naout", [128, NT, 256])
        with mk.scope():
            gq = mk.sb("nagq", [128, 4, 64])
            gk = mk.sb("nagk", [128, 4, 64])
            for h in range(4):
                mk.dma("sp", gq[:, h, :], p.na_q_g[l, :].partition_broadcast(128))
                mk.dma("sp", gk[:, h, :], p.na_k_g[l, :].partition_broadcast(128))
            ones65 = mk.sb("ones65", [128, 4, 2])
            mk.memset(ones65[:], 0.0)
            mk.memset(ones65[:, :, 0:1], 1.0)
            tin = [mk.sb("natin%d" % i, [128, 768]) for i in range(2)]
            sq = mk.sb("nasq", [128, 256])
            ss = mk.sb("nass", [128, 4])
            qn = mk.sb("naqn", [128, 256])
            for n in range(NT):
                t = tin[n % 2]
                mk.dma("sp", t[:], p.ptok[n * 128:(n + 1) * 128, 2048:2816])
                for (src, g, dstT) in ((t[:, 0:256], gq, qT), (t[:, 256:512], gk, kT)):
                    p.headnorm(qn[:], src, 4, 64, g[:].rearrange("q h d -> q (h d)"), sq[:], ss[:])
                    for c in range(2):
                        pt = p.psum[c]
                        mk.tr(pt[:, 0:128], qn[:, c * 128:(c + 1) * 128], p.identS[:])
                        p.evac(dstT[:, c, n * 128:(n + 1) * 128], pt[:, 0:128])
                mk.copy(V[:, n, :, 0:64], t[:, 512:768].rearrange("q (h d) -> q h d", h=4), en="pool")
                mk.copy(V[:, n, :, 64:66], ones65[:], en="pool")
        with mk.scope():
            bias = [mk.sb("nabias%d" % i, [128, 5, 5, 128]) for i in range(2)]
            PT = [mk.sb("naPT%d" % i, [128, 7, 128], F32R) for i in range(2)]
            tmpb = [mk.sb("natmp%d" % i, [128, 5, 128]) for i in range(2)]
            rec = mk.sb("narec", [128, 1])
            PTc = mk.sb("naPTc", [128, 2, 256], F32R)
            it = 0
            for h in range(4):
                hb, hc = (h % 2) * 64, h // 2
                bs = bias[h % 2]
                mk.dma("sp", bs[:], p.na_biasT[l, h, :, :, :, :])
                for pr in range(16):
                    pat = 0 if pr == 0 else 1 if pr == 1 else 3 if pr == 14 else 4 if pr == 15 else 2
                    rs0 = min(max(2 * pr - 4, 0), 24)
                    ws = min((rs0 // 2) * 2, 22)
                    kt0 = ws // 2
                    q0 = pr * 128
                    pa, pb = p.psum[(it % 2) * 2], p.psum[(it % 2) * 2 + 1]
                    P_, tb = PT[it % 2], tmpb[it % 2]
                    for kt in range(4):
                        mk.mm(pa[:, kt * 128:(kt + 1) * 128], lhsT=kT[hb:hb + 64, hc, (kt0 + kt) * 128:(kt0 + kt + 1) * 128],
                              rhs=qT[hb:hb + 64, hc, q0:q0 + 128])
                    mk.mm(pb[:, 0:128], lhsT=kT[hb:hb + 64, hc, (kt0 + 4) * 128:(kt0 + 5) * 128], rhs=qT[hb:hb + 64, hc, q0:q0 + 128])
                    for j in range(2):
                        mk.mm(pb[:, 128 + j * 128:256 + j * 128], lhsT=kT[hb:hb + 64, hc, T_LAT + j * 128:T_LAT + (j + 1) * 128],
                              rhs=qT[hb:hb + 64, hc, q0:q0 + 128])
                    mk.stt(tb[:, 0:4, :], pa[:, 0:512].rearrange("q (a b) -> q a b", a=4), NA_SCALE, bs[:, pat, 0:4, :], ALU.mult, ALU.add)
                    mk.stt(tb[:, 4, :], pb[:, 0:128], NA_SCALE, bs[:, pat, 4, :], ALU.mult, ALU.add)
                    mk.act(P_[:, 0:5, :], tb[:], AF.Exp)
                    mk.act(P_[:, 5:7, :], pb[:, 128:384].rearrange("q (a b) -> q a b", a=2), AF.Exp, scale=NA_SCALE)
                    po = p.psum[4 + it % 2]
                    for kt in range(7):
                        vt = kt0 + kt if kt < 5 else 16 + (kt - 5)
                        mk.mm(po[:, 0:66], lhsT=P_[:, kt, :], rhs=V[:, vt, h, :], start=(kt == 0), stop=(kt == 6))
                    mk.recip(rec[:], po[:, 64:65])
                    mk.tsc(out_all[:, pr, h * 64:(h + 1) * 64], po[:, 0:64], rec[:, 0:1], ALU.mult)
                    it += 1
                pc_ = p.psum[6]
                for j in range(2):
                    mk.mm(pc_[:, j * 256:(j + 1) * 256], lhsT=kT[hb:hb + 64, hc, T_LAT + j * 128:T_LAT + (j + 1) * 128],
                          rhs=qT[hb:hb + 64, hc, T_LAT:T_ALL])
                mk.act(PTc[:], pc_[:, 0:512].rearrange("q (a b) -> q a b", a=2), AF.Exp, scale=NA_SCALE)
                for qi in range(2):
                    po = p.psum[7]
                    for j in range(2):
                        mk.mm(po[:, 0:66], lhsT=PTc[:, j, qi * 128:(qi + 1) * 128], rhs=V[:, 16 + j, h, :], start=(j == 0), stop=(j == 1))
                    mk.recip(rec[:], po[:, 64:65])
                    mk.tsc(out_all[:, 16 + qi, h * 64:(h + 1) * 64], po[:, 0:64], rec[:, 0:1], ALU.mult)
        p.transposes_out(out_all, 512)


def _mla(self, l):
    p, mk = self, self.mk
    with mk.scope():
        qT = mk.sb("mlqT", [96, 4, T_ALL], F32R)
        kT = mk.sb("mlkT", [96, 4, T_ALL], F32R)
        V = mk.sb("mlV", [128, NT, 4, 66], F32R)
        out_all = mk.sb("mlout", [128, NT, 256])
        with mk.scope():
            cqT = mk.sb("cqT", [128, 2, T_ALL], F32R)
            ckvT = mk.sb("ckvT", [128, T_ALL], F32R)
            gqa = mk.sb("gqa", [128, 256])
            gkva = mk.sb("gkva", [128, 128])
            mk.dma("sp", gqa[:], p.mla_q_a_g[l, :].partition_broadcast(128))
            mk.dma("sp", gkva[:], p.mla_kv_a_g[l, :].partition_broadcast(128))
            gq = mk.sb("mgq", [128, 4, 96])
            gk = mk.sb("mgk", [128, 4, 96])
            for h in range(4):
                mk.dma("sp", gq[:, h, :], p.mla_q_g[l, :].partition_broadcast(128))
                mk.dma("sp", gk[:, h, :], p.mla_k_g[l, :].partition_broadcast(128))
            ones65 = mk.sb("mones65", [128, 4, 2])
            mk.memset(ones65[:], 0.0)
            mk.memset(ones65[:, :, 0:1], 1.0)
            wst = mk.sb("mwst", [128, 768])
            wuq = mk.sb("wuq", [128, 2, 384], F32R)
            wukv = mk.sb("wukv", [128, 512], F32R)
            mk.dma("sp", wst[:].rearrange("q (k n) -> q k n", k=2), p.mla_w_uq[l, :, :].rearrange("(k q) n -> q k n", q=128))
            mk.copy(wuq[:], wst[:].rearrange("q (k n) -> q k n", k=2), en="pool")
            mk.dma("sp", wst[:, 0:512], p.mla_w_ukv[l, :, :])
            mk.copy(wukv[:], wst[:, 0:512], en="pool")
            cosT = mk.sb("ropec", [128, 16, 32])
            sinT = mk.sb("ropes", [128, 16, 32])
            mk.dma("sp", cosT[:], p.c_ropecos[:, :, :])
            mk.dma("sp", sinT[:], p.c_ropesin[:, :, :])
            tin = [mk.sb("mltin%d" % i, [128, 416]) for i in range(2)]
            sq = mk.sb("mlsq", [128, 384])
            ss = mk.sb("mlss", [128, 4])
            nrm = mk.sb("mlnrm", [128, 384])
            for n in range(NT):
                t = tin[n % 2]
                mk.dma("sp", t[:], p.ptok[n * 128:(n + 1) * 128, 2816:3232])
                p.headnorm(nrm[:, 0:256], t[:, 0:256], 1, 256, gqa[:], sq[:, 0:256], ss[:, 0:1])
                for c in range(2):
                    pt = p.psum[c]
                    mk.tr(pt[:, 0:128], nrm[:, c * 128:(c + 1) * 128], p.identS[:])
                    p.evac(cqT[:, c, n * 128:(n + 1) * 128], pt[:, 0:128])
                p.headnorm(nrm[:, 256:384], t[:, 256:384], 1, 128, gkva[:], sq[:, 0:128], ss[:, 0:1])
                pt = p.psum[2]
                mk.tr(pt[:, 0:128], nrm[:, 256:384], p.identS[:])
                p.evac(ckvT[:, n * 128:(n + 1) * 128], pt[:, 0:128])
            qk = [mk.sb("mlqk%d" % i, [128, 384]) for i in range(2)]
            sw = mk.sb("mlsw", [128, 4, 32])
            for n in range(NT):
                t = tin[n % 2]
                mk.dma("sp", t[:, 384:416], p.ptok[n * 128:(n + 1) * 128, 3200:3232])
                pq, pkv = p.psum[3], p.psum[4]
                for c in range(2):
                    mk.mm(pq[:, 0:384], lhsT=cqT[:, c, n * 128:(n + 1) * 128], rhs=wuq[:, c, :], start=(c == 0), stop=(c == 1))
                mk.mm(pkv[:, 0:512], lhsT=ckvT[:, n * 128:(n + 1) * 128], rhs=wukv[:], start=True, stop=True)
                kvv = pkv[:, 0:512].rearrange("q (h d) -> q h d", h=4)
                mk.copy(V[:, n, :, 0:64], kvv[:, :, 64:128], en="act")
                mk.copy(V[:, n, :, 64:66], ones65[:], en="pool")
                for which in range(2):
                    raw = qk[which]
                    rv = raw[:].rearrange("q (h d) -> q h d", h=4)
                    if which == 0:
                        mk.copy(raw[:], pq[:, 0:384])
                        g, dstT = gq, qT
                    else:
                        mk.copy(rv[:, :, 0:64], kvv[:, :, 0:64])
                        mk.copy(rv[:, :, 64:96], t[:, 384:416].unsqueeze(1).to_broadcast([128, 4, 32]), en="pool")
                        g, dstT = gk, kT
                    p.headnorm(nrm[:], raw[:], 4, 96, g[:].rearrange("q h d -> q (h d)"), sq[:], ss[:])
                    nv = nrm[:].rearrange("q (h d) -> q h d", h=4)
                    if n < 16:
                        for s0 in (64, 80):
                            o0 = s0 - 64
                            mk.copy(sw[:, :, o0:o0 + 8], nv[:, :, s0 + 8:s0 + 16])
                            mk.copy(sw[:, :, o0 + 8:o0 + 16], nv[:, :, s0:s0 + 8])
                        mk.tt(sw[:], sw[:], sinT[:, n, :].unsqueeze(1).to_broadcast([128, 4, 32]), ALU.mult)
                        mk.tt(nv[:, :, 64:96], nv[:, :, 64:96], cosT[:, n, :].unsqueeze(1).to_broadcast([128, 4, 32]), ALU.mult)
                        mk.tt(nv[:, :, 64:96], nv[:, :, 64:96], sw[:], ALU.add)
                    for h in range(4):
                        pt = p.psum[5 + h % 2]
                        mk.tr(pt[0:96, 0:128], nrm[:, h * 96:(h + 1) * 96], p.identS[:])
                        p.evac(dstT[:, h, n * 128:(n + 1) * 128], pt[0:96, 0:128])
        with mk.scope():
            PT = mk.sb("mlPT", [128, NT, 512], F32R)
            rec = mk.sb("mlrec", [128, 1])
            for h in range(4):
                for bi, (t0, w) in enumerate(TB):
                    kts = list(range(NT)) if t0 < T_LAT else [16, 17]
                    for i, kt in enumerate(kts):
                        ps = p.psum[i % 4]
                        mk.mm(ps[:, 0:w], lhsT=kT[:, h, kt * 128:(kt + 1) * 128], rhs=qT[:, h, t0:t0 + w])
                        mk.act(PT[:, i, 0:w], ps[:, 0:w], AF.Exp, scale=MLA_SCALE)
                    for j in range(w // 128):
                        n = t0 // 128 + j
                        po = p.psum[4 + j % 2]
                        for i, kt in enumerate(kts):
                            mk.mm(po[:, 0:66], lhsT=PT[:, i, j * 128:(j + 1) * 128], rhs=V[:, kt, h, :],
                                  start=(i == 0), stop=(i == len(kts) - 1))
                        mk.recip(rec[:], po[:, 64:65])
                        mk.tsc(out_all[:, n, h * 64:(h + 1) * 64], po[:, 0:64], rec[:, 0:1], ALU.mult)
        p.transposes_out(out_all, 768)


Prog.headnorm = _headnorm
Prog.transposes_out = _transposes_out
Prog.na = _na
Prog.mla = _mla


def _hgrn_gen(self, l):
    p, mk = self, self.mk
    if True:
        lb = mk.sb("lb", [64, 512])
        oml = mk.sb("oml", [64, 512])
        if True:
            lg = mk.sb("lblg", [64, 4, 512])
            mk.dma("sp", lg[:], p.hgrn_lb.ap().rearrange("l a b -> l (a b)").partition_broadcast(64))
            mk.act(lg[:], lg[:], AF.Exp)
            tot = mk.sb("lbtot", [64, 512])
            mk.tt(tot[:], lg[:, 0, :], lg[:, 1, :], ALU.add)
            mk.tt(tot[:], tot[:], lg[:, 2, :], ALU.add)
            mk.tt(tot[:], tot[:], lg[:, 3, :], ALU.add)
            mk.recip(tot[:], tot[:])
            mk.memset(lb[:], 0.0)
            for ll in range(1, l + 1):
                mk.tt(lb[:], lb[:], lg[:, ll, :], ALU.add)
            mk.tt(lb[:], lb[:], tot[:], ALU.mult)
            mk.tsc(oml[:], lb[:], -1.0, ALU.mult, 1.0, ALU.add)
        gn = mk.sb("hgn", [64, 4, 64])
        for h in range(4):
            mk.dma("sp", gn[:, h, :], p.hgrn_norm_g[l, :].partition_broadcast(64))
        tri = [mk.sb("tri%d" % i, [64, 64]) for i in range(2)]
        mk.dma("sp", tri[0][:], p.c_trif[:, :])
        mk.dma("sp", tri[1][:], p.c_trib[:, :])
        ones1 = mk.sb("hones1", [64, 1])
        mk.memset(ones1[:], 1.0)
        S = mk.sb("hS", [64, 4, 64])
        tin = [mk.sb("htin%d" % i, [64, 1280]) for i in range(2)]
        NB = 2
        f_ = [mk.sb("hf%d" % i, [64, 256]) for i in range(NB)]
        lf = [mk.sb("hlf%d" % i, [64, 256]) for i in range(NB)]
        kk = [mk.sb("hkk%d" % i, [64, 256]) for i in range(NB)]
        bc = [mk.sb("hbc%d" % i, [64, 256]) for i in range(NB)]
        eb = [mk.sb("heb%d" % i, [64, 256]) for i in range(NB)]
        qe = [mk.sb("hqe%d" % i, [64, 256]) for i in range(NB)]
        ke = [mk.sb("hke%d" % i, [64, 256]) for i in range(NB)]
        qeT = [mk.sb("hqeT%d" % i, [64, 4, 64]) for i in range(NB)]
        keT = [mk.sb("hkeT%d" % i, [64, 4, 64]) for i in range(NB)]
        ebl = [mk.sb("hebl%d" % i, [64, 4]) for i in range(NB)]
        ATm = [mk.sb("hAT%d" % i, [64, 4, 64]) for i in range(NB)]
        osb = [mk.sb("hosb%d" % i, [64, 256]) for i in range(NB)]
        of_ = [mk.sb("hof%d" % i, [64, 256]) for i in range(NB)]
        sq = mk.sb("hsq", [64, 256])
        ss = mk.sb("hss", [64, 4])
        sg = mk.sb("hsg", [64, 256])
        oT = [mk.sb("hoT%d" % i, [128, 2, 64]) for i in range(NB)]
        tmpS = mk.sb("htmpS", [64, 4, 64])
        it = 0
        for d in range(2):
            mk.memset(S[:], 0.0)
            if d == 0:
                order = [(T_LAT + 64 * c) for c in range(4)] + [64 * c for c in range(32)]
            else:
                order = [(T_LAT + 64 * c) for c in reversed(range(4))] + [64 * c for c in reversed(range(32))]
            for tok0 in order:
                i = it % NB
                it += 1
                t = tin[i]
                mk.dma("sp", t[:], p.ptok[tok0:tok0 + 64, 0:1280])
                z = t[:, 256 + 256 * d:512 + 256 * d]
                mk.act(f_[i][:], z, AF.Sigmoid)
                mk.tt(f_[i][:], f_[i][:], oml[:, d * 256:(d + 1) * 256], ALU.mult)
                mk.tt(f_[i][:], f_[i][:], lb[:, d * 256:(d + 1) * 256], ALU.add)
                mk.act(lf[i][:], f_[i][:], AF.Ln)
                mk.tsc(kk[i][:], f_[i][:], -1.0, ALU.mult, 1.0, ALU.add)
                pb = p.psum[0]
                mk.mm(pb[0:64, 0:256], lhsT=tri[d][:], rhs=lf[i][:])
                mk.tsc(bc[i][:], pb[0:64, 0:256], -80.0, ALU.max)
                pl = p.psum[1]
                for h in range(4):
                    mk.mm(pl[0:64, h:h + 1], lhsT=lf[i][:, h * 64:(h + 1) * 64], rhs=ones1[:])
                mk.tsc(ebl[i][:], pl[0:64, 0:4], -80.0, ALU.max)
                mk.act(ebl[i][:], ebl[i][:], AF.Exp)
                mk.act(eb[i][:], bc[i][:], AF.Exp)
                mk.tt(qe[i][:], t[:, 0:256], eb[i][:], ALU.mult)
                mk.act(eb[i][:], bc[i][:], AF.Exp, scale=-1.0)
                mk.tt(ke[i][:], kk[i][:], eb[i][:], ALU.mult)
                pq, pk = p.psum[2], p.psum[3]
                for h in range(4):
                    mk.tr(pq[0:64, h * 64:(h + 1) * 64], qe[i][:, h * 64:(h + 1) * 64], p.identS[0:64, 0:64])
                    mk.tr(pk[0:64, h * 64:(h + 1) * 64], ke[i][:, h * 64:(h + 1) * 64], p.identS[0:64, 0:64])
                mk.copy(qeT[i][:].rearrange("q h t -> q (h t)"), pq[0:64, 0:256])
                mk.copy(keT[i][:].rearrange("q h t -> q (h t)"), pk[0:64, 0:256], en="act")
                pa = p.psum[4]
                for h in range(4):
                    mk.mm(pa[0:64, h * 64:(h + 1) * 64], lhsT=keT[i][:, h, :], rhs=qeT[i][:, h, :])
                mk.tt(ATm[i][:], pa[0:64, 0:256].rearrange("q (h t) -> q h t", h=4),
                      tri[d][:].unsqueeze(1).to_broadcast([64, 4, 64]), ALU.mult)
                po = p.psum[5]
                v = t[:, 768:1024]
                for h in range(4):
                    mk.mm(po[0:64, h * 64:(h + 1) * 64], lhsT=ATm[i][:, h, :], rhs=v[:, h * 64:(h + 1) * 64], start=True, stop=False)
                    mk.mm(po[0:64, h * 64:(h + 1) * 64], lhsT=qeT[i][:, h, :], rhs=S[:, h, :], start=False, stop=True)
                pd = p.psum[6]
                for h in range(4):
                    mk.mm(pd[0:64, h * 64:(h + 1) * 64], lhsT=ke[i][:, h * 64:(h + 1) * 64], rhs=v[:, h * 64:(h + 1) * 64])
                mk.tt(tmpS[:], S[:], pd[0:64, 0:256].rearrange("q (h t) -> q h t", h=4), ALU.add)
                mk.tt(S[:], tmpS[:], ebl[i][:].unsqueeze(2).to_broadcast([64, 4, 64]), ALU.mult)
                if d == 0:
                    mk.copy(osb[i][:], po[0:64, 0:256], en="act")
                    mk.dma("pool", p.hof[tok0:tok0 + 64, :], osb[i][:])
                else:
                    mk.dma("pool", of_[i][:], p.hof[tok0:tok0 + 64, :])
                    mk.tt(osb[i][:], po[0:64, 0:256], of_[i][:], ALU.add)
                    p.headnorm(osb[i][:], osb[i][:], 4, 64, gn[:].rearrange("q h d -> q (h d)"), sq[:], ss[:], np_=64)
                    mk.act(sg[:], t[:, 1024:1280], AF.Silu)
                    mk.tt(osb[i][:], osb[i][:], sg[:], ALU.mult)
                    pt = p.psum[7]
                    for c in range(2):
                        mk.tr(pt[:, c * 64:(c + 1) * 64], osb[i][:, c * 128:(c + 1) * 128], p.identS[0:64, 0:64])
                    mk.copy(oT[i][:].rearrange("q c t -> q (c t)"), pt[:, 0:128])
                    mk.dma("sp", p.catT[0:256, tok0:tok0 + 64].rearrange("(c q) t -> q c t", q=128), oT[i][:])
                yield


def _hgrn(self, l):
    p, mk = self, self.mk
    G = 4
    with mk.scope():
        lb = mk.sb("lb", [64, 512])
        oml = mk.sb("oml", [64, 512])
        with mk.scope():
            lg = mk.sb("lblg", [64, 4, 512])
            mk.dma("sp", lg[:], p.hgrn_lb.ap().rearrange("l a b -> l (a b)").partition_broadcast(64))
            mk.act(lg[:], lg[:], AF.Exp)
            tot = mk.sb("lbtot", [64, 512])
            mk.tt(tot[:], lg[:, 0, :], lg[:, 1, :], ALU.add)
            mk.tt(tot[:], tot[:], lg[:, 2, :], ALU.add)
            mk.tt(tot[:], tot[:], lg[:, 3, :], ALU.add)
            mk.recip(tot[:], tot[:])
            mk.memset(lb[:], 0.0)
            for ll in range(1, l + 1):
                mk.tt(lb[:], lb[:], lg[:, ll, :], ALU.add)
            mk.tt(lb[:], lb[:], tot[:], ALU.mult)
            mk.tsc(oml[:], lb[:], -1.0, ALU.mult, 1.0, ALU.add)
        gn = mk.sb("hgn", [64, G * 4, 64])
        for h in range(G * 4):
            mk.dma("sp", gn[:, h, :], p.hgrn_norm_g[l, :].partition_broadcast(64))
        tri = [mk.sb("tri%d" % i, [64, 64]) for i in range(2)]
        mk.dma("sp", tri[0][:], p.c_trif[:, :])
        mk.dma("sp", tri[1][:], p.c_trib[:, :])
        ones1 = mk.sb("hones1", [64, 1])
        mk.memset(ones1[:], 1.0)
        S = mk.sb("hS", [64, 4, 64])
        tmpS = mk.sb("htmpS", [64, 4, 64])
        NB = 2
        mkt = lambda nm, shp: [mk.sb("h%s%d" % (nm, i), shp) for i in range(NB)]
        tin = mkt("tin", [64, G, 1280])
        f_ = mkt("f", [64, G, 256]); lf = mkt("lf", [64, G, 256]); kk = mkt("kk", [64, G, 256])
        bc = mkt("bc", [64, G, 256]); eb = mkt("eb", [64, G, 256]); qe = mkt("qe", [64, G, 256]); ke = mkt("ke", [64, G, 256])
        qeT = mkt("qeT", [64, G, 4, 64]); keT = mkt("keT", [64, G, 4, 64]); ATm = mkt("ATm", [64, G, 4, 64])
        ebl = mkt("ebl", [64, G, 4]); osb = mkt("osb", [64, G, 256]); of_ = mkt("of", [64, G, 256])
        sq = mk.sb("hsq", [64, G * 256]); ss = mk.sb("hss", [64, G * 4]); sg = mk.sb("hsg", [64, G, 256])
        oT = mkt("oT", [128, 2, G * 64])
        batches = [T_LAT] + [G * 64 * b for b in range(8)]
        it = 0
        for d in range(2):
            mk.memset(S[:], 0.0)
            blist = batches if d == 0 else [T_LAT] + [G * 64 * b for b in reversed(range(8))]
            for tok0 in blist:
                i = it % NB
                it += 1
                t = tin[i]
                ncol = 1024 if d == 0 else 1280
                mk.dma("sp", t[:, :, 0:ncol], p.ptok[tok0:tok0 + G * 64, 0:ncol].rearrange("(g q) n -> q g n", q=64))
                z = t[:, :, 256 + 256 * d:512 + 256 * d]
                lbd = lb[:, d * 256:(d + 1) * 256].unsqueeze(1).to_broadcast([64, G, 256])
                omd = oml[:, d * 256:(d + 1) * 256].unsqueeze(1).to_broadcast([64, G, 256])
                mk.act(f_[i][:], z, AF.Sigmoid)
                mk.tt(f_[i][:], f_[i][:], omd, ALU.mult)
                mk.tt(f_[i][:], f_[i][:], lbd, ALU.add)
                mk.act(lf[i][:], f_[i][:], AF.Ln)
                mk.tsc(kk[i][:], f_[i][:], -1.0, ALU.mult, 1.0, ALU.add)
                for g2 in range(G // 2):
                    pb = p.psum[g2]
                    mk.mm(pb[0:64, 0:512], lhsT=tri[d][:], rhs=lf[i][:, 2 * g2:2 * g2 + 2, :].rearrange("q g k -> q (g k)"))
                    mk.tsc(bc[i][:, 2 * g2:2 * g2 + 2, :].rearrange("q g k -> q (g k)"), pb[0:64, 0:512], -80.0, ALU.max)
                pl = p.psum[2]
                for g in range(G):
                    for h in range(4):
                        mk.mm(pl[0:64, g * 4 + h:g * 4 + h + 1], lhsT=lf[i][:, g, h * 64:(h + 1) * 64], rhs=ones1[:])
                mk.tsc(ebl[i][:].rearrange("q g h -> q (g h)"), pl[0:64, 0:G * 4], -80.0, ALU.max)
                mk.act(ebl[i][:], ebl[i][:], AF.Exp)
                mk.act(eb[i][:], bc[i][:], AF.Exp)
                mk.tt(qe[i][:], t[:, :, 0:256], eb[i][:], ALU.mult)
                mk.act(eb[i][:], bc[i][:], AF.Exp, scale=-1.0)
                mk.tt(ke[i][:], kk[i][:], eb[i][:], ALU.mult)
                for g in range(G):
                    pq, pk = p.psum[3], p.psum[4]
                    for h in range(4):
                        mk.tr(pq[0:64, h * 64:(h + 1) * 64], qe[i][:, g, h * 64:(h + 1) * 64], p.identS[0:64, 0:64])
                        mk.tr(pk[0:64, h * 64:(h + 1) * 64], ke[i][:, g, h * 64:(h + 1) * 64], p.identS[0:64, 0:64])
                    mk.copy(qeT[i][:, g, :, :].rearrange("q h t -> q (h t)"), pq[0:64, 0:256])
                    mk.copy(keT[i][:, g, :, :].rearrange("q h t -> q (h t)"), pk[0:64, 0:256], en="act")
                    pa = p.psum[5]
                    for h in range(4):
                        mk.mm(pa[0:64, h * 64:(h + 1) * 64], lhsT=keT[i][:, g, h, :], rhs=qeT[i][:, g, h, :])
                    mk.tt(ATm[i][:, g, :, :], pa[0:64, 0:256].rearrange("q (h t) -> q h t", h=4),
                          tri[d][:].unsqueeze(1).to_broadcast([64, 4, 64]), ALU.mult)
                gorder = range(G) if d == 0 else reversed(range(G))
                for g in gorder:
                    v = t[:, g, 768:1024]
                    po = p.psum[6]
                    for h in range(4):
                        mk.mm(po[0:64, h * 64:(h + 1) * 64], lhsT=ATm[i][:, g, h, :], rhs=v[:, h * 64:(h + 1) * 64], start=True, stop=False)
                        mk.mm(po[0:64, h * 64:(h + 1) * 64], lhsT=qeT[i][:, g, h, :], rhs=S[:, h, :], start=False, stop=True)
                    pd = p.psum[7]
                    for h in range(4):
                        mk.mm(pd[0:64, h * 64:(h + 1) * 64], lhsT=ke[i][:, g, h * 64:(h + 1) * 64], rhs=v[:, h * 64:(h + 1) * 64])
                    mk.tt(tmpS[:], S[:], pd[0:64, 0:256].rearrange("q (h t) -> q h t", h=4), ALU.add)
                    mk.tt(S[:], tmpS[:], ebl[i][:, g, :].unsqueeze(2).to_broadcast([64, 4, 64]), ALU.mult)
                    mk.copy(osb[i][:, g, :], po[0:64, 0:256], en="act")
                hv = p.hof[tok0:tok0 + G * 64, :].rearrange("(g q) n -> q g n", q=64)
                if d == 0:
                    mk.dma("sp", hv, osb[i][:])
                else:
                    mk.dma("sp", of_[i][:], hv)
                    o2 = osb[i][:].rearrange("q g n -> q (g n)")
                    mk.tt(o2, o2, of_[i][:].rearrange("q g n -> q (g n)"), ALU.add)
                    p.headnorm(o2, o2, G * 4, 64, gn[:].rearrange("q h d -> q (h d)"), sq[:], ss[:], np_=64)
                    mk.act(sg[:], t[:, :, 1024:1280], AF.Silu)
                    mk.tt(osb[i][:], osb[i][:], sg[:], ALU.mult)
                    for c in range(2):
                        pt = p.psum[c]
                        for g in range(G):
                            mk.tr(pt[:, g * 64:(g + 1) * 64], osb[i][:, g, c * 128:(c + 1) * 128], p.identS[0:64, 0:64])
                        mk.copy(oT[i][:, c, :], pt[:, 0:G * 64], en="dve" if c == 0 else "act")
                    mk.dma("sp", p.catT[0:256, tok0:tok0 + G * 64].rearrange("(c q) t -> q c t", q=128), oT[i][:])


Prog.hgrn_gen = _hgrn_gen
Prog.hgrn = _hgrn


def _hy_sin(self, out, ps, b_col, f_col, w, tmp):
    mk = self.mk
    arg, s4, s2 = tmp
    mk.tsc(arg[:, 0:w], ps, b_col, ALU.add, f_col, ALU.mult)
    mk.act(s4[:, 0:w], arg[:, 0:w], AF.Sin, scale=0.25)
    mk.act(s2[:, 0:w], arg[:, 0:w], AF.Sin, scale=0.5)
    mk.tt(s4[:, 0:w], s4[:, 0:w], s4[:, 0:w], ALU.mult)
    mk.tsc(s4[:, 0:w], s4[:, 0:w], -2.0, ALU.mult, 1.0, ALU.add)
    mk.stt(out, s2[:, 0:w], 2.0, s4[:, 0:w], ALU.mult, ALU.mult)


def _hy_filters(self, l):
    p, mk = self, self.mk
    with mk.scope():
        w1 = mk.sb("hyw1", [33, 64])
        w2 = mk.sb("hyw2", [64, 64])
        w3 = mk.sb("hyw3", [64, 1024])
        mk.dma("sp", w1[:], p.hy_w1[l, :, :])
        mk.dma("sp", w2[:], p.hy_w2[l, :, :])
        mk.dma("sp", w3[:], p.hy_w3[l, :, :])
        cols = mk.sb("hycols", [64, 4])
        mk.dma("sp", cols[:, 0:1], p.hy_b1[l, :].rearrange("(q o) -> q o", o=1))
        mk.dma("sp", cols[:, 1:2], p.hy_freq[l, :].rearrange("(q o) -> q o", o=1))
        mk.dma("sp", cols[:, 2:3], p.hy_b2[l, :].rearrange("(q o) -> q o", o=1))
        b3 = mk.sb("hyb3", [128, 8])
        ndec = mk.sb("hyndec", [128, 8])
        mk.dma("sp", b3[:], p.hy_b3T[l, :, :])
        mk.dma("sp", ndec[:], p.hy_decT[l, :, :])
        mk.tsc(ndec[:], ndec[:], -1.0, ALU.mult)
        feats = mk.sb("hyfeats", [33, 512])
        tun = mk.sb("hytun", [128, 512])
        tmp = [mk.sb("hytmp%d" % i, [64, 512]) for i in range(3)]
        h1 = mk.sb("hyh1", [64, 512])
        h2 = mk.sb("hyh2", [64, 512])
        E = mk.sb("hyE", [128, 512])
        ssq = mk.sb("hyssq", [128, 8, 4])
        junk = mk.sb("hyjunk", [128, 2048])
        inv = mk.sb("hyinv", [128, 4])
        for (L, featsD, tunD, Kd, KW) in ((2048, p.c_feats, p.c_tun, p.Kd, 4096), (256, p.c_featsc, p.c_tunc, p.Kdc, 512)):
            with mk.scope():
                FT = mk.sb("hyFT", [128, 8, L])
                nblk = (L + 511) // 512
                for tdir in range(2):
                    for bi in range(nblk):
                        w = min(512, L)
                        t0 = bi * 512
                        mk.dma("sp", feats[:, 0:w], featsD[tdir, :, t0:t0 + w])
                        mk.dma("sp", tun[:, 0:w], tunD[tdir, t0:t0 + w].partition_broadcast(128))
                        ps = p.psum[0]
                        mk.mm(ps[0:64, 0:w], lhsT=w1[:], rhs=feats[:, 0:w])
                        p.hy_sin(h1[:, 0:w], ps[0:64, 0:w], cols[:, 0:1], cols[:, 1:2], w, tmp)
                        ps = p.psum[1]
                        mk.mm(ps[0:64, 0:w], lhsT=w2[:], rhs=h1[:, 0:w])
                        p.hy_sin(h2[:, 0:w], ps[0:64, 0:w], cols[:, 2:3], cols[:, 1:2], w, tmp)
                        for j in (0, 1, 4, 5):
                            jj = j + 2 * tdir
                            ps = p.psum[2 + jj % 4]
                            mk.mm(ps[:, 0:w], lhsT=w3[:, jj * 128:(jj + 1) * 128], rhs=h2[:, 0:w])
                            mk.act(E[:, 0:w], tun[:, 0:w], AF.Exp, scale=ndec[:, jj:jj + 1])
                            mk.stt(FT[:, jj, t0:t0 + w], ps[:, 0:w], b3[:, jj:jj + 1], E[:, 0:w], ALU.add, ALU.mult)
                for jj in range(8):
                    tdir = (jj // 2) % 2
                    n = L if tdir == 0 else L - 1
                    mk.act(junk[:, 0:n], FT[:, jj, 0:n], AF.Square, accum_out=ssq[:, jj, 0:1])
                for j in (0, 1, 4, 5):
                    col = inv[:, 0:1]
                    mk.tt(col, ssq[:, j, 0:1], ssq[:, j + 2, 0:1], ALU.add)
                    mk.act(col, col, AF.Sqrt, bias=EPS, scale=1.0)
                    mk.recip(col, col)
                    o, ch = j // 4, j % 2
                    r0 = o * 256 + ch * 128
                    mk.tsc(FT[:, j, :], FT[:, j, :], col, ALU.mult)
                    mk.tsc(FT[:, j + 2, :], FT[:, j + 2, :], col, ALU.mult)
                    mk.dma("sp", Kd[r0:r0 + 128, L - 1:2 * L - 1], FT[:, j, :])
                    mk.dma("sp", Kd[r0:r0 + 128, 0:L - 1], FT[:, j + 2, 0:L - 1])


def _hyena(self, l):
    p, mk = self, self.mk
    p.hy_filters(l)
    with mk.scope():
        sw = mk.sb("hysw", [128, 6, 3])
        sbias = mk.sb("hysb", [128, 6])
        mk.dma("sp", sw[:], p.hy_swT[l, :, :, :])
        mk.dma("sp", sbias[:], p.hy_sbT[l, :, :])
        db = mk.sb("hydb", [128, 2, 256])
        mk.dma("sp", db[:], p.hy_bias[l, :, :].partition_broadcast(128))
        Ex = mk.sb("hyEx", [128, 128])
        mk.dma("sp", Ex[:], p.c_exch[:, :])
        uf = [mk.sb("hyuf%d" % i, [128, T_ALL]) for i in range(2)]
        acc = mk.sb("hyacc", [128, T_ALL])
        tk = [mk.sb("hytk%d" % i, [128, NT, 128]) for i in range(3)]
        z1 = mk.sb("hyz1", [128, NT, 128])
        zr = mk.sb("hyzr", [128, NT, 128])
        R = [mk.sb("hyR%d" % i, [128, 3968]) for i in range(3)]
        Rc = [mk.sb("hyRc%d" % i, [128, 384]) for i in range(3)]
        tmpg = [mk.sb("hytg%d" % i, [128, NT]) for i in range(2)]
        ob = [mk.sb("hyob%d" % i, [128, 512]) for i in range(2)]
        ci = 0
        for ch in range(2):
            for part in range(3):
                jt = part * 2 + ch
                u = uf[part % 2]
                mk.dma("sp", u[:], p.uT[jt * 128:(jt + 1) * 128, :])
                mk.tsc(acc[:], u[:], sw[:, jt, 1:2], ALU.mult, sbias[:, jt:jt + 1], ALU.add)
                for (a, b) in ((0, T_LAT), (T_LAT, T_ALL)):
                    mk.stt(acc[:, a + 1:b], u[:, a:b - 1], sw[:, jt, 0:1], acc[:, a + 1:b], ALU.mult, ALU.add)
                    mk.stt(acc[:, a:b - 1], u[:, a + 1:b], sw[:, jt, 2:3], acc[:, a:b - 1], ALU.mult, ALU.add)
                for n in range(NT):
                    pt = p.psum[n % 4]
                    mk.tr(pt[:, 0:128], acc[:, n * 128:(n + 1) * 128], p.identS[:])
                    p.evac(tk[part][:, n, :], pt[:, 0:128])
            for o in range(2):
                z = tk[0] if o == 0 else z1
                gate = tk[1] if o == 0 else tk[2]
                zo = z1 if o == 0 else tk[0]
                for n in range(NT):
                    pt = p.psum[n % 4]
                    mk.mm(pt[:, 0:128], lhsT=Ex[:], rhs=z[:, n, :])
                    p.evac(zr[:, n, :], pt[:, 0:128])
                for c in range(128):
                    row = o * 256 + ch * 128 + c
                    r, rc = R[ci % 3], Rc[ci % 3]
                    mk.dma("sp" if ci % 2 == 0 else "act", r[:],
                           bass.AP(tensor=p.Kd.ap().tensor, offset=row * 4096, ap=[[1, 128], [1, 3968]]))
                    mk.dma("pool", rc[:], bass.AP(tensor=p.Kdc.ap().tensor, offset=row * 512, ap=[[1, 128], [1, 384]]))
                    ps = p.psum[4 + ci % 4]
                    ci += 1
                    Ds = [0] + [d for k in range(1, 16) for d in (k, -k)]
                    for di, Dd in enumerate(Ds):
                        i0, i1 = max(0, Dd), min(16, 16 + Dd)
                        mk.mm(ps[:, i0:i1], lhsT=r[:, 128 * (Dd + 15):128 * (Dd + 16)], rhs=zr[:, i0 - Dd:i1 - Dd, c],
                              start=(di == 0), stop=(di == len(Ds) - 1))
                    for di, Dd in enumerate((0, 1, -1)):
                        i0, i1 = max(0, Dd), min(2, 2 + Dd)
                        mk.mm(ps[:, 16 + i0:16 + i1], lhsT=rc[:, 128 * (Dd + 1):128 * (Dd + 2)], rhs=zr[:, 16 + i0 - Dd:16 + i1 - Dd, c],
                              start=(di == 0), stop=(di == 2))
                    tg = tmpg[c % 2]
                    cc = ch * 128 + c
                    mk.stt(tg[:], z[:, :, c], db[:, o, cc:cc + 1], ps[:, 0:NT], ALU.mult, ALU.add)
                    mk.tt(zo[:, :, c], tg[:], gate[:, :, c], ALU.mult)
            oi = 0
            for bi, (t0, w) in enumerate(TB):
                o_ = ob[oi % 2]
                oi += 1
                for j in range(w // 128):
                    n = t0 // 128 + j
                    pt = p.psum[n % 4]
                    mk.tr(pt[:, 0:128], tk[0][:, n, :], p.identS[:])
                    p.evac(o_[:, j * 128:(j + 1) * 128], pt[:, 0:128])
                mk.dma("sp", p.catT[256 + ch * 128:256 + (ch + 1) * 128, t0:t0 + w], o_[:, 0:w])


Prog.hy_sin = _hy_sin
Prog.hy_filters = _hy_filters
Prog.hyena = _hyena


def _hy_spectrum(self, l):
    p, mk = self, self.mk
    with mk.scope():
        w1 = mk.sb("hyw1", [33, 64])
        w2 = mk.sb("hyw2", [64, 64])
        w3 = mk.sb("hyw3", [64, 1024])
        mk.dma("sp", w1[:], p.hy_w1[l, :, :])
        mk.dma("sp", w2[:], p.hy_w2[l, :, :])
        mk.dma("sp", w3[:], p.hy_w3[l, :, :])
        cols = mk.sb("hycols", [64, 4])
        mk.dma("sp", cols[:, 0:1], p.hy_b1[l, :].rearrange("(q o) -> q o", o=1))
        mk.dma("sp", cols[:, 1:2], p.hy_freq[l, :].rearrange("(q o) -> q o", o=1))
        mk.dma("sp", cols[:, 2:3], p.hy_b2[l, :].rearrange("(q o) -> q o", o=1))
        b3 = mk.sb("hyb3", [128, 1024])
        dec = mk.sb("hydec", [128, 1024])
        mk.dma("sp", b3[:], p.hy_b3[l, :].partition_broadcast(128))
        mk.dma("sp", dec[:], p.hy_decay[l, :, :, :].rearrange("a b c -> (a b c)").partition_broadcast(128))
        feats = mk.sb("hyfeats", [33, 512])
        tmp = [mk.sb("hytmp%d" % i, [64, 512]) for i in range(3)]
        h1 = mk.sb("hyh1", [64, 512])
        h2 = mk.sb("hyh2", [64, 512])
        E = mk.sb("hyE", [128, 1024])
        tot = mk.sb("hytot", [128, 512])
        for (L, featsD, ntunD, dftB, Ksp, nrm) in ((2048, p.c_feats, p.c_ntun, p.c_dftB, p.Ksp, 2.0 / 4096),
                                                 (256, p.c_featsc, p.c_ntunc, p.c_dftBc, p.Kspc, 2.0 / 512)):
            nt = L // 128
            with mk.scope():
                filtR = mk.sb("hyfilt", [128, nt, 1024], F32R)
                filt = filtR[:].bitcast(F32)
                ntun = mk.sb("hyntun", [128, nt])
                mk.dma("sp", ntun[:], ntunD[:, :])
                sq = mk.sb("hysq", [128, 1024], F32R)
                for bi in range((L + 511) // 512):
                    w = min(512, L)
                    t0 = bi * 512
                    mk.dma("sp", feats[:, 0:w], featsD[0, :, t0:t0 + w])
                    ps = p.psum[0]
                    mk.mm(ps[0:64, 0:w], lhsT=w1[:], rhs=feats[:, 0:w])
                    p.hy_sin(h1[:, 0:w], ps[0:64, 0:w], cols[:, 0:1], cols[:, 1:2], w, tmp)
                    ps = p.psum[1]
                    mk.mm(ps[0:64, 0:w], lhsT=w2[:], rhs=h1[:, 0:w])
                    p.hy_sin(h2[:, 0:w], ps[0:64, 0:w], cols[:, 2:3], cols[:, 1:2], w, tmp)
                    for j in range(w // 128):
                        n = t0 // 128 + j
                        p.tick()
                        mk.act(E[:], dec[:], AF.Exp, scale=ntun[:, n:n + 1])
                        if n == 0:
                            mk.memset(E[:].rearrange("q (o d c) -> q o d c", o=2, d=2)[0:1, :, 1, :], 0.0)
                        for hf in range(2):
                            ps = p.psum[2 + hf]
                            mk.mm(ps[:, 0:512], lhsT=h2[:, j * 128:(j + 1) * 128], rhs=w3[:, hf * 512:(hf + 1) * 512])
                            mk.tt(filtR[:, n, hf * 512:(hf + 1) * 512], ps[:, 0:512], b3[:, hf * 512:(hf + 1) * 512], ALU.add)
                        mk.tt(filtR[:, n, :], filt[:, n, :], E[:], ALU.mult, en="pool")
                pss = [p.psum[4], p.psum[5]]
                for n in range(nt):
                    mk.act(sq[:], filt[:, n, :], AF.Square)
                    for hf in range(2):
                        mk.mm(pss[hf][:, 0:512], lhsT=p.onesR[:], rhs=sq[:, hf * 512:(hf + 1) * 512], start=(n == 0), stop=(n == nt - 1))
                for o in range(2):
                    mk.copy(tot[:, o * 256:(o + 1) * 256], pss[o][:, 256:512], en="act")
                    mk.tt(tot[:, o * 256:(o + 1) * 256], tot[:, o * 256:(o + 1) * 256], pss[o][:, 0:256], ALU.add)
                mk.act(tot[:], tot[:], AF.Sqrt, bias=EPS, scale=1.0)
                mk.recip(tot[:], tot[:])
                mk.tsc(tot[:], tot[:], nrm, ALU.mult)
                totv = tot[:].rearrange("q (o c) -> q o c", o=2).unsqueeze(2).to_broadcast([128, 2, 2, 256])
                for n in range(nt):
                    mk.tt(filtR[:, n, :].rearrange("q (o d c) -> q o d c", o=2, d=2),
                          filt[:, n, :].rearrange("q (o d c) -> q o d c", o=2, d=2), totv, ALU.mult,
                          en="dve" if n % 2 == 0 else "pool")
                for n in range(nt):
                    fv4 = filt[:, n, :].rearrange("q (o d c) -> q o d c", o=2, d=2)
                    fr4 = filtR[:, n, :].rearrange("q (o d c) -> q o d c", o=2, d=2)
                    eng = "dve" if n % 2 == 0 else "pool"
                    mk.tt(fr4[:, :, 1, :], fv4[:, :, 0, :], fv4[:, :, 1, :], ALU.subtract, en=eng)
                    if eng == "dve":
                        mk.stt(fr4[:, :, 0, :], fv4[:, :, 0, :], 2.0, fv4[:, :, 1, :], ALU.mult, ALU.subtract)
                    else:
                        mk.tsc(fr4[:, :, 0, :], fv4[:, :, 0, :], 2.0, ALU.mult, en="pool")
                        mk.tt(fr4[:, :, 0, :], fv4[:, :, 0, :], fv4[:, :, 1, :], ALU.subtract, en="pool")
                blk = [mk.sb("hyblk%d" % i, [128, 2, nt, 128], F32R) for i in range(2)]
                ko = [mk.sb("hyko%d" % i, [128, 2, 2, 256]) for i in range(2)]
                for k in range(nt):
                    p.tick()
                    bk = blk[k % 2]
                    mk.dma("pool", bk[:], dftB[k, :, 0:2, :, :], max_dma_last_dim=8192)
                    kk_ = ko[k % 2]
                    for cs in range(2):
                        ps = p.psum[cs]
                        for n in range(nt):
                            rhs = filtR[:, n, :].rearrange("q (o d c) -> q o d c", o=2, d=2)[:, :, cs, :]
                            mk.mm(ps[:, 0:512].rearrange("q (o c) -> q o c", o=2), lhsT=bk[:, 1 - cs, n, :], rhs=rhs,
                                  start=(n == 0), stop=(n == nt - 1))
                        mk.copy(kk_[:, :, cs, :], ps[:, 0:512].rearrange("q (o c) -> q o c", o=2), en="dve" if cs == 0 else "act")
                    if k == 0:
                        ps = p.psum[2]
                        for n in range(nt):
                            rhs = filtR[:, n, :].rearrange("q (o d c) -> q o d c", o=2, d=2)[:, :, 0, :]
                            mk.mm(ps[:, 0:512].rearrange("q (o c) -> q o c", o=2), lhsT=bk[:, 0, n, :], rhs=rhs,
                                  start=(n == 0), stop=(n == nt - 1))
                        mk.copy(kk_[0:1, :, 1, :], ps[0:1, 0:512].rearrange("q (o c) -> q o c", o=2))
                        mk.tsc(kk_[0:1, :, :, :], kk_[0:1, :, :, :], 0.5, ALU.mult)
                    mk.dma("sp", Ksp[:, :, k * 128:(k + 1) * 128, :].rearrange("o s q c -> q o s c"), kk_[:])


def _hy_conv(self, ztok, zch, o, nt, tile0, dftB, dftN, Ksp, gch_o, dbc, zout_ch, blkname):
    p, mk = self, self.mk
    W = nt * 128
    wb = min(512, W)
    ntb = W // wb
    with mk.scope():
        blk = [mk.sb(blkname + "%d" % i, [128, 2, nt, 128], F32R) for i in range(2)]
        rowt = [mk.sb(blkname + "r%d" % i, [128, W], F32R) for i in range(2)]
        kc = [mk.sb("hykc%d" % i, [128, 2, 256]) for i in range(2)]
        YR = mk.sb("hyYR", [128, 2, nt, 256], F32R)
        t1 = [mk.sb("hyt1%d" % i, [128, 256]) for i in range(2)]
        t2 = [mk.sb("hyt2%d" % i, [128, 256]) for i in range(2)]
        t3 = [mk.sb("hyt3%d" % i, [128, 256]) for i in range(2)]
        t4 = [mk.sb("hyt4%d" % i, [128, 256]) for i in range(2)]
        gt = [mk.sb("hygt%d" % i, [128, wb]) for i in range(2)]
        dz = [mk.sb("hydz%d" % i, [128, wb]) for i in range(2)]
        for k in range(nt):
            p.tick()
            bk = blk[k % 2]
            mk.dma("pool", bk[:], dftB[k, :, 0:2, :, :], max_dma_last_dim=8192)
            kk_ = kc[k % 2]
            mk.dma("sp", kk_[:], Ksp[o, :, k * 128:(k + 1) * 128, :].rearrange("s q c -> q s c"))
            psA, psB = p.psum[(k % 2) * 2], p.psum[(k % 2) * 2 + 1]
            for cs, ps in ((0, psA), (1, psB)):
                for n in range(nt):
                    mk.mm(ps[:, 0:256], lhsT=bk[:, 1 - cs, n, :], rhs=ztok[:, tile0 + n, :], start=(n == 0), stop=(n == nt - 1))
            a1, a2, b1_, b2_ = t1[k % 2], t2[k % 2], t3[k % 2], t4[k % 2]
            mk.tt(a1[:], psA[:, 0:256], kk_[:, 0, :], ALU.mult)
            mk.tt(a2[:], psB[:, 0:256], kk_[:, 1, :], ALU.mult)
            mk.tt(YR[:, 0, k, :], a1[:], a2[:], ALU.subtract, en="pool")
            mk.tt(b1_[:], psA[:, 0:256], kk_[:, 1, :], ALU.mult)
            mk.tt(b2_[:], psB[:, 0:256], kk_[:, 0, :], ALU.mult)
            mk.tt(YR[:, 1, k, :], b1_[:], b2_[:], ALU.add, en="pool")
            if k == 0:
                mk.copy(YR[0:1, 0, 0, :], a1[0:1, :])
                mk.copy(YR[0:1, 1, 0, :], a2[0:1, :])
        ri = 0
        for cs in range(2):
            for b in range(nt):
                p.tick()
                rt_ = rowt[ri % 2]
                ri += 1
                mk.dma("pool", rt_[:], dftN[cs, b * 128:(b + 1) * 128, 0:W], max_dma_last_dim=8192)
                for ct in range(2):
                    for tb in range(ntb):
                        ps = p.psum[ct * ntb + tb]
                        mk.mm(ps[:, 0:wb], lhsT=YR[:, cs, b, ct * 128:(ct + 1) * 128], rhs=rt_[:, tb * wb:(tb + 1) * wb],
                              start=(cs == 0 and b == 0), stop=(cs == 1 and b == nt - 1))
        gi = 0
        for ct in range(2):
            for tb in range(ntb):
                ps = p.psum[ct * ntb + tb]
                c0 = tile0 * 128 + tb * wb
                g, d_ = gt[gi % 2], dz[gi % 2]
                gi += 1
                mk.dma("sp", g[:], gch_o[ct * 128:(ct + 1) * 128, c0:c0 + wb])
                mk.stt(d_[:], zch[:, ct, c0:c0 + wb], dbc[:, o, ct:ct + 1], ps[:, 0:wb], ALU.mult, ALU.add)
                mk.tt(zout_ch[:, ct, c0:c0 + wb], d_[:], g[:], ALU.mult, en="pool")


def _hyena2(self, l):
    p, mk = self, self.mk
    with mk.scope():
        if p.bg_hgrn:
            p._bg = p.hgrn_gen(l)
            p.tick()
        p.hyena_body(l)
        while getattr(p, "_bg", None) is not None:
            p.tick()


def _hyena_body(self, l):
    p, mk = self, self.mk
    p.hy_spectrum(l)
    with mk.scope():
        ztok = mk.sb("hyztok", [128, NT, 256], F32R)
        zcA = mk.sb("hyzcA", [128, 2, T_ALL])
        zcB = mk.sb("hyzcB", [128, 2, T_ALL])
        dbc = mk.sb("hydbc", [128, 2, 2])
        mk.dma("sp", dbc[:], p.hy_biasT[l, :, :, :])
        with mk.scope():
            sw = mk.sb("hysw", [128, 6, 3])
            sbias = mk.sb("hysb", [128, 6])
            mk.dma("sp", sw[:], p.hy_swT[l, :, :, :])
            mk.dma("sp", sbias[:], p.hy_sbT[l, :, :])
            uf = [mk.sb("hyuf%d" % i, [128, T_ALL]) for i in range(2)]
            acc = [mk.sb("hyacc%d" % i, [128, T_ALL]) for i in range(2)]
            for jt in range(6):
                part, ch = jt // 2, jt % 2
                u = uf[jt % 2]
                ac = zcA[:, ch, :] if part == 0 else acc[jt % 2][:]
                mk.dma("sp", u[:], p.uT[jt * 128:(jt + 1) * 128, :])
                mk.tsc(ac, u[:], sw[:, jt, 1:2], ALU.mult, sbias[:, jt:jt + 1], ALU.add)
                for (a, b) in ((0, T_LAT), (T_LAT, T_ALL)):
                    mk.stt(ac[:, a + 1:b], u[:, a:b - 1], sw[:, jt, 0:1], ac[:, a + 1:b], ALU.mult, ALU.add)
                    mk.stt(ac[:, a:b - 1], u[:, a + 1:b], sw[:, jt, 2:3], ac[:, a:b - 1], ALU.mult, ALU.add)
                if part == 0:
                    for n in range(NT):
                        pt = p.psum[n % 4]
                        mk.tr(pt[:, 0:128], ac[:, n * 128:(n + 1) * 128], p.identS[:])
                        p.evac(ztok[:, n, ch * 128:(ch + 1) * 128], pt[:, 0:128])
                else:
                    mk.dma("act", p.gch[part - 1, ch * 128:(ch + 1) * 128, :], ac)
        for o in range(2):
            zin_ch = zcA if o == 0 else zcB
            zo_ch = zcB if o == 0 else zcA
            p.hy_conv(ztok, zin_ch, o, 16, 0, p.c_dftB, p.c_dftN, p.Ksp, p.gch[o, :, :], dbc, zo_ch, "hyblkL")
            p.hy_conv(ztok, zin_ch, o, 2, 16, p.c_dftBc, p.c_dftNc, p.Kspc, p.gch[o, :, :], dbc, zo_ch, "hyblkC")
            if o == 0:
                for ct in range(2):
                    for n in range(NT):
                        pt = p.psum[4 + n % 4]
                        mk.tr(pt[:, 0:128], zcB[:, ct, n * 128:(n + 1) * 128], p.identS[:])
                        p.evac(ztok[:, n, ct * 128:(ct + 1) * 128], pt[:, 0:128])
        for ct in range(2):
            mk.dma("sp", p.catT[256 + ct * 128:256 + (ct + 1) * 128, :], zcA[:, ct, :])


Prog.hyena_body = _hyena_body


def _tick(self, n=1):
    g = getattr(self, "_bg", None)
    if g is None:
        return
    for _ in range(n):
        try:
            next(g)
        except StopIteration:
            self._bg = None
            return


Prog.tick = _tick
Prog.hy_spectrum = _hy_spectrum
Prog.hy_conv = _hy_conv
Prog.hyena = _hyena2


def _consts():
    f = np.float32
    c = {}
    s = np.arange(128)
    c["c_lstrict"] = (s[:, None] < s[None, :]).astype(f)
    c["c_iotaE"] = np.tile((np.arange(32) * CAP).astype(f)[None, :], (128, 1))
    s = np.arange(64)
    c["c_trif"] = (s[:, None] <= s[None, :]).astype(f)
    c["c_trib"] = (s[:, None] >= s[None, :]).astype(f)
    c["c_exch"] = np.eye(128, dtype=f)[::-1].copy()
    c["ident"] = np.eye(128, dtype=f)
    t = np.arange(T_LAT)
    row = (t // 64).astype(f)
    col = (t % 64).astype(f)
    inv = (f(10000.0) ** (-np.arange(0, 16, 2, dtype=f) / f(16))).astype(f)
    ar = row[:, None] * inv[None, :]
    ac = col[:, None] * inv[None, :]
    cos = np.concatenate([np.cos(ar), np.cos(ar), np.cos(ac), np.cos(ac)], axis=1).astype(f)
    sin = np.concatenate([-np.sin(ar), np.sin(ar), -np.sin(ac), np.sin(ac)], axis=1).astype(f)
    c["c_ropecos"] = np.ascontiguousarray(cos.reshape(16, 128, 32).transpose(1, 0, 2))
    c["c_ropesin"] = np.ascontiguousarray(sin.reshape(16, 128, 32).transpose(1, 0, 2))
    for nm, tn, L in (("c_feats", "c_ntun", 2048), ("c_featsc", "c_ntunc", 256)):
        tt = np.arange(L, dtype=f)
        tu = np.linspace(0.0, 1.0, L, dtype=f)
        bands = np.linspace(1e-4, 15, 16, dtype=f)
        ang = (f(2.0 * np.pi / L) * tt[:, None] * bands[None, :]).astype(f)
        feats = np.concatenate([tu[:, None], np.cos(ang), -np.sin(ang)], axis=-1).astype(f)
        fT = feats.T
        c[nm] = np.ascontiguousarray(np.stack([fT, fT[:, ::-1]], axis=0))
        c[tn] = np.ascontiguousarray(-tu.reshape(L // 128, 128).T)
        N = 2 * L
        idx = np.arange(L, dtype=np.int64)
        prod = (idx[:, None] * idx[None, :]) % N
        ang64 = prod.astype(np.float64) * (2.0 * np.pi / N)
        Cm = np.cos(ang64)
        Sm = np.sin(ang64)
        sgn = np.where(idx % 2 == 0, 1.0, -1.0)
        Sm[:, 0] = sgn
        M = np.stack([Sm, Cm, Sm.T], axis=0).astype(f)
        nt = L // 128
        Mb = M.reshape(3, nt, 128, nt, 128).transpose(3, 2, 0, 1, 4)
        c["c_dftB" if L == 2048 else "c_dftBc"] = np.ascontiguousarray(Mb)
        c["c_dftN" if L == 2048 else "c_dftNc"] = np.ascontiguousarray(M[1:3])
    return c


def _na_bias_table(rpb):
    Ld, H = rpb.shape[0], rpb.shape[1]
    out = np.full((Ld, H, 5, 5, 128, 128), -30000.0, np.float32)
    pat_pr = {0: 0, 1: 1, 2: 2, 3: 14, 4: 15}
    kp = np.arange(128)
    q = np.arange(128)
    for pat, pr in pat_pr.items():
        rs0 = min(max(2 * pr - 4, 0), 24)
        ws = min((rs0 // 2) * 2, 22)
        for kt in range(5):
            krow = ws + (kt * 128 + kp) // 64
            kcol = (kt * 128 + kp) % 64
            qrow = 2 * pr + q // 64
            qcol = q % 64
            rs = np.clip(qrow - 4, 0, 24)
            cs = np.clip(qcol - 8, 0, 48)
            ok = (krow[:, None] >= rs[None, :]) & (krow[:, None] < rs[None, :] + 8) & \
                 (kcol[:, None] >= cs[None, :]) & (kcol[:, None] < cs[None, :] + 16)
            dr = np.clip(krow[:, None] - qrow[None, :] + 7, 0, 14)
            dc = np.clip(kcol[:, None] - qcol[None, :] + 15, 0, 30)
            g = rpb[:, :, dr, dc]
            out[:, :, pat, kt] = np.where(ok[None, None], g, np.float32(-30000.0))
    return np.ascontiguousarray(out.transpose(0, 1, 4, 2, 3, 5))


def host_shared(inp):
    f = np.float32
    m = dict(_consts())
    Ld = DEPTH
    m["w_ada"] = inp["w_ada"]
    m["b_adaT"] = np.ascontiguousarray(inp["b_ada"].reshape(Ld, 48, 128).transpose(0, 2, 1))
    m["n1g"] = np.ascontiguousarray(inp["norm1_g"].reshape(Ld, 8, 128).transpose(0, 2, 1))
    m["n2g"] = np.ascontiguousarray(inp["norm2_g"].reshape(Ld, 8, 128).transpose(0, 2, 1))
    m["w_in"] = inp["w_in"]
    m["w_out"] = inp["w_out"]
    m["hgrn_lb"] = inp["hgrn_lb_logits"]
    m["hgrn_norm_g"] = inp["hgrn_norm_g"]
    m["hy_swT"] = np.ascontiguousarray(inp["hy_short_w"].reshape(Ld, 3, 6, 128).transpose(0, 3, 2, 1))
    m["hy_sbT"] = np.ascontiguousarray(inp["hy_short_b"].reshape(Ld, 6, 128).transpose(0, 2, 1))
    for k in ("hy_w1", "hy_b1", "hy_freq", "hy_w2", "hy_b2", "hy_w3", "hy_bias", "hy_b3", "hy_decay", "na_q_g", "na_k_g", "mla_q_a_g",
              "mla_kv_a_g", "mla_w_uq", "mla_w_ukv", "mla_q_g", "mla_k_g", "moe_wg", "moe_bg", "moe_we", "moe_be",
              "moe_w_gate", "moe_w_up", "moe_w_down"):
        m[k] = inp[k]
    m["na_biasT"] = _na_bias_table(inp["na_rpb"])
    m["hy_biasT"] = np.ascontiguousarray(inp["hy_bias"].reshape(Ld, 2, 2, 128).transpose(0, 3, 1, 2))
    return m


def host_inputs(inp, b, shared):
    m = dict(shared)
    m["xin"] = np.ascontiguousarray(np.concatenate([inp["x"][b], inp["ctx"][b]], axis=0))
    m["cvecT"] = np.ascontiguousarray(np.stack([inp["c"][b], inp["c_ctx"]], axis=1))
    return m


def run_prog(inp, cores, extra=None, **kw):
    from concourse.bass_utils import run_bass_kernel_spmd
    prog = Prog(**kw)
    prog.build()
    shared = host_shared(inp)
    in_maps = []
    for b in cores:
        m = host_inputs(inp, b, shared)
        if extra:
            m.update(extra)
        in_maps.append({k: np.ascontiguousarray(m[k], dtype=np.float32) for k in prog.inputs})
    res = run_bass_kernel_spmd(prog.nc, in_maps, core_ids=list(range(len(cores))))
    return prog, res


def kernel(**inp):
    inp = {k: np.asarray(v) for k, v in inp.items()}
    prog, res = run_prog(inp, list(range(8)))
    out = np.stack([res.results[b]["xout"] for b in range(8)], axis=0)
    return out.astype(np.float32)
```
